# Optimizing a Trainium2 kernel written in Bass

```python
import jax, jax.numpy as jnp
from jax import lax
import numpy as np

D_MODEL = 1024
BATCH = 16
SEQ = 2048
DEPTH = 1

N_Q_HEADS = 8
N_KV_HEADS = 2
HEAD_DIM = 128
Q_GROUP = N_Q_HEADS // N_KV_HEADS
ROPE_THETA = 10000.0
Q_BLOCK = 128
GRID_W = 64
LRU_WIDTH = 1024
LRU_BLOCKS = 8
LRU_BLOCK_DIM = LRU_WIDTH // LRU_BLOCKS
CONV_WIDTH = 4
LRU_C = 8.0
N_EXPERTS = 32
TOP_K = 4
D_EXPERT = 1024
SWIGLU_LIMIT = 7.0
SWIGLU_ALPHA = 1.702
MOE_BLOCK = 256
PLE_DIM = 256
EPS = 1e-6

Q_W = N_Q_HEADS * HEAD_DIM
KV_W = N_KV_HEADS * HEAD_DIM
IN_SPLITS = (Q_W, KV_W, KV_W, LRU_WIDTH, LRU_WIDTH, D_MODEL, D_MODEL)
IN_WIDTH = Q_W + 2 * KV_W + 2 * LRU_WIDTH + 2 * D_MODEL

kernel_name = "hybrid_gqa_rglru_moe_ple_encoder"


def rms_norm(x, g):
    xf = x.astype(jnp.float32)
    y = xf * lax.rsqrt(jnp.mean(xf * xf, axis=-1, keepdims=True) + EPS)
    return (y * g.astype(jnp.float32)).astype(x.dtype)


def axial_rope_tables(seq_len):
    rows = seq_len // GRID_W
    row_ids = jnp.repeat(jnp.arange(rows), GRID_W).astype(jnp.float32)
    col_ids = jnp.tile(jnp.arange(GRID_W), rows).astype(jnp.float32)
    axis_dim = HEAD_DIM // 2
    inv_freq = ROPE_THETA ** (-jnp.arange(0, axis_dim, 2, dtype=jnp.float32) / axis_dim)
    ang_r = row_ids[:, None] * inv_freq[None, :]
    ang_c = col_ids[:, None] * inv_freq[None, :]
    return (jnp.cos(ang_r), jnp.sin(ang_r), jnp.cos(ang_c), jnp.sin(ang_c))


def rope_rotate_half(x, cos, sin):
    x1, x2 = jnp.split(x, 2, axis=-1)
    c = cos[:, None, :]
    s = sin[:, None, :]
    return jnp.concatenate([x1 * c - x2 * s, x2 * c + x1 * s], axis=-1).astype(x.dtype)


def apply_axial_rope(x, tables):
    cr, sr, cc, sc = tables
    x_row, x_col = jnp.split(x, 2, axis=-1)
    return jnp.concatenate([rope_rotate_half(x_row, cr, sr), rope_rotate_half(x_col, cc, sc)], axis=-1)


def blocked_gqa(q, k, v):
    B, S = q.shape[0], q.shape[1]
    n_blk = S // Q_BLOCK
    qb = q.reshape(B, n_blk, Q_BLOCK, N_KV_HEADS, Q_GROUP, HEAD_DIM).transpose(1, 0, 2, 3, 4, 5)
    scale = HEAD_DIM ** -0.5

    def one_block(q_blk):
        s = jnp.einsum('bqkgd,bskd->bkgqs', q_blk, k, preferred_element_type=jnp.float32) * scale
        pr = jax.nn.softmax(s, axis=-1)
        return jnp.einsum('bkgqs,bskd->bqkgd', pr.astype(v.dtype), v)

    ob = lax.map(one_block, qb)
    return ob.transpose(1, 0, 2, 3, 4, 5).reshape(B, S, Q_W)


def centred_depthwise_conv(x, w, b):
    left = CONV_WIDTH // 2
    right = CONV_WIDTH - 1 - left
    y = lax.conv_general_dilated(
        x, w[:, None, :].astype(x.dtype), window_strides=(1,), padding=[(left, right)],
        dimension_numbers=('NWC', 'WIO', 'NWC'), feature_group_count=x.shape[-1])
    return y + b


def block_diag_linear(x, w, b):
    B, S, C = x.shape
    xb = x.reshape(B, S, LRU_BLOCKS, LRU_BLOCK_DIM)
    return jnp.einsum('bsnd,nde->bsne', xb, w).reshape(B, S, C) + b


def linear_recurrence(a, b):
    def combine(c1, c2):
        a1, b1 = c1
        a2, b2 = c2
        return a1 * a2, a2 * b1 + b2
    _, h = lax.associative_scan(combine, (a, b), axis=1)
    return h


def rg_lru(x, w_a, b_a, w_i, b_i, lam, reverse):
    xf = x.astype(jnp.float32)
    r = jax.nn.sigmoid(block_diag_linear(x, w_a, b_a).astype(jnp.float32))
    i = jax.nn.sigmoid(block_diag_linear(x, w_i, b_i).astype(jnp.float32))
    log_a = -LRU_C * r * jax.nn.softplus(-lam.astype(jnp.float32))
    a = jnp.exp(log_a)
    mult = jnp.sqrt(-jnp.expm1(2.0 * log_a))
    bt = mult * i * xf
    if reverse:
        h = jnp.flip(linear_recurrence(jnp.flip(a, 1), jnp.flip(bt, 1)), 1)
    else:
        h = linear_recurrence(a, bt)
    return h.astype(x.dtype)


def hybrid_mixer(h, rope, g_mix, w_in, q_norm, k_norm, conv_w, conv_b,
                 lru_wa, lru_ba, lru_wi, lru_bi, lru_lam, w_attn_br, w_lru_br, w_out):
    B, S, _ = h.shape
    u = rms_norm(h, g_mix)
    z = u @ w_in
    points = np.cumsum(IN_SPLITS)[:-1].tolist()
    q, k, v, xr, xg, ga, gr = jnp.split(z, points, axis=-1)
    q = q.reshape(B, S, N_Q_HEADS, HEAD_DIM)
    k = k.reshape(B, S, N_KV_HEADS, HEAD_DIM)
    v = v.reshape(B, S, N_KV_HEADS, HEAD_DIM)
    q = apply_axial_rope(rms_norm(q, q_norm), rope)
    k = apply_axial_rope(rms_norm(k, k_norm), rope)
    y_attn = blocked_gqa(q, k, v) @ w_attn_br
    c = centred_depthwise_conv(xr, conv_w, conv_b)
    h_fwd = rg_lru(c, lru_wa[0], lru_ba[0], lru_wi[0], lru_bi[0], lru_lam[0], reverse=False)
    h_bwd = rg_lru(c, lru_wa[1], lru_ba[1], lru_wi[1], lru_bi[1], lru_lam[1], reverse=True)
    y_lru = ((h_fwd + h_bwd) * jax.nn.gelu(xg, approximate=True)) @ w_lru_br
    merged = jax.nn.sigmoid(ga) * y_attn + jax.nn.sigmoid(gr) * y_lru
    return merged @ w_out


def moe_ffn(h, g_moe, w_router, b_router, w_gu, b_gu, w_dn, b_dn):
    B, S, D = h.shape
    T = B * S
    TK = T * TOP_K
    u = rms_norm(h, g_moe).reshape(T, D)
    logits = (u @ w_router + b_router).astype(jnp.float32)
    top_val, top_idx = lax.top_k(logits, TOP_K)
    gates = jax.nn.softmax(top_val, axis=-1)
    e_flat = top_idx.reshape(-1).astype(jnp.int32)
    tok_flat = jnp.arange(TK, dtype=jnp.int32) // TOP_K
    g_flat = gates.reshape(-1)
    order = jnp.argsort(e_flat)
    e_sorted = e_flat[order]
    counts = jnp.zeros((N_EXPERTS,), jnp.int32).at[e_flat].add(1)
    padded = ((counts + MOE_BLOCK - 1) // MOE_BLOCK) * MOE_BLOCK
    start = jnp.cumsum(counts) - counts
    pad_end = jnp.cumsum(padded)
    pad_start = pad_end - padded
    dest = pad_start[e_sorted] + (jnp.arange(TK, dtype=jnp.int32) - start[e_sorted])
    cap = TK + N_EXPERTS * MOE_BLOCK
    n_blocks = cap // MOE_BLOCK
    slot_tok = jnp.zeros((cap,), jnp.int32).at[dest].set(tok_flat[order])
    slot_gate = jnp.zeros((cap,), jnp.float32).at[dest].set(g_flat[order])
    blk_start = jnp.arange(n_blocks, dtype=jnp.int32) * MOE_BLOCK
    blk_exp = jnp.minimum(jnp.sum(blk_start[:, None] >= pad_end[None, :], axis=1), N_EXPERTS - 1)
    xs = u[slot_tok].reshape(n_blocks, MOE_BLOCK, D)

    def expert_block(args):
        xb, e = args
        hu = xb @ w_gu[e] + b_gu[e]
        gate, up = jnp.split(hu, 2, axis=-1)
        gate = jnp.minimum(gate, SWIGLU_LIMIT)
        up = jnp.clip(up, -SWIGLU_LIMIT, SWIGLU_LIMIT)
        act = gate * jax.nn.sigmoid(SWIGLU_ALPHA * gate)
        return ((up + 1.0) * act) @ w_dn[e] + b_dn[e]

    ys = lax.map(expert_block, (xs, blk_exp)).reshape(cap, D)
    out = jnp.zeros((T, D), ys.dtype).at[slot_tok].add(ys * slot_gate[:, None].astype(ys.dtype))
    return out.reshape(B, S, D)


def setup_inputs(seed: int = 0) -> dict:
    key = jax.random.key(seed)
    ks = jax.random.split(key, 32)
    L, D, E, F = DEPTH, D_MODEL, N_EXPERTS, D_EXPERT

    def nrm(k, shape, scale):
        return jax.random.normal(k, shape, jnp.float32) * scale

    def gain(k, shape):
        return 1.0 + 0.05 * jax.random.normal(k, shape, jnp.float32)

    a_c = jax.random.uniform(ks[12], (L, 2, LRU_WIDTH), jnp.float32, 0.9, 0.999)
    s = a_c ** (1.0 / LRU_C)
    lam = jnp.log(s) - jnp.log1p(-s)
    return {
        "x": nrm(ks[0], (BATCH, SEQ, D), 1.0),
        "p": nrm(ks[1], (DEPTH, BATCH, SEQ, PLE_DIM), 1.0),
        "g_mix": gain(ks[2], (L, D)),
        "w_in": nrm(ks[3], (L, D, IN_WIDTH), D ** -0.5),
        "q_norm": gain(ks[4], (L, HEAD_DIM)),
        "k_norm": gain(ks[5], (L, HEAD_DIM)),
        "conv_w": nrm(ks[6], (L, CONV_WIDTH, LRU_WIDTH), CONV_WIDTH ** -0.5),
        "conv_b": nrm(ks[7], (L, LRU_WIDTH), 0.01),
        "lru_wa": nrm(ks[8], (L, 2, LRU_BLOCKS, LRU_BLOCK_DIM, LRU_BLOCK_DIM), LRU_BLOCK_DIM ** -0.5),
        "lru_ba": nrm(ks[9], (L, 2, LRU_WIDTH), 0.01),
        "lru_wi": nrm(ks[10], (L, 2, LRU_BLOCKS, LRU_BLOCK_DIM, LRU_BLOCK_DIM), LRU_BLOCK_DIM ** -0.5),
        "lru_bi": nrm(ks[11], (L, 2, LRU_WIDTH), 0.01),
        "lru_lam": lam,
        "w_attn_br": nrm(ks[13], (L, Q_W, D), Q_W ** -0.5),
        "w_lru_br": nrm(ks[14], (L, LRU_WIDTH, D), LRU_WIDTH ** -0.5),
        "w_out": nrm(ks[15], (L, D, D), D ** -0.5),
        "g_moe": gain(ks[16], (L, D)),
        "w_router": nrm(ks[17], (L, D, E), D ** -0.5),
        "b_router": nrm(ks[18], (L, E), 0.01),
        "w_gu": nrm(ks[19], (L, E, D, 2 * F), D ** -0.5),
        "b_gu": nrm(ks[20], (L, E, 2 * F), 0.01),
        "w_dn": nrm(ks[21], (L, E, F, D), F ** -0.5),
        "b_dn": nrm(ks[22], (L, E, D), 0.01),
        "g_ple": gain(ks[23], (L, D)),
        "w_ple_gate": nrm(ks[24], (L, D, D), D ** -0.5),
        "w_ple_proj": nrm(ks[25], (L, PLE_DIM, D), PLE_DIM ** -0.5),
    }


def reference(x, p, g_mix, w_in, q_norm, k_norm, conv_w, conv_b, lru_wa, lru_ba, lru_wi, lru_bi,
              lru_lam, w_attn_br, w_lru_br, w_out, g_moe, w_router, b_router, w_gu, b_gu, w_dn, b_dn,
              g_ple, w_ple_gate, w_ple_proj):
    S = x.shape[1]
    rope = axial_rope_tables(S)
    h = x
    for l in range(DEPTH):
        h = h + hybrid_mixer(h, rope, g_mix[l], w_in[l], q_norm[l], k_norm[l], conv_w[l], conv_b[l],
                             lru_wa[l], lru_ba[l], lru_wi[l], lru_bi[l], lru_lam[l],
                             w_attn_br[l], w_lru_br[l], w_out[l])
        h = h + moe_ffn(h, g_moe[l], w_router[l], b_router[l], w_gu[l], b_gu[l], w_dn[l], b_dn[l])
        u = rms_norm(h, g_ple[l])
        h = h + jax.nn.sigmoid(u @ w_ple_gate[l]) * (p[l] @ w_ple_proj[l])
    return h
```

```python
import os
import numpy as np
from contextlib import ExitStack
import concourse.bass as bass
import concourse.mybir as mybir
from concourse.bass_utils import run_bass_kernel_spmd

F32 = mybir.dt.float32
BF16 = mybir.dt.bfloat16
I32 = mybir.dt.int32
AF = mybir.ActivationFunctionType
ALU = mybir.AluOpType
AX = mybir.AxisListType

NCORES = 8
D = 1024
SEQ = 2048
NSEQ = 2
T = NSEQ * SEQ
NTILE = T // 128
E = 32
F = 1024
CAP = 640
NSB = CAP // 128
NSLOT = E * CAP
TRASH = NSLOT
PLE = 256
EPS = 1e-6
INW = 5632
NPV = 608
PV_CONVW, PV_CONVB, PV_BA, PV_BI, PV_LAM, PV_QN, PV_KN, PV_BGU = 0, 32, 40, 56, 72, 88, 89, 96

ENGS = ("pe", "act", "dve", "pool", "sp")


class Buf:
    __slots__ = ("name", "last_write", "reads", "sem", "sem_total", "excl")

    def __init__(self, name, excl=False):
        self.name = name
        self.excl = excl
        self.last_write = None
        self.reads = []
        self.sem = None
        self.sem_total = 0


class Sched:
    def __init__(self, nc, stack):
        self.nc = nc
        self.stack = stack
        self.stream = {e: [] for e in ENGS}
        self.sem = {e: stack.enter_context(nc.semaphore("s_" + e)) for e in ENGS}
        self.count = {e: 0 for e in ENGS}
        self.seen = {e: {} for e in ENGS}
        self.dma_bufs = []

    def _wait_tokens(self, e, toks):
        need = {}
        for t in toks:
            if t is None:
                continue
            if t[0] == "e":
                _, src, c = t
                if src == "pe" and e == "pe":
                    continue
                key = ("e", src)
                val = c
                sem = self.sem[src]
            else:
                b = t[1]
                key = ("d", id(b))
                val = b.sem_total
                sem = b.sem
            if self.seen[e].get(key, 0) >= val:
                continue
            if key not in need or need[key][1] < val:
                need[key] = (sem, val)
        for key, (sem, val) in need.items():
            self.seen[e][key] = val
            self.stream[e].append(lambda eng, sem=sem, val=val: eng.wait_ge(sem, val))

    @staticmethod
    def _deps(reads, writes):
        toks = []
        for r in reads:
            toks.append(r.last_write)
            if r.excl:
                toks.extend(r.reads)
        for w in writes:
            toks.append(w.last_write)
            toks.extend(w.reads)
        return toks

    def op(self, e, fn, reads=(), writes=(), signal=True):
        self._wait_tokens(e, self._deps(reads, writes))
        if signal:
            self.count[e] += 1
            tok = ("e", e, self.count[e])
            sem = self.sem[e]
            self.stream[e].append(lambda eng, fn=fn, sem=sem: fn(eng).then_inc(sem, 1))
        else:
            tok = ("e", e, self.count[e] + 1)
            self.stream[e].append(lambda eng, fn=fn: fn(eng))
        for w in writes:
            w.last_write = tok
            w.reads = []
        for r in reads:
            r.reads.append(tok)
        return tok

    def dma(self, e, fn, reads=(), writes=(), owner=None):
        if owner is None:
            owner = writes[0] if writes else reads[0]
        if owner.sem is None:
            owner.sem = self.stack.enter_context(self.nc.semaphore("d%d_%s" % (len(self.dma_bufs), owner.name)))
            self.dma_bufs.append(owner)
        self._wait_tokens(e, self._deps(reads, writes))
        owner.sem_total += 16
        sem = owner.sem
        self.stream[e].append(lambda eng, fn=fn, sem=sem: fn(eng).then_inc(sem, 16))
        tok = ("d", owner)
        for w in writes:
            w.last_write = tok
            w.reads = []
        for r in reads:
            r.reads.append(tok)
        return tok

    def barrier(self):
        toks = [("e", s, self.count[s]) for s in ENGS if self.count[s] > 0]
        toks += [("d", b) for b in self.dma_bufs]
        for e in ENGS:
            self._wait_tokens(e, toks)

    def emit(self):
        nc = self.nc
        self.barrier()
        with nc.Block() as block:
            for e, reg in (("sp", block.sync), ("act", block.scalar), ("pe", block.tensor),
                           ("dve", block.vector), ("pool", block.gpsimd)):
                lst = self.stream[e]
                if not lst:
                    continue

                def body(eng, lst=lst):
                    for f in lst:
                        f(eng)
                reg(body)


def _dsize(dt):
    return 2 if dt == BF16 else 4


class Arena:
    def __init__(self, nc, stack, nbytes):
        self.t = stack.enter_context(nc.sbuf_tensor("arena", [128, nbytes // 4], F32))
        self.off = 0
        self.nbytes = nbytes
        self.peak = 0

    def alloc(self, name, free, dt=F32):
        n = 1
        for f in free:
            n *= f
        sz = (n * _dsize(dt) + 31) // 32 * 32
        assert self.off + sz <= self.nbytes, (name, self.off, sz, self.nbytes)
        a = self.t[:, self.off // 4:(self.off + sz) // 4]
        if dt != F32:
            a = a.bitcast(dt)
        a = a[:, 0:n]
        if len(free) == 2:
            a = a.rearrange("p (a b) -> p a b", a=free[0])
        self.off += sz
        self.peak = max(self.peak, self.off)
        return a, Buf(name)

    def mark(self):
        return self.off

    def release(self, m):
        self.off = m


class Ctx:
    pass


def build_program(dbg=False):
    nc = bass.Bass("TRN2", target_bir_lowering=False)
    K = Ctx()
    K.nc = nc

    def din(name, shape, dt=F32):
        return nc.dram_tensor(name, list(shape), dt, kind="ExternalInput")

    K.x = din("x", [T, D]).ap()
    K.p = din("p", [T, PLE]).ap()
    K.w_in = din("w_in", [D, INW]).ap()
    K.lru_wa = din("lru_wa", [2, 8, 128, 128]).ap()
    K.lru_wi = din("lru_wi", [2, 8, 128, 128]).ap()
    K.w_attn_br = din("w_attn_br", [D, D]).ap()
    K.w_lru_br = din("w_lru_br", [D, D]).ap()
    K.w_out = din("w_out", [D, D]).ap()
    K.w_router = din("w_router", [D, E]).ap()
    K.w_gu = din("w_gu", [E, D, 2 * F]).ap()
    K.w_dn = din("w_dn", [E, F, D]).ap()
    K.b_dn = din("b_dn", [E, D]).ap()
    K.w_ple_gate = din("w_ple_gate", [D, D]).ap()
    K.w_ple_proj = din("w_ple_proj", [PLE, D]).ap()
    K.g_mix = din("g_mix", [1, D])
    K.g_moe = din("g_moe", [1, D])
    K.g_ple = din("g_ple", [1, D])
    K.b_router = din("b_router", [1, E])
    K.pv = din("pv", [128, NPV]).ap()
    K.ropeC = din("ropeC", [128, SEQ]).ap()
    K.ropeS = din("ropeS", [128, SEQ]).ap()
    K.perm = din("perm", [128, 128]).ap()
    K.ident = din("ident", [128, 128]).ap()
    K.tri = din("tri", [128, 128]).ap()
    K.eC = din("eC", [128, E]).ap()
    K.y = nc.dram_tensor("y", [T, D], F32, kind="ExternalOutput").ap()

    kind = "ExternalOutput" if dbg else "Internal"

    def dscr(name, shape, dt):
        if dbg:
            return nc.dram_tensor(name, list(shape), dt, kind="ExternalOutput").ap()
        return nc.dram_tensor(name, list(shape), dt).ap()

    K.qT_s = dscr("qT_s", [NSEQ, 8, 128, SEQ], BF16)
    K.kT_s = dscr("kT_s", [NSEQ, 2, 128, SEQ], BF16)
    K.V_s = dscr("V_s", [NSEQ, 128, 16, 256], BF16)
    K.yl_s = dscr("yl_s", [NSEQ, 8, 128, SEQ], BF16)
    K.sga_s = dscr("sga_s", [NSEQ, 8, 128, SEQ], BF16)
    K.sgr_s = dscr("sgr_s", [NSEQ, 8, 128, SEQ], BF16)
    K.h1_s = dscr("h1_s", [T, D], F32)
    K.xs = dscr("xs_s", [NSLOT + 128, D], BF16)
    K.ys = dscr("ys_s", [NSLOT + 128, D], F32)

    with ExitStack() as st:
        S = Sched(nc, st)
        K.S = S
        A = Arena(nc, st, 196 * 1024)
        K.A = A
        K.ps = []
        K.Bps = []
        for i in range(8):
            K.ps.append(st.enter_context(nc.psum_tensor(f"ps{i}", [128, 512], F32)))
            K.Bps.append(Buf(f"ps{i}", excl=True))
        stop = int(os.environ.get("MK_STOP", "9"))
        phase0_consts(K)
        S.barrier()
        m0 = A.mark()
        if stop >= 1:
            phase1_inproj(K)
            S.barrier()
        A.release(m0)
        if stop >= 2:
            phase2_attn(K)
            S.barrier()
        A.release(m0)
        if stop >= 3:
            phase3_router(K)
            S.barrier()
        A.release(m0)
        if stop >= 4:
            phase4_experts(K)
            S.barrier()
        A.release(m0)
        if stop >= 5:
            phase5_combine(K)
        S.emit()
    return nc


def bcast_row(dt_tensor, n):
    return bass.AP(dt_tensor, 0, [[0, 128], [1, n]])


def phase0_consts(K):
    S, A = K.S, K.A
    K.ident_f, K.Bident_f = A.alloc("ident_f", [128])
    K.ident_b, K.Bident_b = A.alloc("ident_b", [128], BF16)
    K.ones_b, K.Bones_b = A.alloc("ones_b", [128], BF16)
    K.ones_f, K.Bones_f = A.alloc("ones_f", [128])
    K.pvt, K.Bpv = A.alloc("pvt", [NPV])
    K.kk, K.Bkk = A.alloc("kk", [16])
    K.dest_i, K.Bdest = A.alloc("dest_i", [NTILE * 4], I32)
    K.gate_a, K.Bgate = A.alloc("gate_a", [NTILE * 4])
    S.dma("sp", lambda e: e.dma_start(out=K.ident_f, in_=K.ident), writes=[K.Bident_f])
    S.dma("pool", lambda e: e.dma_start(out=K.ident_b, in_=K.ident), writes=[K.Bident_b])
    S.dma("sp", lambda e: e.dma_start(out=K.pvt, in_=K.pv), writes=[K.Bpv])
    S.op("dve", lambda e: e.memset(K.ones_b, 1.0), writes=[K.Bones_b])
    S.op("dve", lambda e: e.memset(K.ones_f, 1.0), writes=[K.Bones_f])
    lam = K.pvt[:, PV_LAM:PV_LAM + 16]
    S.op("act", lambda e: e.activation(out=K.kk, in_=lam, func=AF.Exp, scale=-1.0), reads=[K.Bpv], writes=[K.Bkk])
    S.op("act", lambda e: e.activation(out=K.kk, in_=K.kk, func=AF.Ln, bias=1.0), reads=[K.Bkk], writes=[K.Bkk])
    S.op("dve", lambda e: e.tensor_scalar(out=K.kk, in0=K.kk, scalar1=-8.0, scalar2=None, op0=ALU.mult),
         reads=[K.Bkk], writes=[K.Bkk])


def mm_group(S, out, Bout, pairs, reads):
    n = len(pairs)
    for i, (l, r) in enumerate(pairs):
        S.op("pe", lambda e, l=l, r=r, i=i: e.matmul(out, l, r, start=(i == 0), stop=(i == n - 1)),
             reads=reads, writes=[Bout], signal=(i == n - 1))


def rms_rstd(K, src, Bsrc, junk, Bjunk, ss, Bss, n):
    S = K.S
    S.op("act", lambda e: e.activation(out=junk, in_=src, func=AF.Square, accum_out=ss),
         reads=[Bsrc], writes=[Bjunk, Bss])
    S.op("act", lambda e: e.activation(out=ss, in_=ss, func=AF.Sqrt, bias=EPS, scale=1.0 / n),
         reads=[Bss], writes=[Bss])
    S.op("dve", lambda e: e.reciprocal(out=ss, in_=ss), reads=[Bss], writes=[Bss])


def interleave(gens):
    gens = list(gens)
    while gens:
        for g in list(gens):
            try:
                next(g)
            except StopIteration:
                gens.remove(g)


def phase1_inproj(K):
    S, A, nc = K.S, K.A, K.nc
    ps, Bps = K.ps, K.Bps
    uT, BuT = A.alloc("uT", [8, SEQ], BF16)
    wtA = [A.alloc(f"wtA{i}", [8, 512], BF16) for i in range(2)]
    wtB = [A.alloc(f"wtB{i}", [8, 256], BF16) for i in range(2)]
    gmix, Bgmix = A.alloc("gmix", [D])
    ropeS, BropeS = A.alloc("ropeS", [SEQ])
    cq, Bcq = A.alloc("cq", [SEQ])
    ck, Bck = A.alloc("ck", [SEQ])
    permf, Bpermf = A.alloc("permf", [128])
    permq, Bpermq = A.alloc("permq", [128], BF16)
    permk, Bpermk = A.alloc("permk", [128], BF16)
    od_b, Bod = A.alloc("od_b", [128], BF16)
    wa_b, Bwa = A.alloc("wa_b", [16, 128], BF16)
    wi_b, Bwi = A.alloc("wi_b", [16, 128], BF16)
    junk, Bjunk = A.alloc("junk", [D], BF16)
    xn, Bxn = A.alloc("xn", [D], BF16)
    ss = [A.alloc(f"ss{i}", [1]) for i in range(2)]
    stgA = [A.alloc(f"stgA{i}", [SEQ], BF16) for i in range(2)]
    stgB = [A.alloc(f"stgB{i}", [SEQ], BF16) for i in range(2)]
    xq = [A.alloc(f"xq{i}", [512], BF16) for i in range(2)]
    sq = [A.alloc(f"sq{i}", [512], BF16) for i in range(2)]
    ta = [A.alloc(f"ta{i}", [512]) for i in range(2)]
    tb_ = [A.alloc(f"tb{i}", [512]) for i in range(2)]
    rst = [A.alloc(f"rst{i}", [512]) for i in range(2)]
    xrp, Bxrp = A.alloc("xrp", [SEQ + 4])
    cc, Bcc = A.alloc("cc", [SEQ])
    ccb, Bccb = A.alloc("ccb", [SEQ], BF16)
    aa, Baa = A.alloc("aa", [SEQ])
    bt, Bbt = A.alloc("bt", [SEQ])
    t1, Bt1 = A.alloc("t1", [SEQ])
    hf, Bhf = A.alloc("hf", [SEQ])
    hb, Bhb = A.alloc("hb", [SEQ])
    xt = [(hb[:, 0:D], Bhb), (hf[:, 0:D], Bhf)]
    vst, Bvst = aa.bitcast(BF16).rearrange("p (a b) -> p a b", a=16), Baa

    pvt = K.pvt
    S.dma("sp", lambda e: e.dma_start(out=gmix, in_=bcast_row(K.g_mix, D)), writes=[Bgmix])
    S.dma("sp", lambda e: e.dma_start(out=cq, in_=K.ropeC), writes=[Bcq])
    S.dma("sp", lambda e: e.dma_start(out=ck, in_=K.ropeC), writes=[Bck])
    S.dma("sp", lambda e: e.dma_start(out=ropeS, in_=K.ropeS), writes=[BropeS])
    S.dma("sp", lambda e: e.dma_start(out=permf, in_=K.perm), writes=[Bpermf])
    S.dma("pool", lambda e: e.dma_start(out=wa_b, in_=K.lru_wa.rearrange("d c p n -> p (d c) n")), writes=[Bwa])
    S.dma("pool", lambda e: e.dma_start(out=wi_b, in_=K.lru_wi.rearrange("d c p n -> p (d c) n")), writes=[Bwi])
    S.op("dve", lambda e: e.memset(od_b, 1.0 / 128.0), writes=[Bod])
    S.op("dve", lambda e: e.memset(xrp, 0.0), writes=[Bxrp])
    qn = pvt[:, PV_QN:PV_QN + 1]
    kn = pvt[:, PV_KN:PV_KN + 1]
    S.op("dve", lambda e: e.tensor_scalar(out=cq, in0=cq, scalar1=qn, scalar2=None, op0=ALU.mult),
         reads=[Bcq, K.Bpv], writes=[Bcq])
    S.op("dve", lambda e: e.tensor_scalar(out=ck, in0=ck, scalar1=kn, scalar2=None, op0=ALU.mult),
         reads=[Bck, K.Bpv], writes=[Bck])
    S.op("dve", lambda e: e.tensor_scalar(out=permq, in0=permf, scalar1=qn, scalar2=None, op0=ALU.mult),
         reads=[Bpermf, K.Bpv], writes=[Bpermq])
    S.op("dve", lambda e: e.tensor_scalar(out=permk, in0=permf, scalar1=kn, scalar2=None, op0=ALU.mult),
         reads=[Bpermf, K.Bpv], writes=[Bpermk])

    w_in_v = K.w_in.rearrange("(c p) n -> p c n", p=128)
    wcnt = {"A": 0, "B": 0}

    def load_w(which, cols):
        tiles = wtA if which == "A" else wtB
        i = wcnt[which] % 2
        wcnt[which] += 1
        w, Bw = tiles[i]
        off = 0
        for (c0, wd) in cols:
            S.dma("pool", lambda e, w=w, off=off, c0=c0, wd=wd: e.dma_start(
                out=w[:, :, off:off + wd], in_=w_in_v[:, :, c0:c0 + wd]), writes=[Bw])
            off += wd
        return w, Bw

    def inproj(w, Bw, woff, tb, bank):
        mm_group(S, ps[bank][:], Bps[bank],
                 [(w[:, k, woff:woff + 128], uT[:, k, tb * 512:(tb + 1) * 512]) for k in range(8)],
                 reads=[Bw, BuT])

    scnt = {"A": 0, "B": 0}

    def build_uT(s):
        for i in range(16):
            x_t, Bx = xt[i % 2]
            ss_t, Bss = ss[i % 2]
            r0 = s * SEQ + i * 128
            S.dma("sp", lambda e, x_t=x_t, r0=r0: e.dma_start(out=x_t, in_=K.x[r0:r0 + 128, :]), writes=[Bx])
            rms_rstd(K, x_t, Bx, junk, Bjunk, ss_t, Bss, D)
            S.op("dve", lambda e, x_t=x_t, ss_t=ss_t: e.scalar_tensor_tensor(
                out=xn, in0=x_t, scalar=ss_t, in1=gmix, op0=ALU.mult, op1=ALU.mult),
                reads=[Bx, Bss, Bgmix], writes=[Bxn])
            bank = i % 2
            pv16 = ps[bank][:].bitcast(BF16)
            for c in range(8):
                S.op("pe", lambda e, c=c, pv16=pv16: e.transpose(pv16[:, c * 128:(c + 1) * 128], xn[:, c * 128:(c + 1) * 128], K.ident_b),
                     reads=[Bxn, K.Bident_b], writes=[Bps[bank]], signal=(c == 7))
            S.op("act", lambda e, pv16=pv16, i=i: e.activation(
                out=uT[:, :, i * 128:(i + 1) * 128], in_=pv16.rearrange("p (c t) -> p c t", c=8), func=AF.Copy),
                reads=[Bps[bank]], writes=[BuT])

    def qk_head(s, w, Bw, woff, is_q, hidx):
        cg, Bcg = (cq, Bcq) if is_q else (ck, Bck)
        pm, Bpm = (permq, Bpermq) if is_q else (permk, Bpermk)
        st_t, Bst = stgA[scnt["A"] % 2]
        scnt["A"] += 1
        for tb in range(4):
            p = tb % 2
            sl = slice(tb * 512, (tb + 1) * 512)
            xq_, Bxq = xq[p]
            sq_, Bsq = sq[p]
            ta_, Bta = ta[p]
            tb2, Btb = tb_[p]
            rs_, Brst = rst[p]
            zb = p
            inproj(w, Bw, woff, tb, zb)
            S.op("act", lambda e, xq_=xq_, zb=zb: e.activation(out=xq_, in_=ps[zb][:], func=AF.Copy), reads=[Bps[zb]], writes=[Bxq])
            S.op("act", lambda e, sq_=sq_, zb=zb: e.activation(out=sq_, in_=ps[zb][:], func=AF.Square), reads=[Bps[zb]], writes=[Bsq])
            S.op("dve", lambda e, sl=sl, cg=cg, ta_=ta_, zb=zb: e.tensor_tensor(out=ta_, in0=ps[zb][:], in1=cg[:, sl], op=ALU.mult),
                 reads=[Bps[zb], Bcg], writes=[Bta])
            yield
            mm_group(S, ps[2][:], Bps[2], [(od_b, sq_)], reads=[Bod, Bsq])
            mm_group(S, ps[3][:], Bps[3], [(pm, xq_)], reads=[Bpm, Bxq])
            S.op("act", lambda e, rs_=rs_: e.activation(out=rs_, in_=ps[2][:], func=AF.Sqrt, bias=EPS), reads=[Bps[2]], writes=[Brst])
            S.op("dve", lambda e, sl=sl, tb2=tb2: e.tensor_tensor(out=tb2, in0=ps[3][:], in1=ropeS[:, sl], op=ALU.mult),
                 reads=[Bps[3], BropeS], writes=[Btb])
            yield
            S.op("dve", lambda e, rs_=rs_: e.reciprocal(out=rs_, in_=rs_), reads=[Brst], writes=[Brst])
            S.op("dve", lambda e, ta_=ta_, tb2=tb2: e.tensor_tensor(out=ta_, in0=ta_, in1=tb2, op=ALU.add), reads=[Bta, Btb], writes=[Bta])
            S.op("dve", lambda e, sl=sl, st_t=st_t, ta_=ta_, rs_=rs_: e.tensor_tensor(out=st_t[:, sl], in0=ta_, in1=rs_, op=ALU.mult),
                 reads=[Bta, Brst], writes=[Bst])
            yield
        dst = (K.qT_s if is_q else K.kT_s)[s, hidx]
        S.dma("sp", lambda e, st_t=st_t, dst=dst: e.dma_start(out=dst, in_=st_t), reads=[Bst], owner=Bst)

    def stream_A(s):
        for t2 in range(2):
            w, Bw = load_w("A", [(t2 * 512, 512)])
            for j in range(4):
                yield from qk_head(s, w, Bw, j * 128, True, t2 * 4 + j)
        w, Bw = load_w("A", [(1024, 512)])
        for j in range(2):
            yield from qk_head(s, w, Bw, j * 128, False, j)
        for gi, dst_s in ((0, K.sga_s), (1, K.sgr_s)):
            for t2 in range(2):
                wg, Bwg = load_w("A", [(3584 + gi * 1024 + t2 * 512, 512)])
                for j in range(4):
                    st_t, Bst = stgA[scnt["A"] % 2]
                    scnt["A"] += 1
                    for tb in range(4):
                        bank = tb % 2
                        inproj(wg, Bwg, j * 128, tb, bank)
                        S.op("act", lambda e, bank=bank, tb=tb, st_t=st_t: e.activation(out=st_t[:, tb * 512:(tb + 1) * 512], in_=ps[bank][:], func=AF.Sigmoid),
                             reads=[Bps[bank]], writes=[Bst])
                        yield
                    S.dma("sp", lambda e, st_t=st_t, dst=dst_s[s, t2 * 4 + j]: e.dma_start(out=dst, in_=st_t), reads=[Bst], owner=Bst)

    def do_V(s):
        w, Bw = load_w("A", [(1024, 512)])
        for tk in range(16):
            bank = 2 + tk % 2
            mm_group(S, ps[bank][:, 0:256], Bps[bank],
                     [(uT[:, k, tk * 128:(tk + 1) * 128], w[:, k, 256:512]) for k in range(8)], reads=[Bw, BuT])
            S.op("act", lambda e, bank=bank, tk=tk: e.activation(out=vst[:, tk, :], in_=ps[bank][:, 0:256], func=AF.Copy),
                 reads=[Bps[bank]], writes=[Bvst])
        S.dma("sp", lambda e, s=s: e.dma_start(out=K.V_s[s], in_=vst), reads=[Bvst], owner=Bvst)

    def stream_B(s):
        for j in range(8):
            w, Bw = load_w("B", [(1536 + j * 128, 128), (2560 + j * 128, 128)])
            for tb in range(4):
                bank = 4 + tb % 2
                inproj(w, Bw, 0, tb, bank)
                S.op("act", lambda e, bank=bank, tb=tb: e.activation(out=xrp[:, 2 + tb * 512:2 + (tb + 1) * 512], in_=ps[bank][:], func=AF.Copy),
                     reads=[Bps[bank]], writes=[Bxrp])
                yield
            cw = lambda jj, j=j: K.pvt[:, PV_CONVW + j * 4 + jj:PV_CONVW + j * 4 + jj + 1]
            cb = K.pvt[:, PV_CONVB + j:PV_CONVB + j + 1]
            S.op("act", lambda e, cw=cw, cb=cb: e.activation(out=cc, in_=xrp[:, 0:SEQ], func=AF.Identity, bias=cb, scale=cw(0)),
                 reads=[Bxrp, K.Bpv], writes=[Bcc])
            for jj in range(1, 4):
                S.op("dve", lambda e, cw=cw, jj=jj: e.scalar_tensor_tensor(out=cc, in0=xrp[:, jj:jj + SEQ], scalar=cw(jj), in1=cc, op0=ALU.mult, op1=ALU.add),
                     reads=[Bxrp, Bcc, K.Bpv], writes=[Bcc])
                yield
            S.op("act", lambda e: e.activation(out=ccb, in_=cc, func=AF.Copy), reads=[Bcc], writes=[Bccb])
            for d in range(2):
                ba = K.pvt[:, PV_BA + d * 8 + j:PV_BA + d * 8 + j + 1]
                bi = K.pvt[:, PV_BI + d * 8 + j:PV_BI + d * 8 + j + 1]
                kkc = K.kk[:, d * 8 + j:d * 8 + j + 1]
                for tb in range(4):
                    sl = slice(tb * 512, (tb + 1) * 512)
                    mm_group(S, ps[6][:], Bps[6], [(wa_b[:, d * 8 + j, :], ccb[:, sl])], reads=[Bwa, Bccb])
                    S.op("act", lambda e, sl=sl, ba=ba: e.activation(out=aa[:, sl], in_=ps[6][:], func=AF.Sigmoid, bias=ba),
                         reads=[Bps[6], K.Bpv], writes=[Baa])
                    mm_group(S, ps[7][:], Bps[7], [(wi_b[:, d * 8 + j, :], ccb[:, sl])], reads=[Bwi, Bccb])
                    S.op("act", lambda e, sl=sl, bi=bi: e.activation(out=bt[:, sl], in_=ps[7][:], func=AF.Sigmoid, bias=bi),
                         reads=[Bps[7], K.Bpv], writes=[Bbt])
                    yield
                S.op("act", lambda e, kkc=kkc: e.activation(out=aa, in_=aa, func=AF.Exp, scale=kkc), reads=[Baa, K.Bkk], writes=[Baa])
                S.op("dve", lambda e: e.tensor_tensor(out=bt, in0=bt, in1=cc, op=ALU.mult), reads=[Bbt, Bcc], writes=[Bbt])
                yield
                S.op("act", lambda e: e.activation(out=t1, in_=aa, func=AF.Square), reads=[Baa], writes=[Bt1])
                S.op("act", lambda e: e.activation(out=t1, in_=t1, func=AF.Sqrt, bias=1.0, scale=-1.0), reads=[Bt1], writes=[Bt1])
                yield
                S.op("dve", lambda e: e.tensor_tensor(out=bt, in0=bt, in1=t1, op=ALU.mult), reads=[Bbt, Bt1], writes=[Bbt])
                yield
                if d == 0:
                    S.op("dve", lambda e: e.tensor_tensor_scan(out=hf, data0=aa, data1=bt, initial=0.0, op0=ALU.mult, op1=ALU.add),
                         reads=[Baa, Bbt], writes=[Bhf])
                else:
                    S.op("dve", lambda e: e.tensor_tensor_scan(out=hb[:, ::-1], data0=aa[:, ::-1], data1=bt[:, ::-1], initial=0.0, op0=ALU.mult, op1=ALU.add),
                         reads=[Baa, Bbt], writes=[Bhb])
                yield
            S.op("dve", lambda e: e.tensor_tensor(out=hf, in0=hf, in1=hb, op=ALU.add), reads=[Bhf, Bhb], writes=[Bhf])
            st_t, Bst = stgB[scnt["B"] % 2]
            scnt["B"] += 1
            for tb in range(4):
                sl = slice(tb * 512, (tb + 1) * 512)
                bank = 4 + tb % 2
                inproj(w, Bw, 128, tb, bank)
                S.op("act", lambda e, bank=bank, sl=sl: e.activation(out=t1[:, sl], in_=ps[bank][:], func=AF.Square), reads=[Bps[bank]], writes=[Bt1])
                S.op("act", lambda e, sl=sl: e.activation(out=t1[:, sl], in_=t1[:, sl], func=AF.Identity, bias=1.0, scale=0.044715),
                     reads=[Bt1], writes=[Bt1])
                S.op("dve", lambda e, bank=bank, sl=sl: e.tensor_tensor(out=t1[:, sl], in0=t1[:, sl], in1=ps[bank][:], op=ALU.mult),
                     reads=[Bt1, Bps[bank]], writes=[Bt1])
                yield
                S.op("act", lambda e, sl=sl: e.activation(out=t1[:, sl], in_=t1[:, sl], func=AF.Sigmoid, scale=1.5957691216), reads=[Bt1], writes=[Bt1])
                S.op("dve", lambda e, bank=bank, sl=sl: e.tensor_tensor(out=t1[:, sl], in0=t1[:, sl], in1=ps[bank][:], op=ALU.mult),
                     reads=[Bt1, Bps[bank]], writes=[Bt1])
                S.op("dve", lambda e, sl=sl, st_t=st_t: e.tensor_tensor(out=st_t[:, sl], in0=t1[:, sl], in1=hf[:, sl], op=ALU.mult),
                     reads=[Bt1, Bhf], writes=[Bst])
                yield
            S.dma("sp", lambda e, st_t=st_t, j=j, s=s: e.dma_start(out=K.yl_s[s, j], in_=st_t), reads=[Bst], owner=Bst)

    for s in range(NSEQ):
        build_uT(s)
        interleave([stream_A(s), stream_B(s)])
        do_V(s)


def phase2_attn(K):
    S, A, nc = K.S, K.A, K.nc
    ps, Bps = K.ps, K.Bps
    wab, Bwab = A.alloc("wab", [8, D], BF16)
    wlb, Bwlb = A.alloc("wlb", [8, D], BF16)
    wo, Bwo = A.alloc("wo", [8, D], BF16)
    kT, BkT = A.alloc("kT", [2, SEQ], BF16)
    V, BV = A.alloc("V", [16, 256], BF16)
    qT = [A.alloc(f"qT{i}", [8, 512], BF16) for i in range(2)]
    ylb = [A.alloc(f"ylb{i}", [8, 512], BF16) for i in range(2)]
    gab = [A.alloc(f"gab{i}", [8, 512], BF16) for i in range(2)]
    grb = [A.alloc(f"grb{i}", [8, 512], BF16) for i in range(2)]
    PT = [A.alloc(f"PT{i}", [512], BF16) for i in range(6)]
    SBANK = (0, 1, 2, 7)
    attnT, BattnT = A.alloc("attnT", [8, 512], BF16)
    mrg, Bmrg = A.alloc("mrg", [8, 512], BF16)
    rz, Brz = A.alloc("rz", [512])
    zacc = [A.alloc(f"zacc{i}", [512]) for i in range(2)]
    zab = [A.alloc(f"zab{i}", [512], BF16) for i in range(2)]
    m1, Bm1 = A.alloc("m1", [512])
    m2, Bm2 = A.alloc("m2", [512])
    xt = [A.alloc(f"xt{i}", [D]) for i in range(2)]
    ho = [A.alloc(f"ho{i}", [D]) for i in range(2)]
    S.dma("pool", lambda e: e.dma_start(out=wab, in_=K.w_attn_br.rearrange("(c p) n -> p c n", p=128)), writes=[Bwab])
    S.dma("pool", lambda e: e.dma_start(out=wlb, in_=K.w_lru_br.rearrange("(c p) n -> p c n", p=128)), writes=[Bwlb])
    S.dma("pool", lambda e: e.dma_start(out=wo, in_=K.w_out.rearrange("(c p) n -> p c n", p=128)), writes=[Bwo])
    zt, Bzt = A.alloc("zt", [D], BF16)
    S.op("dve", lambda e: e.memset(zt, 0.0), writes=[Bzt])
    zrows = list(range(0, NSLOT + 128, 128))

    def zero_some(n):
        for _ in range(n):
            if zrows:
                r0 = zrows.pop(0)
                S.dma("sp", lambda e, r0=r0: e.dma_start(out=K.xs[r0:r0 + 128, :], in_=zt), reads=[Bzt], owner=Bzt)
    scale = 128.0 ** -0.5
    cnt = 0
    for s in range(NSEQ):
        S.dma("sp", lambda e, s=s: e.dma_start(out=kT, in_=K.kT_s[s].rearrange("h p t -> p h t")), writes=[BkT])
        S.dma("sp", lambda e, s=s: e.dma_start(out=V, in_=K.V_s[s]), writes=[BV])
        for qb in range(4):
            q, Bq = qT[cnt % 2]
            yl, Byl = ylb[cnt % 2]
            ga, Bga = gab[cnt % 2]
            gr, Bgr = grb[cnt % 2]
            cnt += 1
            tsl = slice(qb * 512, (qb + 1) * 512)
            S.dma("sp", lambda e, q=q, s=s, tsl=tsl: e.dma_start(out=q, in_=K.qT_s[s].rearrange("h p t -> p h t")[:, :, tsl]), writes=[Bq])
            S.dma("sp", lambda e, yl=yl, s=s, tsl=tsl: e.dma_start(out=yl, in_=K.yl_s[s].rearrange("h p t -> p h t")[:, :, tsl]), writes=[Byl])
            S.dma("sp", lambda e, ga=ga, s=s, tsl=tsl: e.dma_start(out=ga, in_=K.sga_s[s].rearrange("h p t -> p h t")[:, :, tsl]), writes=[Bga])
            S.dma("sp", lambda e, gr=gr, s=s, tsl=tsl: e.dma_start(out=gr, in_=K.sgr_s[s].rearrange("h p t -> p h t")[:, :, tsl]), writes=[Bgr])
            for h in range(8):
                kv = h // 4
                ob, zb = 3 + h % 2, 5 + h % 2

                def score(kc):
                    bank = SBANK[kc % 4]
                    mm_group(S, ps[bank][:], Bps[bank], [(kT[:, kv, kc * 128:(kc + 1) * 128], q[:, h, :])], reads=[BkT, Bq])
                    pt, Bpt = PT[kc % 6]
                    S.op("act", lambda e, bank=bank, pt=pt: e.activation(out=pt, in_=ps[bank][:], func=AF.Exp, scale=scale),
                         reads=[Bps[bank]], writes=[Bpt])

                za, Bza = zacc[h % 2]
                zb16, Bzb16 = zab[h % 2]

                def pv(kc):
                    pt, Bpt = PT[kc % 6]
                    S.op("pe", lambda e, kc=kc, pt=pt, ob=ob, kv=kv: e.matmul(ps[ob][:], V[:, kc, kv * 128:(kv + 1) * 128], pt, start=(kc == 0), stop=(kc == 15)),
                         reads=[BV, Bpt], writes=[Bps[ob]], signal=(kc == 15))
                    if kc % 2 == 1:
                        S.op("pe", lambda e, kc=kc, pt=pt, zb=zb: e.matmul(ps[zb][:], K.ones_b, pt, start=(kc == 1), stop=False),
                             reads=[K.Bones_b, Bpt], writes=[Bps[zb]], signal=False)
                    elif kc == 0:
                        S.op("dve", lambda e, pt=pt, za=za: e.tensor_copy(out=za, in_=pt), reads=[Bpt], writes=[Bza])
                    else:
                        S.op("dve", lambda e, pt=pt, za=za: e.tensor_tensor(out=za, in0=za, in1=pt, op=ALU.add), reads=[Bpt, Bza], writes=[Bza])
                    if kc == 15:
                        S.op("pe", lambda e, za=za, zb=zb: e.matmul(ps[zb][:], K.ones_f, za, start=False, stop=True),
                             reads=[K.Bones_f, Bza], writes=[Bps[zb]], signal=True)

                score(0)
                score(1)
                score(2)
                for kc in range(16):
                    if kc + 3 < 16:
                        score(kc + 3)
                    pv(kc)
                S.op("dve", lambda e, zb=zb: e.reciprocal(out=rz, in_=ps[zb][:]), reads=[Bps[zb]], writes=[Brz])
                S.op("dve", lambda e, ob=ob, h=h: e.tensor_tensor(out=attnT[:, h, :], in0=ps[ob][:], in1=rz, op=ALU.mult),
                     reads=[Bps[ob], Brz], writes=[BattnT])
                zero_some(6)
            for m in range(8):
                b1, b2 = 0 + m % 2, 2 + m % 2
                mm_group(S, ps[b1][:], Bps[b1], [(wab[:, k, m * 128:(m + 1) * 128], attnT[:, k, :]) for k in range(8)], reads=[Bwab, BattnT])
                mm_group(S, ps[b2][:], Bps[b2], [(wlb[:, k, m * 128:(m + 1) * 128], yl[:, k, :]) for k in range(8)], reads=[Bwlb, Byl])
                S.op("dve", lambda e, b1=b1, m=m, ga=ga: e.tensor_tensor(out=m1, in0=ps[b1][:], in1=ga[:, m, :], op=ALU.mult),
                     reads=[Bps[b1], Bga], writes=[Bm1])
                S.op("dve", lambda e, b2=b2, m=m, gr=gr: e.tensor_tensor(out=m2, in0=ps[b2][:], in1=gr[:, m, :], op=ALU.mult),
                     reads=[Bps[b2], Bgr], writes=[Bm2])
                S.op("dve", lambda e, m=m: e.tensor_tensor(out=mrg[:, m, :], in0=m1, in1=m2, op=ALU.add),
                     reads=[Bm1, Bm2], writes=[Bmrg])
            for tk in range(4):
                x_t, Bx = xt[tk % 2]
                h_t, Bh = ho[tk % 2]
                r0 = s * SEQ + qb * 512 + tk * 128
                S.dma("sp", lambda e, x_t=x_t, r0=r0: e.dma_start(out=x_t, in_=K.x[r0:r0 + 128, :]), writes=[Bx])
                for nh in range(2):
                    bank = 5 + nh
                    mm_group(S, ps[bank][:], Bps[bank],
                             [(mrg[:, k, tk * 128:(tk + 1) * 128], wo[:, k, nh * 512:(nh + 1) * 512]) for k in range(8)], reads=[Bmrg, Bwo])
                    S.op("dve", lambda e, bank=bank, nh=nh, x_t=x_t, h_t=h_t: e.tensor_tensor(
                        out=h_t[:, nh * 512:(nh + 1) * 512], in0=ps[bank][:], in1=x_t[:, nh * 512:(nh + 1) * 512], op=ALU.add),
                        reads=[Bps[bank], Bx], writes=[Bh])
                S.dma("sp", lambda e, h_t=h_t, r0=r0: e.dma_start(out=K.h1_s[r0:r0 + 128, :], in_=h_t), reads=[Bh], owner=Bh)
    zero_some(len(zrows))


def phase3_router(K):
    S, A, nc = K.S, K.A, K.nc
    ps, Bps = K.ps, K.Bps
    gmoe, Bgmoe = A.alloc("gmoe", [D])
    wr, Bwr = A.alloc("wr", [8, E])
    brt, Bbr = A.alloc("brt", [E])
    tri, Btri = A.alloc("tri", [128])
    eC, BeC = A.alloc("eC", [E])
    msum, Bmsum = A.alloc("msum", [E])
    S.dma("sp", lambda e: e.dma_start(out=gmoe, in_=bcast_row(K.g_moe, D)), writes=[Bgmoe])
    S.dma("sp", lambda e: e.dma_start(out=wr, in_=K.w_router.rearrange("(c p) n -> p c n", p=128)), writes=[Bwr])
    S.dma("sp", lambda e: e.dma_start(out=brt, in_=bcast_row(K.b_router, E)), writes=[Bbr])
    S.dma("sp", lambda e: e.dma_start(out=tri, in_=K.tri), writes=[Btri])
    S.dma("sp", lambda e: e.dma_start(out=eC, in_=K.eC), writes=[BeC])
    S.op("dve", lambda e: e.memset(msum, 0.0), writes=[Bmsum])

    def tile_stream(par):
        h_t, Bh = A.alloc(f"ht{par}", [D])
        junk, Bjunk = A.alloc(f"junk{par}", [D], BF16)
        ss_t, Bss = A.alloc(f"ss{par}", [1])
        u2, Bu2 = A.alloc(f"u2{par}", [D])
        ub, Bub = A.alloc(f"u2b{par}", [D], BF16)
        u2T, Bu2T = A.alloc(f"u2T{par}", [8, 128])
        lg, Blg = A.alloc(f"lg{par}", [E])
        top8, Btop8 = A.alloc(f"top8{par}", [8])
        nm, Bnm = A.alloc(f"nm{par}", [1])
        mask, Bmask = A.alloc(f"mask{par}", [E])
        ex, Bex = A.alloc(f"ex{par}", [E])
        den, Bden = A.alloc(f"den{par}", [1])
        gf, Bgf = A.alloc(f"gf{par}", [E])
        rank, Brank = A.alloc(f"rank{par}", [E])
        okm, Bok = A.alloc(f"okm{par}", [E])
        dst, Bdst = A.alloc(f"dst{par}", [E])
        dk, Bdk = A.alloc(f"dk{par}", [4])
        oh, Boh = A.alloc(f"oh{par}", [E])
        b0 = par * 2

        def gen():
            for i in range(par, NTILE, 4):
                r0 = i * 128
                S.dma("sp", lambda e, r0=r0: e.dma_start(out=h_t, in_=K.h1_s[r0:r0 + 128, :]), writes=[Bh])
                rms_rstd(K, h_t, Bh, junk, Bjunk, ss_t, Bss, D)
                S.op("dve", lambda e: e.scalar_tensor_tensor(out=u2, in0=h_t, scalar=ss_t, in1=gmoe, op0=ALU.mult, op1=ALU.mult),
                     reads=[Bh, Bss, Bgmoe], writes=[Bu2])
                yield
                S.op("act", lambda e: e.activation(out=ub, in_=u2, func=AF.Copy), reads=[Bu2], writes=[Bub])
                for hc in range(2):
                    bank = b0
                    for c4 in range(4):
                        c = hc * 4 + c4
                        S.op("pe", lambda e, c=c, c4=c4, bank=bank: e.transpose(ps[bank][:, c4 * 128:(c4 + 1) * 128], u2[:, c * 128:(c + 1) * 128], K.ident_f),
                             reads=[Bu2, K.Bident_f], writes=[Bps[bank]], signal=(c4 == 3))
                    S.op("act", lambda e, bank=bank, hc=hc: e.activation(out=u2T[:, hc * 4:(hc + 1) * 4, :], in_=ps[bank][:].rearrange("p (c t) -> p c t", c=4), func=AF.Copy),
                         reads=[Bps[bank]], writes=[Bu2T])
                yield
                lb, rb = b0 + 1, b0 + 1
                mm_group(S, ps[lb][:, 0:E], Bps[lb], [(u2T[:, k, :], wr[:, k, :]) for k in range(8)], reads=[Bu2T, Bwr])
                S.op("dve", lambda e, lb=lb: e.tensor_tensor(out=lg, in0=ps[lb][:, 0:E], in1=brt, op=ALU.add), reads=[Bps[lb], Bbr], writes=[Blg])
                S.op("dve", lambda e: e.max(out=top8, in_=lg), reads=[Blg], writes=[Btop8])
                S.op("dve", lambda e: e.tensor_scalar(out=mask, in0=lg, scalar1=top8[:, 3:4], scalar2=None, op0=ALU.is_ge),
                     reads=[Blg, Btop8], writes=[Bmask])
                yield
                mm_group(S, ps[rb][:, 64:64 + E], Bps[rb], [(tri, mask), (K.ones_f, msum)], reads=[Btri, Bmask, K.Bones_f, Bmsum])
                S.op("dve", lambda e: e.tensor_tensor(out=msum, in0=msum, in1=mask, op=ALU.add), reads=[Bmsum, Bmask], writes=[Bmsum])
                yield
                S.op("dve", lambda e: e.tensor_scalar(out=nm, in0=top8[:, 0:1], scalar1=-1.0, scalar2=None, op0=ALU.mult),
                     reads=[Btop8], writes=[Bnm])
                S.op("act", lambda e: e.activation(out=ex, in_=lg, func=AF.Exp, bias=nm), reads=[Blg, Bnm], writes=[Bex])
                S.op("dve", lambda e, rb=rb: e.tensor_copy(out=rank, in_=ps[rb][:, 64:64 + E]), reads=[Bps[rb]], writes=[Brank])
                S.op("dve", lambda e: e.tensor_scalar(out=okm, in0=rank, scalar1=float(CAP), scalar2=None, op0=ALU.is_lt),
                     reads=[Brank], writes=[Bok])
                S.op("dve", lambda e: e.tensor_tensor(out=dst, in0=rank, in1=eC, op=ALU.add), reads=[Brank, BeC], writes=[Bdst])
                S.op("dve", lambda e: e.scalar_tensor_tensor(out=dst, in0=dst, scalar=float(-TRASH), in1=okm, op0=ALU.add, op1=ALU.mult),
                     reads=[Bdst, Bok], writes=[Bdst])
                S.op("dve", lambda e: e.tensor_scalar(out=dst, in0=dst, scalar1=float(TRASH), scalar2=None, op0=ALU.add),
                     reads=[Bdst], writes=[Bdst])
                yield
                S.op("dve", lambda e: e.tensor_tensor(out=ex, in0=ex, in1=mask, op=ALU.mult), reads=[Bex, Bmask], writes=[Bex])
                S.op("dve", lambda e: e.reduce_sum(out=den, in_=ex, axis=AX.X), reads=[Bex], writes=[Bden])
                S.op("dve", lambda e: e.reciprocal(out=den, in_=den), reads=[Bden], writes=[Bden])
                S.op("dve", lambda e: e.scalar_tensor_tensor(out=gf, in0=ex, scalar=den, in1=okm, op0=ALU.mult, op1=ALU.mult),
                     reads=[Bex, Bden, Bok], writes=[Bgf])
                yield
                for k in range(4):
                    S.op("dve", lambda e, k=k: e.scalar_tensor_tensor(out=oh, in0=lg, scalar=top8[:, k:k + 1], in1=dst, op0=ALU.is_equal, op1=ALU.mult,
                                                                      accum_out=dk[:, k:k + 1]),
                         reads=[Blg, Btop8, Bdst], writes=[Boh, Bdk])
                    S.op("dve", lambda e, k=k, i=i: e.scalar_tensor_tensor(out=oh, in0=lg, scalar=top8[:, k:k + 1], in1=gf, op0=ALU.is_equal, op1=ALU.mult,
                                                                           accum_out=K.gate_a[:, i * 4 + k:i * 4 + k + 1]),
                         reads=[Blg, Btop8, Bgf], writes=[Boh, K.Bgate])
                S.op("dve", lambda e, i=i: e.tensor_copy(out=K.dest_i[:, i * 4:(i + 1) * 4], in_=dk), reads=[Bdk], writes=[K.Bdest])
                for k in range(4):
                    S.dma("pool", lambda e, k=k, i=i: e.indirect_dma_start(
                        out=K.xs, out_offset=bass.IndirectOffsetOnAxis(ap=K.dest_i[:, i * 4 + k:i * 4 + k + 1], axis=0),
                        in_=ub, in_offset=None), reads=[Bub, K.Bdest], owner=Bub)
                yield
        return gen()

    interleave([tile_stream(q) for q in range(4)])


def phase4_experts(K):
    S, A, nc = K.S, K.A, K.nc
    ps, Bps = K.ps, K.Bps
    wgu = [A.alloc(f"wgu{i}", [8, 2 * F], BF16) for i in range(2)]
    wdn = [A.alloc(f"wdn{i}", [8, D], BF16) for i in range(2)]
    bdn = [A.alloc(f"bdn{i}", [D], BF16) for i in range(2)]
    xTs = [A.alloc(f"xT{i}", [8, CAP], BF16) for i in range(2)]
    resTs = [A.alloc(f"resT{i}", [8, CAP], BF16) for i in range(2)]
    xst = [A.alloc(f"xst{i}", [D], BF16) for i in range(3)]
    yst = [A.alloc(f"yst{i}", [D]) for i in range(2)]
    HW = CAP // 2
    gt = [A.alloc(f"gt{i}", [HW]) for i in range(2)]
    sg = [A.alloc(f"sg{i}", [HW]) for i in range(2)]
    ut = [A.alloc(f"ut{i}", [HW]) for i in range(2)]
    cnts = {"y": 0, "x": 0, "u": 0}
    S.op("dve", lambda e: e.memset(yst[1][0], 0.0), writes=[yst[1][1]])
    S.dma("sp", lambda e: e.dma_start(out=K.ys[NSLOT:NSLOT + 128, :], in_=yst[1][0]), reads=[yst[1][1]], owner=yst[1][1])

    def load_expert(e_):
        w, Bw = wgu[e_ % 2]
        wd, Bwd = wdn[e_ % 2]
        bd, Bbd = bdn[e_ % 2]
        src = K.w_gu[e_].rearrange("(c p) n -> p c n", p=128)
        for hh in range(2):
            S.dma("pool", lambda e, w=w, src=src, hh=hh: e.dma_start(out=w[:, hh * 4:(hh + 1) * 4, :], in_=src[:, hh * 4:(hh + 1) * 4, :]), writes=[Bw])
        S.dma("pool", lambda e, wd=wd, e_=e_: e.dma_start(out=wd, in_=K.w_dn[e_].rearrange("(c p) n -> p c n", p=128)), writes=[Bwd])
        S.dma("pool", lambda e, bd=bd, e_=e_: e.dma_start(out=bd[0:1, :], in_=K.b_dn[e_:e_ + 1, :]), writes=[Bbd])

    def build_xT(e_):
        xT, BxT = xTs[e_ % 2]
        for sb in range(NSB):
            xs_t, Bxs = xst[cnts["x"] % 3]
            cnts["x"] += 1
            r0 = e_ * CAP + sb * 128
            S.dma("sp", lambda e, xs_t=xs_t, r0=r0: e.dma_start(out=xs_t, in_=K.xs[r0:r0 + 128, :]), writes=[Bxs])
            bank = sb % 2
            pv16 = ps[bank][:].bitcast(BF16)
            for c in range(8):
                S.op("pe", lambda e, c=c, pv16=pv16, xs_t=xs_t: e.transpose(pv16[:, c * 128:(c + 1) * 128], xs_t[:, c * 128:(c + 1) * 128], K.ident_b),
                     reads=[Bxs, K.Bident_b], writes=[Bps[bank]], signal=(c == 7))
            S.op("act", lambda e, pv16=pv16, sb=sb, xT=xT: e.activation(out=xT[:, :, sb * 128:(sb + 1) * 128], in_=pv16.rearrange("p (c t) -> p c t", c=8), func=AF.Copy),
                 reads=[Bps[bank]], writes=[BxT])

    def gate_up(e_):
        w, Bw = wgu[e_ % 2]
        xT, BxT = xTs[e_ % 2]
        resT, BresT = resTs[e_ % 2]
        for f in range(8):
            bgc = K.pvt[:, PV_BGU + e_ * 16 + f:PV_BGU + e_ * 16 + f + 1]
            buc = K.pvt[:, PV_BGU + e_ * 16 + 8 + f:PV_BGU + e_ * 16 + 8 + f + 1]
            for hv in range(2):
                nsl = slice(hv * HW, (hv + 1) * HW)
                cnt = cnts["u"]
                cnts["u"] += 1
                gb, ub_ = 2 + cnt % 2, 4 + cnt % 2
                g_t, Bg = gt[cnt % 2]
                s_t, Bs = sg[cnt % 2]
                u_t, Bu = ut[cnt % 2]
                mm_group(S, ps[gb][:, 0:HW], Bps[gb], [(w[:, k, f * 128:(f + 1) * 128], xT[:, k, nsl]) for k in range(8)], reads=[Bw, BxT])
                mm_group(S, ps[ub_][:, 0:HW], Bps[ub_], [(w[:, k, F + f * 128:F + (f + 1) * 128], xT[:, k, nsl]) for k in range(8)], reads=[Bw, BxT])
                S.op("dve", lambda e, gb=gb, g_t=g_t, bgc=bgc: e.tensor_scalar(out=g_t, in0=ps[gb][:, 0:HW], scalar1=bgc, scalar2=7.0, op0=ALU.add, op1=ALU.min),
                     reads=[Bps[gb], K.Bpv], writes=[Bg])
                S.op("act", lambda e, ub_=ub_, u_t=u_t, buc=buc: e.activation(out=u_t, in_=ps[ub_][:, 0:HW], func=AF.Identity, bias=buc),
                     reads=[Bps[ub_], K.Bpv], writes=[Bu])
                S.op("act", lambda e, g_t=g_t, s_t=s_t: e.activation(out=s_t, in_=g_t, func=AF.Sigmoid, scale=1.702), reads=[Bg], writes=[Bs])
                S.op("dve", lambda e, u_t=u_t: e.tensor_scalar(out=u_t, in0=u_t, scalar1=7.0, scalar2=-7.0, op0=ALU.min, op1=ALU.max),
                     reads=[Bu], writes=[Bu])
                S.op("dve", lambda e, g_t=g_t, s_t=s_t: e.tensor_tensor(out=g_t, in0=g_t, in1=s_t, op=ALU.mult), reads=[Bg, Bs], writes=[Bg])
                S.op("dve", lambda e, g_t=g_t, u_t=u_t, f=f, nsl=nsl, resT=resT: e.scalar_tensor_tensor(
                    out=resT[:, f, nsl], in0=u_t, scalar=1.0, in1=g_t, op0=ALU.add, op1=ALU.mult),
                    reads=[Bg, Bu], writes=[BresT])

    def down(e_):
        wd, Bwd = wdn[e_ % 2]
        bd, Bbd = bdn[e_ % 2]
        resT, BresT = resTs[e_ % 2]
        for sb in range(NSB):
            y_t, By = yst[cnts["y"] % 2]
            cnts["y"] += 1
            for nh in range(2):
                bank = 6 + nh
                pairs = [(resT[:, k, sb * 128:(sb + 1) * 128], wd[:, k, nh * 512:(nh + 1) * 512]) for k in range(8)]
                pairs.append((K.ones_b[0:1, :], bd[0:1, nh * 512:(nh + 1) * 512]))
                mm_group(S, ps[bank][:], Bps[bank], pairs, reads=[BresT, Bwd, Bbd, K.Bones_b])
                S.op("act", lambda e, bank=bank, y_t=y_t, nh=nh: e.activation(out=y_t[:, nh * 512:(nh + 1) * 512], in_=ps[bank][:], func=AF.Copy),
                     reads=[Bps[bank]], writes=[By])
            r0 = e_ * CAP + sb * 128
            S.dma("sp", lambda e, y_t=y_t, r0=r0: e.dma_start(out=K.ys[r0:r0 + 128, :], in_=y_t), reads=[By], owner=By)

    load_expert(0)
    build_xT(0)
    for e_ in range(E):
        if e_ + 1 < E:
            load_expert(e_ + 1)
        gate_up(e_)
        if e_ + 1 < E:
            build_xT(e_ + 1)
        down(e_)


def phase5_combine(K):
    S, A, nc = K.S, K.A, K.nc
    ps, Bps = K.ps, K.Bps
    wpg, Bwpg = A.alloc("wpg", [8, D], BF16)
    wpp, Bwpp = A.alloc("wpp", [2, D], BF16)
    gple, Bgple = A.alloc("gple", [D])
    S.dma("pool", lambda e: e.dma_start(out=wpg, in_=K.w_ple_gate.rearrange("(c p) n -> p c n", p=128)), writes=[Bwpg])
    S.dma("pool", lambda e: e.dma_start(out=wpp, in_=K.w_ple_proj.rearrange("(c p) n -> p c n", p=128)), writes=[Bwpp])
    S.dma("sp", lambda e: e.dma_start(out=gple, in_=bcast_row(K.g_ple, D)), writes=[Bgple])

    def tile_stream(par):
        h_t, Bh = A.alloc(f"ht{par}", [D])
        yg = [A.alloc(f"yg{par}_{k}", [D]) for k in range(4)]
        p_t, Bp = A.alloc(f"pt{par}", [PLE])
        ptb, Bptb = A.alloc(f"ptb{par}", [PLE], BF16)
        pT, BpT = A.alloc(f"pT{par}", [2, 128], BF16)
        junk, Bjunk = A.alloc(f"junk{par}", [D], BF16)
        ss_t, Bss = A.alloc(f"ss{par}", [1])
        u3, Bu3 = A.alloc(f"u3{par}", [D], BF16)
        u3T, Bu3T = A.alloc(f"u3T{par}", [8, 128], BF16)
        sgm, Bsgm = A.alloc(f"sgm{par}", [D])
        o_t, Bo = A.alloc(f"ot{par}", [D])
        b0 = par * 2

        def gen():
            for i in range(par, NTILE, 4):
                r0 = i * 128
                S.dma("sp", lambda e, r0=r0: e.dma_start(out=h_t, in_=K.h1_s[r0:r0 + 128, :]), writes=[Bh])
                S.dma("sp", lambda e, r0=r0: e.dma_start(out=p_t, in_=K.p[r0:r0 + 128, :]), writes=[Bp])
                for k in range(4):
                    y_t, By = yg[k]
                    S.dma("pool", lambda e, y_t=y_t, i=i, k=k: e.indirect_dma_start(
                        out=y_t, out_offset=None, in_=K.ys,
                        in_offset=bass.IndirectOffsetOnAxis(ap=K.dest_i[:, i * 4 + k:i * 4 + k + 1], axis=0)),
                        reads=[K.Bdest], writes=[By])
                yield
                S.op("act", lambda e: e.activation(out=ptb, in_=p_t, func=AF.Copy), reads=[Bp], writes=[Bptb])
                pv1 = ps[b0 + 1][:].bitcast(BF16)
                for c in range(2):
                    S.op("pe", lambda e, c=c, pv1=pv1: e.transpose(pv1[:, c * 128:(c + 1) * 128], ptb[:, c * 128:(c + 1) * 128], K.ident_b),
                         reads=[Bptb, K.Bident_b], writes=[Bps[b0 + 1]], signal=(c == 1))
                S.op("act", lambda e, pv1=pv1: e.activation(out=pT, in_=pv1[:, 0:256].rearrange("p (c t) -> p c t", c=2), func=AF.Copy),
                     reads=[Bps[b0 + 1]], writes=[BpT])
                yield
                for k in range(4):
                    y_t, By = yg[k]
                    S.op("dve", lambda e, y_t=y_t, i=i, k=k: e.scalar_tensor_tensor(
                        out=h_t, in0=y_t, scalar=K.gate_a[:, i * 4 + k:i * 4 + k + 1], in1=h_t, op0=ALU.mult, op1=ALU.add),
                        reads=[By, Bh, K.Bgate], writes=[Bh])
                yield
                rms_rstd(K, h_t, Bh, junk, Bjunk, ss_t, Bss, D)
                S.op("dve", lambda e: e.scalar_tensor_tensor(out=u3, in0=h_t, scalar=ss_t, in1=gple, op0=ALU.mult, op1=ALU.mult),
                     reads=[Bh, Bss, Bgple], writes=[Bu3])
                yield
                pv16 = ps[b0][:].bitcast(BF16)
                for c in range(8):
                    S.op("pe", lambda e, c=c, pv16=pv16: e.transpose(pv16[:, c * 128:(c + 1) * 128], u3[:, c * 128:(c + 1) * 128], K.ident_b),
                         reads=[Bu3, K.Bident_b], writes=[Bps[b0]], signal=(c == 7))
                S.op("act", lambda e, pv16=pv16: e.activation(out=u3T, in_=pv16.rearrange("p (c t) -> p c t", c=8), func=AF.Copy),
                     reads=[Bps[b0]], writes=[Bu3T])
                yield
                for nh in range(2):
                    nsl = slice(nh * 512, (nh + 1) * 512)
                    gbk, pbk = b0, b0 + 1
                    mm_group(S, ps[gbk][:], Bps[gbk], [(u3T[:, k, :], wpg[:, k, nsl]) for k in range(8)], reads=[Bu3T, Bwpg])
                    mm_group(S, ps[pbk][:], Bps[pbk], [(pT[:, k, :], wpp[:, k, nsl]) for k in range(2)], reads=[BpT, Bwpp])
                    S.op("act", lambda e, gbk=gbk, nsl=nsl: e.activation(out=sgm[:, nsl], in_=ps[gbk][:], func=AF.Sigmoid), reads=[Bps[gbk]], writes=[Bsgm])
                    S.op("dve", lambda e, pbk=pbk, nsl=nsl: e.tensor_tensor(out=sgm[:, nsl], in0=sgm[:, nsl], in1=ps[pbk][:], op=ALU.mult),
                         reads=[Bsgm, Bps[pbk]], writes=[Bsgm])
                    S.op("dve", lambda e, nsl=nsl: e.tensor_tensor(out=o_t[:, nsl], in0=sgm[:, nsl], in1=h_t[:, nsl], op=ALU.add),
                         reads=[Bsgm, Bh], writes=[Bo])
                    yield
                S.dma("sp", lambda e, r0=r0: e.dma_start(out=K.y[r0:r0 + 128, :], in_=o_t), reads=[Bo], owner=Bo)
        return gen()

    interleave([tile_stream(q) for q in range(4)])


def _rope_tables():
    pos = np.arange(SEQ)
    row = (pos // 64).astype(np.float32)
    col = (pos % 64).astype(np.float32)
    inv = (10000.0 ** (-np.arange(0, 64, 2, dtype=np.float32) / 64.0)).astype(np.float32)
    C = np.zeros((128, SEQ), np.float32)
    Sg = np.zeros((128, SEQ), np.float32)
    for p in range(128):
        ids = row if p < 64 else col
        j = p % 32
        ang = (ids * inv[j]).astype(np.float32)
        C[p] = np.cos(ang)
        sgn = -1.0 if (p % 64) < 32 else 1.0
        Sg[p] = sgn * np.sin(ang)
    perm = np.zeros((128, 128), np.float32)
    for m in range(128):
        partner = m + 32 if (m % 64) < 32 else m - 32
        perm[partner, m] = 1.0
    return C, Sg, perm


_NC_CACHE = {}


def _prep_common(inp):
    f = lambda a: np.ascontiguousarray(np.asarray(a, dtype=np.float32))
    pv = np.zeros((128, NPV), np.float32)
    cw = f(inp["conv_w"])[0]
    pv[:, PV_CONVW:PV_CONVW + 32] = cw.reshape(4, 8, 128).transpose(2, 1, 0).reshape(128, 32)
    pv[:, PV_CONVB:PV_CONVB + 8] = f(inp["conv_b"])[0].reshape(8, 128).T
    pv[:, PV_BA:PV_BA + 16] = f(inp["lru_ba"])[0].reshape(16, 128).T
    pv[:, PV_BI:PV_BI + 16] = f(inp["lru_bi"])[0].reshape(16, 128).T
    pv[:, PV_LAM:PV_LAM + 16] = f(inp["lru_lam"])[0].reshape(16, 128).T
    pv[:, PV_QN] = f(inp["q_norm"])[0]
    pv[:, PV_KN] = f(inp["k_norm"])[0]
    pv[:, PV_BGU:PV_BGU + 512] = f(inp["b_gu"])[0].reshape(E * 16, 128).T
    C, Sg, perm = _rope_tables()
    tri = np.triu(np.ones((128, 128), np.float32), 1)
    eC = np.tile((np.arange(E, dtype=np.float32) * CAP)[None, :], (128, 1))
    com = {
        "w_in": f(inp["w_in"])[0], "lru_wa": f(inp["lru_wa"])[0], "lru_wi": f(inp["lru_wi"])[0],
        "w_attn_br": f(inp["w_attn_br"])[0], "w_lru_br": f(inp["w_lru_br"])[0], "w_out": f(inp["w_out"])[0],
        "w_router": f(inp["w_router"])[0], "w_gu": f(inp["w_gu"])[0], "w_dn": f(inp["w_dn"])[0],
        "b_dn": f(inp["b_dn"])[0], "w_ple_gate": f(inp["w_ple_gate"])[0], "w_ple_proj": f(inp["w_ple_proj"])[0],
        "g_mix": f(inp["g_mix"]), "g_moe": f(inp["g_moe"]), "g_ple": f(inp["g_ple"]), "b_router": f(inp["b_router"]),
        "pv": pv, "ropeC": C, "ropeS": Sg, "perm": perm, "ident": np.eye(128, dtype=np.float32), "tri": tri, "eC": eC,
    }
    return com


def kernel(**inputs):
    dbg = bool(int(os.environ.get("MK_DBG", "0")))
    ncores = int(os.environ.get("MK_NCORES", str(NCORES)))
    key = dbg
    if key not in _NC_CACHE:
        _NC_CACHE[key] = build_program(dbg)
    nc = _NC_CACHE[key]
    com = _prep_common(inputs)
    x = np.asarray(inputs["x"], dtype=np.float32)
    p = np.asarray(inputs["p"], dtype=np.float32)[0]
    in_maps = []
    for c in range(ncores):
        m = dict(com)
        m["x"] = np.ascontiguousarray(x[2 * c:2 * c + 2].reshape(T, D))
        m["p"] = np.ascontiguousarray(p[2 * c:2 * c + 2].reshape(T, PLE))
        in_maps.append(m)
    res = run_bass_kernel_spmd(nc, in_maps, core_ids=list(range(ncores)))
    if dbg:
        kernel.last = res
    out = np.zeros((16, SEQ, D), np.float32)
    for c in range(ncores):
        out[2 * c:2 * c + 2] = np.asarray(res.results[c]["y"], dtype=np.float32).reshape(2, SEQ, D)
    return out
```

```python
import os
import numpy as np
from contextlib import ExitStack
import concourse.bass as bass
import concourse.mybir as mybir
from concourse.bass_utils import run_bass_kernel_spmd

F32 = mybir.dt.float32
BF16 = mybir.dt.bfloat16
I32 = mybir.dt.int32
AF = mybir.ActivationFunctionType
ALU = mybir.AluOpType
AX = mybir.AxisListType

NCORES = 8
D = 1024
SEQ = 2048
NSEQ = 2
T = NSEQ * SEQ
NTILE = T // 128
E = 32
F = 1024
CAP = 640
NSB = CAP // 128
NSLOT = E * CAP
TRASH = NSLOT
PLE = 256
EPS = 1e-6
INW = 5632
NPV = 608
PV_CONVW, PV_CONVB, PV_BA, PV_BI, PV_LAM, PV_QN, PV_KN, PV_BGU = 0, 32, 40, 56, 72, 88, 89, 96

ENGS = ("pe", "act", "dve", "pool", "sp")


class Buf:
    __slots__ = ("name", "last_write", "reads", "sem", "sem_total", "excl")

    def __init__(self, name, excl=False):
        self.name = name
        self.excl = excl
        self.last_write = None
        self.reads = []
        self.sem = None
        self.sem_total = 0


class Sched:
    def __init__(self, nc, stack):
        self.nc = nc
        self.stack = stack
        self.stream = {e: [] for e in ENGS}
        self.sem = {e: stack.enter_context(nc.semaphore("s_" + e)) for e in ENGS}
        self.count = {e: 0 for e in ENGS}
        self.seen = {e: {} for e in ENGS}
        self.dma_bufs = []

    def _wait_tokens(self, e, toks):
        need = {}
        for t in toks:
            if t is None:
                continue
            if t[0] == "e":
                _, src, c = t
                if src == "pe" and e == "pe":
                    continue
                key = ("e", src)
                val = c
                sem = self.sem[src]
            else:
                b = t[1]
                key = ("d", id(b))
                val = b.sem_total
                sem = b.sem
            if self.seen[e].get(key, 0) >= val:
                continue
            if key not in need or need[key][1] < val:
                need[key] = (sem, val)
        for key, (sem, val) in need.items():
            self.seen[e][key] = val
            self.stream[e].append(lambda eng, sem=sem, val=val: eng.wait_ge(sem, val))

    @staticmethod
    def _deps(reads, writes):
        toks = []
        for r in reads:
            toks.append(r.last_write)
            if r.excl:
                toks.extend(r.reads)
        for w in writes:
            toks.append(w.last_write)
            toks.extend(w.reads)
        return toks

    def op(self, e, fn, reads=(), writes=(), signal=True):
        self._wait_tokens(e, self._deps(reads, writes))
        if signal:
            self.count[e] += 1
            tok = ("e", e, self.count[e])
            sem = self.sem[e]
            self.stream[e].append(lambda eng, fn=fn, sem=sem: fn(eng).then_inc(sem, 1))
        else:
            tok = ("e", e, self.count[e] + 1)
            self.stream[e].append(lambda eng, fn=fn: fn(eng))
        for w in writes:
            w.last_write = tok
            w.reads = []
        for r in reads:
            r.reads.append(tok)
        return tok

    def dma(self, e, fn, reads=(), writes=(), owner=None):
        if owner is None:
            owner = writes[0] if writes else reads[0]
        if owner.sem is None:
            owner.sem = self.stack.enter_context(self.nc.semaphore("d%d_%s" % (len(self.dma_bufs), owner.name)))
            self.dma_bufs.append(owner)
        self._wait_tokens(e, self._deps(reads, writes))
        owner.sem_total += 16
        sem = owner.sem
        self.stream[e].append(lambda eng, fn=fn, sem=sem: fn(eng).then_inc(sem, 16))
        tok = ("d", owner)
        for w in writes:
            w.last_write = tok
            w.reads = []
        for r in reads:
            r.reads.append(tok)
        return tok

    def barrier(self):
        toks = [("e", s, self.count[s]) for s in ENGS if self.count[s] > 0]
        toks += [("d", b) for b in self.dma_bufs]
        for e in ENGS:
            self._wait_tokens(e, toks)

    def emit(self):
        nc = self.nc
        self.barrier()
        with nc.Block() as block:
            for e, reg in (("sp", block.sync), ("act", block.scalar), ("pe", block.tensor),
                           ("dve", block.vector), ("pool", block.gpsimd)):
                lst = self.stream[e]
                if not lst:
                    continue

                def body(eng, lst=lst):
                    for f in lst:
                        f(eng)
                reg(body)


def _dsize(dt):
    return 2 if dt == BF16 else 4


class Arena:
    def __init__(self, nc, stack, nbytes):
        self.t = stack.enter_context(nc.sbuf_tensor("arena", [128, nbytes // 4], F32))
        self.off = 0
        self.nbytes = nbytes
        self.peak = 0

    def alloc(self, name, free, dt=F32):
        n = 1
        for f in free:
            n *= f
        sz = (n * _dsize(dt) + 31) // 32 * 32
        assert self.off + sz <= self.nbytes, (name, self.off, sz, self.nbytes)
        a = self.t[:, self.off // 4:(self.off + sz) // 4]
        if dt != F32:
            a = a.bitcast(dt)
        a = a[:, 0:n]
        if len(free) == 2:
            a = a.rearrange("p (a b) -> p a b", a=free[0])
        self.off += sz
        self.peak = max(self.peak, self.off)
        return a, Buf(name)

    def mark(self):
        return self.off

    def release(self, m):
        self.off = m


class Ctx:
    pass


def build_program(dbg=False):
    nc = bass.Bass("TRN2", target_bir_lowering=False)
    K = Ctx()
    K.nc = nc

    def din(name, shape, dt=F32):
        return nc.dram_tensor(name, list(shape), dt, kind="ExternalInput")

    K.x = din("x", [T, D]).ap()
    K.p = din("p", [T, PLE]).ap()
    K.w_in = din("w_in", [D, INW]).ap()
    K.lru_wa = din("lru_wa", [2, 8, 128, 128]).ap()
    K.lru_wi = din("lru_wi", [2, 8, 128, 128]).ap()
    K.w_attn_br = din("w_attn_br", [D, D]).ap()
    K.w_lru_br = din("w_lru_br", [D, D]).ap()
    K.w_out = din("w_out", [D, D]).ap()
    K.w_router = din("w_router", [D, E]).ap()
    K.w_gu = din("w_gu", [E, D, 2 * F]).ap()
    K.w_dn = din("w_dn", [E, F, D]).ap()
    K.b_dn = din("b_dn", [E, D]).ap()
    K.w_ple_gate = din("w_ple_gate", [D, D]).ap()
    K.w_ple_proj = din("w_ple_proj", [PLE, D]).ap()
    K.g_mix = din("g_mix", [1, D])
    K.g_moe = din("g_moe", [1, D])
    K.g_ple = din("g_ple", [1, D])
    K.b_router = din("b_router", [1, E])
    K.pv = din("pv", [128, NPV]).ap()
    K.ropeC = din("ropeC", [128, SEQ]).ap()
    K.ropeS = din("ropeS", [128, SEQ]).ap()
    K.perm = din("perm", [128, 128]).ap()
    K.ident = din("ident", [128, 128]).ap()
    K.tri = din("tri", [128, 128]).ap()
    K.eC = din("eC", [128, E]).ap()
    K.y = nc.dram_tensor("y", [T, D], F32, kind="ExternalOutput").ap()

    kind = "ExternalOutput" if dbg else "Internal"

    def dscr(name, shape, dt):
        if dbg:
            return nc.dram_tensor(name, list(shape), dt, kind="ExternalOutput").ap()
        return nc.dram_tensor(name, list(shape), dt).ap()

    K.qT_s = dscr("qT_s", [NSEQ, 8, 128, SEQ], BF16)
    K.kT_s = dscr("kT_s", [NSEQ, 2, 128, SEQ], BF16)
    K.V_s = dscr("V_s", [NSEQ, 128, 16, 256], BF16)
    K.yl_s = dscr("yl_s", [NSEQ, 8, 128, SEQ], BF16)
    K.sga_s = dscr("sga_s", [NSEQ, 8, 128, SEQ], BF16)
    K.sgr_s = dscr("sgr_s", [NSEQ, 8, 128, SEQ], BF16)
    K.h1_s = dscr("h1_s", [T, D], F32)
    K.xs = dscr("xs_s", [NSLOT + 128, D], BF16)
    K.ys = dscr("ys_s", [NSLOT + 128, D], F32)

    with ExitStack() as st:
        S = Sched(nc, st)
        K.S = S
        A = Arena(nc, st, 196 * 1024)
        K.A = A
        K.ps = []
        K.Bps = []
        for i in range(8):
            K.ps.append(st.enter_context(nc.psum_tensor(f"ps{i}", [128, 512], F32)))
            K.Bps.append(Buf(f"ps{i}", excl=True))
        stop = int(os.environ.get("MK_STOP", "9"))
        phase0_consts(K)
        S.barrier()
        m0 = A.mark()
        if stop >= 1:
            phase1_inproj(K)
            S.barrier()
        A.release(m0)
        if stop >= 2:
            phase2_attn(K)
            S.barrier()
        A.release(m0)
        if stop >= 3:
            phase3_router(K)
            S.barrier()
        A.release(m0)
        if stop >= 4:
            phase4_experts(K)
            S.barrier()
        A.release(m0)
        if stop >= 5:
            phase5_combine(K)
        S.emit()
    return nc


def bcast_row(dt_tensor, n):
    return bass.AP(dt_tensor, 0, [[0, 128], [1, n]])


def phase0_consts(K):
    S, A = K.S, K.A
    K.ident_f, K.Bident_f = A.alloc("ident_f", [128])
    K.ident_b, K.Bident_b = A.alloc("ident_b", [128], BF16)
    K.ones_b, K.Bones_b = A.alloc("ones_b", [128], BF16)
    K.ones_f, K.Bones_f = A.alloc("ones_f", [128])
    K.pvt, K.Bpv = A.alloc("pvt", [NPV])
    K.kk, K.Bkk = A.alloc("kk", [16])
    K.dest_i, K.Bdest = A.alloc("dest_i", [NTILE * 4], I32)
    K.gate_a, K.Bgate = A.alloc("gate_a", [NTILE * 4])
    S.dma("sp", lambda e: e.dma_start(out=K.ident_f, in_=K.ident), writes=[K.Bident_f])
    S.dma("pool", lambda e: e.dma_start(out=K.ident_b, in_=K.ident), writes=[K.Bident_b])
    S.dma("sp", lambda e: e.dma_start(out=K.pvt, in_=K.pv), writes=[K.Bpv])
    S.op("dve", lambda e: e.memset(K.ones_b, 1.0), writes=[K.Bones_b])
    S.op("dve", lambda e: e.memset(K.ones_f, 1.0), writes=[K.Bones_f])
    lam = K.pvt[:, PV_LAM:PV_LAM + 16]
    S.op("act", lambda e: e.activation(out=K.kk, in_=lam, func=AF.Exp, scale=-1.0), reads=[K.Bpv], writes=[K.Bkk])
    S.op("act", lambda e: e.activation(out=K.kk, in_=K.kk, func=AF.Ln, bias=1.0), reads=[K.Bkk], writes=[K.Bkk])
    S.op("dve", lambda e: e.tensor_scalar(out=K.kk, in0=K.kk, scalar1=-8.0, scalar2=None, op0=ALU.mult),
         reads=[K.Bkk], writes=[K.Bkk])


def mm_group(S, out, Bout, pairs, reads):
    n = len(pairs)
    for i, (l, r) in enumerate(pairs):
        S.op("pe", lambda e, l=l, r=r, i=i: e.matmul(out, l, r, start=(i == 0), stop=(i == n - 1)),
             reads=reads, writes=[Bout], signal=(i == n - 1))


def rms_rstd(K, src, Bsrc, junk, Bjunk, ss, Bss, n):
    S = K.S
    S.op("act", lambda e: e.activation(out=junk, in_=src, func=AF.Square, accum_out=ss),
         reads=[Bsrc], writes=[Bjunk, Bss])
    S.op("act", lambda e: e.activation(out=ss, in_=ss, func=AF.Sqrt, bias=EPS, scale=1.0 / n),
         reads=[Bss], writes=[Bss])
    S.op("dve", lambda e: e.reciprocal(out=ss, in_=ss), reads=[Bss], writes=[Bss])


def interleave(gens, skew=0):
    gens = list(gens)
    start = {id(g): i * skew for i, g in enumerate(gens)}
    rnd = 0
    while gens:
        for g in list(gens):
            if rnd < start[id(g)]:
                continue
            try:
                next(g)
            except StopIteration:
                gens.remove(g)
        rnd += 1


def phase1_inproj(K):
    S, A, nc = K.S, K.A, K.nc
    ps, Bps = K.ps, K.Bps
    uT, BuT = A.alloc("uT", [8, SEQ], BF16)
    wtA = [A.alloc(f"wtA{i}", [8, 512], BF16) for i in range(2)]
    wtB = [A.alloc(f"wtB{i}", [8, 256], BF16) for i in range(2)]
    gmix, Bgmix = A.alloc("gmix", [D])
    ropeS, BropeS = A.alloc("ropeS", [SEQ])
    cq, Bcq = A.alloc("cq", [SEQ])
    ck, Bck = A.alloc("ck", [SEQ])
    permf, Bpermf = A.alloc("permf", [128])
    permq, Bpermq = A.alloc("permq", [128], BF16)
    permk, Bpermk = A.alloc("permk", [128], BF16)
    od_b, Bod = A.alloc("od_b", [128], BF16)
    wa_b, Bwa = A.alloc("wa_b", [16, 128], BF16)
    wi_b, Bwi = A.alloc("wi_b", [16, 128], BF16)
    junk, Bjunk = A.alloc("junk", [D], BF16)
    xn, Bxn = A.alloc("xn", [D], BF16)
    ss = [A.alloc(f"ss{i}", [1]) for i in range(2)]
    stgA = [A.alloc(f"stgA{i}", [SEQ], BF16) for i in range(2)]
    stgB = [A.alloc(f"stgB{i}", [SEQ], BF16) for i in range(2)]
    xq = [A.alloc(f"xq{i}", [512], BF16) for i in range(2)]
    sq = [A.alloc(f"sq{i}", [512], BF16) for i in range(2)]
    ta = [A.alloc(f"ta{i}", [512]) for i in range(2)]
    tb_ = [A.alloc(f"tb{i}", [512]) for i in range(2)]
    rst = [A.alloc(f"rst{i}", [512]) for i in range(2)]
    xrp, Bxrp = A.alloc("xrp", [SEQ + 4])
    cc, Bcc = A.alloc("cc", [SEQ])
    ccb, Bccb = A.alloc("ccb", [SEQ], BF16)
    aa, Baa = A.alloc("aa", [SEQ])
    bt, Bbt = A.alloc("bt", [SEQ])
    t1, Bt1 = A.alloc("t1", [SEQ])
    hf, Bhf = A.alloc("hf", [SEQ])
    hb, Bhb = A.alloc("hb", [SEQ])
    xt = [(hb[:, 0:D], Bhb), (hf[:, 0:D], Bhf)]
    vst, Bvst = aa.bitcast(BF16).rearrange("p (a b) -> p a b", a=16), Baa

    pvt = K.pvt
    S.dma("sp", lambda e: e.dma_start(out=gmix, in_=bcast_row(K.g_mix, D)), writes=[Bgmix])
    S.dma("sp", lambda e: e.dma_start(out=cq, in_=K.ropeC), writes=[Bcq])
    S.dma("sp", lambda e: e.dma_start(out=ck, in_=K.ropeC), writes=[Bck])
    S.dma("sp", lambda e: e.dma_start(out=ropeS, in_=K.ropeS), writes=[BropeS])
    S.dma("sp", lambda e: e.dma_start(out=permf, in_=K.perm), writes=[Bpermf])
    S.dma("pool", lambda e: e.dma_start(out=wa_b, in_=K.lru_wa.rearrange("d c p n -> p (d c) n")), writes=[Bwa])
    S.dma("pool", lambda e: e.dma_start(out=wi_b, in_=K.lru_wi.rearrange("d c p n -> p (d c) n")), writes=[Bwi])
    S.op("dve", lambda e: e.memset(od_b, 1.0 / 128.0), writes=[Bod])
    S.op("dve", lambda e: e.memset(xrp, 0.0), writes=[Bxrp])
    qn = pvt[:, PV_QN:PV_QN + 1]
    kn = pvt[:, PV_KN:PV_KN + 1]
    S.op("dve", lambda e: e.tensor_scalar(out=cq, in0=cq, scalar1=qn, scalar2=None, op0=ALU.mult),
         reads=[Bcq, K.Bpv], writes=[Bcq])
    S.op("dve", lambda e: e.tensor_scalar(out=ck, in0=ck, scalar1=kn, scalar2=None, op0=ALU.mult),
         reads=[Bck, K.Bpv], writes=[Bck])
    S.op("dve", lambda e: e.tensor_scalar(out=permq, in0=permf, scalar1=qn, scalar2=None, op0=ALU.mult),
         reads=[Bpermf, K.Bpv], writes=[Bpermq])
    S.op("dve", lambda e: e.tensor_scalar(out=permk, in0=permf, scalar1=kn, scalar2=None, op0=ALU.mult),
         reads=[Bpermf, K.Bpv], writes=[Bpermk])

    w_in_v = K.w_in.rearrange("(c p) n -> p c n", p=128)
    wcnt = {"A": 0, "B": 0}

    def load_w(which, cols):
        tiles = wtA if which == "A" else wtB
        i = wcnt[which] % 2
        wcnt[which] += 1
        w, Bw = tiles[i]
        off = 0
        for (c0, wd) in cols:
            S.dma("pool", lambda e, w=w, off=off, c0=c0, wd=wd: e.dma_start(
                out=w[:, :, off:off + wd], in_=w_in_v[:, :, c0:c0 + wd]), writes=[Bw])
            off += wd
        return w, Bw

    def inproj(w, Bw, woff, tb, bank):
        mm_group(S, ps[bank][:], Bps[bank],
                 [(w[:, k, woff:woff + 128], uT[:, k, tb * 512:(tb + 1) * 512]) for k in range(8)],
                 reads=[Bw, BuT])

    scnt = {"A": 0, "B": 0}

    def build_uT(s):
        for i in range(16):
            x_t, Bx = xt[i % 2]
            ss_t, Bss = ss[i % 2]
            r0 = s * SEQ + i * 128
            S.dma("sp", lambda e, x_t=x_t, r0=r0: e.dma_start(out=x_t, in_=K.x[r0:r0 + 128, :]), writes=[Bx])
            rms_rstd(K, x_t, Bx, junk, Bjunk, ss_t, Bss, D)
            S.op("dve", lambda e, x_t=x_t, ss_t=ss_t: e.scalar_tensor_tensor(
                out=xn, in0=x_t, scalar=ss_t, in1=gmix, op0=ALU.mult, op1=ALU.mult),
                reads=[Bx, Bss, Bgmix], writes=[Bxn])
            bank = i % 2
            pv16 = ps[bank][:].bitcast(BF16)
            for c in range(8):
                S.op("pe", lambda e, c=c, pv16=pv16: e.transpose(pv16[:, c * 128:(c + 1) * 128], xn[:, c * 128:(c + 1) * 128], K.ident_b),
                     reads=[Bxn, K.Bident_b], writes=[Bps[bank]], signal=(c == 7))
            S.op("act", lambda e, pv16=pv16, i=i: e.activation(
                out=uT[:, :, i * 128:(i + 1) * 128], in_=pv16.rearrange("p (c t) -> p c t", c=8), func=AF.Copy),
                reads=[Bps[bank]], writes=[BuT])

    def qk_head(s, w, Bw, woff, is_q, hidx):
        cg, Bcg = (cq, Bcq) if is_q else (ck, Bck)
        pm, Bpm = (permq, Bpermq) if is_q else (permk, Bpermk)
        st_t, Bst = stgA[scnt["A"] % 2]
        scnt["A"] += 1
        for tb in range(4):
            p = tb % 2
            sl = slice(tb * 512, (tb + 1) * 512)
            xq_, Bxq = xq[p]
            sq_, Bsq = sq[p]
            ta_, Bta = ta[p]
            tb2, Btb = tb_[p]
            rs_, Brst = rst[p]
            zb = p
            inproj(w, Bw, woff, tb, zb)
            S.op("act", lambda e, xq_=xq_, zb=zb: e.activation(out=xq_, in_=ps[zb][:], func=AF.Copy), reads=[Bps[zb]], writes=[Bxq])
            S.op("act", lambda e, sq_=sq_, zb=zb: e.activation(out=sq_, in_=ps[zb][:], func=AF.Square), reads=[Bps[zb]], writes=[Bsq])
            S.op("dve", lambda e, sl=sl, cg=cg, ta_=ta_, zb=zb: e.tensor_tensor(out=ta_, in0=ps[zb][:], in1=cg[:, sl], op=ALU.mult),
                 reads=[Bps[zb], Bcg], writes=[Bta])
            yield
            mm_group(S, ps[2][:], Bps[2], [(od_b, sq_)], reads=[Bod, Bsq])
            mm_group(S, ps[3][:], Bps[3], [(pm, xq_)], reads=[Bpm, Bxq])
            S.op("act", lambda e, rs_=rs_: e.activation(out=rs_, in_=ps[2][:], func=AF.Sqrt, bias=EPS), reads=[Bps[2]], writes=[Brst])
            S.op("dve", lambda e, sl=sl, tb2=tb2: e.tensor_tensor(out=tb2, in0=ps[3][:], in1=ropeS[:, sl], op=ALU.mult),
                 reads=[Bps[3], BropeS], writes=[Btb])
            yield
            S.op("dve", lambda e, rs_=rs_: e.reciprocal(out=rs_, in_=rs_), reads=[Brst], writes=[Brst])
            S.op("dve", lambda e, ta_=ta_, tb2=tb2: e.tensor_tensor(out=ta_, in0=ta_, in1=tb2, op=ALU.add), reads=[Bta, Btb], writes=[Bta])
            S.op("dve", lambda e, sl=sl, st_t=st_t, ta_=ta_, rs_=rs_: e.tensor_tensor(out=st_t[:, sl], in0=ta_, in1=rs_, op=ALU.mult),
                 reads=[Bta, Brst], writes=[Bst])
            yield
        dst = (K.qT_s if is_q else K.kT_s)[s, hidx]
        S.dma("sp", lambda e, st_t=st_t, dst=dst: e.dma_start(out=dst, in_=st_t), reads=[Bst], owner=Bst)

    def stream_A(s):
        for t2 in range(2):
            w, Bw = load_w("A", [(t2 * 512, 512)])
            for j in range(4):
                yield from qk_head(s, w, Bw, j * 128, True, t2 * 4 + j)
        w, Bw = load_w("A", [(1024, 512)])
        for j in range(2):
            yield from qk_head(s, w, Bw, j * 128, False, j)
        for gi, dst_s in ((0, K.sga_s), (1, K.sgr_s)):
            for t2 in range(2):
                wg, Bwg = load_w("A", [(3584 + gi * 1024 + t2 * 512, 512)])
                for j in range(4):
                    st_t, Bst = stgA[scnt["A"] % 2]
                    scnt["A"] += 1
                    for tb in range(4):
                        bank = tb % 2
                        inproj(wg, Bwg, j * 128, tb, bank)
                        S.op("act", lambda e, bank=bank, tb=tb, st_t=st_t: e.activation(out=st_t[:, tb * 512:(tb + 1) * 512], in_=ps[bank][:], func=AF.Sigmoid),
                             reads=[Bps[bank]], writes=[Bst])
                        yield
                    S.dma("sp", lambda e, st_t=st_t, dst=dst_s[s, t2 * 4 + j]: e.dma_start(out=dst, in_=st_t), reads=[Bst], owner=Bst)

    def do_V(s):
        w, Bw = load_w("A", [(1024, 512)])
        for tk in range(16):
            bank = 2 + tk % 2
            mm_group(S, ps[bank][:, 0:256], Bps[bank],
                     [(uT[:, k, tk * 128:(tk + 1) * 128], w[:, k, 256:512]) for k in range(8)], reads=[Bw, BuT])
            S.op("act", lambda e, bank=bank, tk=tk: e.activation(out=vst[:, tk, :], in_=ps[bank][:, 0:256], func=AF.Copy),
                 reads=[Bps[bank]], writes=[Bvst])
        S.dma("sp", lambda e, s=s: e.dma_start(out=K.V_s[s], in_=vst), reads=[Bvst], owner=Bvst)

    def stream_B(s):
        for j in range(8):
            w, Bw = load_w("B", [(1536 + j * 128, 128), (2560 + j * 128, 128)])
            for tb in range(4):
                bank = 4 + tb % 2
                inproj(w, Bw, 0, tb, bank)
                S.op("act", lambda e, bank=bank, tb=tb: e.activation(out=xrp[:, 2 + tb * 512:2 + (tb + 1) * 512], in_=ps[bank][:], func=AF.Copy),
                     reads=[Bps[bank]], writes=[Bxrp])
                yield
            cw = lambda jj, j=j: K.pvt[:, PV_CONVW + j * 4 + jj:PV_CONVW + j * 4 + jj + 1]
            cb = K.pvt[:, PV_CONVB + j:PV_CONVB + j + 1]
            S.op("act", lambda e, cw=cw, cb=cb: e.activation(out=cc, in_=xrp[:, 0:SEQ], func=AF.Identity, bias=cb, scale=cw(0)),
                 reads=[Bxrp, K.Bpv], writes=[Bcc])
            for jj in range(1, 4):
                S.op("dve", lambda e, cw=cw, jj=jj: e.scalar_tensor_tensor(out=cc, in0=xrp[:, jj:jj + SEQ], scalar=cw(jj), in1=cc, op0=ALU.mult, op1=ALU.add),
                     reads=[Bxrp, Bcc, K.Bpv], writes=[Bcc])
                yield
            S.op("act", lambda e: e.activation(out=ccb, in_=cc, func=AF.Copy), reads=[Bcc], writes=[Bccb])
            for d in range(2):
                ba = K.pvt[:, PV_BA + d * 8 + j:PV_BA + d * 8 + j + 1]
                bi = K.pvt[:, PV_BI + d * 8 + j:PV_BI + d * 8 + j + 1]
                kkc = K.kk[:, d * 8 + j:d * 8 + j + 1]
                for tb in range(4):
                    sl = slice(tb * 512, (tb + 1) * 512)
                    mm_group(S, ps[6][:], Bps[6], [(wa_b[:, d * 8 + j, :], ccb[:, sl])], reads=[Bwa, Bccb])
                    S.op("act", lambda e, sl=sl, ba=ba: e.activation(out=aa[:, sl], in_=ps[6][:], func=AF.Sigmoid, bias=ba),
                         reads=[Bps[6], K.Bpv], writes=[Baa])
                    mm_group(S, ps[7][:], Bps[7], [(wi_b[:, d * 8 + j, :], ccb[:, sl])], reads=[Bwi, Bccb])
                    S.op("act", lambda e, sl=sl, bi=bi: e.activation(out=bt[:, sl], in_=ps[7][:], func=AF.Sigmoid, bias=bi),
                         reads=[Bps[7], K.Bpv], writes=[Bbt])
                    yield
                S.op("act", lambda e, kkc=kkc: e.activation(out=aa, in_=aa, func=AF.Exp, scale=kkc), reads=[Baa, K.Bkk], writes=[Baa])
                S.op("dve", lambda e: e.tensor_tensor(out=bt, in0=bt, in1=cc, op=ALU.mult), reads=[Bbt, Bcc], writes=[Bbt])
                yield
                S.op("act", lambda e: e.activation(out=t1, in_=aa, func=AF.Square), reads=[Baa], writes=[Bt1])
                S.op("act", lambda e: e.activation(out=t1, in_=t1, func=AF.Sqrt, bias=1.0, scale=-1.0), reads=[Bt1], writes=[Bt1])
                yield
                S.op("dve", lambda e: e.tensor_tensor(out=bt, in0=bt, in1=t1, op=ALU.mult), reads=[Bbt, Bt1], writes=[Bbt])
                yield
                if d == 0:
                    S.op("dve", lambda e: e.tensor_tensor_scan(out=hf, data0=aa, data1=bt, initial=0.0, op0=ALU.mult, op1=ALU.add),
                         reads=[Baa, Bbt], writes=[Bhf])
                else:
                    S.op("dve", lambda e: e.tensor_tensor_scan(out=hb[:, ::-1], data0=aa[:, ::-1], data1=bt[:, ::-1], initial=0.0, op0=ALU.mult, op1=ALU.add),
                         reads=[Baa, Bbt], writes=[Bhb])
                yield
            S.op("dve", lambda e: e.tensor_tensor(out=hf, in0=hf, in1=hb, op=ALU.add), reads=[Bhf, Bhb], writes=[Bhf])
            st_t, Bst = stgB[scnt["B"] % 2]
            scnt["B"] += 1
            for tb in range(4):
                sl = slice(tb * 512, (tb + 1) * 512)
                bank = 4 + tb % 2
                inproj(w, Bw, 128, tb, bank)
                S.op("act", lambda e, bank=bank, sl=sl: e.activation(out=t1[:, sl], in_=ps[bank][:], func=AF.Square), reads=[Bps[bank]], writes=[Bt1])
                S.op("act", lambda e, sl=sl: e.activation(out=t1[:, sl], in_=t1[:, sl], func=AF.Identity, bias=1.0, scale=0.044715),
                     reads=[Bt1], writes=[Bt1])
                S.op("dve", lambda e, bank=bank, sl=sl: e.tensor_tensor(out=t1[:, sl], in0=t1[:, sl], in1=ps[bank][:], op=ALU.mult),
                     reads=[Bt1, Bps[bank]], writes=[Bt1])
                yield
                S.op("act", lambda e, sl=sl: e.activation(out=t1[:, sl], in_=t1[:, sl], func=AF.Sigmoid, scale=1.5957691216), reads=[Bt1], writes=[Bt1])
                S.op("dve", lambda e, bank=bank, sl=sl: e.tensor_tensor(out=t1[:, sl], in0=t1[:, sl], in1=ps[bank][:], op=ALU.mult),
                     reads=[Bt1, Bps[bank]], writes=[Bt1])
                S.op("dve", lambda e, sl=sl, st_t=st_t: e.tensor_tensor(out=st_t[:, sl], in0=t1[:, sl], in1=hf[:, sl], op=ALU.mult),
                     reads=[Bt1, Bhf], writes=[Bst])
                yield
            S.dma("sp", lambda e, st_t=st_t, j=j, s=s: e.dma_start(out=K.yl_s[s, j], in_=st_t), reads=[Bst], owner=Bst)

    for s in range(NSEQ):
        build_uT(s)
        interleave([stream_A(s), stream_B(s)])
        do_V(s)


def phase2_attn(K):
    S, A, nc = K.S, K.A, K.nc
    ps, Bps = K.ps, K.Bps
    wab, Bwab = A.alloc("wab", [8, D], BF16)
    wlb, Bwlb = A.alloc("wlb", [8, D], BF16)
    wo, Bwo = A.alloc("wo", [8, D], BF16)
    kT, BkT = A.alloc("kT", [2, SEQ], BF16)
    V, BV = A.alloc("V", [16, 256], BF16)
    qT = [A.alloc(f"qT{i}", [8, 512], BF16) for i in range(2)]
    ylb = [A.alloc(f"ylb{i}", [8, 512], BF16) for i in range(2)]
    gab = [A.alloc(f"gab{i}", [8, 512], BF16) for i in range(2)]
    grb = [A.alloc(f"grb{i}", [8, 512], BF16) for i in range(2)]
    PT = [A.alloc(f"PT{i}", [512], BF16) for i in range(6)]
    SBANK = (0, 1, 2, 7)
    attnT, BattnT = A.alloc("attnT", [8, 512], BF16)
    mrg, Bmrg = A.alloc("mrg", [8, 512], BF16)
    rz, Brz = A.alloc("rz", [512])
    zacc = [A.alloc(f"zacc{i}", [512]) for i in range(2)]
    zab = [A.alloc(f"zab{i}", [512], BF16) for i in range(2)]
    m1, Bm1 = A.alloc("m1", [512])
    m2, Bm2 = A.alloc("m2", [512])
    xt = [A.alloc(f"xt{i}", [D]) for i in range(2)]
    ho = [A.alloc(f"ho{i}", [D]) for i in range(2)]
    S.dma("pool", lambda e: e.dma_start(out=wab, in_=K.w_attn_br.rearrange("(c p) n -> p c n", p=128)), writes=[Bwab])
    S.dma("pool", lambda e: e.dma_start(out=wlb, in_=K.w_lru_br.rearrange("(c p) n -> p c n", p=128)), writes=[Bwlb])
    S.dma("pool", lambda e: e.dma_start(out=wo, in_=K.w_out.rearrange("(c p) n -> p c n", p=128)), writes=[Bwo])
    zt, Bzt = A.alloc("zt", [D], BF16)
    S.op("dve", lambda e: e.memset(zt, 0.0), writes=[Bzt])
    zrows = list(range(0, NSLOT + 128, 128))

    def zero_some(n):
        for _ in range(n):
            if zrows:
                r0 = zrows.pop(0)
                S.dma("sp", lambda e, r0=r0: e.dma_start(out=K.xs[r0:r0 + 128, :], in_=zt), reads=[Bzt], owner=Bzt)
    scale = 128.0 ** -0.5
    cnt = 0
    for s in range(NSEQ):
        S.dma("sp", lambda e, s=s: e.dma_start(out=kT, in_=K.kT_s[s].rearrange("h p t -> p h t")), writes=[BkT])
        S.dma("sp", lambda e, s=s: e.dma_start(out=V, in_=K.V_s[s]), writes=[BV])
        for qb in range(4):
            q, Bq = qT[cnt % 2]
            yl, Byl = ylb[cnt % 2]
            ga, Bga = gab[cnt % 2]
            gr, Bgr = grb[cnt % 2]
            cnt += 1
            tsl = slice(qb * 512, (qb + 1) * 512)
            S.dma("sp", lambda e, q=q, s=s, tsl=tsl: e.dma_start(out=q, in_=K.qT_s[s].rearrange("h p t -> p h t")[:, :, tsl]), writes=[Bq])
            S.dma("sp", lambda e, yl=yl, s=s, tsl=tsl: e.dma_start(out=yl, in_=K.yl_s[s].rearrange("h p t -> p h t")[:, :, tsl]), writes=[Byl])
            S.dma("sp", lambda e, ga=ga, s=s, tsl=tsl: e.dma_start(out=ga, in_=K.sga_s[s].rearrange("h p t -> p h t")[:, :, tsl]), writes=[Bga])
            S.dma("sp", lambda e, gr=gr, s=s, tsl=tsl: e.dma_start(out=gr, in_=K.sgr_s[s].rearrange("h p t -> p h t")[:, :, tsl]), writes=[Bgr])
            for h in range(8):
                kv = h // 4
                ob, zb = 3 + h % 2, 5 + h % 2

                def score(kc):
                    bank = SBANK[kc % 4]
                    mm_group(S, ps[bank][:], Bps[bank], [(kT[:, kv, kc * 128:(kc + 1) * 128], q[:, h, :])], reads=[BkT, Bq])
                    pt, Bpt = PT[kc % 6]
                    S.op("act", lambda e, bank=bank, pt=pt: e.activation(out=pt, in_=ps[bank][:], func=AF.Exp, scale=scale),
                         reads=[Bps[bank]], writes=[Bpt])

                za, Bza = zacc[h % 2]
                zb16, Bzb16 = zab[h % 2]

                def pv(kc):
                    pt, Bpt = PT[kc % 6]
                    S.op("pe", lambda e, kc=kc, pt=pt, ob=ob, kv=kv: e.matmul(ps[ob][:], V[:, kc, kv * 128:(kv + 1) * 128], pt, start=(kc == 0), stop=(kc == 15)),
                         reads=[BV, Bpt], writes=[Bps[ob]], signal=(kc == 15))
                    if kc % 2 == 1:
                        S.op("pe", lambda e, kc=kc, pt=pt, zb=zb: e.matmul(ps[zb][:], K.ones_b, pt, start=(kc == 1), stop=False),
                             reads=[K.Bones_b, Bpt], writes=[Bps[zb]], signal=False)
                    elif kc == 0:
                        S.op("dve", lambda e, pt=pt, za=za: e.tensor_copy(out=za, in_=pt), reads=[Bpt], writes=[Bza])
                    else:
                        S.op("dve", lambda e, pt=pt, za=za: e.tensor_tensor(out=za, in0=za, in1=pt, op=ALU.add), reads=[Bpt, Bza], writes=[Bza])
                    if kc == 15:
                        S.op("pe", lambda e, za=za, zb=zb: e.matmul(ps[zb][:], K.ones_f, za, start=False, stop=True),
                             reads=[K.Bones_f, Bza], writes=[Bps[zb]], signal=True)

                score(0)
                score(1)
                score(2)
                for kc in range(16):
                    if kc + 3 < 16:
                        score(kc + 3)
                    pv(kc)
                S.op("dve", lambda e, zb=zb: e.reciprocal(out=rz, in_=ps[zb][:]), reads=[Bps[zb]], writes=[Brz])
                S.op("dve", lambda e, ob=ob, h=h: e.tensor_tensor(out=attnT[:, h, :], in0=ps[ob][:], in1=rz, op=ALU.mult),
                     reads=[Bps[ob], Brz], writes=[BattnT])
                zero_some(6)
            for m in range(8):
                b1, b2 = 0 + m % 2, 2 + m % 2
                mm_group(S, ps[b1][:], Bps[b1], [(wab[:, k, m * 128:(m + 1) * 128], attnT[:, k, :]) for k in range(8)], reads=[Bwab, BattnT])
                mm_group(S, ps[b2][:], Bps[b2], [(wlb[:, k, m * 128:(m + 1) * 128], yl[:, k, :]) for k in range(8)], reads=[Bwlb, Byl])
                S.op("dve", lambda e, b1=b1, m=m, ga=ga: e.tensor_tensor(out=m1, in0=ps[b1][:], in1=ga[:, m, :], op=ALU.mult),
                     reads=[Bps[b1], Bga], writes=[Bm1])
                S.op("dve", lambda e, b2=b2, m=m, gr=gr: e.tensor_tensor(out=m2, in0=ps[b2][:], in1=gr[:, m, :], op=ALU.mult),
                     reads=[Bps[b2], Bgr], writes=[Bm2])
                S.op("dve", lambda e, m=m: e.tensor_tensor(out=mrg[:, m, :], in0=m1, in1=m2, op=ALU.add),
                     reads=[Bm1, Bm2], writes=[Bmrg])
            for tk in range(4):
                x_t, Bx = xt[tk % 2]
                h_t, Bh = ho[tk % 2]
                r0 = s * SEQ + qb * 512 + tk * 128
                S.dma("sp", lambda e, x_t=x_t, r0=r0: e.dma_start(out=x_t, in_=K.x[r0:r0 + 128, :]), writes=[Bx])
                for nh in range(2):
                    bank = 5 + nh
                    mm_group(S, ps[bank][:], Bps[bank],
                             [(mrg[:, k, tk * 128:(tk + 1) * 128], wo[:, k, nh * 512:(nh + 1) * 512]) for k in range(8)], reads=[Bmrg, Bwo])
                    S.op("dve", lambda e, bank=bank, nh=nh, x_t=x_t, h_t=h_t: e.tensor_tensor(
                        out=h_t[:, nh * 512:(nh + 1) * 512], in0=ps[bank][:], in1=x_t[:, nh * 512:(nh + 1) * 512], op=ALU.add),
                        reads=[Bps[bank], Bx], writes=[Bh])
                S.dma("sp", lambda e, h_t=h_t, r0=r0: e.dma_start(out=K.h1_s[r0:r0 + 128, :], in_=h_t), reads=[Bh], owner=Bh)
    zero_some(len(zrows))


def phase3_router(K):
    S, A, nc = K.S, K.A, K.nc
    ps, Bps = K.ps, K.Bps
    gmoe, Bgmoe = A.alloc("gmoe", [D])
    wr, Bwr = A.alloc("wr", [8, E])
    brt, Bbr = A.alloc("brt", [E])
    tri, Btri = A.alloc("tri", [128])
    eC, BeC = A.alloc("eC", [E])
    msum, Bmsum = A.alloc("msum", [E])
    S.dma("sp", lambda e: e.dma_start(out=gmoe, in_=bcast_row(K.g_moe, D)), writes=[Bgmoe])
    S.dma("sp", lambda e: e.dma_start(out=wr, in_=K.w_router.rearrange("(c p) n -> p c n", p=128)), writes=[Bwr])
    S.dma("sp", lambda e: e.dma_start(out=brt, in_=bcast_row(K.b_router, E)), writes=[Bbr])
    S.dma("sp", lambda e: e.dma_start(out=tri, in_=K.tri), writes=[Btri])
    S.dma("sp", lambda e: e.dma_start(out=eC, in_=K.eC), writes=[BeC])
    S.op("dve", lambda e: e.memset(msum, 0.0), writes=[Bmsum])

    def tile_stream(par):
        h_t, Bh = A.alloc(f"ht{par}", [D])
        junk, Bjunk = A.alloc(f"junk{par}", [D], BF16)
        ss_t, Bss = A.alloc(f"ss{par}", [1])
        u2, Bu2 = A.alloc(f"u2{par}", [D])
        ub, Bub = A.alloc(f"u2b{par}", [D], BF16)
        u2T, Bu2T = A.alloc(f"u2T{par}", [8, 128])
        lg, Blg = A.alloc(f"lg{par}", [E])
        top8, Btop8 = A.alloc(f"top8{par}", [8])
        nm, Bnm = A.alloc(f"nm{par}", [1])
        mask, Bmask = A.alloc(f"mask{par}", [E])
        ex, Bex = A.alloc(f"ex{par}", [E])
        den, Bden = A.alloc(f"den{par}", [1])
        gf, Bgf = A.alloc(f"gf{par}", [E])
        rank, Brank = A.alloc(f"rank{par}", [E])
        okm, Bok = A.alloc(f"okm{par}", [E])
        dst, Bdst = A.alloc(f"dst{par}", [E])
        dk, Bdk = A.alloc(f"dk{par}", [4])
        oh, Boh = A.alloc(f"oh{par}", [E])
        b0 = par * 2

        def gen():
            for i in range(par, NTILE, 4):
                r0 = i * 128
                S.dma("sp", lambda e, r0=r0: e.dma_start(out=h_t, in_=K.h1_s[r0:r0 + 128, :]), writes=[Bh])
                rms_rstd(K, h_t, Bh, junk, Bjunk, ss_t, Bss, D)
                S.op("dve", lambda e: e.scalar_tensor_tensor(out=u2, in0=h_t, scalar=ss_t, in1=gmoe, op0=ALU.mult, op1=ALU.mult),
                     reads=[Bh, Bss, Bgmoe], writes=[Bu2])
                yield
                S.op("act", lambda e: e.activation(out=ub, in_=u2, func=AF.Copy), reads=[Bu2], writes=[Bub])
                for hc in range(2):
                    bank = b0
                    for c4 in range(4):
                        c = hc * 4 + c4
                        S.op("pe", lambda e, c=c, c4=c4, bank=bank: e.transpose(ps[bank][:, c4 * 128:(c4 + 1) * 128], u2[:, c * 128:(c + 1) * 128], K.ident_f),
                             reads=[Bu2, K.Bident_f], writes=[Bps[bank]], signal=(c4 == 3))
                    S.op("act", lambda e, bank=bank, hc=hc: e.activation(out=u2T[:, hc * 4:(hc + 1) * 4, :], in_=ps[bank][:].rearrange("p (c t) -> p c t", c=4), func=AF.Copy),
                         reads=[Bps[bank]], writes=[Bu2T])
                yield
                lb, rb = b0 + 1, b0 + 1
                mm_group(S, ps[lb][:, 0:E], Bps[lb], [(u2T[:, k, :], wr[:, k, :]) for k in range(8)], reads=[Bu2T, Bwr])
                S.op("dve", lambda e, lb=lb: e.tensor_tensor(out=lg, in0=ps[lb][:, 0:E], in1=brt, op=ALU.add), reads=[Bps[lb], Bbr], writes=[Blg])
                S.op("dve", lambda e: e.max(out=top8, in_=lg), reads=[Blg], writes=[Btop8])
                S.op("dve", lambda e: e.tensor_scalar(out=mask, in0=lg, scalar1=top8[:, 3:4], scalar2=None, op0=ALU.is_ge),
                     reads=[Blg, Btop8], writes=[Bmask])
                yield
                mm_group(S, ps[rb][:, 64:64 + E], Bps[rb], [(tri, mask), (K.ones_f, msum)], reads=[Btri, Bmask, K.Bones_f, Bmsum])
                S.op("dve", lambda e: e.tensor_tensor(out=msum, in0=msum, in1=mask, op=ALU.add), reads=[Bmsum, Bmask], writes=[Bmsum])
                yield
                S.op("dve", lambda e: e.tensor_scalar(out=nm, in0=top8[:, 0:1], scalar1=-1.0, scalar2=None, op0=ALU.mult),
                     reads=[Btop8], writes=[Bnm])
                S.op("act", lambda e: e.activation(out=ex, in_=lg, func=AF.Exp, bias=nm), reads=[Blg, Bnm], writes=[Bex])
                S.op("dve", lambda e, rb=rb: e.tensor_copy(out=rank, in_=ps[rb][:, 64:64 + E]), reads=[Bps[rb]], writes=[Brank])
                S.op("dve", lambda e: e.tensor_scalar(out=okm, in0=rank, scalar1=float(CAP), scalar2=None, op0=ALU.is_lt),
                     reads=[Brank], writes=[Bok])
                S.op("dve", lambda e: e.tensor_tensor(out=dst, in0=rank, in1=eC, op=ALU.add), reads=[Brank, BeC], writes=[Bdst])
                S.op("dve", lambda e: e.scalar_tensor_tensor(out=dst, in0=dst, scalar=float(-TRASH), in1=okm, op0=ALU.add, op1=ALU.mult),
                     reads=[Bdst, Bok], writes=[Bdst])
                S.op("dve", lambda e: e.tensor_scalar(out=dst, in0=dst, scalar1=float(TRASH), scalar2=None, op0=ALU.add),
                     reads=[Bdst], writes=[Bdst])
                yield
                S.op("dve", lambda e: e.tensor_tensor(out=ex, in0=ex, in1=mask, op=ALU.mult), reads=[Bex, Bmask], writes=[Bex])
                S.op("dve", lambda e: e.reduce_sum(out=den, in_=ex, axis=AX.X), reads=[Bex], writes=[Bden])
                S.op("dve", lambda e: e.reciprocal(out=den, in_=den), reads=[Bden], writes=[Bden])
                S.op("dve", lambda e: e.scalar_tensor_tensor(out=gf, in0=ex, scalar=den, in1=okm, op0=ALU.mult, op1=ALU.mult),
                     reads=[Bex, Bden, Bok], writes=[Bgf])
                yield
                for k in range(4):
                    S.op("dve", lambda e, k=k: e.scalar_tensor_tensor(out=oh, in0=lg, scalar=top8[:, k:k + 1], in1=dst, op0=ALU.is_equal, op1=ALU.mult,
                                                                      accum_out=dk[:, k:k + 1]),
                         reads=[Blg, Btop8, Bdst], writes=[Boh, Bdk])
                    S.op("dve", lambda e, k=k, i=i: e.scalar_tensor_tensor(out=oh, in0=lg, scalar=top8[:, k:k + 1], in1=gf, op0=ALU.is_equal, op1=ALU.mult,
                                                                           accum_out=K.gate_a[:, i * 4 + k:i * 4 + k + 1]),
                         reads=[Blg, Btop8, Bgf], writes=[Boh, K.Bgate])
                S.op("dve", lambda e, i=i: e.tensor_copy(out=K.dest_i[:, i * 4:(i + 1) * 4], in_=dk), reads=[Bdk], writes=[K.Bdest])
                for k in range(4):
                    S.dma("pool", lambda e, k=k, i=i: e.indirect_dma_start(
                        out=K.xs, out_offset=bass.IndirectOffsetOnAxis(ap=K.dest_i[:, i * 4 + k:i * 4 + k + 1], axis=0),
                        in_=ub, in_offset=None), reads=[Bub, K.Bdest], owner=Bub)
                yield
        return gen()

    interleave([tile_stream(q) for q in range(4)], skew=2)


def phase4_experts(K):
    S, A, nc = K.S, K.A, K.nc
    ps, Bps = K.ps, K.Bps
    wgu = [A.alloc(f"wgu{i}", [8, 2 * F], BF16) for i in range(2)]
    wdn = [A.alloc(f"wdn{i}", [8, D], BF16) for i in range(2)]
    bdn = [A.alloc(f"bdn{i}", [D], BF16) for i in range(2)]
    xTs = [A.alloc(f"xT{i}", [8, CAP], BF16) for i in range(2)]
    resTs = [A.alloc(f"resT{i}", [8, CAP], BF16) for i in range(2)]
    xst = [A.alloc(f"xst{i}", [D], BF16) for i in range(3)]
    yst = [A.alloc(f"yst{i}", [D]) for i in range(2)]
    HW = CAP // 2
    gt = [A.alloc(f"gt{i}", [HW]) for i in range(2)]
    sg = [A.alloc(f"sg{i}", [HW]) for i in range(2)]
    ut = [A.alloc(f"ut{i}", [HW]) for i in range(2)]
    cnts = {"y": 0, "x": 0, "u": 0}
    S.op("dve", lambda e: e.memset(yst[1][0], 0.0), writes=[yst[1][1]])
    S.dma("sp", lambda e: e.dma_start(out=K.ys[NSLOT:NSLOT + 128, :], in_=yst[1][0]), reads=[yst[1][1]], owner=yst[1][1])

    def load_expert(e_):
        w, Bw = wgu[e_ % 2]
        wd, Bwd = wdn[e_ % 2]
        bd, Bbd = bdn[e_ % 2]
        src = K.w_gu[e_].rearrange("(c p) n -> p c n", p=128)
        for hh in range(2):
            S.dma("pool", lambda e, w=w, src=src, hh=hh: e.dma_start(out=w[:, hh * 4:(hh + 1) * 4, :], in_=src[:, hh * 4:(hh + 1) * 4, :]), writes=[Bw])
        S.dma("pool", lambda e, wd=wd, e_=e_: e.dma_start(out=wd, in_=K.w_dn[e_].rearrange("(c p) n -> p c n", p=128)), writes=[Bwd])
        S.dma("pool", lambda e, bd=bd, e_=e_: e.dma_start(out=bd[0:1, :], in_=K.b_dn[e_:e_ + 1, :]), writes=[Bbd])

    def build_xT(e_):
        xT, BxT = xTs[e_ % 2]
        for sb in range(NSB):
            xs_t, Bxs = xst[cnts["x"] % 3]
            cnts["x"] += 1
            r0 = e_ * CAP + sb * 128
            S.dma("sp", lambda e, xs_t=xs_t, r0=r0: e.dma_start(out=xs_t, in_=K.xs[r0:r0 + 128, :]), writes=[Bxs])
            bank = sb % 2
            pv16 = ps[bank][:].bitcast(BF16)
            for c in range(8):
                S.op("pe", lambda e, c=c, pv16=pv16, xs_t=xs_t: e.transpose(pv16[:, c * 128:(c + 1) * 128], xs_t[:, c * 128:(c + 1) * 128], K.ident_b),
                     reads=[Bxs, K.Bident_b], writes=[Bps[bank]], signal=(c == 7))
            S.op("act", lambda e, pv16=pv16, sb=sb, xT=xT: e.activation(out=xT[:, :, sb * 128:(sb + 1) * 128], in_=pv16.rearrange("p (c t) -> p c t", c=8), func=AF.Copy),
                 reads=[Bps[bank]], writes=[BxT])

    def gate_up(e_):
        w, Bw = wgu[e_ % 2]
        xT, BxT = xTs[e_ % 2]
        resT, BresT = resTs[e_ % 2]
        for f in range(8):
            bgc = K.pvt[:, PV_BGU + e_ * 16 + f:PV_BGU + e_ * 16 + f + 1]
            buc = K.pvt[:, PV_BGU + e_ * 16 + 8 + f:PV_BGU + e_ * 16 + 8 + f + 1]
            for hv in range(2):
                nsl = slice(hv * HW, (hv + 1) * HW)
                cnt = cnts["u"]
                cnts["u"] += 1
                gb, ub_ = 2 + cnt % 2, 4 + cnt % 2
                g_t, Bg = gt[cnt % 2]
                s_t, Bs = sg[cnt % 2]
                u_t, Bu = ut[cnt % 2]
                mm_group(S, ps[gb][:, 0:HW], Bps[gb], [(w[:, k, f * 128:(f + 1) * 128], xT[:, k, nsl]) for k in range(8)], reads=[Bw, BxT])
                mm_group(S, ps[ub_][:, 0:HW], Bps[ub_], [(w[:, k, F + f * 128:F + (f + 1) * 128], xT[:, k, nsl]) for k in range(8)], reads=[Bw, BxT])
                S.op("dve", lambda e, gb=gb, g_t=g_t, bgc=bgc: e.tensor_scalar(out=g_t, in0=ps[gb][:, 0:HW], scalar1=bgc, scalar2=7.0, op0=ALU.add, op1=ALU.min),
                     reads=[Bps[gb], K.Bpv], writes=[Bg])
                S.op("act", lambda e, ub_=ub_, u_t=u_t, buc=buc: e.activation(out=u_t, in_=ps[ub_][:, 0:HW], func=AF.Identity, bias=buc),
                     reads=[Bps[ub_], K.Bpv], writes=[Bu])
                S.op("act", lambda e, g_t=g_t, s_t=s_t: e.activation(out=s_t, in_=g_t, func=AF.Sigmoid, scale=1.702), reads=[Bg], writes=[Bs])
                S.op("dve", lambda e, u_t=u_t: e.tensor_scalar(out=u_t, in0=u_t, scalar1=7.0, scalar2=-7.0, op0=ALU.min, op1=ALU.max),
                     reads=[Bu], writes=[Bu])
                S.op("dve", lambda e, g_t=g_t, s_t=s_t: e.tensor_tensor(out=g_t, in0=g_t, in1=s_t, op=ALU.mult), reads=[Bg, Bs], writes=[Bg])
                S.op("dve", lambda e, g_t=g_t, u_t=u_t, f=f, nsl=nsl, resT=resT: e.scalar_tensor_tensor(
                    out=resT[:, f, nsl], in0=u_t, scalar=1.0, in1=g_t, op0=ALU.add, op1=ALU.mult),
                    reads=[Bg, Bu], writes=[BresT])

    def down(e_):
        wd, Bwd = wdn[e_ % 2]
        bd, Bbd = bdn[e_ % 2]
        resT, BresT = resTs[e_ % 2]
        for sb in range(NSB):
            y_t, By = yst[cnts["y"] % 2]
            cnts["y"] += 1
            for nh in range(2):
                bank = 6 + nh
                pairs = [(resT[:, k, sb * 128:(sb + 1) * 128], wd[:, k, nh * 512:(nh + 1) * 512]) for k in range(8)]
                pairs.append((K.ones_b[0:1, :], bd[0:1, nh * 512:(nh + 1) * 512]))
                mm_group(S, ps[bank][:], Bps[bank], pairs, reads=[BresT, Bwd, Bbd, K.Bones_b])
                S.op("act", lambda e, bank=bank, y_t=y_t, nh=nh: e.activation(out=y_t[:, nh * 512:(nh + 1) * 512], in_=ps[bank][:], func=AF.Copy),
                     reads=[Bps[bank]], writes=[By])
            r0 = e_ * CAP + sb * 128
            S.dma("sp", lambda e, y_t=y_t, r0=r0: e.dma_start(out=K.ys[r0:r0 + 128, :], in_=y_t), reads=[By], owner=By)

    load_expert(0)
    build_xT(0)
    for e_ in range(E):
        if e_ + 1 < E:
            load_expert(e_ + 1)
        gate_up(e_)
        if e_ + 1 < E:
            build_xT(e_ + 1)
        down(e_)


def phase5_combine(K):
    S, A, nc = K.S, K.A, K.nc
    ps, Bps = K.ps, K.Bps
    wpg, Bwpg = A.alloc("wpg", [8, D], BF16)
    wpp, Bwpp = A.alloc("wpp", [2, D], BF16)
    gple, Bgple = A.alloc("gple", [D])
    S.dma("pool", lambda e: e.dma_start(out=wpg, in_=K.w_ple_gate.rearrange("(c p) n -> p c n", p=128)), writes=[Bwpg])
    S.dma("pool", lambda e: e.dma_start(out=wpp, in_=K.w_ple_proj.rearrange("(c p) n -> p c n", p=128)), writes=[Bwpp])
    S.dma("sp", lambda e: e.dma_start(out=gple, in_=bcast_row(K.g_ple, D)), writes=[Bgple])

    def tile_stream(par):
        h_t, Bh = A.alloc(f"ht{par}", [D])
        yg = [A.alloc(f"yg{par}_{k}", [D]) for k in range(4)]
        p_t, Bp = A.alloc(f"pt{par}", [PLE])
        ptb, Bptb = A.alloc(f"ptb{par}", [PLE], BF16)
        pT, BpT = A.alloc(f"pT{par}", [2, 128], BF16)
        junk, Bjunk = A.alloc(f"junk{par}", [D], BF16)
        ss_t, Bss = A.alloc(f"ss{par}", [1])
        u3, Bu3 = A.alloc(f"u3{par}", [D], BF16)
        u3T, Bu3T = A.alloc(f"u3T{par}", [8, 128], BF16)
        sgm, Bsgm = A.alloc(f"sgm{par}", [D])
        o_t, Bo = A.alloc(f"ot{par}", [D])
        b0 = par * 2

        def gen():
            for i in range(par, NTILE, 4):
                r0 = i * 128
                S.dma("sp", lambda e, r0=r0: e.dma_start(out=h_t, in_=K.h1_s[r0:r0 + 128, :]), writes=[Bh])
                S.dma("sp", lambda e, r0=r0: e.dma_start(out=p_t, in_=K.p[r0:r0 + 128, :]), writes=[Bp])
                for k in range(4):
                    y_t, By = yg[k]
                    S.dma("pool", lambda e, y_t=y_t, i=i, k=k: e.indirect_dma_start(
                        out=y_t, out_offset=None, in_=K.ys,
                        in_offset=bass.IndirectOffsetOnAxis(ap=K.dest_i[:, i * 4 + k:i * 4 + k + 1], axis=0)),
                        reads=[K.Bdest], writes=[By])
                yield
                S.op("act", lambda e: e.activation(out=ptb, in_=p_t, func=AF.Copy), reads=[Bp], writes=[Bptb])
                pv1 = ps[b0 + 1][:].bitcast(BF16)
                for c in range(2):
                    S.op("pe", lambda e, c=c, pv1=pv1: e.transpose(pv1[:, c * 128:(c + 1) * 128], ptb[:, c * 128:(c + 1) * 128], K.ident_b),
                         reads=[Bptb, K.Bident_b], writes=[Bps[b0 + 1]], signal=(c == 1))
                S.op("act", lambda e, pv1=pv1: e.activation(out=pT, in_=pv1[:, 0:256].rearrange("p (c t) -> p c t", c=2), func=AF.Copy),
                     reads=[Bps[b0 + 1]], writes=[BpT])
                yield
                for k in range(4):
                    y_t, By = yg[k]
                    S.op("dve", lambda e, y_t=y_t, i=i, k=k: e.scalar_tensor_tensor(
                        out=h_t, in0=y_t, scalar=K.gate_a[:, i * 4 + k:i * 4 + k + 1], in1=h_t, op0=ALU.mult, op1=ALU.add),
                        reads=[By, Bh, K.Bgate], writes=[Bh])
                yield
                rms_rstd(K, h_t, Bh, junk, Bjunk, ss_t, Bss, D)
                S.op("dve", lambda e: e.scalar_tensor_tensor(out=u3, in0=h_t, scalar=ss_t, in1=gple, op0=ALU.mult, op1=ALU.mult),
                     reads=[Bh, Bss, Bgple], writes=[Bu3])
                yield
                pv16 = ps[b0][:].bitcast(BF16)
                for c in range(8):
                    S.op("pe", lambda e, c=c, pv16=pv16: e.transpose(pv16[:, c * 128:(c + 1) * 128], u3[:, c * 128:(c + 1) * 128], K.ident_b),
                         reads=[Bu3, K.Bident_b], writes=[Bps[b0]], signal=(c == 7))
                S.op("act", lambda e, pv16=pv16: e.activation(out=u3T, in_=pv16.rearrange("p (c t) -> p c t", c=8), func=AF.Copy),
                     reads=[Bps[b0]], writes=[Bu3T])
                yield
                for nh in range(2):
                    nsl = slice(nh * 512, (nh + 1) * 512)
                    gbk, pbk = b0, b0 + 1
                    mm_group(S, ps[gbk][:], Bps[gbk], [(u3T[:, k, :], wpg[:, k, nsl]) for k in range(8)], reads=[Bu3T, Bwpg])
                    mm_group(S, ps[pbk][:], Bps[pbk], [(pT[:, k, :], wpp[:, k, nsl]) for k in range(2)], reads=[BpT, Bwpp])
                    S.op("act", lambda e, gbk=gbk, nsl=nsl: e.activation(out=sgm[:, nsl], in_=ps[gbk][:], func=AF.Sigmoid), reads=[Bps[gbk]], writes=[Bsgm])
                    S.op("dve", lambda e, pbk=pbk, nsl=nsl: e.tensor_tensor(out=sgm[:, nsl], in0=sgm[:, nsl], in1=ps[pbk][:], op=ALU.mult),
                         reads=[Bsgm, Bps[pbk]], writes=[Bsgm])
                    S.op("dve", lambda e, nsl=nsl: e.tensor_tensor(out=o_t[:, nsl], in0=sgm[:, nsl], in1=h_t[:, nsl], op=ALU.add),
                         reads=[Bsgm, Bh], writes=[Bo])
                    yield
                S.dma("sp", lambda e, r0=r0: e.dma_start(out=K.y[r0:r0 + 128, :], in_=o_t), reads=[Bo], owner=Bo)
        return gen()

    interleave([tile_stream(q) for q in range(4)], skew=2)


def _rope_tables():
    pos = np.arange(SEQ)
    row = (pos // 64).astype(np.float32)
    col = (pos % 64).astype(np.float32)
    inv = (10000.0 ** (-np.arange(0, 64, 2, dtype=np.float32) / 64.0)).astype(np.float32)
    C = np.zeros((128, SEQ), np.float32)
    Sg = np.zeros((128, SEQ), np.float32)
    for p in range(128):
        ids = row if p < 64 else col
        j = p % 32
        ang = (ids * inv[j]).astype(np.float32)
        C[p] = np.cos(ang)
        sgn = -1.0 if (p % 64) < 32 else 1.0
        Sg[p] = sgn * np.sin(ang)
    perm = np.zeros((128, 128), np.float32)
    for m in range(128):
        partner = m + 32 if (m % 64) < 32 else m - 32
        perm[partner, m] = 1.0
    return C, Sg, perm


_NC_CACHE = {}


def _prep_common(inp):
    f = lambda a: np.ascontiguousarray(np.asarray(a, dtype=np.float32))
    pv = np.zeros((128, NPV), np.float32)
    cw = f(inp["conv_w"])[0]
    pv[:, PV_CONVW:PV_CONVW + 32] = cw.reshape(4, 8, 128).transpose(2, 1, 0).reshape(128, 32)
    pv[:, PV_CONVB:PV_CONVB + 8] = f(inp["conv_b"])[0].reshape(8, 128).T
    pv[:, PV_BA:PV_BA + 16] = f(inp["lru_ba"])[0].reshape(16, 128).T
    pv[:, PV_BI:PV_BI + 16] = f(inp["lru_bi"])[0].reshape(16, 128).T
    pv[:, PV_LAM:PV_LAM + 16] = f(inp["lru_lam"])[0].reshape(16, 128).T
    pv[:, PV_QN] = f(inp["q_norm"])[0]
    pv[:, PV_KN] = f(inp["k_norm"])[0]
    pv[:, PV_BGU:PV_BGU + 512] = f(inp["b_gu"])[0].reshape(E * 16, 128).T
    C, Sg, perm = _rope_tables()
    tri = np.triu(np.ones((128, 128), np.float32), 1)
    eC = np.tile((np.arange(E, dtype=np.float32) * CAP)[None, :], (128, 1))
    com = {
        "w_in": f(inp["w_in"])[0], "lru_wa": f(inp["lru_wa"])[0], "lru_wi": f(inp["lru_wi"])[0],
        "w_attn_br": f(inp["w_attn_br"])[0], "w_lru_br": f(inp["w_lru_br"])[0], "w_out": f(inp["w_out"])[0],
        "w_router": f(inp["w_router"])[0], "w_gu": f(inp["w_gu"])[0], "w_dn": f(inp["w_dn"])[0],
        "b_dn": f(inp["b_dn"])[0], "w_ple_gate": f(inp["w_ple_gate"])[0], "w_ple_proj": f(inp["w_ple_proj"])[0],
        "g_mix": f(inp["g_mix"]), "g_moe": f(inp["g_moe"]), "g_ple": f(inp["g_ple"]), "b_router": f(inp["b_router"]),
        "pv": pv, "ropeC": C, "ropeS": Sg, "perm": perm, "ident": np.eye(128, dtype=np.float32), "tri": tri, "eC": eC,
    }
    return com


def kernel(**inputs):
    dbg = bool(int(os.environ.get("MK_DBG", "0")))
    ncores = int(os.environ.get("MK_NCORES", str(NCORES)))
    key = dbg
    if key not in _NC_CACHE:
        _NC_CACHE[key] = build_program(dbg)
    nc = _NC_CACHE[key]
    com = _prep_common(inputs)
    x = np.asarray(inputs["x"], dtype=np.float32)
    p = np.asarray(inputs["p"], dtype=np.float32)[0]
    in_maps = []
    for c in range(ncores):
        m = dict(com)
        m["x"] = np.ascontiguousarray(x[2 * c:2 * c + 2].reshape(T, D))
        m["p"] = np.ascontiguousarray(p[2 * c:2 * c + 2].reshape(T, PLE))
        in_maps.append(m)
    res = run_bass_kernel_spmd(nc, in_maps, core_ids=list(range(ncores)))
    if dbg:
        kernel.last = res
    out = np.zeros((16, SEQ, D), np.float32)
    for c in range(ncores):
        out[2 * c:2 * c + 2] = np.asarray(res.results[c]["y"], dtype=np.float32).reshape(2, SEQ, D)
    return out
```

```python
import os
import numpy as np
from contextlib import ExitStack
import concourse.bass as bass
import concourse.mybir as mybir
from concourse.bass_utils import run_bass_kernel_spmd

F32 = mybir.dt.float32
BF16 = mybir.dt.bfloat16
I32 = mybir.dt.int32
AF = mybir.ActivationFunctionType
ALU = mybir.AluOpType
AX = mybir.AxisListType

NCORES = 8
D = 1024
SEQ = 2048
NSEQ = 2
T = NSEQ * SEQ
NTILE = T // 128
E = 32
F = 1024
CAP = 640
NSB = CAP // 128
NSLOT = E * CAP
TRASH = NSLOT
PLE = 256
EPS = 1e-6
INW = 5632
NPV = 608
PV_CONVW, PV_CONVB, PV_BA, PV_BI, PV_LAM, PV_QN, PV_KN, PV_BGU = 0, 32, 40, 56, 72, 88, 89, 96

ENGS = ("pe", "act", "dve", "pool", "sp")


class Buf:
    __slots__ = ("name", "last_write", "reads", "sem", "sem_total", "excl")

    def __init__(self, name, excl=False):
        self.name = name
        self.excl = excl
        self.last_write = None
        self.reads = []
        self.sem = None
        self.sem_total = 0


class Sched:
    def __init__(self, nc, stack):
        self.nc = nc
        self.stack = stack
        self.stream = {e: [] for e in ENGS}
        self.sem = {e: stack.enter_context(nc.semaphore("s_" + e)) for e in ENGS}
        self.count = {e: 0 for e in ENGS}
        self.seen = {e: {} for e in ENGS}
        self.dma_bufs = []

    def _wait_tokens(self, e, toks):
        need = {}
        for t in toks:
            if t is None:
                continue
            if t[0] == "e":
                _, src, c = t
                if src == "pe" and e == "pe":
                    continue
                key = ("e", src)
                val = c
                sem = self.sem[src]
            else:
                b = t[1]
                key = ("d", id(b))
                val = b.sem_total
                sem = b.sem
            if self.seen[e].get(key, 0) >= val:
                continue
            if key not in need or need[key][1] < val:
                need[key] = (sem, val)
        for key, (sem, val) in need.items():
            self.seen[e][key] = val
            self.stream[e].append(lambda eng, sem=sem, val=val: eng.wait_ge(sem, val))

    @staticmethod
    def _deps(reads, writes):
        toks = []
        for r in reads:
            toks.append(r.last_write)
            if r.excl:
                toks.extend(r.reads)
        for w in writes:
            toks.append(w.last_write)
            toks.extend(w.reads)
        return toks

    def op(self, e, fn, reads=(), writes=(), signal=True):
        self._wait_tokens(e, self._deps(reads, writes))
        if signal:
            self.count[e] += 1
            tok = ("e", e, self.count[e])
            sem = self.sem[e]
            self.stream[e].append(lambda eng, fn=fn, sem=sem: fn(eng).then_inc(sem, 1))
        else:
            tok = ("e", e, self.count[e] + 1)
            self.stream[e].append(lambda eng, fn=fn: fn(eng))
        for w in writes:
            w.last_write = tok
            w.reads = []
        for r in reads:
            r.reads.append(tok)
        return tok

    def dma(self, e, fn, reads=(), writes=(), owner=None):
        if owner is None:
            owner = writes[0] if writes else reads[0]
        if owner.sem is None:
            owner.sem = self.stack.enter_context(self.nc.semaphore("d%d_%s" % (len(self.dma_bufs), owner.name)))
            self.dma_bufs.append(owner)
        self._wait_tokens(e, self._deps(reads, writes))
        owner.sem_total += 16
        sem = owner.sem
        self.stream[e].append(lambda eng, fn=fn, sem=sem: fn(eng).then_inc(sem, 16))
        tok = ("d", owner)
        for w in writes:
            w.last_write = tok
            w.reads = []
        for r in reads:
            r.reads.append(tok)
        return tok

    def barrier(self):
        toks = [("e", s, self.count[s]) for s in ENGS if self.count[s] > 0]
        toks += [("d", b) for b in self.dma_bufs]
        for e in ENGS:
            self._wait_tokens(e, toks)

    def emit(self):
        nc = self.nc
        self.barrier()
        with nc.Block() as block:
            for e, reg in (("sp", block.sync), ("act", block.scalar), ("pe", block.tensor),
                           ("dve", block.vector), ("pool", block.gpsimd)):
                lst = self.stream[e]
                if not lst:
                    continue

                def body(eng, lst=lst):
                    for f in lst:
                        f(eng)
                reg(body)


def _dsize(dt):
    return 2 if dt == BF16 else 4


class Arena:
    def __init__(self, nc, stack, nbytes):
        self.t = stack.enter_context(nc.sbuf_tensor("arena", [128, nbytes // 4], F32))
        self.off = 0
        self.nbytes = nbytes
        self.peak = 0

    def alloc(self, name, free, dt=F32):
        n = 1
        for f in free:
            n *= f
        sz = (n * _dsize(dt) + 31) // 32 * 32
        assert self.off + sz <= self.nbytes, (name, self.off, sz, self.nbytes)
        a = self.t[:, self.off // 4:(self.off + sz) // 4]
        if dt != F32:
            a = a.bitcast(dt)
        a = a[:, 0:n]
        if len(free) == 2:
            a = a.rearrange("p (a b) -> p a b", a=free[0])
        self.off += sz
        self.peak = max(self.peak, self.off)
        return a, Buf(name)

    def mark(self):
        return self.off

    def release(self, m):
        self.off = m


class Ctx:
    pass


def build_program(dbg=False):
    nc = bass.Bass("TRN2", target_bir_lowering=False)
    K = Ctx()
    K.nc = nc

    def din(name, shape, dt=F32):
        return nc.dram_tensor(name, list(shape), dt, kind="ExternalInput")

    K.x = din("x", [T, D]).ap()
    K.p = din("p", [T, PLE]).ap()
    K.w_in = din("w_in", [D, INW]).ap()
    K.lru_wa = din("lru_wa", [2, 8, 128, 128]).ap()
    K.lru_wi = din("lru_wi", [2, 8, 128, 128]).ap()
    K.w_attn_br = din("w_attn_br", [D, D]).ap()
    K.w_lru_br = din("w_lru_br", [D, D]).ap()
    K.w_out = din("w_out", [D, D]).ap()
    K.w_router = din("w_router", [D, E]).ap()
    K.w_gu = din("w_gu", [E, D, 2 * F]).ap()
    K.w_dn = din("w_dn", [E, F, D]).ap()
    K.b_dn = din("b_dn", [E, D]).ap()
    K.w_ple_gate = din("w_ple_gate", [D, D]).ap()
    K.w_ple_proj = din("w_ple_proj", [PLE, D]).ap()
    K.g_mix = din("g_mix", [1, D])
    K.g_moe = din("g_moe", [1, D])
    K.g_ple = din("g_ple", [1, D])
    K.b_router = din("b_router", [1, E])
    K.pv = din("pv", [128, NPV]).ap()
    K.ropeC = din("ropeC", [128, SEQ]).ap()
    K.ropeS = din("ropeS", [128, SEQ]).ap()
    K.perm = din("perm", [128, 128]).ap()
    K.ident = din("ident", [128, 128]).ap()
    K.tri = din("tri", [128, 128]).ap()
    K.eC = din("eC", [128, E]).ap()
    K.y = nc.dram_tensor("y", [T, D], F32, kind="ExternalOutput").ap()

    kind = "ExternalOutput" if dbg else "Internal"

    def dscr(name, shape, dt):
        if dbg:
            return nc.dram_tensor(name, list(shape), dt, kind="ExternalOutput").ap()
        return nc.dram_tensor(name, list(shape), dt).ap()

    K.qT_s = dscr("qT_s", [NSEQ, 8, 128, SEQ], BF16)
    K.kT_s = dscr("kT_s", [NSEQ, 2, 128, SEQ], BF16)
    K.V_s = dscr("V_s", [NSEQ, 128, 16, 256], BF16)
    K.yl_s = dscr("yl_s", [NSEQ, 8, 128, SEQ], BF16)
    K.sga_s = dscr("sga_s", [NSEQ, 8, 128, SEQ], BF16)
    K.sgr_s = dscr("sgr_s", [NSEQ, 8, 128, SEQ], BF16)
    K.h1_s = dscr("h1_s", [T, D], F32)
    K.xs = dscr("xs_s", [NSLOT + 128, D], BF16)
    K.ys = dscr("ys_s", [NSLOT + 128, D], F32)

    with ExitStack() as st:
        S = Sched(nc, st)
        K.S = S
        A = Arena(nc, st, 202 * 1024)
        K.A = A
        K.ps = []
        K.Bps = []
        for i in range(8):
            K.ps.append(st.enter_context(nc.psum_tensor(f"ps{i}", [128, 512], F32)))
            K.Bps.append(Buf(f"ps{i}", excl=True))
        stop = int(os.environ.get("MK_STOP", "9"))
        phase0_consts(K)
        S.barrier()
        m0 = A.mark()
        if stop >= 1:
            phase1_inproj(K)
            S.barrier()
        A.release(m0)
        if stop >= 2:
            phase2_attn(K)
            S.barrier()
        A.release(m0)
        if stop >= 3:
            phase3_router(K)
            S.barrier()
        A.release(m0)
        if stop >= 4:
            phase4_experts(K)
            S.barrier()
        A.release(m0)
        if stop >= 5:
            phase5_combine(K)
        S.emit()
    return nc


def bcast_row(dt_tensor, n):
    return bass.AP(dt_tensor, 0, [[0, 128], [1, n]])


def phase0_consts(K):
    S, A = K.S, K.A
    K.ident_f, K.Bident_f = A.alloc("ident_f", [128])
    K.ident_b, K.Bident_b = A.alloc("ident_b", [128], BF16)
    K.ones_b, K.Bones_b = A.alloc("ones_b", [128], BF16)
    K.ones_f, K.Bones_f = A.alloc("ones_f", [128])
    K.pvt, K.Bpv = A.alloc("pvt", [NPV])
    K.kk, K.Bkk = A.alloc("kk", [16])
    K.dest_i, K.Bdest = A.alloc("dest_i", [NTILE * 4], I32)
    K.gate_a, K.Bgate = A.alloc("gate_a", [NTILE * 4])
    S.dma("sp", lambda e: e.dma_start(out=K.ident_f, in_=K.ident), writes=[K.Bident_f])
    S.dma("pool", lambda e: e.dma_start(out=K.ident_b, in_=K.ident), writes=[K.Bident_b])
    S.dma("sp", lambda e: e.dma_start(out=K.pvt, in_=K.pv), writes=[K.Bpv])
    S.op("dve", lambda e: e.memset(K.ones_b, 1.0), writes=[K.Bones_b])
    S.op("dve", lambda e: e.memset(K.ones_f, 1.0), writes=[K.Bones_f])
    lam = K.pvt[:, PV_LAM:PV_LAM + 16]
    S.op("act", lambda e: e.activation(out=K.kk, in_=lam, func=AF.Exp, scale=-1.0), reads=[K.Bpv], writes=[K.Bkk])
    S.op("act", lambda e: e.activation(out=K.kk, in_=K.kk, func=AF.Ln, bias=1.0), reads=[K.Bkk], writes=[K.Bkk])
    S.op("dve", lambda e: e.tensor_scalar(out=K.kk, in0=K.kk, scalar1=-8.0, scalar2=None, op0=ALU.mult),
         reads=[K.Bkk], writes=[K.Bkk])


def mm_group(S, out, Bout, pairs, reads):
    n = len(pairs)
    for i, (l, r) in enumerate(pairs):
        S.op("pe", lambda e, l=l, r=r, i=i: e.matmul(out, l, r, start=(i == 0), stop=(i == n - 1)),
             reads=reads, writes=[Bout], signal=(i == n - 1))


def rms_rstd(K, src, Bsrc, junk, Bjunk, ss, Bss, n):
    S = K.S
    S.op("act", lambda e: e.activation(out=junk, in_=src, func=AF.Square, accum_out=ss),
         reads=[Bsrc], writes=[Bjunk, Bss])
    S.op("act", lambda e: e.activation(out=ss, in_=ss, func=AF.Sqrt, bias=EPS, scale=1.0 / n),
         reads=[Bss], writes=[Bss])
    S.op("dve", lambda e: e.reciprocal(out=ss, in_=ss), reads=[Bss], writes=[Bss])


def interleave(gens, skew=0):
    gens = list(gens)
    start = {id(g): i * skew for i, g in enumerate(gens)}
    rnd = 0
    while gens:
        for g in list(gens):
            if rnd < start[id(g)]:
                continue
            try:
                next(g)
            except StopIteration:
                gens.remove(g)
        rnd += 1


def phase1_inproj(K):
    S, A, nc = K.S, K.A, K.nc
    ps, Bps = K.ps, K.Bps
    uT, BuT = A.alloc("uT", [8, SEQ], BF16)
    wtA = [A.alloc(f"wtA{i}", [8, 512], BF16) for i in range(2)]
    wtB = [A.alloc(f"wtB{i}", [8, 256], BF16) for i in range(2)]
    ropeS, BropeS = A.alloc("ropeS", [SEQ])
    cq, Bcq = A.alloc("cq", [SEQ])
    ck, Bck = A.alloc("ck", [SEQ])
    permf, Bpermf = A.alloc("permf", [128])
    permq, Bpermq = A.alloc("permq", [128], BF16)
    permk, Bpermk = A.alloc("permk", [128], BF16)
    od_b, Bod = A.alloc("od_b", [128], BF16)
    wa_b, Bwa = A.alloc("wa_b", [16, 128], BF16)
    wi_b, Bwi = A.alloc("wi_b", [16, 128], BF16)
    xn, Bxn = A.alloc("xn", [D], BF16)
    junk, Bjunk = xn, Bxn
    ss = [A.alloc(f"ss{i}", [1]) for i in range(2)]
    stgA = [A.alloc(f"stgA{i}", [SEQ], BF16) for i in range(2)]
    stgB = [A.alloc(f"stgB{i}", [SEQ], BF16) for i in range(1)]
    xq = [A.alloc(f"xq{i}", [512], BF16) for i in range(2)]
    sq = [A.alloc(f"sq{i}", [512], BF16) for i in range(2)]
    ta = [A.alloc(f"ta{i}", [512]) for i in range(2)]
    tb_ = [A.alloc(f"tb{i}", [512]) for i in range(2)]
    rst = [A.alloc(f"rst{i}", [512]) for i in range(2)]
    xrp, Bxrp = A.alloc("xrp", [SEQ + 4])
    cc, Bcc = A.alloc("cc", [SEQ])
    ccb, Bccb = A.alloc("ccb", [SEQ], BF16)
    aas = [A.alloc(f"aa{i}", [SEQ]) for i in range(2)]
    bts = [A.alloc(f"bt{i}", [SEQ]) for i in range(2)]
    aa, Baa = aas[0]
    t1, Bt1 = A.alloc("t1", [SEQ])
    gmix, Bgmix = cc[:, 0:D], Bcc
    hf, Bhf = A.alloc("hf", [SEQ])
    hb, Bhb = A.alloc("hb", [SEQ])
    xt = [(hb[:, 0:D], Bhb), (hf[:, 0:D], Bhf)]
    vst, Bvst = aa.bitcast(BF16).rearrange("p (a b) -> p a b", a=16), Baa

    pvt = K.pvt
    S.dma("sp", lambda e: e.dma_start(out=cq, in_=K.ropeC), writes=[Bcq])
    S.dma("sp", lambda e: e.dma_start(out=ck, in_=K.ropeC), writes=[Bck])
    S.dma("sp", lambda e: e.dma_start(out=ropeS, in_=K.ropeS), writes=[BropeS])
    S.dma("sp", lambda e: e.dma_start(out=permf, in_=K.perm), writes=[Bpermf])
    S.dma("pool", lambda e: e.dma_start(out=wa_b, in_=K.lru_wa.rearrange("d c p n -> p (d c) n")), writes=[Bwa])
    S.dma("pool", lambda e: e.dma_start(out=wi_b, in_=K.lru_wi.rearrange("d c p n -> p (d c) n")), writes=[Bwi])
    S.op("dve", lambda e: e.memset(od_b, 1.0 / 128.0), writes=[Bod])
    S.op("dve", lambda e: e.memset(xrp, 0.0), writes=[Bxrp])
    qn = pvt[:, PV_QN:PV_QN + 1]
    kn = pvt[:, PV_KN:PV_KN + 1]
    S.op("dve", lambda e: e.tensor_scalar(out=cq, in0=cq, scalar1=qn, scalar2=None, op0=ALU.mult),
         reads=[Bcq, K.Bpv], writes=[Bcq])
    S.op("dve", lambda e: e.tensor_scalar(out=ck, in0=ck, scalar1=kn, scalar2=None, op0=ALU.mult),
         reads=[Bck, K.Bpv], writes=[Bck])
    S.op("dve", lambda e: e.tensor_scalar(out=permq, in0=permf, scalar1=qn, scalar2=None, op0=ALU.mult),
         reads=[Bpermf, K.Bpv], writes=[Bpermq])
    S.op("dve", lambda e: e.tensor_scalar(out=permk, in0=permf, scalar1=kn, scalar2=None, op0=ALU.mult),
         reads=[Bpermf, K.Bpv], writes=[Bpermk])

    w_in_v = K.w_in.rearrange("(c p) n -> p c n", p=128)
    wcnt = {"A": 0, "B": 0}

    def load_w(which, cols):
        tiles = wtA if which == "A" else wtB
        i = wcnt[which] % 2
        wcnt[which] += 1
        w, Bw = tiles[i]
        off = 0
        for (c0, wd) in cols:
            S.dma("pool", lambda e, w=w, off=off, c0=c0, wd=wd: e.dma_start(
                out=w[:, :, off:off + wd], in_=w_in_v[:, :, c0:c0 + wd]), writes=[Bw])
            off += wd
        return w, Bw

    def inproj(w, Bw, woff, tb, bank):
        mm_group(S, ps[bank][:], Bps[bank],
                 [(w[:, k, woff:woff + 128], uT[:, k, tb * 512:(tb + 1) * 512]) for k in range(8)],
                 reads=[Bw, BuT])

    scnt = {"A": 0, "B": 0}

    def build_uT(s):
        S.dma("sp", lambda e: e.dma_start(out=gmix, in_=bcast_row(K.g_mix, D)), writes=[Bgmix])
        for i in range(16):
            x_t, Bx = xt[i % 2]
            ss_t, Bss = ss[i % 2]
            r0 = s * SEQ + i * 128
            S.dma("sp", lambda e, x_t=x_t, r0=r0: e.dma_start(out=x_t, in_=K.x[r0:r0 + 128, :]), writes=[Bx])
            rms_rstd(K, x_t, Bx, junk, Bjunk, ss_t, Bss, D)
            S.op("dve", lambda e, x_t=x_t, ss_t=ss_t: e.scalar_tensor_tensor(
                out=xn, in0=x_t, scalar=ss_t, in1=gmix, op0=ALU.mult, op1=ALU.mult),
                reads=[Bx, Bss, Bgmix], writes=[Bxn])
            bank = i % 2
            pv16 = ps[bank][:].bitcast(BF16)
            for c in range(8):
                S.op("pe", lambda e, c=c, pv16=pv16: e.transpose(pv16[:, c * 128:(c + 1) * 128], xn[:, c * 128:(c + 1) * 128], K.ident_b),
                     reads=[Bxn, K.Bident_b], writes=[Bps[bank]], signal=(c == 7))
            S.op("act", lambda e, pv16=pv16, i=i: e.activation(
                out=uT[:, :, i * 128:(i + 1) * 128], in_=pv16.rearrange("p (c t) -> p c t", c=8), func=AF.Copy),
                reads=[Bps[bank]], writes=[BuT])

    def qk_head(s, w, Bw, woff, is_q, hidx):
        cg, Bcg = (cq, Bcq) if is_q else (ck, Bck)
        pm, Bpm = (permq, Bpermq) if is_q else (permk, Bpermk)
        st_t, Bst = stgA[scnt["A"] % 2]
        scnt["A"] += 1
        for tb in range(4):
            p = tb % 2
            sl = slice(tb * 512, (tb + 1) * 512)
            xq_, Bxq = xq[p]
            sq_, Bsq = sq[p]
            ta_, Bta = ta[p]
            tb2, Btb = tb_[p]
            rs_, Brst = rst[p]
            zb = p
            inproj(w, Bw, woff, tb, zb)
            S.op("act", lambda e, xq_=xq_, zb=zb: e.activation(out=xq_, in_=ps[zb][:], func=AF.Copy), reads=[Bps[zb]], writes=[Bxq])
            S.op("act", lambda e, sq_=sq_, zb=zb: e.activation(out=sq_, in_=ps[zb][:], func=AF.Square), reads=[Bps[zb]], writes=[Bsq])
            S.op("dve", lambda e, sl=sl, cg=cg, ta_=ta_, zb=zb: e.tensor_tensor(out=ta_, in0=ps[zb][:], in1=cg[:, sl], op=ALU.mult),
                 reads=[Bps[zb], Bcg], writes=[Bta])
            yield
            mm_group(S, ps[2][:], Bps[2], [(od_b, sq_)], reads=[Bod, Bsq])
            mm_group(S, ps[3][:], Bps[3], [(pm, xq_)], reads=[Bpm, Bxq])
            S.op("act", lambda e, rs_=rs_: e.activation(out=rs_, in_=ps[2][:], func=AF.Sqrt, bias=EPS), reads=[Bps[2]], writes=[Brst])
            S.op("dve", lambda e, sl=sl, tb2=tb2: e.tensor_tensor(out=tb2, in0=ps[3][:], in1=ropeS[:, sl], op=ALU.mult),
                 reads=[Bps[3], BropeS], writes=[Btb])
            yield
            S.op("dve", lambda e, rs_=rs_: e.reciprocal(out=rs_, in_=rs_), reads=[Brst], writes=[Brst])
            S.op("dve", lambda e, ta_=ta_, tb2=tb2: e.tensor_tensor(out=ta_, in0=ta_, in1=tb2, op=ALU.add), reads=[Bta, Btb], writes=[Bta])
            S.op("dve", lambda e, sl=sl, st_t=st_t, ta_=ta_, rs_=rs_: e.tensor_tensor(out=st_t[:, sl], in0=ta_, in1=rs_, op=ALU.mult),
                 reads=[Bta, Brst], writes=[Bst])
            yield
        dst = (K.qT_s if is_q else K.kT_s)[s, hidx]
        S.dma("sp", lambda e, st_t=st_t, dst=dst: e.dma_start(out=dst, in_=st_t), reads=[Bst], owner=Bst)

    def stream_A(s):
        for t2 in range(2):
            w, Bw = load_w("A", [(t2 * 512, 512)])
            for j in range(4):
                yield from qk_head(s, w, Bw, j * 128, True, t2 * 4 + j)
        w, Bw = load_w("A", [(1024, 512)])
        for j in range(2):
            yield from qk_head(s, w, Bw, j * 128, False, j)
        for gi, dst_s in ((0, K.sga_s), (1, K.sgr_s)):
            for t2 in range(2):
                wg, Bwg = load_w("A", [(3584 + gi * 1024 + t2 * 512, 512)])
                for j in range(4):
                    st_t, Bst = stgA[scnt["A"] % 2]
                    scnt["A"] += 1
                    for tb in range(4):
                        bank = tb % 2
                        inproj(wg, Bwg, j * 128, tb, bank)
                        S.op("act", lambda e, bank=bank, tb=tb, st_t=st_t: e.activation(out=st_t[:, tb * 512:(tb + 1) * 512], in_=ps[bank][:], func=AF.Sigmoid),
                             reads=[Bps[bank]], writes=[Bst])
                        yield
                    S.dma("sp", lambda e, st_t=st_t, dst=dst_s[s, t2 * 4 + j]: e.dma_start(out=dst, in_=st_t), reads=[Bst], owner=Bst)

    def do_V(s):
        w, Bw = load_w("A", [(1024, 512)])
        for tk in range(16):
            bank = 2 + tk % 2
            mm_group(S, ps[bank][:, 0:256], Bps[bank],
                     [(uT[:, k, tk * 128:(tk + 1) * 128], w[:, k, 256:512]) for k in range(8)], reads=[Bw, BuT])
            S.op("act", lambda e, bank=bank, tk=tk: e.activation(out=vst[:, tk, :], in_=ps[bank][:, 0:256], func=AF.Copy),
                 reads=[Bps[bank]], writes=[Bvst])
        S.dma("sp", lambda e, s=s: e.dma_start(out=K.V_s[s], in_=vst), reads=[Bvst], owner=Bvst)

    def stream_B(s):
        for j in range(8):
            w, Bw = load_w("B", [(1536 + j * 128, 128), (2560 + j * 128, 128)])
            for tb in range(4):
                bank = 4 + tb % 2
                inproj(w, Bw, 0, tb, bank)
                S.op("act", lambda e, bank=bank, tb=tb: e.activation(out=xrp[:, 2 + tb * 512:2 + (tb + 1) * 512], in_=ps[bank][:], func=AF.Copy),
                     reads=[Bps[bank]], writes=[Bxrp])
                yield
            cw = lambda jj, j=j: K.pvt[:, PV_CONVW + j * 4 + jj:PV_CONVW + j * 4 + jj + 1]
            cb = K.pvt[:, PV_CONVB + j:PV_CONVB + j + 1]
            S.op("act", lambda e, cw=cw, cb=cb: e.activation(out=cc, in_=xrp[:, 0:SEQ], func=AF.Identity, bias=cb, scale=cw(0)),
                 reads=[Bxrp, K.Bpv], writes=[Bcc])
            for jj in range(1, 4):
                S.op("dve", lambda e, cw=cw, jj=jj: e.scalar_tensor_tensor(out=cc, in0=xrp[:, jj:jj + SEQ], scalar=cw(jj), in1=cc, op0=ALU.mult, op1=ALU.add),
                     reads=[Bxrp, Bcc, K.Bpv], writes=[Bcc])
                yield
            S.op("act", lambda e: e.activation(out=ccb, in_=cc, func=AF.Copy), reads=[Bcc], writes=[Bccb])

            def dir_gen(d, j=j):
                aa_, Baa_ = aas[d]
                bt_, Bbt_ = bts[d]
                hh, Bhh = (hf, Bhf) if d == 0 else (hb, Bhb)
                ba = K.pvt[:, PV_BA + d * 8 + j:PV_BA + d * 8 + j + 1]
                bi = K.pvt[:, PV_BI + d * 8 + j:PV_BI + d * 8 + j + 1]
                kkc = K.kk[:, d * 8 + j:d * 8 + j + 1]
                for tb in range(4):
                    sl = slice(tb * 512, (tb + 1) * 512)
                    mm_group(S, ps[6][:], Bps[6], [(wa_b[:, d * 8 + j, :], ccb[:, sl])], reads=[Bwa, Bccb])
                    S.op("act", lambda e, sl=sl: e.activation(out=aa_[:, sl], in_=ps[6][:], func=AF.Sigmoid, bias=ba),
                         reads=[Bps[6], K.Bpv], writes=[Baa_])
                    mm_group(S, ps[7][:], Bps[7], [(wi_b[:, d * 8 + j, :], ccb[:, sl])], reads=[Bwi, Bccb])
                    S.op("act", lambda e, sl=sl: e.activation(out=bt_[:, sl], in_=ps[7][:], func=AF.Sigmoid, bias=bi),
                         reads=[Bps[7], K.Bpv], writes=[Bbt_])
                    yield
                S.op("act", lambda e: e.activation(out=aa_, in_=aa_, func=AF.Exp, scale=kkc), reads=[Baa_, K.Bkk], writes=[Baa_])
                S.op("dve", lambda e: e.tensor_tensor(out=bt_, in0=bt_, in1=cc, op=ALU.mult), reads=[Bbt_, Bcc], writes=[Bbt_])
                yield
                S.op("act", lambda e: e.activation(out=hh, in_=aa_, func=AF.Square), reads=[Baa_], writes=[Bhh])
                S.op("act", lambda e: e.activation(out=hh, in_=hh, func=AF.Sqrt, bias=1.0, scale=-1.0), reads=[Bhh], writes=[Bhh])
                yield
                S.op("dve", lambda e: e.tensor_tensor(out=bt_, in0=bt_, in1=hh, op=ALU.mult), reads=[Bbt_, Bhh], writes=[Bbt_])
                yield
                if d == 0:
                    S.op("dve", lambda e: e.tensor_tensor_scan(out=hh, data0=aa_, data1=bt_, initial=0.0, op0=ALU.mult, op1=ALU.add),
                         reads=[Baa_, Bbt_], writes=[Bhh])
                else:
                    S.op("dve", lambda e: e.tensor_tensor_scan(out=hh[:, ::-1], data0=aa_[:, ::-1], data1=bt_[:, ::-1], initial=0.0, op0=ALU.mult, op1=ALU.add),
                         reads=[Baa_, Bbt_], writes=[Bhh])
                yield

            def gelu_gen(w=w, Bw=Bw):
                for tb in range(4):
                    sl = slice(tb * 512, (tb + 1) * 512)
                    bank = 4 + tb % 2
                    inproj(w, Bw, 128, tb, bank)
                    S.op("act", lambda e, bank=bank, sl=sl: e.activation(out=t1[:, sl], in_=ps[bank][:], func=AF.Square), reads=[Bps[bank]], writes=[Bt1])
                    S.op("act", lambda e, sl=sl: e.activation(out=t1[:, sl], in_=t1[:, sl], func=AF.Identity, bias=1.0, scale=0.044715),
                         reads=[Bt1], writes=[Bt1])
                    S.op("dve", lambda e, bank=bank, sl=sl: e.tensor_tensor(out=t1[:, sl], in0=t1[:, sl], in1=ps[bank][:], op=ALU.mult),
                         reads=[Bt1, Bps[bank]], writes=[Bt1])
                    S.op("act", lambda e, sl=sl: e.activation(out=t1[:, sl], in_=t1[:, sl], func=AF.Sigmoid, scale=1.5957691216), reads=[Bt1], writes=[Bt1])
                    S.op("dve", lambda e, bank=bank, sl=sl: e.tensor_tensor(out=t1[:, sl], in0=t1[:, sl], in1=ps[bank][:], op=ALU.mult),
                         reads=[Bt1, Bps[bank]], writes=[Bt1])
                    yield

            subs = [dir_gen(0), dir_gen(1), gelu_gen()]
            while subs:
                for g in list(subs):
                    try:
                        next(g)
                    except StopIteration:
                        subs.remove(g)
                yield
            S.op("dve", lambda e: e.tensor_tensor(out=hf, in0=hf, in1=hb, op=ALU.add), reads=[Bhf, Bhb], writes=[Bhf])
            st_t, Bst = stgB[0]
            S.op("dve", lambda e, st_t=st_t: e.tensor_tensor(out=st_t, in0=hf, in1=t1, op=ALU.mult), reads=[Bhf, Bt1], writes=[Bst])
            S.dma("sp", lambda e, st_t=st_t, j=j, s=s: e.dma_start(out=K.yl_s[s, j], in_=st_t), reads=[Bst], owner=Bst)
            yield

    for s in range(NSEQ):
        build_uT(s)
        interleave([stream_A(s), stream_B(s)])
        do_V(s)


def phase2_attn(K):
    S, A, nc = K.S, K.A, K.nc
    ps, Bps = K.ps, K.Bps
    wab, Bwab = A.alloc("wab", [8, D], BF16)
    wlb, Bwlb = A.alloc("wlb", [8, D], BF16)
    wo, Bwo = A.alloc("wo", [8, D], BF16)
    kT, BkT = A.alloc("kT", [2, SEQ], BF16)
    V, BV = A.alloc("V", [16, 256], BF16)
    qT = [A.alloc(f"qT{i}", [8, 512], BF16) for i in range(2)]
    ylb = [A.alloc(f"ylb{i}", [8, 512], BF16) for i in range(2)]
    gab = [A.alloc(f"gab{i}", [8, 512], BF16) for i in range(2)]
    grb = [A.alloc(f"grb{i}", [8, 512], BF16) for i in range(2)]
    PT = [A.alloc(f"PT{i}", [512], BF16) for i in range(6)]
    SBANK = (0, 1, 2, 7)
    attnT, BattnT = A.alloc("attnT", [8, 512], BF16)
    mrg, Bmrg = A.alloc("mrg", [8, 512], BF16)
    rz, Brz = A.alloc("rz", [512])
    zacc = [A.alloc(f"zacc{i}", [512]) for i in range(2)]
    zab = [A.alloc(f"zab{i}", [512], BF16) for i in range(2)]
    m1, Bm1 = A.alloc("m1", [512])
    m2, Bm2 = A.alloc("m2", [512])
    xt = [A.alloc(f"xt{i}", [D]) for i in range(2)]
    ho = [A.alloc(f"ho{i}", [D]) for i in range(2)]
    S.dma("pool", lambda e: e.dma_start(out=wab, in_=K.w_attn_br.rearrange("(c p) n -> p c n", p=128)), writes=[Bwab])
    S.dma("pool", lambda e: e.dma_start(out=wlb, in_=K.w_lru_br.rearrange("(c p) n -> p c n", p=128)), writes=[Bwlb])
    S.dma("pool", lambda e: e.dma_start(out=wo, in_=K.w_out.rearrange("(c p) n -> p c n", p=128)), writes=[Bwo])
    zt, Bzt = A.alloc("zt", [D], BF16)
    S.op("dve", lambda e: e.memset(zt, 0.0), writes=[Bzt])
    zrows = list(range(0, NSLOT + 128, 128))

    def zero_some(n):
        for _ in range(n):
            if zrows:
                r0 = zrows.pop(0)
                S.dma("sp", lambda e, r0=r0: e.dma_start(out=K.xs[r0:r0 + 128, :], in_=zt), reads=[Bzt], owner=Bzt)
    scale = 128.0 ** -0.5
    cnt = 0
    for s in range(NSEQ):
        S.dma("sp", lambda e, s=s: e.dma_start(out=kT, in_=K.kT_s[s].rearrange("h p t -> p h t")), writes=[BkT])
        S.dma("sp", lambda e, s=s: e.dma_start(out=V, in_=K.V_s[s]), writes=[BV])
        for qb in range(4):
            q, Bq = qT[cnt % 2]
            yl, Byl = ylb[cnt % 2]
            ga, Bga = gab[cnt % 2]
            gr, Bgr = grb[cnt % 2]
            cnt += 1
            tsl = slice(qb * 512, (qb + 1) * 512)
            S.dma("sp", lambda e, q=q, s=s, tsl=tsl: e.dma_start(out=q, in_=K.qT_s[s].rearrange("h p t -> p h t")[:, :, tsl]), writes=[Bq])
            S.dma("sp", lambda e, yl=yl, s=s, tsl=tsl: e.dma_start(out=yl, in_=K.yl_s[s].rearrange("h p t -> p h t")[:, :, tsl]), writes=[Byl])
            S.dma("sp", lambda e, ga=ga, s=s, tsl=tsl: e.dma_start(out=ga, in_=K.sga_s[s].rearrange("h p t -> p h t")[:, :, tsl]), writes=[Bga])
            S.dma("sp", lambda e, gr=gr, s=s, tsl=tsl: e.dma_start(out=gr, in_=K.sgr_s[s].rearrange("h p t -> p h t")[:, :, tsl]), writes=[Bgr])
            for h in range(8):
                kv = h // 4
                ob, zb = 3 + h % 2, 5 + h % 2

                def score(kc):
                    bank = SBANK[kc % 4]
                    mm_group(S, ps[bank][:], Bps[bank], [(kT[:, kv, kc * 128:(kc + 1) * 128], q[:, h, :])], reads=[BkT, Bq])
                    pt, Bpt = PT[kc % 6]
                    S.op("act", lambda e, bank=bank, pt=pt: e.activation(out=pt, in_=ps[bank][:], func=AF.Exp, scale=scale),
                         reads=[Bps[bank]], writes=[Bpt])

                za, Bza = zacc[h % 2]
                zb16, Bzb16 = zab[h % 2]

                def pv(kc):
                    pt, Bpt = PT[kc % 6]
                    S.op("pe", lambda e, kc=kc, pt=pt, ob=ob, kv=kv: e.matmul(ps[ob][:], V[:, kc, kv * 128:(kv + 1) * 128], pt, start=(kc == 0), stop=(kc == 15)),
                         reads=[BV, Bpt], writes=[Bps[ob]], signal=(kc == 15))
                    if kc % 2 == 1:
                        S.op("pe", lambda e, kc=kc, pt=pt, zb=zb: e.matmul(ps[zb][:], K.ones_b, pt, start=(kc == 1), stop=False),
                             reads=[K.Bones_b, Bpt], writes=[Bps[zb]], signal=False)
                    elif kc == 0:
                        S.op("dve", lambda e, pt=pt, za=za: e.tensor_copy(out=za, in_=pt), reads=[Bpt], writes=[Bza])
                    else:
                        S.op("dve", lambda e, pt=pt, za=za: e.tensor_tensor(out=za, in0=za, in1=pt, op=ALU.add), reads=[Bpt, Bza], writes=[Bza])
                    if kc == 15:
                        S.op("pe", lambda e, za=za, zb=zb: e.matmul(ps[zb][:], K.ones_f, za, start=False, stop=True),
                             reads=[K.Bones_f, Bza], writes=[Bps[zb]], signal=True)

                score(0)
                score(1)
                score(2)
                for kc in range(16):
                    if kc + 3 < 16:
                        score(kc + 3)
                    pv(kc)
                S.op("dve", lambda e, zb=zb: e.reciprocal(out=rz, in_=ps[zb][:]), reads=[Bps[zb]], writes=[Brz])
                S.op("dve", lambda e, ob=ob, h=h: e.tensor_tensor(out=attnT[:, h, :], in0=ps[ob][:], in1=rz, op=ALU.mult),
                     reads=[Bps[ob], Brz], writes=[BattnT])
                zero_some(6)
            for m in range(8):
                b1, b2 = 0 + m % 2, 2 + m % 2
                mm_group(S, ps[b1][:], Bps[b1], [(wab[:, k, m * 128:(m + 1) * 128], attnT[:, k, :]) for k in range(8)], reads=[Bwab, BattnT])
                mm_group(S, ps[b2][:], Bps[b2], [(wlb[:, k, m * 128:(m + 1) * 128], yl[:, k, :]) for k in range(8)], reads=[Bwlb, Byl])
                S.op("dve", lambda e, b1=b1, m=m, ga=ga: e.tensor_tensor(out=m1, in0=ps[b1][:], in1=ga[:, m, :], op=ALU.mult),
                     reads=[Bps[b1], Bga], writes=[Bm1])
                S.op("dve", lambda e, b2=b2, m=m, gr=gr: e.tensor_tensor(out=m2, in0=ps[b2][:], in1=gr[:, m, :], op=ALU.mult),
                     reads=[Bps[b2], Bgr], writes=[Bm2])
                S.op("dve", lambda e, m=m: e.tensor_tensor(out=mrg[:, m, :], in0=m1, in1=m2, op=ALU.add),
                     reads=[Bm1, Bm2], writes=[Bmrg])
            for tk in range(4):
                x_t, Bx = xt[tk % 2]
                h_t, Bh = ho[tk % 2]
                r0 = s * SEQ + qb * 512 + tk * 128
                S.dma("sp", lambda e, x_t=x_t, r0=r0: e.dma_start(out=x_t, in_=K.x[r0:r0 + 128, :]), writes=[Bx])
                for nh in range(2):
                    bank = 5 + nh
                    mm_group(S, ps[bank][:], Bps[bank],
                             [(mrg[:, k, tk * 128:(tk + 1) * 128], wo[:, k, nh * 512:(nh + 1) * 512]) for k in range(8)], reads=[Bmrg, Bwo])
                    S.op("dve", lambda e, bank=bank, nh=nh, x_t=x_t, h_t=h_t: e.tensor_tensor(
                        out=h_t[:, nh * 512:(nh + 1) * 512], in0=ps[bank][:], in1=x_t[:, nh * 512:(nh + 1) * 512], op=ALU.add),
                        reads=[Bps[bank], Bx], writes=[Bh])
                S.dma("sp", lambda e, h_t=h_t, r0=r0: e.dma_start(out=K.h1_s[r0:r0 + 128, :], in_=h_t), reads=[Bh], owner=Bh)
    zero_some(len(zrows))


def phase3_router(K):
    S, A, nc = K.S, K.A, K.nc
    ps, Bps = K.ps, K.Bps
    gmoe, Bgmoe = A.alloc("gmoe", [D])
    wr, Bwr = A.alloc("wr", [8, E])
    brt, Bbr = A.alloc("brt", [E])
    tri, Btri = A.alloc("tri", [128])
    eC, BeC = A.alloc("eC", [E])
    msum, Bmsum = A.alloc("msum", [E])
    S.dma("sp", lambda e: e.dma_start(out=gmoe, in_=bcast_row(K.g_moe, D)), writes=[Bgmoe])
    S.dma("sp", lambda e: e.dma_start(out=wr, in_=K.w_router.rearrange("(c p) n -> p c n", p=128)), writes=[Bwr])
    S.dma("sp", lambda e: e.dma_start(out=brt, in_=bcast_row(K.b_router, E)), writes=[Bbr])
    S.dma("sp", lambda e: e.dma_start(out=tri, in_=K.tri), writes=[Btri])
    S.dma("sp", lambda e: e.dma_start(out=eC, in_=K.eC), writes=[BeC])
    S.op("dve", lambda e: e.memset(msum, 0.0), writes=[Bmsum])

    def tile_stream(par):
        h_t, Bh = A.alloc(f"ht{par}", [D])
        junk, Bjunk = A.alloc(f"junk{par}", [D], BF16)
        ss_t, Bss = A.alloc(f"ss{par}", [1])
        u2, Bu2 = A.alloc(f"u2{par}", [D])
        ub, Bub = A.alloc(f"u2b{par}", [D], BF16)
        u2T, Bu2T = A.alloc(f"u2T{par}", [8, 128])
        lg, Blg = A.alloc(f"lg{par}", [E])
        top8, Btop8 = A.alloc(f"top8{par}", [8])
        nm, Bnm = A.alloc(f"nm{par}", [1])
        mask, Bmask = A.alloc(f"mask{par}", [E])
        ex, Bex = A.alloc(f"ex{par}", [E])
        den, Bden = A.alloc(f"den{par}", [1])
        gf, Bgf = A.alloc(f"gf{par}", [E])
        rank, Brank = A.alloc(f"rank{par}", [E])
        okm, Bok = A.alloc(f"okm{par}", [E])
        dst, Bdst = A.alloc(f"dst{par}", [E])
        dk, Bdk = A.alloc(f"dk{par}", [4])
        oh, Boh = A.alloc(f"oh{par}", [E])
        b0 = par * 2

        def gen():
            for i in range(par, NTILE, 4):
                r0 = i * 128
                S.dma("sp", lambda e, r0=r0: e.dma_start(out=h_t, in_=K.h1_s[r0:r0 + 128, :]), writes=[Bh])
                rms_rstd(K, h_t, Bh, junk, Bjunk, ss_t, Bss, D)
                S.op("dve", lambda e: e.scalar_tensor_tensor(out=u2, in0=h_t, scalar=ss_t, in1=gmoe, op0=ALU.mult, op1=ALU.mult),
                     reads=[Bh, Bss, Bgmoe], writes=[Bu2])
                yield
                S.op("act", lambda e: e.activation(out=ub, in_=u2, func=AF.Copy), reads=[Bu2], writes=[Bub])
                for hc in range(2):
                    bank = b0
                    for c4 in range(4):
                        c = hc * 4 + c4
                        S.op("pe", lambda e, c=c, c4=c4, bank=bank: e.transpose(ps[bank][:, c4 * 128:(c4 + 1) * 128], u2[:, c * 128:(c + 1) * 128], K.ident_f),
                             reads=[Bu2, K.Bident_f], writes=[Bps[bank]], signal=(c4 == 3))
                    S.op("act", lambda e, bank=bank, hc=hc: e.activation(out=u2T[:, hc * 4:(hc + 1) * 4, :], in_=ps[bank][:].rearrange("p (c t) -> p c t", c=4), func=AF.Copy),
                         reads=[Bps[bank]], writes=[Bu2T])
                yield
                lb, rb = b0 + 1, b0 + 1
                mm_group(S, ps[lb][:, 0:E], Bps[lb], [(u2T[:, k, :], wr[:, k, :]) for k in range(8)], reads=[Bu2T, Bwr])
                S.op("dve", lambda e, lb=lb: e.tensor_tensor(out=lg, in0=ps[lb][:, 0:E], in1=brt, op=ALU.add), reads=[Bps[lb], Bbr], writes=[Blg])
                S.op("dve", lambda e: e.max(out=top8, in_=lg), reads=[Blg], writes=[Btop8])
                S.op("dve", lambda e: e.tensor_scalar(out=mask, in0=lg, scalar1=top8[:, 3:4], scalar2=None, op0=ALU.is_ge),
                     reads=[Blg, Btop8], writes=[Bmask])
                yield
                mm_group(S, ps[rb][:, 64:64 + E], Bps[rb], [(tri, mask), (K.ones_f, msum)], reads=[Btri, Bmask, K.Bones_f, Bmsum])
                S.op("dve", lambda e: e.tensor_tensor(out=msum, in0=msum, in1=mask, op=ALU.add), reads=[Bmsum, Bmask], writes=[Bmsum])
                yield
                S.op("dve", lambda e: e.tensor_scalar(out=nm, in0=top8[:, 0:1], scalar1=-1.0, scalar2=None, op0=ALU.mult),
                     reads=[Btop8], writes=[Bnm])
                S.op("act", lambda e: e.activation(out=ex, in_=lg, func=AF.Exp, bias=nm), reads=[Blg, Bnm], writes=[Bex])
                S.op("dve", lambda e, rb=rb: e.tensor_copy(out=rank, in_=ps[rb][:, 64:64 + E]), reads=[Bps[rb]], writes=[Brank])
                S.op("dve", lambda e: e.tensor_scalar(out=okm, in0=rank, scalar1=float(CAP), scalar2=None, op0=ALU.is_lt),
                     reads=[Brank], writes=[Bok])
                S.op("dve", lambda e: e.tensor_tensor(out=dst, in0=rank, in1=eC, op=ALU.add), reads=[Brank, BeC], writes=[Bdst])
                S.op("dve", lambda e: e.scalar_tensor_tensor(out=dst, in0=dst, scalar=float(-TRASH), in1=okm, op0=ALU.add, op1=ALU.mult),
                     reads=[Bdst, Bok], writes=[Bdst])
                S.op("dve", lambda e: e.tensor_scalar(out=dst, in0=dst, scalar1=float(TRASH), scalar2=None, op0=ALU.add),
                     reads=[Bdst], writes=[Bdst])
                yield
                S.op("dve", lambda e: e.tensor_tensor(out=ex, in0=ex, in1=mask, op=ALU.mult), reads=[Bex, Bmask], writes=[Bex])
                S.op("dve", lambda e: e.reduce_sum(out=den, in_=ex, axis=AX.X), reads=[Bex], writes=[Bden])
                S.op("dve", lambda e: e.reciprocal(out=den, in_=den), reads=[Bden], writes=[Bden])
                S.op("dve", lambda e: e.scalar_tensor_tensor(out=gf, in0=ex, scalar=den, in1=okm, op0=ALU.mult, op1=ALU.mult),
                     reads=[Bex, Bden, Bok], writes=[Bgf])
                yield
                for k in range(4):
                    S.op("dve", lambda e, k=k: e.scalar_tensor_tensor(out=oh, in0=lg, scalar=top8[:, k:k + 1], in1=dst, op0=ALU.is_equal, op1=ALU.mult,
                                                                      accum_out=dk[:, k:k + 1]),
                         reads=[Blg, Btop8, Bdst], writes=[Boh, Bdk])
                    S.op("dve", lambda e, k=k, i=i: e.scalar_tensor_tensor(out=oh, in0=lg, scalar=top8[:, k:k + 1], in1=gf, op0=ALU.is_equal, op1=ALU.mult,
                                                                           accum_out=K.gate_a[:, i * 4 + k:i * 4 + k + 1]),
                         reads=[Blg, Btop8, Bgf], writes=[Boh, K.Bgate])
                S.op("dve", lambda e, i=i: e.tensor_copy(out=K.dest_i[:, i * 4:(i + 1) * 4], in_=dk), reads=[Bdk], writes=[K.Bdest])
                for k in range(4):
                    S.dma("pool", lambda e, k=k, i=i: e.indirect_dma_start(
                        out=K.xs, out_offset=bass.IndirectOffsetOnAxis(ap=K.dest_i[:, i * 4 + k:i * 4 + k + 1], axis=0),
                        in_=ub, in_offset=None), reads=[Bub, K.Bdest], owner=Bub)
                yield
        return gen()

    interleave([tile_stream(q) for q in range(4)], skew=2)


def phase4_experts(K):
    S, A, nc = K.S, K.A, K.nc
    ps, Bps = K.ps, K.Bps
    wgu = [A.alloc(f"wgu{i}", [8, 2 * F], BF16) for i in range(2)]
    wdn = [A.alloc(f"wdn{i}", [8, D], BF16) for i in range(2)]
    bdn = [A.alloc(f"bdn{i}", [D], BF16) for i in range(2)]
    xTs = [A.alloc(f"xT{i}", [8, CAP], BF16) for i in range(2)]
    resTs = [A.alloc(f"resT{i}", [8, CAP], BF16) for i in range(2)]
    xst = [A.alloc(f"xst{i}", [D], BF16) for i in range(3)]
    yst = [A.alloc(f"yst{i}", [D]) for i in range(2)]
    HW = CAP // 2
    gt = [A.alloc(f"gt{i}", [HW]) for i in range(2)]
    sg = [A.alloc(f"sg{i}", [HW]) for i in range(2)]
    ut = [A.alloc(f"ut{i}", [HW]) for i in range(2)]
    cnts = {"y": 0, "x": 0, "u": 0}
    S.op("dve", lambda e: e.memset(yst[1][0], 0.0), writes=[yst[1][1]])
    S.dma("sp", lambda e: e.dma_start(out=K.ys[NSLOT:NSLOT + 128, :], in_=yst[1][0]), reads=[yst[1][1]], owner=yst[1][1])

    def load_expert(e_):
        w, Bw = wgu[e_ % 2]
        wd, Bwd = wdn[e_ % 2]
        bd, Bbd = bdn[e_ % 2]
        src = K.w_gu[e_].rearrange("(c p) n -> p c n", p=128)
        for hh in range(2):
            S.dma("pool", lambda e, w=w, src=src, hh=hh: e.dma_start(out=w[:, hh * 4:(hh + 1) * 4, :], in_=src[:, hh * 4:(hh + 1) * 4, :]), writes=[Bw])
        S.dma("pool", lambda e, wd=wd, e_=e_: e.dma_start(out=wd, in_=K.w_dn[e_].rearrange("(c p) n -> p c n", p=128)), writes=[Bwd])
        S.dma("pool", lambda e, bd=bd, e_=e_: e.dma_start(out=bd[0:1, :], in_=K.b_dn[e_:e_ + 1, :]), writes=[Bbd])

    def build_xT(e_):
        xT, BxT = xTs[e_ % 2]
        for sb in range(NSB):
            xs_t, Bxs = xst[cnts["x"] % 3]
            cnts["x"] += 1
            r0 = e_ * CAP + sb * 128
            S.dma("sp", lambda e, xs_t=xs_t, r0=r0: e.dma_start(out=xs_t, in_=K.xs[r0:r0 + 128, :]), writes=[Bxs])
            bank = sb % 2
            pv16 = ps[bank][:].bitcast(BF16)
            for c in range(8):
                S.op("pe", lambda e, c=c, pv16=pv16, xs_t=xs_t: e.transpose(pv16[:, c * 128:(c + 1) * 128], xs_t[:, c * 128:(c + 1) * 128], K.ident_b),
                     reads=[Bxs, K.Bident_b], writes=[Bps[bank]], signal=(c == 7))
            S.op("act", lambda e, pv16=pv16, sb=sb, xT=xT: e.activation(out=xT[:, :, sb * 128:(sb + 1) * 128], in_=pv16.rearrange("p (c t) -> p c t", c=8), func=AF.Copy),
                 reads=[Bps[bank]], writes=[BxT])

    def gate_up(e_):
        w, Bw = wgu[e_ % 2]
        xT, BxT = xTs[e_ % 2]
        resT, BresT = resTs[e_ % 2]
        for f in range(8):
            bgc = K.pvt[:, PV_BGU + e_ * 16 + f:PV_BGU + e_ * 16 + f + 1]
            buc = K.pvt[:, PV_BGU + e_ * 16 + 8 + f:PV_BGU + e_ * 16 + 8 + f + 1]
            for hv in range(2):
                nsl = slice(hv * HW, (hv + 1) * HW)
                cnt = cnts["u"]
                cnts["u"] += 1
                gb, ub_ = 2 + cnt % 2, 4 + cnt % 2
                g_t, Bg = gt[cnt % 2]
                s_t, Bs = sg[cnt % 2]
                u_t, Bu = ut[cnt % 2]
                mm_group(S, ps[gb][:, 0:HW], Bps[gb], [(w[:, k, f * 128:(f + 1) * 128], xT[:, k, nsl]) for k in range(8)], reads=[Bw, BxT])
                mm_group(S, ps[ub_][:, 0:HW], Bps[ub_], [(w[:, k, F + f * 128:F + (f + 1) * 128], xT[:, k, nsl]) for k in range(8)], reads=[Bw, BxT])
                S.op("dve", lambda e, gb=gb, g_t=g_t, bgc=bgc: e.tensor_scalar(out=g_t, in0=ps[gb][:, 0:HW], scalar1=bgc, scalar2=7.0, op0=ALU.add, op1=ALU.min),
                     reads=[Bps[gb], K.Bpv], writes=[Bg])
                S.op("act", lambda e, ub_=ub_, u_t=u_t, buc=buc: e.activation(out=u_t, in_=ps[ub_][:, 0:HW], func=AF.Identity, bias=buc),
                     reads=[Bps[ub_], K.Bpv], writes=[Bu])
                S.op("act", lambda e, g_t=g_t, s_t=s_t: e.activation(out=s_t, in_=g_t, func=AF.Sigmoid, scale=1.702), reads=[Bg], writes=[Bs])
                S.op("dve", lambda e, u_t=u_t: e.tensor_scalar(out=u_t, in0=u_t, scalar1=7.0, scalar2=-7.0, op0=ALU.min, op1=ALU.max),
                     reads=[Bu], writes=[Bu])
                S.op("dve", lambda e, g_t=g_t, s_t=s_t: e.tensor_tensor(out=g_t, in0=g_t, in1=s_t, op=ALU.mult), reads=[Bg, Bs], writes=[Bg])
                S.op("dve", lambda e, g_t=g_t, u_t=u_t, f=f, nsl=nsl, resT=resT: e.scalar_tensor_tensor(
                    out=resT[:, f, nsl], in0=u_t, scalar=1.0, in1=g_t, op0=ALU.add, op1=ALU.mult),
                    reads=[Bg, Bu], writes=[BresT])

    def down(e_):
        wd, Bwd = wdn[e_ % 2]
        bd, Bbd = bdn[e_ % 2]
        resT, BresT = resTs[e_ % 2]
        for sb in range(NSB):
            y_t, By = yst[cnts["y"] % 2]
            cnts["y"] += 1
            for nh in range(2):
                bank = 6 + nh
                pairs = [(resT[:, k, sb * 128:(sb + 1) * 128], wd[:, k, nh * 512:(nh + 1) * 512]) for k in range(8)]
                pairs.append((K.ones_b[0:1, :], bd[0:1, nh * 512:(nh + 1) * 512]))
                mm_group(S, ps[bank][:], Bps[bank], pairs, reads=[BresT, Bwd, Bbd, K.Bones_b])
                S.op("act", lambda e, bank=bank, y_t=y_t, nh=nh: e.activation(out=y_t[:, nh * 512:(nh + 1) * 512], in_=ps[bank][:], func=AF.Copy),
                     reads=[Bps[bank]], writes=[By])
            r0 = e_ * CAP + sb * 128
            S.dma("sp", lambda e, y_t=y_t, r0=r0: e.dma_start(out=K.ys[r0:r0 + 128, :], in_=y_t), reads=[By], owner=By)

    load_expert(0)
    build_xT(0)
    for e_ in range(E):
        if e_ + 1 < E:
            load_expert(e_ + 1)
        gate_up(e_)
        if e_ + 1 < E:
            build_xT(e_ + 1)
        down(e_)


def phase5_combine(K):
    S, A, nc = K.S, K.A, K.nc
    ps, Bps = K.ps, K.Bps
    wpg, Bwpg = A.alloc("wpg", [8, D], BF16)
    wpp, Bwpp = A.alloc("wpp", [2, D], BF16)
    gple, Bgple = A.alloc("gple", [D])
    S.dma("pool", lambda e: e.dma_start(out=wpg, in_=K.w_ple_gate.rearrange("(c p) n -> p c n", p=128)), writes=[Bwpg])
    S.dma("pool", lambda e: e.dma_start(out=wpp, in_=K.w_ple_proj.rearrange("(c p) n -> p c n", p=128)), writes=[Bwpp])
    S.dma("sp", lambda e: e.dma_start(out=gple, in_=bcast_row(K.g_ple, D)), writes=[Bgple])

    def tile_stream(par):
        h_t, Bh = A.alloc(f"ht{par}", [D])
        yg = [A.alloc(f"yg{par}_{k}", [D]) for k in range(4)]
        p_t, Bp = A.alloc(f"pt{par}", [PLE])
        ptb, Bptb = A.alloc(f"ptb{par}", [PLE], BF16)
        pT, BpT = A.alloc(f"pT{par}", [2, 128], BF16)
        junk, Bjunk = A.alloc(f"junk{par}", [D], BF16)
        ss_t, Bss = A.alloc(f"ss{par}", [1])
        u3, Bu3 = A.alloc(f"u3{par}", [D], BF16)
        u3T, Bu3T = A.alloc(f"u3T{par}", [8, 128], BF16)
        sgm, Bsgm = A.alloc(f"sgm{par}", [D])
        o_t, Bo = A.alloc(f"ot{par}", [D])
        b0 = par * 2

        def gen():
            for i in range(par, NTILE, 4):
                r0 = i * 128
                S.dma("sp", lambda e, r0=r0: e.dma_start(out=h_t, in_=K.h1_s[r0:r0 + 128, :]), writes=[Bh])
                S.dma("sp", lambda e, r0=r0: e.dma_start(out=p_t, in_=K.p[r0:r0 + 128, :]), writes=[Bp])
                for k in range(4):
                    y_t, By = yg[k]
                    S.dma("pool", lambda e, y_t=y_t, i=i, k=k: e.indirect_dma_start(
                        out=y_t, out_offset=None, in_=K.ys,
                        in_offset=bass.IndirectOffsetOnAxis(ap=K.dest_i[:, i * 4 + k:i * 4 + k + 1], axis=0)),
                        reads=[K.Bdest], writes=[By])
                yield
                S.op("act", lambda e: e.activation(out=ptb, in_=p_t, func=AF.Copy), reads=[Bp], writes=[Bptb])
                pv1 = ps[b0 + 1][:].bitcast(BF16)
                for c in range(2):
                    S.op("pe", lambda e, c=c, pv1=pv1: e.transpose(pv1[:, c * 128:(c + 1) * 128], ptb[:, c * 128:(c + 1) * 128], K.ident_b),
                         reads=[Bptb, K.Bident_b], writes=[Bps[b0 + 1]], signal=(c == 1))
                S.op("act", lambda e, pv1=pv1: e.activation(out=pT, in_=pv1[:, 0:256].rearrange("p (c t) -> p c t", c=2), func=AF.Copy),
                     reads=[Bps[b0 + 1]], writes=[BpT])
                yield
                for k in range(4):
                    y_t, By = yg[k]
                    S.op("dve", lambda e, y_t=y_t, i=i, k=k: e.scalar_tensor_tensor(
                        out=h_t, in0=y_t, scalar=K.gate_a[:, i * 4 + k:i * 4 + k + 1], in1=h_t, op0=ALU.mult, op1=ALU.add),
                        reads=[By, Bh, K.Bgate], writes=[Bh])
                yield
                rms_rstd(K, h_t, Bh, junk, Bjunk, ss_t, Bss, D)
                S.op("dve", lambda e: e.scalar_tensor_tensor(out=u3, in0=h_t, scalar=ss_t, in1=gple, op0=ALU.mult, op1=ALU.mult),
                     reads=[Bh, Bss, Bgple], writes=[Bu3])
                yield
                pv16 = ps[b0][:].bitcast(BF16)
                for c in range(8):
                    S.op("pe", lambda e, c=c, pv16=pv16: e.transpose(pv16[:, c * 128:(c + 1) * 128], u3[:, c * 128:(c + 1) * 128], K.ident_b),
                         reads=[Bu3, K.Bident_b], writes=[Bps[b0]], signal=(c == 7))
                S.op("act", lambda e, pv16=pv16: e.activation(out=u3T, in_=pv16.rearrange("p (c t) -> p c t", c=8), func=AF.Copy),
                     reads=[Bps[b0]], writes=[Bu3T])
                yield
                for nh in range(2):
                    nsl = slice(nh * 512, (nh + 1) * 512)
                    gbk, pbk = b0, b0 + 1
                    mm_group(S, ps[gbk][:], Bps[gbk], [(u3T[:, k, :], wpg[:, k, nsl]) for k in range(8)], reads=[Bu3T, Bwpg])
                    mm_group(S, ps[pbk][:], Bps[pbk], [(pT[:, k, :], wpp[:, k, nsl]) for k in range(2)], reads=[BpT, Bwpp])
                    S.op("act", lambda e, gbk=gbk, nsl=nsl: e.activation(out=sgm[:, nsl], in_=ps[gbk][:], func=AF.Sigmoid), reads=[Bps[gbk]], writes=[Bsgm])
                    S.op("dve", lambda e, pbk=pbk, nsl=nsl: e.tensor_tensor(out=sgm[:, nsl], in0=sgm[:, nsl], in1=ps[pbk][:], op=ALU.mult),
                         reads=[Bsgm, Bps[pbk]], writes=[Bsgm])
                    S.op("dve", lambda e, nsl=nsl: e.tensor_tensor(out=o_t[:, nsl], in0=sgm[:, nsl], in1=h_t[:, nsl], op=ALU.add),
                         reads=[Bsgm, Bh], writes=[Bo])
                    yield
                S.dma("sp", lambda e, r0=r0: e.dma_start(out=K.y[r0:r0 + 128, :], in_=o_t), reads=[Bo], owner=Bo)
        return gen()

    interleave([tile_stream(q) for q in range(4)], skew=2)


def _rope_tables():
    pos = np.arange(SEQ)
    row = (pos // 64).astype(np.float32)
    col = (pos % 64).astype(np.float32)
    inv = (10000.0 ** (-np.arange(0, 64, 2, dtype=np.float32) / 64.0)).astype(np.float32)
    C = np.zeros((128, SEQ), np.float32)
    Sg = np.zeros((128, SEQ), np.float32)
    for p in range(128):
        ids = row if p < 64 else col
        j = p % 32
        ang = (ids * inv[j]).astype(np.float32)
        C[p] = np.cos(ang)
        sgn = -1.0 if (p % 64) < 32 else 1.0
        Sg[p] = sgn * np.sin(ang)
    perm = np.zeros((128, 128), np.float32)
    for m in range(128):
        partner = m + 32 if (m % 64) < 32 else m - 32
        perm[partner, m] = 1.0
    return C, Sg, perm


_NC_CACHE = {}


def _prep_common(inp):
    f = lambda a: np.ascontiguousarray(np.asarray(a, dtype=np.float32))
    pv = np.zeros((128, NPV), np.float32)
    cw = f(inp["conv_w"])[0]
    pv[:, PV_CONVW:PV_CONVW + 32] = cw.reshape(4, 8, 128).transpose(2, 1, 0).reshape(128, 32)
    pv[:, PV_CONVB:PV_CONVB + 8] = f(inp["conv_b"])[0].reshape(8, 128).T
    pv[:, PV_BA:PV_BA + 16] = f(inp["lru_ba"])[0].reshape(16, 128).T
    pv[:, PV_BI:PV_BI + 16] = f(inp["lru_bi"])[0].reshape(16, 128).T
    pv[:, PV_LAM:PV_LAM + 16] = f(inp["lru_lam"])[0].reshape(16, 128).T
    pv[:, PV_QN] = f(inp["q_norm"])[0]
    pv[:, PV_KN] = f(inp["k_norm"])[0]
    pv[:, PV_BGU:PV_BGU + 512] = f(inp["b_gu"])[0].reshape(E * 16, 128).T
    C, Sg, perm = _rope_tables()
    tri = np.triu(np.ones((128, 128), np.float32), 1)
    eC = np.tile((np.arange(E, dtype=np.float32) * CAP)[None, :], (128, 1))
    com = {
        "w_in": f(inp["w_in"])[0], "lru_wa": f(inp["lru_wa"])[0], "lru_wi": f(inp["lru_wi"])[0],
        "w_attn_br": f(inp["w_attn_br"])[0], "w_lru_br": f(inp["w_lru_br"])[0], "w_out": f(inp["w_out"])[0],
        "w_router": f(inp["w_router"])[0], "w_gu": f(inp["w_gu"])[0], "w_dn": f(inp["w_dn"])[0],
        "b_dn": f(inp["b_dn"])[0], "w_ple_gate": f(inp["w_ple_gate"])[0], "w_ple_proj": f(inp["w_ple_proj"])[0],
        "g_mix": f(inp["g_mix"]), "g_moe": f(inp["g_moe"]), "g_ple": f(inp["g_ple"]), "b_router": f(inp["b_router"]),
        "pv": pv, "ropeC": C, "ropeS": Sg, "perm": perm, "ident": np.eye(128, dtype=np.float32), "tri": tri, "eC": eC,
    }
    return com


def kernel(**inputs):
    dbg = bool(int(os.environ.get("MK_DBG", "0")))
    ncores = int(os.environ.get("MK_NCORES", str(NCORES)))
    key = dbg
    if key not in _NC_CACHE:
        _NC_CACHE[key] = build_program(dbg)
    nc = _NC_CACHE[key]
    com = _prep_common(inputs)
    x = np.asarray(inputs["x"], dtype=np.float32)
    p = np.asarray(inputs["p"], dtype=np.float32)[0]
    in_maps = []
    for c in range(ncores):
        m = dict(com)
        m["x"] = np.ascontiguousarray(x[2 * c:2 * c + 2].reshape(T, D))
        m["p"] = np.ascontiguousarray(p[2 * c:2 * c + 2].reshape(T, PLE))
        in_maps.append(m)
    res = run_bass_kernel_spmd(nc, in_maps, core_ids=list(range(ncores)))
    if dbg:
        kernel.last = res
    out = np.zeros((16, SEQ, D), np.float32)
    for c in range(ncores):
        out[2 * c:2 * c + 2] = np.asarray(res.results[c]["y"], dtype=np.float32).reshape(2, SEQ, D)
    return out
```

```python
import os
import numpy as np
from contextlib import ExitStack
import concourse.bass as bass
import concourse.mybir as mybir
from concourse.bass_utils import run_bass_kernel_spmd

F32 = mybir.dt.float32
BF16 = mybir.dt.bfloat16
I32 = mybir.dt.int32
AF = mybir.ActivationFunctionType
ALU = mybir.AluOpType
AX = mybir.AxisListType

NCORES = 8
D = 1024
SEQ = 2048
NSEQ = 2
T = NSEQ * SEQ
NTILE = T // 128
E = 32
F = 1024
CAP = 640
NSB = CAP // 128
NSLOT = E * CAP
TRASH = NSLOT
PLE = 256
EPS = 1e-6
INW = 5632
NPV = 608
PV_CONVW, PV_CONVB, PV_BA, PV_BI, PV_LAM, PV_QN, PV_KN, PV_BGU = 0, 32, 40, 56, 72, 88, 89, 96

ENGS = ("pe", "act", "dve", "pool", "sp")


class Buf:
    __slots__ = ("name", "last_write", "reads", "sem", "sem_total", "excl")

    def __init__(self, name, excl=False):
        self.name = name
        self.excl = excl
        self.last_write = None
        self.reads = []
        self.sem = None
        self.sem_total = 0


class Sched:
    def __init__(self, nc, stack):
        self.nc = nc
        self.stack = stack
        self.stream = {e: [] for e in ENGS}
        self.sem = {e: stack.enter_context(nc.semaphore("s_" + e)) for e in ENGS}
        self.count = {e: 0 for e in ENGS}
        self.seen = {e: {} for e in ENGS}
        self.dma_bufs = []

    def _wait_tokens(self, e, toks):
        need = {}
        for t in toks:
            if t is None:
                continue
            if t[0] == "e":
                _, src, c = t
                if src == "pe" and e == "pe":
                    continue
                key = ("e", src)
                val = c
                sem = self.sem[src]
            else:
                b = t[1]
                key = ("d", id(b))
                val = b.sem_total
                sem = b.sem
            if self.seen[e].get(key, 0) >= val:
                continue
            if key not in need or need[key][1] < val:
                need[key] = (sem, val)
        for key, (sem, val) in need.items():
            self.seen[e][key] = val
            self.stream[e].append(lambda eng, sem=sem, val=val: eng.wait_ge(sem, val))

    @staticmethod
    def _deps(reads, writes):
        toks = []
        for r in reads:
            toks.append(r.last_write)
            if r.excl:
                toks.extend(r.reads)
        for w in writes:
            toks.append(w.last_write)
            toks.extend(w.reads)
        return toks

    def op(self, e, fn, reads=(), writes=(), signal=True):
        self._wait_tokens(e, self._deps(reads, writes))
        if signal:
            self.count[e] += 1
            tok = ("e", e, self.count[e])
            sem = self.sem[e]
            self.stream[e].append(lambda eng, fn=fn, sem=sem: fn(eng).then_inc(sem, 1))
        else:
            tok = ("e", e, self.count[e] + 1)
            self.stream[e].append(lambda eng, fn=fn: fn(eng))
        for w in writes:
            w.last_write = tok
            w.reads = []
        for r in reads:
            r.reads.append(tok)
        return tok

    def dma(self, e, fn, reads=(), writes=(), owner=None):
        if owner is None:
            owner = writes[0] if writes else reads[0]
        if owner.sem is None:
            owner.sem = self.stack.enter_context(self.nc.semaphore("d%d_%s" % (len(self.dma_bufs), owner.name)))
            self.dma_bufs.append(owner)
        self._wait_tokens(e, self._deps(reads, writes))
        owner.sem_total += 16
        sem = owner.sem
        self.stream[e].append(lambda eng, fn=fn, sem=sem: fn(eng).then_inc(sem, 16))
        tok = ("d", owner)
        for w in writes:
            w.last_write = tok
            w.reads = []
        for r in reads:
            r.reads.append(tok)
        return tok

    def barrier(self):
        toks = [("e", s, self.count[s]) for s in ENGS if self.count[s] > 0]
        toks += [("d", b) for b in self.dma_bufs]
        for e in ENGS:
            self._wait_tokens(e, toks)

    def emit(self):
        nc = self.nc
        self.barrier()
        with nc.Block() as block:
            for e, reg in (("sp", block.sync), ("act", block.scalar), ("pe", block.tensor),
                           ("dve", block.vector), ("pool", block.gpsimd)):
                lst = self.stream[e]
                if not lst:
                    continue

                def body(eng, lst=lst):
                    for f in lst:
                        f(eng)
                reg(body)


def _dsize(dt):
    return 2 if dt == BF16 else 4


class Arena:
    def __init__(self, nc, stack, nbytes):
        self.t = stack.enter_context(nc.sbuf_tensor("arena", [128, nbytes // 4], F32))
        self.off = 0
        self.nbytes = nbytes
        self.peak = 0

    def alloc(self, name, free, dt=F32):
        n = 1
        for f in free:
            n *= f
        sz = (n * _dsize(dt) + 31) // 32 * 32
        assert self.off + sz <= self.nbytes, (name, self.off, sz, self.nbytes)
        a = self.t[:, self.off // 4:(self.off + sz) // 4]
        if dt != F32:
            a = a.bitcast(dt)
        a = a[:, 0:n]
        if len(free) == 2:
            a = a.rearrange("p (a b) -> p a b", a=free[0])
        self.off += sz
        self.peak = max(self.peak, self.off)
        return a, Buf(name)

    def mark(self):
        return self.off

    def release(self, m):
        self.off = m


class Ctx:
    pass


def build_program(dbg=False):
    nc = bass.Bass("TRN2", target_bir_lowering=False)
    K = Ctx()
    K.nc = nc

    def din(name, shape, dt=F32):
        return nc.dram_tensor(name, list(shape), dt, kind="ExternalInput")

    K.x = din("x", [T, D]).ap()
    K.p = din("p", [T, PLE]).ap()
    K.w_in = din("w_in", [D, INW]).ap()
    K.lru_wa = din("lru_wa", [2, 8, 128, 128]).ap()
    K.lru_wi = din("lru_wi", [2, 8, 128, 128]).ap()
    K.w_attn_br = din("w_attn_br", [D, D]).ap()
    K.w_lru_br = din("w_lru_br", [D, D]).ap()
    K.w_out = din("w_out", [D, D]).ap()
    K.w_router = din("w_router", [D, E]).ap()
    K.w_gu = din("w_gu", [E, D, 2 * F]).ap()
    K.w_dn = din("w_dn", [E, F, D]).ap()
    K.b_dn = din("b_dn", [E, D]).ap()
    K.w_ple_gate = din("w_ple_gate", [D, D]).ap()
    K.w_ple_proj = din("w_ple_proj", [PLE, D]).ap()
    K.g_mix = din("g_mix", [1, D])
    K.g_moe = din("g_moe", [1, D])
    K.g_ple = din("g_ple", [1, D])
    K.b_router = din("b_router", [1, E])
    K.pv = din("pv", [128, NPV]).ap()
    K.ropeC = din("ropeC", [128, SEQ]).ap()
    K.ropeS = din("ropeS", [128, SEQ]).ap()
    K.perm = din("perm", [128, 128]).ap()
    K.ident = din("ident", [128, 128]).ap()
    K.tri = din("tri", [128, 128]).ap()
    K.eC = din("eC", [128, E]).ap()
    K.y = nc.dram_tensor("y", [T, D], F32, kind="ExternalOutput").ap()

    kind = "ExternalOutput" if dbg else "Internal"

    def dscr(name, shape, dt):
        if dbg:
            return nc.dram_tensor(name, list(shape), dt, kind="ExternalOutput").ap()
        return nc.dram_tensor(name, list(shape), dt).ap()

    K.qT_s = dscr("qT_s", [NSEQ, 8, 128, SEQ], BF16)
    K.kT_s = dscr("kT_s", [NSEQ, 2, 128, SEQ], BF16)
    K.V_s = dscr("V_s", [NSEQ, 128, 16, 256], BF16)
    K.yl_s = dscr("yl_s", [NSEQ, 8, 128, SEQ], BF16)
    K.sga_s = dscr("sga_s", [NSEQ, 8, 128, SEQ], BF16)
    K.sgr_s = dscr("sgr_s", [NSEQ, 8, 128, SEQ], BF16)
    K.h1_s = dscr("h1_s", [T, D], F32)
    K.xs = dscr("xs_s", [NSLOT + 128, D], BF16)
    K.ys = dscr("ys_s", [NSLOT + 128, D], F32)

    with ExitStack() as st:
        S = Sched(nc, st)
        K.S = S
        A = Arena(nc, st, 204 * 1024)
        K.A = A
        K.ps = []
        K.Bps = []
        for i in range(8):
            K.ps.append(st.enter_context(nc.psum_tensor(f"ps{i}", [128, 512], F32)))
            K.Bps.append(Buf(f"ps{i}", excl=True))
        stop = int(os.environ.get("MK_STOP", "9"))
        phase0_consts(K)
        S.barrier()
        m0 = A.mark()
        if stop >= 1:
            phase1_inproj(K)
            S.barrier()
        A.release(m0)
        if stop >= 2:
            phase2_attn(K)
            S.barrier()
        A.release(m0)
        if stop >= 3:
            phase3_router(K)
            S.barrier()
        A.release(m0)
        if stop >= 4:
            phase4_experts(K)
            S.barrier()
        A.release(m0)
        if stop >= 5:
            phase5_combine(K)
        S.emit()
    return nc


def bcast_row(dt_tensor, n):
    return bass.AP(dt_tensor, 0, [[0, 128], [1, n]])


def phase0_consts(K):
    S, A = K.S, K.A
    K.ident_f, K.Bident_f = A.alloc("ident_f", [128])
    K.ident_b, K.Bident_b = A.alloc("ident_b", [128], BF16)
    K.ones_b, K.Bones_b = A.alloc("ones_b", [128], BF16)
    K.ones_f, K.Bones_f = A.alloc("ones_f", [128])
    K.pvt, K.Bpv = A.alloc("pvt", [NPV])
    K.kk, K.Bkk = A.alloc("kk", [16])
    K.dest_i, K.Bdest = A.alloc("dest_i", [NTILE * 4], I32)
    K.gate_a, K.Bgate = A.alloc("gate_a", [NTILE * 4])
    S.dma("sp", lambda e: e.dma_start(out=K.ident_f, in_=K.ident), writes=[K.Bident_f])
    S.dma("pool", lambda e: e.dma_start(out=K.ident_b, in_=K.ident), writes=[K.Bident_b])
    S.dma("sp", lambda e: e.dma_start(out=K.pvt, in_=K.pv), writes=[K.Bpv])
    S.op("dve", lambda e: e.memset(K.ones_b, 1.0), writes=[K.Bones_b])
    S.op("dve", lambda e: e.memset(K.ones_f, 1.0), writes=[K.Bones_f])
    lam = K.pvt[:, PV_LAM:PV_LAM + 16]
    S.op("act", lambda e: e.activation(out=K.kk, in_=lam, func=AF.Exp, scale=-1.0), reads=[K.Bpv], writes=[K.Bkk])
    S.op("act", lambda e: e.activation(out=K.kk, in_=K.kk, func=AF.Ln, bias=1.0), reads=[K.Bkk], writes=[K.Bkk])
    S.op("dve", lambda e: e.tensor_scalar(out=K.kk, in0=K.kk, scalar1=-8.0, scalar2=None, op0=ALU.mult),
         reads=[K.Bkk], writes=[K.Bkk])


def mm_group(S, out, Bout, pairs, reads):
    n = len(pairs)
    for i, (l, r) in enumerate(pairs):
        S.op("pe", lambda e, l=l, r=r, i=i: e.matmul(out, l, r, start=(i == 0), stop=(i == n - 1)),
             reads=reads, writes=[Bout], signal=(i == n - 1))


def rms_rstd(K, src, Bsrc, junk, Bjunk, ss, Bss, n):
    S = K.S
    S.op("act", lambda e: e.activation(out=junk, in_=src, func=AF.Square, accum_out=ss),
         reads=[Bsrc], writes=[Bjunk, Bss])
    S.op("act", lambda e: e.activation(out=ss, in_=ss, func=AF.Sqrt, bias=EPS, scale=1.0 / n),
         reads=[Bss], writes=[Bss])
    S.op("dve", lambda e: e.reciprocal(out=ss, in_=ss), reads=[Bss], writes=[Bss])


def interleave(gens, skew=0):
    gens = list(gens)
    start = {id(g): i * skew for i, g in enumerate(gens)}
    rnd = 0
    while gens:
        for g in list(gens):
            if rnd < start[id(g)]:
                continue
            try:
                next(g)
            except StopIteration:
                gens.remove(g)
        rnd += 1


def phase1_inproj(K):
    S, A, nc = K.S, K.A, K.nc
    ps, Bps = K.ps, K.Bps
    uT, BuT = A.alloc("uT", [8, SEQ], BF16)
    wtA = [A.alloc(f"wtA{i}", [8, 512], BF16) for i in range(2)]
    wtB = [A.alloc(f"wtB{i}", [8, 256], BF16) for i in range(2)]
    ropeS, BropeS = A.alloc("ropeS", [SEQ])
    cq, Bcq = A.alloc("cq", [SEQ])
    ck, Bck = A.alloc("ck", [SEQ])
    permf, Bpermf = A.alloc("permf", [128])
    permq, Bpermq = A.alloc("permq", [128], BF16)
    permk, Bpermk = A.alloc("permk", [128], BF16)
    od_b, Bod = A.alloc("od_b", [128], BF16)
    wa_b, Bwa = A.alloc("wa_b", [16, 128], BF16)
    wi_b, Bwi = A.alloc("wi_b", [16, 128], BF16)
    xn, Bxn = A.alloc("xn", [D], BF16)
    junk, Bjunk = xn, Bxn
    xns = [(xn, Bxn), A.alloc("xn2", [D], BF16)]
    ss = [A.alloc(f"ss{i}", [1]) for i in range(2)]
    stgA = [A.alloc(f"stgA{i}", [SEQ], BF16) for i in range(2)]
    stgB = [A.alloc(f"stgB{i}", [SEQ], BF16) for i in range(1)]
    xq = [A.alloc(f"xq{i}", [512], BF16) for i in range(2)]
    sq = [A.alloc(f"sq{i}", [512], BF16) for i in range(2)]
    ta = [A.alloc(f"ta{i}", [512]) for i in range(2)]
    tb_ = [A.alloc(f"tb{i}", [512]) for i in range(2)]
    rst = [A.alloc(f"rst{i}", [512]) for i in range(2)]
    xrp, Bxrp = A.alloc("xrp", [SEQ + 4])
    cc, Bcc = A.alloc("cc", [SEQ])
    ccb, Bccb = A.alloc("ccb", [SEQ], BF16)
    aas = [A.alloc(f"aa{i}", [SEQ]) for i in range(2)]
    bts = [A.alloc(f"bt{i}", [SEQ]) for i in range(2)]
    aa, Baa = aas[0]
    t1, Bt1 = A.alloc("t1", [SEQ])
    gmix, Bgmix = cc[:, 0:D], Bcc
    hf, Bhf = A.alloc("hf", [SEQ])
    hb, Bhb = A.alloc("hb", [SEQ])
    xt = [(hb[:, 0:D], Bhb), (hf[:, 0:D], Bhf)]
    vst, Bvst = aa.bitcast(BF16).rearrange("p (a b) -> p a b", a=16), Baa

    pvt = K.pvt
    S.dma("sp", lambda e: e.dma_start(out=cq, in_=K.ropeC), writes=[Bcq])
    S.dma("sp", lambda e: e.dma_start(out=ck, in_=K.ropeC), writes=[Bck])
    S.dma("sp", lambda e: e.dma_start(out=ropeS, in_=K.ropeS), writes=[BropeS])
    S.dma("sp", lambda e: e.dma_start(out=permf, in_=K.perm), writes=[Bpermf])
    S.dma("pool", lambda e: e.dma_start(out=wa_b, in_=K.lru_wa.rearrange("d c p n -> p (d c) n")), writes=[Bwa])
    S.dma("pool", lambda e: e.dma_start(out=wi_b, in_=K.lru_wi.rearrange("d c p n -> p (d c) n")), writes=[Bwi])
    S.op("dve", lambda e: e.memset(od_b, 1.0 / 128.0), writes=[Bod])
    S.op("dve", lambda e: e.memset(xrp, 0.0), writes=[Bxrp])
    qn = pvt[:, PV_QN:PV_QN + 1]
    kn = pvt[:, PV_KN:PV_KN + 1]
    S.op("dve", lambda e: e.tensor_scalar(out=cq, in0=cq, scalar1=qn, scalar2=None, op0=ALU.mult),
         reads=[Bcq, K.Bpv], writes=[Bcq])
    S.op("dve", lambda e: e.tensor_scalar(out=ck, in0=ck, scalar1=kn, scalar2=None, op0=ALU.mult),
         reads=[Bck, K.Bpv], writes=[Bck])
    S.op("dve", lambda e: e.tensor_scalar(out=permq, in0=permf, scalar1=qn, scalar2=None, op0=ALU.mult),
         reads=[Bpermf, K.Bpv], writes=[Bpermq])
    S.op("dve", lambda e: e.tensor_scalar(out=permk, in0=permf, scalar1=kn, scalar2=None, op0=ALU.mult),
         reads=[Bpermf, K.Bpv], writes=[Bpermk])

    w_in_v = K.w_in.rearrange("(c p) n -> p c n", p=128)
    wcnt = {"A": 0, "B": 0}

    def load_w(which, cols):
        tiles = wtA if which == "A" else wtB
        i = wcnt[which] % 2
        wcnt[which] += 1
        w, Bw = tiles[i]
        off = 0
        for (c0, wd) in cols:
            S.dma("pool", lambda e, w=w, off=off, c0=c0, wd=wd: e.dma_start(
                out=w[:, :, off:off + wd], in_=w_in_v[:, :, c0:c0 + wd]), writes=[Bw])
            off += wd
        return w, Bw

    def inproj(w, Bw, woff, tb, bank):
        mm_group(S, ps[bank][:], Bps[bank],
                 [(w[:, k, woff:woff + 128], uT[:, k, tb * 512:(tb + 1) * 512]) for k in range(8)],
                 reads=[Bw, BuT])

    scnt = {"A": 0, "B": 0}

    def build_uT(s):
        S.dma("sp", lambda e: e.dma_start(out=gmix, in_=bcast_row(K.g_mix, D)), writes=[Bgmix])

        def tiles(par):
            x_t, Bx = xt[par]
            ss_t, Bss = ss[par]
            xn_, Bxn_ = xns[par]
            bank = par
            pv16 = ps[bank][:].bitcast(BF16)
            for i in range(par, 16, 2):
                r0 = s * SEQ + i * 128
                S.dma("sp", lambda e, r0=r0: e.dma_start(out=x_t, in_=K.x[r0:r0 + 128, :]), writes=[Bx])
                rms_rstd(K, x_t, Bx, xn_, Bxn_, ss_t, Bss, D)
                yield
                S.op("dve", lambda e: e.scalar_tensor_tensor(
                    out=xn_, in0=x_t, scalar=ss_t, in1=gmix, op0=ALU.mult, op1=ALU.mult),
                    reads=[Bx, Bss, Bgmix], writes=[Bxn_])
                yield
                for c in range(8):
                    S.op("pe", lambda e, c=c: e.transpose(pv16[:, c * 128:(c + 1) * 128], xn_[:, c * 128:(c + 1) * 128], K.ident_b),
                         reads=[Bxn_, K.Bident_b], writes=[Bps[bank]], signal=(c == 7))
                S.op("act", lambda e, i=i: e.activation(
                    out=uT[:, :, i * 128:(i + 1) * 128], in_=pv16.rearrange("p (c t) -> p c t", c=8), func=AF.Copy),
                    reads=[Bps[bank]], writes=[BuT])
                yield

        interleave([tiles(0), tiles(1)], skew=1)

    def qk_head(s, w, Bw, woff, is_q, hidx):
        cg, Bcg = (cq, Bcq) if is_q else (ck, Bck)
        pm, Bpm = (permq, Bpermq) if is_q else (permk, Bpermk)
        st_t, Bst = stgA[scnt["A"] % 2]
        scnt["A"] += 1
        for tb in range(4):
            p = tb % 2
            sl = slice(tb * 512, (tb + 1) * 512)
            xq_, Bxq = xq[p]
            sq_, Bsq = sq[p]
            ta_, Bta = ta[p]
            tb2, Btb = tb_[p]
            rs_, Brst = rst[p]
            zb = p
            inproj(w, Bw, woff, tb, zb)
            S.op("act", lambda e, xq_=xq_, zb=zb: e.activation(out=xq_, in_=ps[zb][:], func=AF.Copy), reads=[Bps[zb]], writes=[Bxq])
            S.op("act", lambda e, sq_=sq_, zb=zb: e.activation(out=sq_, in_=ps[zb][:], func=AF.Square), reads=[Bps[zb]], writes=[Bsq])
            S.op("dve", lambda e, sl=sl, cg=cg, ta_=ta_, zb=zb: e.tensor_tensor(out=ta_, in0=ps[zb][:], in1=cg[:, sl], op=ALU.mult),
                 reads=[Bps[zb], Bcg], writes=[Bta])
            yield
            mm_group(S, ps[2][:], Bps[2], [(od_b, sq_)], reads=[Bod, Bsq])
            mm_group(S, ps[3][:], Bps[3], [(pm, xq_)], reads=[Bpm, Bxq])
            S.op("act", lambda e, rs_=rs_: e.activation(out=rs_, in_=ps[2][:], func=AF.Sqrt, bias=EPS), reads=[Bps[2]], writes=[Brst])
            S.op("dve", lambda e, sl=sl, tb2=tb2: e.tensor_tensor(out=tb2, in0=ps[3][:], in1=ropeS[:, sl], op=ALU.mult),
                 reads=[Bps[3], BropeS], writes=[Btb])
            yield
            S.op("dve", lambda e, rs_=rs_: e.reciprocal(out=rs_, in_=rs_), reads=[Brst], writes=[Brst])
            S.op("dve", lambda e, ta_=ta_, tb2=tb2: e.tensor_tensor(out=ta_, in0=ta_, in1=tb2, op=ALU.add), reads=[Bta, Btb], writes=[Bta])
            S.op("dve", lambda e, sl=sl, st_t=st_t, ta_=ta_, rs_=rs_: e.tensor_tensor(out=st_t[:, sl], in0=ta_, in1=rs_, op=ALU.mult),
                 reads=[Bta, Brst], writes=[Bst])
            yield
        dst = (K.qT_s if is_q else K.kT_s)[s, hidx]
        S.dma("sp", lambda e, st_t=st_t, dst=dst: e.dma_start(out=dst, in_=st_t), reads=[Bst], owner=Bst)

    def stream_A(s):
        for t2 in range(2):
            w, Bw = load_w("A", [(t2 * 512, 512)])
            for j in range(4):
                yield from qk_head(s, w, Bw, j * 128, True, t2 * 4 + j)
        w, Bw = load_w("A", [(1024, 512)])
        for j in range(2):
            yield from qk_head(s, w, Bw, j * 128, False, j)
        for gi, dst_s in ((0, K.sga_s), (1, K.sgr_s)):
            for t2 in range(2):
                wg, Bwg = load_w("A", [(3584 + gi * 1024 + t2 * 512, 512)])
                for j in range(4):
                    st_t, Bst = stgA[scnt["A"] % 2]
                    scnt["A"] += 1
                    for tb in range(4):
                        bank = tb % 2
                        inproj(wg, Bwg, j * 128, tb, bank)
                        S.op("act", lambda e, bank=bank, tb=tb, st_t=st_t: e.activation(out=st_t[:, tb * 512:(tb + 1) * 512], in_=ps[bank][:], func=AF.Sigmoid),
                             reads=[Bps[bank]], writes=[Bst])
                        yield
                    S.dma("sp", lambda e, st_t=st_t, dst=dst_s[s, t2 * 4 + j]: e.dma_start(out=dst, in_=st_t), reads=[Bst], owner=Bst)

    def do_V(s):
        w, Bw = load_w("A", [(1024, 512)])
        for tk in range(16):
            bank = 2 + tk % 2
            mm_group(S, ps[bank][:, 0:256], Bps[bank],
                     [(uT[:, k, tk * 128:(tk + 1) * 128], w[:, k, 256:512]) for k in range(8)], reads=[Bw, BuT])
            S.op("act", lambda e, bank=bank, tk=tk: e.activation(out=vst[:, tk, :], in_=ps[bank][:, 0:256], func=AF.Copy),
                 reads=[Bps[bank]], writes=[Bvst])
        S.dma("sp", lambda e, s=s: e.dma_start(out=K.V_s[s], in_=vst), reads=[Bvst], owner=Bvst)

    def stream_B(s):
        for j in range(8):
            w, Bw = load_w("B", [(1536 + j * 128, 128), (2560 + j * 128, 128)])
            for tb in range(4):
                bank = 4 + tb % 2
                inproj(w, Bw, 0, tb, bank)
                S.op("act", lambda e, bank=bank, tb=tb: e.activation(out=xrp[:, 2 + tb * 512:2 + (tb + 1) * 512], in_=ps[bank][:], func=AF.Copy),
                     reads=[Bps[bank]], writes=[Bxrp])
                yield
            cw = lambda jj, j=j: K.pvt[:, PV_CONVW + j * 4 + jj:PV_CONVW + j * 4 + jj + 1]
            cb = K.pvt[:, PV_CONVB + j:PV_CONVB + j + 1]
            S.op("act", lambda e, cw=cw, cb=cb: e.activation(out=cc, in_=xrp[:, 0:SEQ], func=AF.Identity, bias=cb, scale=cw(0)),
                 reads=[Bxrp, K.Bpv], writes=[Bcc])
            for jj in range(1, 4):
                S.op("dve", lambda e, cw=cw, jj=jj: e.scalar_tensor_tensor(out=cc, in0=xrp[:, jj:jj + SEQ], scalar=cw(jj), in1=cc, op0=ALU.mult, op1=ALU.add),
                     reads=[Bxrp, Bcc, K.Bpv], writes=[Bcc])
                yield
            S.op("act", lambda e: e.activation(out=ccb, in_=cc, func=AF.Copy), reads=[Bcc], writes=[Bccb])

            def dir_gen(d, j=j):
                aa_, Baa_ = aas[d]
                bt_, Bbt_ = bts[d]
                hh, Bhh = (hf, Bhf) if d == 0 else (hb, Bhb)
                ba = K.pvt[:, PV_BA + d * 8 + j:PV_BA + d * 8 + j + 1]
                bi = K.pvt[:, PV_BI + d * 8 + j:PV_BI + d * 8 + j + 1]
                kkc = K.kk[:, d * 8 + j:d * 8 + j + 1]
                for tb in range(4):
                    sl = slice(tb * 512, (tb + 1) * 512)
                    mm_group(S, ps[6][:], Bps[6], [(wa_b[:, d * 8 + j, :], ccb[:, sl])], reads=[Bwa, Bccb])
                    S.op("act", lambda e, sl=sl: e.activation(out=aa_[:, sl], in_=ps[6][:], func=AF.Sigmoid, bias=ba),
                         reads=[Bps[6], K.Bpv], writes=[Baa_])
                    mm_group(S, ps[7][:], Bps[7], [(wi_b[:, d * 8 + j, :], ccb[:, sl])], reads=[Bwi, Bccb])
                    S.op("act", lambda e, sl=sl: e.activation(out=bt_[:, sl], in_=ps[7][:], func=AF.Sigmoid, bias=bi),
                         reads=[Bps[7], K.Bpv], writes=[Bbt_])
                    yield
                S.op("act", lambda e: e.activation(out=aa_, in_=aa_, func=AF.Exp, scale=kkc), reads=[Baa_, K.Bkk], writes=[Baa_])
                S.op("dve", lambda e: e.tensor_tensor(out=bt_, in0=bt_, in1=cc, op=ALU.mult), reads=[Bbt_, Bcc], writes=[Bbt_])
                yield
                S.op("act", lambda e: e.activation(out=hh, in_=aa_, func=AF.Square), reads=[Baa_], writes=[Bhh])
                S.op("act", lambda e: e.activation(out=hh, in_=hh, func=AF.Sqrt, bias=1.0, scale=-1.0), reads=[Bhh], writes=[Bhh])
                yield
                S.op("dve", lambda e: e.tensor_tensor(out=bt_, in0=bt_, in1=hh, op=ALU.mult), reads=[Bbt_, Bhh], writes=[Bbt_])
                yield
                if d == 0:
                    S.op("dve", lambda e: e.tensor_tensor_scan(out=hh, data0=aa_, data1=bt_, initial=0.0, op0=ALU.mult, op1=ALU.add),
                         reads=[Baa_, Bbt_], writes=[Bhh])
                else:
                    S.op("dve", lambda e: e.tensor_tensor_scan(out=hh[:, ::-1], data0=aa_[:, ::-1], data1=bt_[:, ::-1], initial=0.0, op0=ALU.mult, op1=ALU.add),
                         reads=[Baa_, Bbt_], writes=[Bhh])
                yield

            def gelu_gen(w=w, Bw=Bw):
                for tb in range(4):
                    sl = slice(tb * 512, (tb + 1) * 512)
                    bank = 4 + tb % 2
                    inproj(w, Bw, 128, tb, bank)
                    S.op("act", lambda e, bank=bank, sl=sl: e.activation(out=t1[:, sl], in_=ps[bank][:], func=AF.Square), reads=[Bps[bank]], writes=[Bt1])
                    S.op("act", lambda e, sl=sl: e.activation(out=t1[:, sl], in_=t1[:, sl], func=AF.Identity, bias=1.0, scale=0.044715),
                         reads=[Bt1], writes=[Bt1])
                    S.op("dve", lambda e, bank=bank, sl=sl: e.tensor_tensor(out=t1[:, sl], in0=t1[:, sl], in1=ps[bank][:], op=ALU.mult),
                         reads=[Bt1, Bps[bank]], writes=[Bt1])
                    S.op("act", lambda e, sl=sl: e.activation(out=t1[:, sl], in_=t1[:, sl], func=AF.Sigmoid, scale=1.5957691216), reads=[Bt1], writes=[Bt1])
                    S.op("dve", lambda e, bank=bank, sl=sl: e.tensor_tensor(out=t1[:, sl], in0=t1[:, sl], in1=ps[bank][:], op=ALU.mult),
                         reads=[Bt1, Bps[bank]], writes=[Bt1])
                    yield

            subs = [dir_gen(0), dir_gen(1), gelu_gen()]
            while subs:
                for g in list(subs):
                    try:
                        next(g)
                    except StopIteration:
                        subs.remove(g)
                yield
            S.op("dve", lambda e: e.tensor_tensor(out=hf, in0=hf, in1=hb, op=ALU.add), reads=[Bhf, Bhb], writes=[Bhf])
            st_t, Bst = stgB[0]
            S.op("dve", lambda e, st_t=st_t: e.tensor_tensor(out=st_t, in0=hf, in1=t1, op=ALU.mult), reads=[Bhf, Bt1], writes=[Bst])
            S.dma("sp", lambda e, st_t=st_t, j=j, s=s: e.dma_start(out=K.yl_s[s, j], in_=st_t), reads=[Bst], owner=Bst)
            yield

    for s in range(NSEQ):
        build_uT(s)
        interleave([stream_A(s), stream_B(s)])
        do_V(s)


def phase2_attn(K):
    S, A, nc = K.S, K.A, K.nc
    ps, Bps = K.ps, K.Bps
    wab, Bwab = A.alloc("wab", [8, D], BF16)
    wlb, Bwlb = A.alloc("wlb", [8, D], BF16)
    wo, Bwo = A.alloc("wo", [8, D], BF16)
    kT, BkT = A.alloc("kT", [2, SEQ], BF16)
    V, BV = A.alloc("V", [16, 256], BF16)
    qT = [A.alloc(f"qT{i}", [8, 512], BF16) for i in range(2)]
    ylb = [A.alloc(f"ylb{i}", [8, 512], BF16) for i in range(2)]
    gab = [A.alloc(f"gab{i}", [8, 512], BF16) for i in range(2)]
    grb = [A.alloc(f"grb{i}", [8, 512], BF16) for i in range(2)]
    PT = [A.alloc(f"PT{i}", [512], BF16) for i in range(6)]
    SBANK = (0, 1, 2, 7)
    attnT, BattnT = A.alloc("attnT", [8, 512], BF16)
    mrg, Bmrg = A.alloc("mrg", [8, 512], BF16)
    rz, Brz = A.alloc("rz", [512])
    zacc = [A.alloc(f"zacc{i}", [512]) for i in range(2)]
    zab = [A.alloc(f"zab{i}", [512], BF16) for i in range(2)]
    m1, Bm1 = A.alloc("m1", [512])
    m2, Bm2 = A.alloc("m2", [512])
    xt = [A.alloc(f"xt{i}", [D]) for i in range(2)]
    ho = [A.alloc(f"ho{i}", [D]) for i in range(2)]
    S.dma("pool", lambda e: e.dma_start(out=wab, in_=K.w_attn_br.rearrange("(c p) n -> p c n", p=128)), writes=[Bwab])
    S.dma("pool", lambda e: e.dma_start(out=wlb, in_=K.w_lru_br.rearrange("(c p) n -> p c n", p=128)), writes=[Bwlb])
    S.dma("pool", lambda e: e.dma_start(out=wo, in_=K.w_out.rearrange("(c p) n -> p c n", p=128)), writes=[Bwo])
    zt, Bzt = A.alloc("zt", [D], BF16)
    S.op("dve", lambda e: e.memset(zt, 0.0), writes=[Bzt])
    zrows = list(range(0, NSLOT + 128, 128))

    def zero_some(n):
        for _ in range(n):
            if zrows:
                r0 = zrows.pop(0)
                S.dma("sp", lambda e, r0=r0: e.dma_start(out=K.xs[r0:r0 + 128, :], in_=zt), reads=[Bzt], owner=Bzt)
    scale = 128.0 ** -0.5
    cnt = 0
    for s in range(NSEQ):
        S.dma("sp", lambda e, s=s: e.dma_start(out=kT, in_=K.kT_s[s].rearrange("h p t -> p h t")), writes=[BkT])
        S.dma("sp", lambda e, s=s: e.dma_start(out=V, in_=K.V_s[s]), writes=[BV])
        for qb in range(4):
            q, Bq = qT[cnt % 2]
            yl, Byl = ylb[cnt % 2]
            ga, Bga = gab[cnt % 2]
            gr, Bgr = grb[cnt % 2]
            cnt += 1
            tsl = slice(qb * 512, (qb + 1) * 512)
            S.dma("sp", lambda e, q=q, s=s, tsl=tsl: e.dma_start(out=q, in_=K.qT_s[s].rearrange("h p t -> p h t")[:, :, tsl]), writes=[Bq])
            S.dma("sp", lambda e, yl=yl, s=s, tsl=tsl: e.dma_start(out=yl, in_=K.yl_s[s].rearrange("h p t -> p h t")[:, :, tsl]), writes=[Byl])
            S.dma("sp", lambda e, ga=ga, s=s, tsl=tsl: e.dma_start(out=ga, in_=K.sga_s[s].rearrange("h p t -> p h t")[:, :, tsl]), writes=[Bga])
            S.dma("sp", lambda e, gr=gr, s=s, tsl=tsl: e.dma_start(out=gr, in_=K.sgr_s[s].rearrange("h p t -> p h t")[:, :, tsl]), writes=[Bgr])
            for h in range(8):
                kv = h // 4
                ob, zb = 3 + h % 2, 5 + h % 2

                def score(kc):
                    bank = SBANK[kc % 4]
                    mm_group(S, ps[bank][:], Bps[bank], [(kT[:, kv, kc * 128:(kc + 1) * 128], q[:, h, :])], reads=[BkT, Bq])
                    pt, Bpt = PT[kc % 6]
                    S.op("act", lambda e, bank=bank, pt=pt: e.activation(out=pt, in_=ps[bank][:], func=AF.Exp, scale=scale),
                         reads=[Bps[bank]], writes=[Bpt])

                za, Bza = zacc[h % 2]
                zb16, Bzb16 = zab[h % 2]

                def pv(kc):
                    pt, Bpt = PT[kc % 6]
                    S.op("pe", lambda e, kc=kc, pt=pt, ob=ob, kv=kv: e.matmul(ps[ob][:], V[:, kc, kv * 128:(kv + 1) * 128], pt, start=(kc == 0), stop=(kc == 15)),
                         reads=[BV, Bpt], writes=[Bps[ob]], signal=(kc == 15))
                    if kc % 2 == 1:
                        S.op("pe", lambda e, kc=kc, pt=pt, zb=zb: e.matmul(ps[zb][:], K.ones_b, pt, start=(kc == 1), stop=False),
                             reads=[K.Bones_b, Bpt], writes=[Bps[zb]], signal=False)
                    elif kc == 0:
                        S.op("dve", lambda e, pt=pt, za=za: e.tensor_copy(out=za, in_=pt), reads=[Bpt], writes=[Bza])
                    else:
                        S.op("dve", lambda e, pt=pt, za=za: e.tensor_tensor(out=za, in0=za, in1=pt, op=ALU.add), reads=[Bpt, Bza], writes=[Bza])
                    if kc == 15:
                        S.op("pe", lambda e, za=za, zb=zb: e.matmul(ps[zb][:], K.ones_f, za, start=False, stop=True),
                             reads=[K.Bones_f, Bza], writes=[Bps[zb]], signal=True)

                score(0)
                score(1)
                score(2)
                for kc in range(16):
                    if kc + 3 < 16:
                        score(kc + 3)
                    pv(kc)
                S.op("dve", lambda e, zb=zb: e.reciprocal(out=rz, in_=ps[zb][:]), reads=[Bps[zb]], writes=[Brz])
                S.op("dve", lambda e, ob=ob, h=h: e.tensor_tensor(out=attnT[:, h, :], in0=ps[ob][:], in1=rz, op=ALU.mult),
                     reads=[Bps[ob], Brz], writes=[BattnT])
                zero_some(6)
            for m in range(8):
                b1, b2 = 0 + m % 2, 2 + m % 2
                mm_group(S, ps[b1][:], Bps[b1], [(wab[:, k, m * 128:(m + 1) * 128], attnT[:, k, :]) for k in range(8)], reads=[Bwab, BattnT])
                mm_group(S, ps[b2][:], Bps[b2], [(wlb[:, k, m * 128:(m + 1) * 128], yl[:, k, :]) for k in range(8)], reads=[Bwlb, Byl])
                S.op("dve", lambda e, b1=b1, m=m, ga=ga: e.tensor_tensor(out=m1, in0=ps[b1][:], in1=ga[:, m, :], op=ALU.mult),
                     reads=[Bps[b1], Bga], writes=[Bm1])
                S.op("dve", lambda e, b2=b2, m=m, gr=gr: e.tensor_tensor(out=m2, in0=ps[b2][:], in1=gr[:, m, :], op=ALU.mult),
                     reads=[Bps[b2], Bgr], writes=[Bm2])
                S.op("dve", lambda e, m=m: e.tensor_tensor(out=mrg[:, m, :], in0=m1, in1=m2, op=ALU.add),
                     reads=[Bm1, Bm2], writes=[Bmrg])
            for tk in range(4):
                x_t, Bx = xt[tk % 2]
                h_t, Bh = ho[tk % 2]
                r0 = s * SEQ + qb * 512 + tk * 128
                S.dma("sp", lambda e, x_t=x_t, r0=r0: e.dma_start(out=x_t, in_=K.x[r0:r0 + 128, :]), writes=[Bx])
                for nh in range(2):
                    bank = 5 + nh
                    mm_group(S, ps[bank][:], Bps[bank],
                             [(mrg[:, k, tk * 128:(tk + 1) * 128], wo[:, k, nh * 512:(nh + 1) * 512]) for k in range(8)], reads=[Bmrg, Bwo])
                    S.op("dve", lambda e, bank=bank, nh=nh, x_t=x_t, h_t=h_t: e.tensor_tensor(
                        out=h_t[:, nh * 512:(nh + 1) * 512], in0=ps[bank][:], in1=x_t[:, nh * 512:(nh + 1) * 512], op=ALU.add),
                        reads=[Bps[bank], Bx], writes=[Bh])
                S.dma("sp", lambda e, h_t=h_t, r0=r0: e.dma_start(out=K.h1_s[r0:r0 + 128, :], in_=h_t), reads=[Bh], owner=Bh)
    zero_some(len(zrows))


def phase3_router(K):
    S, A, nc = K.S, K.A, K.nc
    ps, Bps = K.ps, K.Bps
    gmoe, Bgmoe = A.alloc("gmoe", [D])
    wr, Bwr = A.alloc("wr", [8, E])
    brt, Bbr = A.alloc("brt", [E])
    tri, Btri = A.alloc("tri", [128])
    eC, BeC = A.alloc("eC", [E])
    msum, Bmsum = A.alloc("msum", [E])
    S.dma("sp", lambda e: e.dma_start(out=gmoe, in_=bcast_row(K.g_moe, D)), writes=[Bgmoe])
    S.dma("sp", lambda e: e.dma_start(out=wr, in_=K.w_router.rearrange("(c p) n -> p c n", p=128)), writes=[Bwr])
    S.dma("sp", lambda e: e.dma_start(out=brt, in_=bcast_row(K.b_router, E)), writes=[Bbr])
    S.dma("sp", lambda e: e.dma_start(out=tri, in_=K.tri), writes=[Btri])
    S.dma("sp", lambda e: e.dma_start(out=eC, in_=K.eC), writes=[BeC])
    S.op("dve", lambda e: e.memset(msum, 0.0), writes=[Bmsum])

    def tile_stream(par):
        h_t, Bh = A.alloc(f"ht{par}", [D])
        junk, Bjunk = A.alloc(f"junk{par}", [D], BF16)
        ss_t, Bss = A.alloc(f"ss{par}", [1])
        u2, Bu2 = A.alloc(f"u2{par}", [D])
        ub, Bub = A.alloc(f"u2b{par}", [D], BF16)
        u2T, Bu2T = A.alloc(f"u2T{par}", [8, 128])
        lg, Blg = A.alloc(f"lg{par}", [E])
        top8, Btop8 = A.alloc(f"top8{par}", [8])
        nm, Bnm = A.alloc(f"nm{par}", [1])
        mask, Bmask = A.alloc(f"mask{par}", [E])
        ex, Bex = A.alloc(f"ex{par}", [E])
        den, Bden = A.alloc(f"den{par}", [1])
        gf, Bgf = A.alloc(f"gf{par}", [E])
        rank, Brank = A.alloc(f"rank{par}", [E])
        okm, Bok = A.alloc(f"okm{par}", [E])
        dst, Bdst = A.alloc(f"dst{par}", [E])
        dk, Bdk = A.alloc(f"dk{par}", [4])
        oh, Boh = A.alloc(f"oh{par}", [E])
        b0 = par * 2

        def gen():
            for i in range(par, NTILE, 4):
                r0 = i * 128
                S.dma("sp", lambda e, r0=r0: e.dma_start(out=h_t, in_=K.h1_s[r0:r0 + 128, :]), writes=[Bh])
                rms_rstd(K, h_t, Bh, junk, Bjunk, ss_t, Bss, D)
                S.op("dve", lambda e: e.scalar_tensor_tensor(out=u2, in0=h_t, scalar=ss_t, in1=gmoe, op0=ALU.mult, op1=ALU.mult),
                     reads=[Bh, Bss, Bgmoe], writes=[Bu2])
                yield
                S.op("act", lambda e: e.activation(out=ub, in_=u2, func=AF.Copy), reads=[Bu2], writes=[Bub])
                for hc in range(2):
                    bank = b0
                    for c4 in range(4):
                        c = hc * 4 + c4
                        S.op("pe", lambda e, c=c, c4=c4, bank=bank: e.transpose(ps[bank][:, c4 * 128:(c4 + 1) * 128], u2[:, c * 128:(c + 1) * 128], K.ident_f),
                             reads=[Bu2, K.Bident_f], writes=[Bps[bank]], signal=(c4 == 3))
                    S.op("act", lambda e, bank=bank, hc=hc: e.activation(out=u2T[:, hc * 4:(hc + 1) * 4, :], in_=ps[bank][:].rearrange("p (c t) -> p c t", c=4), func=AF.Copy),
                         reads=[Bps[bank]], writes=[Bu2T])
                yield
                lb, rb = b0 + 1, b0 + 1
                mm_group(S, ps[lb][:, 0:E], Bps[lb], [(u2T[:, k, :], wr[:, k, :]) for k in range(8)], reads=[Bu2T, Bwr])
                S.op("dve", lambda e, lb=lb: e.tensor_tensor(out=lg, in0=ps[lb][:, 0:E], in1=brt, op=ALU.add), reads=[Bps[lb], Bbr], writes=[Blg])
                S.op("dve", lambda e: e.max(out=top8, in_=lg), reads=[Blg], writes=[Btop8])
                S.op("dve", lambda e: e.tensor_scalar(out=mask, in0=lg, scalar1=top8[:, 3:4], scalar2=None, op0=ALU.is_ge),
                     reads=[Blg, Btop8], writes=[Bmask])
                yield
                mm_group(S, ps[rb][:, 64:64 + E], Bps[rb], [(tri, mask), (K.ones_f, msum)], reads=[Btri, Bmask, K.Bones_f, Bmsum])
                S.op("dve", lambda e: e.tensor_tensor(out=msum, in0=msum, in1=mask, op=ALU.add), reads=[Bmsum, Bmask], writes=[Bmsum])
                yield
                S.op("dve", lambda e: e.tensor_scalar(out=nm, in0=top8[:, 0:1], scalar1=-1.0, scalar2=None, op0=ALU.mult),
                     reads=[Btop8], writes=[Bnm])
                S.op("act", lambda e: e.activation(out=ex, in_=lg, func=AF.Exp, bias=nm), reads=[Blg, Bnm], writes=[Bex])
                S.op("dve", lambda e, rb=rb: e.tensor_copy(out=rank, in_=ps[rb][:, 64:64 + E]), reads=[Bps[rb]], writes=[Brank])
                S.op("dve", lambda e: e.tensor_scalar(out=okm, in0=rank, scalar1=float(CAP), scalar2=None, op0=ALU.is_lt),
                     reads=[Brank], writes=[Bok])
                S.op("dve", lambda e: e.tensor_tensor(out=dst, in0=rank, in1=eC, op=ALU.add), reads=[Brank, BeC], writes=[Bdst])
                S.op("dve", lambda e: e.scalar_tensor_tensor(out=dst, in0=dst, scalar=float(-TRASH), in1=okm, op0=ALU.add, op1=ALU.mult),
                     reads=[Bdst, Bok], writes=[Bdst])
                S.op("dve", lambda e: e.tensor_scalar(out=dst, in0=dst, scalar1=float(TRASH), scalar2=None, op0=ALU.add),
                     reads=[Bdst], writes=[Bdst])
                yield
                S.op("dve", lambda e: e.tensor_tensor(out=ex, in0=ex, in1=mask, op=ALU.mult), reads=[Bex, Bmask], writes=[Bex])
                S.op("dve", lambda e: e.reduce_sum(out=den, in_=ex, axis=AX.X), reads=[Bex], writes=[Bden])
                S.op("dve", lambda e: e.reciprocal(out=den, in_=den), reads=[Bden], writes=[Bden])
                S.op("dve", lambda e: e.scalar_tensor_tensor(out=gf, in0=ex, scalar=den, in1=okm, op0=ALU.mult, op1=ALU.mult),
                     reads=[Bex, Bden, Bok], writes=[Bgf])
                yield
                for k in range(4):
                    S.op("dve", lambda e, k=k: e.scalar_tensor_tensor(out=oh, in0=lg, scalar=top8[:, k:k + 1], in1=dst, op0=ALU.is_equal, op1=ALU.mult,
                                                                      accum_out=dk[:, k:k + 1]),
                         reads=[Blg, Btop8, Bdst], writes=[Boh, Bdk])
                    S.op("dve", lambda e, k=k, i=i: e.scalar_tensor_tensor(out=oh, in0=lg, scalar=top8[:, k:k + 1], in1=gf, op0=ALU.is_equal, op1=ALU.mult,
                                                                           accum_out=K.gate_a[:, i * 4 + k:i * 4 + k + 1]),
                         reads=[Blg, Btop8, Bgf], writes=[Boh, K.Bgate])
                S.op("dve", lambda e, i=i: e.tensor_copy(out=K.dest_i[:, i * 4:(i + 1) * 4], in_=dk), reads=[Bdk], writes=[K.Bdest])
                for k in range(4):
                    S.dma("pool", lambda e, k=k, i=i: e.indirect_dma_start(
                        out=K.xs, out_offset=bass.IndirectOffsetOnAxis(ap=K.dest_i[:, i * 4 + k:i * 4 + k + 1], axis=0),
                        in_=ub, in_offset=None), reads=[Bub, K.Bdest], owner=Bub)
                yield
        return gen()

    interleave([tile_stream(q) for q in range(4)], skew=2)


def phase4_experts(K):
    S, A, nc = K.S, K.A, K.nc
    ps, Bps = K.ps, K.Bps
    wgu = [A.alloc(f"wgu{i}", [8, 2 * F], BF16) for i in range(2)]
    wdn = [A.alloc(f"wdn{i}", [8, D], BF16) for i in range(2)]
    bdn = [A.alloc(f"bdn{i}", [D], BF16) for i in range(2)]
    xTs = [A.alloc(f"xT{i}", [8, CAP], BF16) for i in range(2)]
    resTs = [A.alloc(f"resT{i}", [8, CAP], BF16) for i in range(2)]
    xst = [A.alloc(f"xst{i}", [D], BF16) for i in range(3)]
    yst = [A.alloc(f"yst{i}", [D]) for i in range(2)]
    HW = CAP // 2
    gt = [A.alloc(f"gt{i}", [HW]) for i in range(2)]
    sg = [A.alloc(f"sg{i}", [HW]) for i in range(2)]
    ut = [A.alloc(f"ut{i}", [HW]) for i in range(2)]
    cnts = {"y": 0, "x": 0, "u": 0}
    S.op("dve", lambda e: e.memset(yst[1][0], 0.0), writes=[yst[1][1]])
    S.dma("sp", lambda e: e.dma_start(out=K.ys[NSLOT:NSLOT + 128, :], in_=yst[1][0]), reads=[yst[1][1]], owner=yst[1][1])

    def load_expert(e_):
        w, Bw = wgu[e_ % 2]
        wd, Bwd = wdn[e_ % 2]
        bd, Bbd = bdn[e_ % 2]
        src = K.w_gu[e_].rearrange("(c p) n -> p c n", p=128)
        for hh in range(2):
            S.dma("pool", lambda e, w=w, src=src, hh=hh: e.dma_start(out=w[:, hh * 4:(hh + 1) * 4, :], in_=src[:, hh * 4:(hh + 1) * 4, :]), writes=[Bw])
        S.dma("pool", lambda e, wd=wd, e_=e_: e.dma_start(out=wd, in_=K.w_dn[e_].rearrange("(c p) n -> p c n", p=128)), writes=[Bwd])
        S.dma("pool", lambda e, bd=bd, e_=e_: e.dma_start(out=bd[0:1, :], in_=K.b_dn[e_:e_ + 1, :]), writes=[Bbd])

    def build_xT(e_):
        xT, BxT = xTs[e_ % 2]
        for sb in range(NSB):
            xs_t, Bxs = xst[cnts["x"] % 3]
            cnts["x"] += 1
            r0 = e_ * CAP + sb * 128
            S.dma("sp", lambda e, xs_t=xs_t, r0=r0: e.dma_start(out=xs_t, in_=K.xs[r0:r0 + 128, :]), writes=[Bxs])
            bank = sb % 2
            pv16 = ps[bank][:].bitcast(BF16)
            for c in range(8):
                S.op("pe", lambda e, c=c, pv16=pv16, xs_t=xs_t: e.transpose(pv16[:, c * 128:(c + 1) * 128], xs_t[:, c * 128:(c + 1) * 128], K.ident_b),
                     reads=[Bxs, K.Bident_b], writes=[Bps[bank]], signal=(c == 7))
            S.op("act", lambda e, pv16=pv16, sb=sb, xT=xT: e.activation(out=xT[:, :, sb * 128:(sb + 1) * 128], in_=pv16.rearrange("p (c t) -> p c t", c=8), func=AF.Copy),
                 reads=[Bps[bank]], writes=[BxT])

    def gate_up(e_):
        w, Bw = wgu[e_ % 2]
        xT, BxT = xTs[e_ % 2]
        resT, BresT = resTs[e_ % 2]
        for f in range(8):
            bgc = K.pvt[:, PV_BGU + e_ * 16 + f:PV_BGU + e_ * 16 + f + 1]
            buc = K.pvt[:, PV_BGU + e_ * 16 + 8 + f:PV_BGU + e_ * 16 + 8 + f + 1]
            for hv in range(2):
                nsl = slice(hv * HW, (hv + 1) * HW)
                cnt = cnts["u"]
                cnts["u"] += 1
                gb, ub_ = 2 + cnt % 2, 4 + cnt % 2
                g_t, Bg = gt[cnt % 2]
                s_t, Bs = sg[cnt % 2]
                u_t, Bu = ut[cnt % 2]
                mm_group(S, ps[gb][:, 0:HW], Bps[gb], [(w[:, k, f * 128:(f + 1) * 128], xT[:, k, nsl]) for k in range(8)], reads=[Bw, BxT])
                mm_group(S, ps[ub_][:, 0:HW], Bps[ub_], [(w[:, k, F + f * 128:F + (f + 1) * 128], xT[:, k, nsl]) for k in range(8)], reads=[Bw, BxT])
                S.op("dve", lambda e, gb=gb, g_t=g_t, bgc=bgc: e.tensor_scalar(out=g_t, in0=ps[gb][:, 0:HW], scalar1=bgc, scalar2=7.0, op0=ALU.add, op1=ALU.min),
                     reads=[Bps[gb], K.Bpv], writes=[Bg])
                S.op("act", lambda e, ub_=ub_, u_t=u_t, buc=buc: e.activation(out=u_t, in_=ps[ub_][:, 0:HW], func=AF.Identity, bias=buc),
                     reads=[Bps[ub_], K.Bpv], writes=[Bu])
                S.op("act", lambda e, g_t=g_t, s_t=s_t: e.activation(out=s_t, in_=g_t, func=AF.Sigmoid, scale=1.702), reads=[Bg], writes=[Bs])
                S.op("dve", lambda e, u_t=u_t: e.tensor_scalar(out=u_t, in0=u_t, scalar1=7.0, scalar2=-7.0, op0=ALU.min, op1=ALU.max),
                     reads=[Bu], writes=[Bu])
                S.op("dve", lambda e, g_t=g_t, s_t=s_t: e.tensor_tensor(out=g_t, in0=g_t, in1=s_t, op=ALU.mult), reads=[Bg, Bs], writes=[Bg])
                S.op("dve", lambda e, g_t=g_t, u_t=u_t, f=f, nsl=nsl, resT=resT: e.scalar_tensor_tensor(
                    out=resT[:, f, nsl], in0=u_t, scalar=1.0, in1=g_t, op0=ALU.add, op1=ALU.mult),
                    reads=[Bg, Bu], writes=[BresT])

    def down(e_):
        wd, Bwd = wdn[e_ % 2]
        bd, Bbd = bdn[e_ % 2]
        resT, BresT = resTs[e_ % 2]
        for sb in range(NSB):
            y_t, By = yst[cnts["y"] % 2]
            cnts["y"] += 1
            for nh in range(2):
                bank = 6 + nh
                pairs = [(resT[:, k, sb * 128:(sb + 1) * 128], wd[:, k, nh * 512:(nh + 1) * 512]) for k in range(8)]
                pairs.append((K.ones_b[0:1, :], bd[0:1, nh * 512:(nh + 1) * 512]))
                mm_group(S, ps[bank][:], Bps[bank], pairs, reads=[BresT, Bwd, Bbd, K.Bones_b])
                S.op("act", lambda e, bank=bank, y_t=y_t, nh=nh: e.activation(out=y_t[:, nh * 512:(nh + 1) * 512], in_=ps[bank][:], func=AF.Copy),
                     reads=[Bps[bank]], writes=[By])
            r0 = e_ * CAP + sb * 128
            S.dma("sp", lambda e, y_t=y_t, r0=r0: e.dma_start(out=K.ys[r0:r0 + 128, :], in_=y_t), reads=[By], owner=By)

    load_expert(0)
    build_xT(0)
    for e_ in range(E):
        if e_ + 1 < E:
            load_expert(e_ + 1)
        gate_up(e_)
        if e_ + 1 < E:
            build_xT(e_ + 1)
        down(e_)


def phase5_combine(K):
    S, A, nc = K.S, K.A, K.nc
    ps, Bps = K.ps, K.Bps
    wpg, Bwpg = A.alloc("wpg", [8, D], BF16)
    wpp, Bwpp = A.alloc("wpp", [2, D], BF16)
    gple, Bgple = A.alloc("gple", [D])
    S.dma("pool", lambda e: e.dma_start(out=wpg, in_=K.w_ple_gate.rearrange("(c p) n -> p c n", p=128)), writes=[Bwpg])
    S.dma("pool", lambda e: e.dma_start(out=wpp, in_=K.w_ple_proj.rearrange("(c p) n -> p c n", p=128)), writes=[Bwpp])
    S.dma("sp", lambda e: e.dma_start(out=gple, in_=bcast_row(K.g_ple, D)), writes=[Bgple])

    def tile_stream(par):
        h_t, Bh = A.alloc(f"ht{par}", [D])
        yg = [A.alloc(f"yg{par}_{k}", [D]) for k in range(4)]
        p_t, Bp = A.alloc(f"pt{par}", [PLE])
        ptb, Bptb = A.alloc(f"ptb{par}", [PLE], BF16)
        pT, BpT = A.alloc(f"pT{par}", [2, 128], BF16)
        junk, Bjunk = A.alloc(f"junk{par}", [D], BF16)
        ss_t, Bss = A.alloc(f"ss{par}", [1])
        u3, Bu3 = A.alloc(f"u3{par}", [D], BF16)
        u3T, Bu3T = A.alloc(f"u3T{par}", [8, 128], BF16)
        sgm, Bsgm = A.alloc(f"sgm{par}", [D])
        o_t, Bo = A.alloc(f"ot{par}", [D])
        b0 = par * 2

        def gen():
            for i in range(par, NTILE, 4):
                r0 = i * 128
                S.dma("sp", lambda e, r0=r0: e.dma_start(out=h_t, in_=K.h1_s[r0:r0 + 128, :]), writes=[Bh])
                S.dma("sp", lambda e, r0=r0: e.dma_start(out=p_t, in_=K.p[r0:r0 + 128, :]), writes=[Bp])
                for k in range(4):
                    y_t, By = yg[k]
                    S.dma("pool", lambda e, y_t=y_t, i=i, k=k: e.indirect_dma_start(
                        out=y_t, out_offset=None, in_=K.ys,
                        in_offset=bass.IndirectOffsetOnAxis(ap=K.dest_i[:, i * 4 + k:i * 4 + k + 1], axis=0)),
                        reads=[K.Bdest], writes=[By])
                yield
                S.op("act", lambda e: e.activation(out=ptb, in_=p_t, func=AF.Copy), reads=[Bp], writes=[Bptb])
                pv1 = ps[b0 + 1][:].bitcast(BF16)
                for c in range(2):
                    S.op("pe", lambda e, c=c, pv1=pv1: e.transpose(pv1[:, c * 128:(c + 1) * 128], ptb[:, c * 128:(c + 1) * 128], K.ident_b),
                         reads=[Bptb, K.Bident_b], writes=[Bps[b0 + 1]], signal=(c == 1))
                S.op("act", lambda e, pv1=pv1: e.activation(out=pT, in_=pv1[:, 0:256].rearrange("p (c t) -> p c t", c=2), func=AF.Copy),
                     reads=[Bps[b0 + 1]], writes=[BpT])
                yield
                for k in range(4):
                    y_t, By = yg[k]
                    S.op("dve", lambda e, y_t=y_t, i=i, k=k: e.scalar_tensor_tensor(
                        out=h_t, in0=y_t, scalar=K.gate_a[:, i * 4 + k:i * 4 + k + 1], in1=h_t, op0=ALU.mult, op1=ALU.add),
                        reads=[By, Bh, K.Bgate], writes=[Bh])
                yield
                rms_rstd(K, h_t, Bh, junk, Bjunk, ss_t, Bss, D)
                S.op("dve", lambda e: e.scalar_tensor_tensor(out=u3, in0=h_t, scalar=ss_t, in1=gple, op0=ALU.mult, op1=ALU.mult),
                     reads=[Bh, Bss, Bgple], writes=[Bu3])
                yield
                pv16 = ps[b0][:].bitcast(BF16)
                for c in range(8):
                    S.op("pe", lambda e, c=c, pv16=pv16: e.transpose(pv16[:, c * 128:(c + 1) * 128], u3[:, c * 128:(c + 1) * 128], K.ident_b),
                         reads=[Bu3, K.Bident_b], writes=[Bps[b0]], signal=(c == 7))
                S.op("act", lambda e, pv16=pv16: e.activation(out=u3T, in_=pv16.rearrange("p (c t) -> p c t", c=8), func=AF.Copy),
                     reads=[Bps[b0]], writes=[Bu3T])
                yield
                for nh in range(2):
                    nsl = slice(nh * 512, (nh + 1) * 512)
                    gbk, pbk = b0, b0 + 1
                    mm_group(S, ps[gbk][:], Bps[gbk], [(u3T[:, k, :], wpg[:, k, nsl]) for k in range(8)], reads=[Bu3T, Bwpg])
                    mm_group(S, ps[pbk][:], Bps[pbk], [(pT[:, k, :], wpp[:, k, nsl]) for k in range(2)], reads=[BpT, Bwpp])
                    S.op("act", lambda e, gbk=gbk, nsl=nsl: e.activation(out=sgm[:, nsl], in_=ps[gbk][:], func=AF.Sigmoid), reads=[Bps[gbk]], writes=[Bsgm])
                    S.op("dve", lambda e, pbk=pbk, nsl=nsl: e.tensor_tensor(out=sgm[:, nsl], in0=sgm[:, nsl], in1=ps[pbk][:], op=ALU.mult),
                         reads=[Bsgm, Bps[pbk]], writes=[Bsgm])
                    S.op("dve", lambda e, nsl=nsl: e.tensor_tensor(out=o_t[:, nsl], in0=sgm[:, nsl], in1=h_t[:, nsl], op=ALU.add),
                         reads=[Bsgm, Bh], writes=[Bo])
                    yield
                S.dma("sp", lambda e, r0=r0: e.dma_start(out=K.y[r0:r0 + 128, :], in_=o_t), reads=[Bo], owner=Bo)
        return gen()

    interleave([tile_stream(q) for q in range(4)], skew=2)


def _rope_tables():
    pos = np.arange(SEQ)
    row = (pos // 64).astype(np.float32)
    col = (pos % 64).astype(np.float32)
    inv = (10000.0 ** (-np.arange(0, 64, 2, dtype=np.float32) / 64.0)).astype(np.float32)
    C = np.zeros((128, SEQ), np.float32)
    Sg = np.zeros((128, SEQ), np.float32)
    for p in range(128):
        ids = row if p < 64 else col
        j = p % 32
        ang = (ids * inv[j]).astype(np.float32)
        C[p] = np.cos(ang)
        sgn = -1.0 if (p % 64) < 32 else 1.0
        Sg[p] = sgn * np.sin(ang)
    perm = np.zeros((128, 128), np.float32)
    for m in range(128):
        partner = m + 32 if (m % 64) < 32 else m - 32
        perm[partner, m] = 1.0
    return C, Sg, perm


_NC_CACHE = {}


def _prep_common(inp):
    f = lambda a: np.ascontiguousarray(np.asarray(a, dtype=np.float32))
    pv = np.zeros((128, NPV), np.float32)
    cw = f(inp["conv_w"])[0]
    pv[:, PV_CONVW:PV_CONVW + 32] = cw.reshape(4, 8, 128).transpose(2, 1, 0).reshape(128, 32)
    pv[:, PV_CONVB:PV_CONVB + 8] = f(inp["conv_b"])[0].reshape(8, 128).T
    pv[:, PV_BA:PV_BA + 16] = f(inp["lru_ba"])[0].reshape(16, 128).T
    pv[:, PV_BI:PV_BI + 16] = f(inp["lru_bi"])[0].reshape(16, 128).T
    pv[:, PV_LAM:PV_LAM + 16] = f(inp["lru_lam"])[0].reshape(16, 128).T
    pv[:, PV_QN] = f(inp["q_norm"])[0]
    pv[:, PV_KN] = f(inp["k_norm"])[0]
    pv[:, PV_BGU:PV_BGU + 512] = f(inp["b_gu"])[0].reshape(E * 16, 128).T
    C, Sg, perm = _rope_tables()
    tri = np.triu(np.ones((128, 128), np.float32), 1)
    eC = np.tile((np.arange(E, dtype=np.float32) * CAP)[None, :], (128, 1))
    com = {
        "w_in": f(inp["w_in"])[0], "lru_wa": f(inp["lru_wa"])[0], "lru_wi": f(inp["lru_wi"])[0],
        "w_attn_br": f(inp["w_attn_br"])[0], "w_lru_br": f(inp["w_lru_br"])[0], "w_out": f(inp["w_out"])[0],
        "w_router": f(inp["w_router"])[0], "w_gu": f(inp["w_gu"])[0], "w_dn": f(inp["w_dn"])[0],
        "b_dn": f(inp["b_dn"])[0], "w_ple_gate": f(inp["w_ple_gate"])[0], "w_ple_proj": f(inp["w_ple_proj"])[0],
        "g_mix": f(inp["g_mix"]), "g_moe": f(inp["g_moe"]), "g_ple": f(inp["g_ple"]), "b_router": f(inp["b_router"]),
        "pv": pv, "ropeC": C, "ropeS": Sg, "perm": perm, "ident": np.eye(128, dtype=np.float32), "tri": tri, "eC": eC,
    }
    return com


def kernel(**inputs):
    dbg = bool(int(os.environ.get("MK_DBG", "0")))
    ncores = int(os.environ.get("MK_NCORES", str(NCORES)))
    key = dbg
    if key not in _NC_CACHE:
        _NC_CACHE[key] = build_program(dbg)
    nc = _NC_CACHE[key]
    com = _prep_common(inputs)
    x = np.asarray(inputs["x"], dtype=np.float32)
    p = np.asarray(inputs["p"], dtype=np.float32)[0]
    in_maps = []
    for c in range(ncores):
        m = dict(com)
        m["x"] = np.ascontiguousarray(x[2 * c:2 * c + 2].reshape(T, D))
        m["p"] = np.ascontiguousarray(p[2 * c:2 * c + 2].reshape(T, PLE))
        in_maps.append(m)
    res = run_bass_kernel_spmd(nc, in_maps, core_ids=list(range(ncores)))
    if dbg:
        kernel.last = res
    out = np.zeros((16, SEQ, D), np.float32)
    for c in range(ncores):
        out[2 * c:2 * c + 2] = np.asarray(res.results[c]["y"], dtype=np.float32).reshape(2, SEQ, D)
    return out
```

```python
import os
import numpy as np
from contextlib import ExitStack
import concourse.bass as bass
import concourse.mybir as mybir
from concourse.bass_utils import run_bass_kernel_spmd

F32 = mybir.dt.float32
BF16 = mybir.dt.bfloat16
I32 = mybir.dt.int32
AF = mybir.ActivationFunctionType
ALU = mybir.AluOpType
AX = mybir.AxisListType

NCORES = 8
D = 1024
SEQ = 2048
NSEQ = 2
T = NSEQ * SEQ
NTILE = T // 128
E = 32
F = 1024
CAP = 640
NSB = CAP // 128
NSLOT = E * CAP
TRASH = NSLOT
PLE = 256
EPS = 1e-6
INW = 5632
NPV = 608
PV_CONVW, PV_CONVB, PV_BA, PV_BI, PV_LAM, PV_QN, PV_KN, PV_BGU = 0, 32, 40, 56, 72, 88, 89, 96

ENGS = ("pe", "act", "dve", "pool", "sp")


class Buf:
    __slots__ = ("name", "last_write", "reads", "sem", "sem_total", "excl")

    def __init__(self, name, excl=False):
        self.name = name
        self.excl = excl
        self.last_write = None
        self.reads = []
        self.sem = None
        self.sem_total = 0


class Sched:
    def __init__(self, nc, stack):
        self.nc = nc
        self.stack = stack
        self.stream = {e: [] for e in ENGS}
        self.sem = {e: stack.enter_context(nc.semaphore("s_" + e)) for e in ENGS}
        self.count = {e: 0 for e in ENGS}
        self.seen = {e: {} for e in ENGS}
        self.dma_bufs = []

    def _wait_tokens(self, e, toks):
        need = {}
        for t in toks:
            if t is None:
                continue
            if t[0] == "e":
                _, src, c = t
                if src == "pe" and e == "pe":
                    continue
                key = ("e", src)
                val = c
                sem = self.sem[src]
            else:
                b = t[1]
                key = ("d", id(b))
                val = b.sem_total
                sem = b.sem
            if self.seen[e].get(key, 0) >= val:
                continue
            if key not in need or need[key][1] < val:
                need[key] = (sem, val)
        for key, (sem, val) in need.items():
            self.seen[e][key] = val
            self.stream[e].append(lambda eng, sem=sem, val=val: eng.wait_ge(sem, val))

    @staticmethod
    def _deps(reads, writes):
        toks = []
        for r in reads:
            toks.append(r.last_write)
            if r.excl:
                toks.extend(r.reads)
        for w in writes:
            toks.append(w.last_write)
            toks.extend(w.reads)
        return toks

    def op(self, e, fn, reads=(), writes=(), signal=True):
        self._wait_tokens(e, self._deps(reads, writes))
        if signal:
            self.count[e] += 1
            tok = ("e", e, self.count[e])
            sem = self.sem[e]
            self.stream[e].append(lambda eng, fn=fn, sem=sem: fn(eng).then_inc(sem, 1))
        else:
            tok = ("e", e, self.count[e] + 1)
            self.stream[e].append(lambda eng, fn=fn: fn(eng))
        for w in writes:
            w.last_write = tok
            w.reads = []
        for r in reads:
            r.reads.append(tok)
        return tok

    def dma(self, e, fn, reads=(), writes=(), owner=None):
        if owner is None:
            owner = writes[0] if writes else reads[0]
        if owner.sem is None:
            owner.sem = self.stack.enter_context(self.nc.semaphore("d%d_%s" % (len(self.dma_bufs), owner.name)))
            self.dma_bufs.append(owner)
        self._wait_tokens(e, self._deps(reads, writes))
        owner.sem_total += 16
        sem = owner.sem
        self.stream[e].append(lambda eng, fn=fn, sem=sem: fn(eng).then_inc(sem, 16))
        tok = ("d", owner)
        for w in writes:
            w.last_write = tok
            w.reads = []
        for r in reads:
            r.reads.append(tok)
        return tok

    def barrier(self):
        toks = [("e", s, self.count[s]) for s in ENGS if self.count[s] > 0]
        toks += [("d", b) for b in self.dma_bufs]
        for e in ENGS:
            self._wait_tokens(e, toks)

    def emit(self):
        nc = self.nc
        self.barrier()
        with nc.Block() as block:
            for e, reg in (("sp", block.sync), ("act", block.scalar), ("pe", block.tensor),
                           ("dve", block.vector), ("pool", block.gpsimd)):
                lst = self.stream[e]
                if not lst:
                    continue

                def body(eng, lst=lst):
                    for f in lst:
                        f(eng)
                reg(body)


def _dsize(dt):
    return 2 if dt == BF16 else 4


class Arena:
    def __init__(self, nc, stack, nbytes):
        self.t = stack.enter_context(nc.sbuf_tensor("arena", [128, nbytes // 4], F32))
        self.off = 0
        self.nbytes = nbytes
        self.peak = 0

    def alloc(self, name, free, dt=F32):
        n = 1
        for f in free:
            n *= f
        sz = (n * _dsize(dt) + 31) // 32 * 32
        assert self.off + sz <= self.nbytes, (name, self.off, sz, self.nbytes)
        a = self.t[:, self.off // 4:(self.off + sz) // 4]
        if dt != F32:
            a = a.bitcast(dt)
        a = a[:, 0:n]
        if len(free) == 2:
            a = a.rearrange("p (a b) -> p a b", a=free[0])
        self.off += sz
        self.peak = max(self.peak, self.off)
        return a, Buf(name)

    def mark(self):
        return self.off

    def release(self, m):
        self.off = m


class Ctx:
    pass


def build_program(dbg=False):
    nc = bass.Bass("TRN2", target_bir_lowering=False)
    K = Ctx()
    K.nc = nc

    def din(name, shape, dt=F32):
        return nc.dram_tensor(name, list(shape), dt, kind="ExternalInput")

    K.x = din("x", [T, D]).ap()
    K.p = din("p", [T, PLE]).ap()
    K.w_in = din("w_in", [D, INW]).ap()
    K.lru_wa = din("lru_wa", [2, 8, 128, 128]).ap()
    K.lru_wi = din("lru_wi", [2, 8, 128, 128]).ap()
    K.w_attn_br = din("w_attn_br", [D, D]).ap()
    K.w_lru_br = din("w_lru_br", [D, D]).ap()
    K.w_out = din("w_out", [D, D]).ap()
    K.w_router = din("w_router", [D, E]).ap()
    K.w_gu = din("w_gu", [E, D, 2 * F]).ap()
    K.w_dn = din("w_dn", [E, F, D]).ap()
    K.b_dn = din("b_dn", [E, D]).ap()
    K.w_ple_gate = din("w_ple_gate", [D, D]).ap()
    K.w_ple_proj = din("w_ple_proj", [PLE, D]).ap()
    K.g_mix = din("g_mix", [1, D])
    K.g_moe = din("g_moe", [1, D])
    K.g_ple = din("g_ple", [1, D])
    K.b_router = din("b_router", [1, E])
    K.pv = din("pv", [128, NPV]).ap()
    K.ropeC = din("ropeC", [128, SEQ]).ap()
    K.ropeS = din("ropeS", [128, SEQ]).ap()
    K.perm = din("perm", [128, 128]).ap()
    K.ident = din("ident", [128, 128]).ap()
    K.tri = din("tri", [128, 128]).ap()
    K.eC = din("eC", [128, E]).ap()
    K.y = nc.dram_tensor("y", [T, D], F32, kind="ExternalOutput").ap()

    kind = "ExternalOutput" if dbg else "Internal"

    def dscr(name, shape, dt):
        if dbg:
            return nc.dram_tensor(name, list(shape), dt, kind="ExternalOutput").ap()
        return nc.dram_tensor(name, list(shape), dt).ap()

    K.qT_s = dscr("qT_s", [NSEQ, 8, 128, SEQ], BF16)
    K.kT_s = dscr("kT_s", [NSEQ, 2, 128, SEQ], BF16)
    K.V_s = dscr("V_s", [NSEQ, 128, 16, 256], BF16)
    K.yl_s = dscr("yl_s", [NSEQ, 8, 128, SEQ], BF16)
    K.sga_s = dscr("sga_s", [NSEQ, 8, 128, SEQ], BF16)
    K.sgr_s = dscr("sgr_s", [NSEQ, 8, 128, SEQ], BF16)
    K.h1_s = dscr("h1_s", [T, D], F32)
    K.xs = dscr("xs_s", [NSLOT + 128, D], BF16)
    K.ys = dscr("ys_s", [NSLOT + 128, D], F32)

    with ExitStack() as st:
        S = Sched(nc, st)
        K.S = S
        A = Arena(nc, st, 204 * 1024)
        K.A = A
        K.ps = []
        K.Bps = []
        for i in range(8):
            K.ps.append(st.enter_context(nc.psum_tensor(f"ps{i}", [128, 512], F32)))
            K.Bps.append(Buf(f"ps{i}", excl=True))
        stop = int(os.environ.get("MK_STOP", "9"))
        phase0_consts(K)
        S.barrier()
        m0 = A.mark()
        if stop >= 1:
            phase1_inproj(K)
            S.barrier()
        A.release(m0)
        if stop >= 2:
            phase2_attn(K)
            S.barrier()
        A.release(m0)
        K.pre5 = {"wpg": A.alloc("wpg", [8, D], BF16), "wpp": A.alloc("wpp", [2, D], BF16), "gple": A.alloc("gple", [D])}
        m5 = A.mark()
        K.pre4 = {"wgu": A.alloc("wgu0", [8, 2 * F], BF16), "wdn": A.alloc("wdn0", [8, D], BF16), "bdn": A.alloc("bdn0", [D], BF16)}
        m3 = A.mark()
        if stop >= 3:
            issue_expert_load(K, 0, *K.pre4["wgu"], *K.pre4["wdn"], *K.pre4["bdn"])
            phase3_router(K)
            S.barrier()
        A.release(m3)
        if stop >= 4:
            phase4_experts(K)
            S.barrier()
        A.release(m5)
        if stop >= 5:
            phase5_combine(K)
        S.emit()
    return nc


def bcast_row(dt_tensor, n):
    return bass.AP(dt_tensor, 0, [[0, 128], [1, n]])


def phase0_consts(K):
    S, A = K.S, K.A
    K.ident_f, K.Bident_f = A.alloc("ident_f", [128])
    K.ident_b, K.Bident_b = A.alloc("ident_b", [128], BF16)
    K.ones_b, K.Bones_b = A.alloc("ones_b", [128], BF16)
    K.ones_f, K.Bones_f = A.alloc("ones_f", [128])
    K.pvt, K.Bpv = A.alloc("pvt", [NPV])
    K.kk, K.Bkk = A.alloc("kk", [16])
    K.dest_i, K.Bdest = A.alloc("dest_i", [NTILE * 4], I32)
    K.gate_a, K.Bgate = A.alloc("gate_a", [NTILE * 4])
    S.dma("sp", lambda e: e.dma_start(out=K.ident_f, in_=K.ident), writes=[K.Bident_f])
    S.dma("pool", lambda e: e.dma_start(out=K.ident_b, in_=K.ident), writes=[K.Bident_b])
    S.dma("sp", lambda e: e.dma_start(out=K.pvt, in_=K.pv), writes=[K.Bpv])
    S.op("dve", lambda e: e.memset(K.ones_b, 1.0), writes=[K.Bones_b])
    S.op("dve", lambda e: e.memset(K.ones_f, 1.0), writes=[K.Bones_f])
    lam = K.pvt[:, PV_LAM:PV_LAM + 16]
    S.op("act", lambda e: e.activation(out=K.kk, in_=lam, func=AF.Exp, scale=-1.0), reads=[K.Bpv], writes=[K.Bkk])
    S.op("act", lambda e: e.activation(out=K.kk, in_=K.kk, func=AF.Ln, bias=1.0), reads=[K.Bkk], writes=[K.Bkk])
    S.op("dve", lambda e: e.tensor_scalar(out=K.kk, in0=K.kk, scalar1=-8.0, scalar2=None, op0=ALU.mult),
         reads=[K.Bkk], writes=[K.Bkk])


def mm_group(S, out, Bout, pairs, reads):
    n = len(pairs)
    for i, (l, r) in enumerate(pairs):
        S.op("pe", lambda e, l=l, r=r, i=i: e.matmul(out, l, r, start=(i == 0), stop=(i == n - 1)),
             reads=reads, writes=[Bout], signal=(i == n - 1))


def rms_rstd(K, src, Bsrc, junk, Bjunk, ss, Bss, n):
    S = K.S
    S.op("act", lambda e: e.activation(out=junk, in_=src, func=AF.Square, accum_out=ss),
         reads=[Bsrc], writes=[Bjunk, Bss])
    S.op("act", lambda e: e.activation(out=ss, in_=ss, func=AF.Sqrt, bias=EPS, scale=1.0 / n),
         reads=[Bss], writes=[Bss])
    S.op("dve", lambda e: e.reciprocal(out=ss, in_=ss), reads=[Bss], writes=[Bss])


def interleave(gens, skew=0):
    gens = list(gens)
    start = {id(g): i * skew for i, g in enumerate(gens)}
    rnd = 0
    while gens:
        for g in list(gens):
            if rnd < start[id(g)]:
                continue
            try:
                next(g)
            except StopIteration:
                gens.remove(g)
        rnd += 1


def phase1_inproj(K):
    S, A, nc = K.S, K.A, K.nc
    ps, Bps = K.ps, K.Bps
    uT, BuT = A.alloc("uT", [8, SEQ], BF16)
    wtA = [A.alloc(f"wtA{i}", [8, 512], BF16) for i in range(2)]
    wtB = [A.alloc(f"wtB{i}", [8, 256], BF16) for i in range(2)]
    ropeS, BropeS = A.alloc("ropeS", [SEQ])
    cq, Bcq = A.alloc("cq", [SEQ])
    ck, Bck = A.alloc("ck", [SEQ])
    permf, Bpermf = A.alloc("permf", [128])
    permq, Bpermq = A.alloc("permq", [128], BF16)
    permk, Bpermk = A.alloc("permk", [128], BF16)
    od_b, Bod = A.alloc("od_b", [128], BF16)
    wa_b, Bwa = A.alloc("wa_b", [16, 128], BF16)
    wi_b, Bwi = A.alloc("wi_b", [16, 128], BF16)
    xn, Bxn = A.alloc("xn", [D], BF16)
    junk, Bjunk = xn, Bxn
    xns = [(xn, Bxn), A.alloc("xn2", [D], BF16)]
    ss = [A.alloc(f"ss{i}", [1]) for i in range(2)]
    stgA = [A.alloc(f"stgA{i}", [SEQ], BF16) for i in range(2)]
    stgB = [A.alloc(f"stgB{i}", [SEQ], BF16) for i in range(1)]
    xq = [A.alloc(f"xq{i}", [512], BF16) for i in range(2)]
    sq = [A.alloc(f"sq{i}", [512], BF16) for i in range(2)]
    ta = [A.alloc(f"ta{i}", [512]) for i in range(2)]
    tb_ = [A.alloc(f"tb{i}", [512]) for i in range(2)]
    rst = [A.alloc(f"rst{i}", [512]) for i in range(2)]
    xrp, Bxrp = A.alloc("xrp", [SEQ + 4])
    cc, Bcc = A.alloc("cc", [SEQ])
    ccb, Bccb = A.alloc("ccb", [SEQ], BF16)
    aas = [A.alloc(f"aa{i}", [SEQ]) for i in range(2)]
    bts = [A.alloc(f"bt{i}", [SEQ]) for i in range(2)]
    aa, Baa = aas[0]
    t1, Bt1 = A.alloc("t1", [SEQ])
    gmix, Bgmix = cc[:, 0:D], Bcc
    hf, Bhf = A.alloc("hf", [SEQ])
    hb, Bhb = A.alloc("hb", [SEQ])
    xt = [(hb[:, 0:D], Bhb), (hf[:, 0:D], Bhf)]
    vst, Bvst = aa.bitcast(BF16).rearrange("p (a b) -> p a b", a=16), Baa

    pvt = K.pvt
    S.dma("sp", lambda e: e.dma_start(out=cq, in_=K.ropeC), writes=[Bcq])
    S.dma("sp", lambda e: e.dma_start(out=ck, in_=K.ropeC), writes=[Bck])
    S.dma("sp", lambda e: e.dma_start(out=ropeS, in_=K.ropeS), writes=[BropeS])
    S.dma("sp", lambda e: e.dma_start(out=permf, in_=K.perm), writes=[Bpermf])
    S.dma("pool", lambda e: e.dma_start(out=wa_b, in_=K.lru_wa.rearrange("d c p n -> p (d c) n")), writes=[Bwa])
    S.dma("pool", lambda e: e.dma_start(out=wi_b, in_=K.lru_wi.rearrange("d c p n -> p (d c) n")), writes=[Bwi])
    S.op("dve", lambda e: e.memset(od_b, 1.0 / 128.0), writes=[Bod])
    S.op("dve", lambda e: e.memset(xrp, 0.0), writes=[Bxrp])
    qn = pvt[:, PV_QN:PV_QN + 1]
    kn = pvt[:, PV_KN:PV_KN + 1]
    S.op("dve", lambda e: e.tensor_scalar(out=cq, in0=cq, scalar1=qn, scalar2=None, op0=ALU.mult),
         reads=[Bcq, K.Bpv], writes=[Bcq])
    S.op("dve", lambda e: e.tensor_scalar(out=ck, in0=ck, scalar1=kn, scalar2=None, op0=ALU.mult),
         reads=[Bck, K.Bpv], writes=[Bck])
    S.op("dve", lambda e: e.tensor_scalar(out=permq, in0=permf, scalar1=qn, scalar2=None, op0=ALU.mult),
         reads=[Bpermf, K.Bpv], writes=[Bpermq])
    S.op("dve", lambda e: e.tensor_scalar(out=permk, in0=permf, scalar1=kn, scalar2=None, op0=ALU.mult),
         reads=[Bpermf, K.Bpv], writes=[Bpermk])

    w_in_v = K.w_in.rearrange("(c p) n -> p c n", p=128)
    wcnt = {"A": 0, "B": 0}

    def load_w(which, cols):
        tiles = wtA if which == "A" else wtB
        i = wcnt[which] % 2
        wcnt[which] += 1
        w, Bw = tiles[i]
        off = 0
        for (c0, wd) in cols:
            S.dma("pool", lambda e, w=w, off=off, c0=c0, wd=wd: e.dma_start(
                out=w[:, :, off:off + wd], in_=w_in_v[:, :, c0:c0 + wd]), writes=[Bw])
            off += wd
        return w, Bw

    def inproj(w, Bw, woff, tb, bank):
        mm_group(S, ps[bank][:], Bps[bank],
                 [(w[:, k, woff:woff + 128], uT[:, k, tb * 512:(tb + 1) * 512]) for k in range(8)],
                 reads=[Bw, BuT])

    scnt = {"A": 0, "B": 0}

    def build_uT(s):
        S.dma("sp", lambda e: e.dma_start(out=gmix, in_=bcast_row(K.g_mix, D)), writes=[Bgmix])

        def tiles(par):
            x_t, Bx = xt[par]
            ss_t, Bss = ss[par]
            xn_, Bxn_ = xns[par]
            bank = par
            pv16 = ps[bank][:].bitcast(BF16)
            for i in range(par, 16, 2):
                r0 = s * SEQ + i * 128
                S.dma("sp", lambda e, r0=r0: e.dma_start(out=x_t, in_=K.x[r0:r0 + 128, :]), writes=[Bx])
                rms_rstd(K, x_t, Bx, xn_, Bxn_, ss_t, Bss, D)
                yield
                S.op("dve", lambda e: e.scalar_tensor_tensor(
                    out=xn_, in0=x_t, scalar=ss_t, in1=gmix, op0=ALU.mult, op1=ALU.mult),
                    reads=[Bx, Bss, Bgmix], writes=[Bxn_])
                yield
                for c in range(8):
                    S.op("pe", lambda e, c=c: e.transpose(pv16[:, c * 128:(c + 1) * 128], xn_[:, c * 128:(c + 1) * 128], K.ident_b),
                         reads=[Bxn_, K.Bident_b], writes=[Bps[bank]], signal=(c == 7))
                S.op("act", lambda e, i=i: e.activation(
                    out=uT[:, :, i * 128:(i + 1) * 128], in_=pv16.rearrange("p (c t) -> p c t", c=8), func=AF.Copy),
                    reads=[Bps[bank]], writes=[BuT])
                yield

        interleave([tiles(0), tiles(1)], skew=1)

    def qk_head(s, w, Bw, woff, is_q, hidx):
        cg, Bcg = (cq, Bcq) if is_q else (ck, Bck)
        pm, Bpm = (permq, Bpermq) if is_q else (permk, Bpermk)
        st_t, Bst = stgA[scnt["A"] % 2]
        scnt["A"] += 1
        for tb in range(4):
            p = tb % 2
            sl = slice(tb * 512, (tb + 1) * 512)
            xq_, Bxq = xq[p]
            sq_, Bsq = sq[p]
            ta_, Bta = ta[p]
            tb2, Btb = tb_[p]
            rs_, Brst = rst[p]
            zb = p
            inproj(w, Bw, woff, tb, zb)
            S.op("act", lambda e, xq_=xq_, zb=zb: e.activation(out=xq_, in_=ps[zb][:], func=AF.Copy), reads=[Bps[zb]], writes=[Bxq])
            S.op("act", lambda e, sq_=sq_, zb=zb: e.activation(out=sq_, in_=ps[zb][:], func=AF.Square), reads=[Bps[zb]], writes=[Bsq])
            S.op("dve", lambda e, sl=sl, cg=cg, ta_=ta_, zb=zb: e.tensor_tensor(out=ta_, in0=ps[zb][:], in1=cg[:, sl], op=ALU.mult),
                 reads=[Bps[zb], Bcg], writes=[Bta])
            yield
            mm_group(S, ps[2][:], Bps[2], [(od_b, sq_)], reads=[Bod, Bsq])
            mm_group(S, ps[3][:], Bps[3], [(pm, xq_)], reads=[Bpm, Bxq])
            S.op("act", lambda e, rs_=rs_: e.activation(out=rs_, in_=ps[2][:], func=AF.Sqrt, bias=EPS), reads=[Bps[2]], writes=[Brst])
            S.op("dve", lambda e, sl=sl, tb2=tb2: e.tensor_tensor(out=tb2, in0=ps[3][:], in1=ropeS[:, sl], op=ALU.mult),
                 reads=[Bps[3], BropeS], writes=[Btb])
            yield
            S.op("dve", lambda e, rs_=rs_: e.reciprocal(out=rs_, in_=rs_), reads=[Brst], writes=[Brst])
            S.op("dve", lambda e, ta_=ta_, tb2=tb2: e.tensor_tensor(out=ta_, in0=ta_, in1=tb2, op=ALU.add), reads=[Bta, Btb], writes=[Bta])
            S.op("dve", lambda e, sl=sl, st_t=st_t, ta_=ta_, rs_=rs_: e.tensor_tensor(out=st_t[:, sl], in0=ta_, in1=rs_, op=ALU.mult),
                 reads=[Bta, Brst], writes=[Bst])
            yield
        dst = (K.qT_s if is_q else K.kT_s)[s, hidx]
        S.dma("sp", lambda e, st_t=st_t, dst=dst: e.dma_start(out=dst, in_=st_t), reads=[Bst], owner=Bst)

    def stream_A(s):
        for t2 in range(2):
            w, Bw = load_w("A", [(t2 * 512, 512)])
            for j in range(4):
                yield from qk_head(s, w, Bw, j * 128, True, t2 * 4 + j)
        w, Bw = load_w("A", [(1024, 512)])
        for j in range(2):
            yield from qk_head(s, w, Bw, j * 128, False, j)
        for gi, dst_s in ((0, K.sga_s), (1, K.sgr_s)):
            for t2 in range(2):
                wg, Bwg = load_w("A", [(3584 + gi * 1024 + t2 * 512, 512)])
                for j in range(4):
                    st_t, Bst = stgA[scnt["A"] % 2]
                    scnt["A"] += 1
                    for tb in range(4):
                        bank = tb % 2
                        inproj(wg, Bwg, j * 128, tb, bank)
                        S.op("act", lambda e, bank=bank, tb=tb, st_t=st_t: e.activation(out=st_t[:, tb * 512:(tb + 1) * 512], in_=ps[bank][:], func=AF.Sigmoid),
                             reads=[Bps[bank]], writes=[Bst])
                        yield
                    S.dma("sp", lambda e, st_t=st_t, dst=dst_s[s, t2 * 4 + j]: e.dma_start(out=dst, in_=st_t), reads=[Bst], owner=Bst)

    def do_V(s):
        w, Bw = load_w("A", [(1024, 512)])
        for tk in range(16):
            bank = 2 + tk % 2
            mm_group(S, ps[bank][:, 0:256], Bps[bank],
                     [(uT[:, k, tk * 128:(tk + 1) * 128], w[:, k, 256:512]) for k in range(8)], reads=[Bw, BuT])
            S.op("act", lambda e, bank=bank, tk=tk: e.activation(out=vst[:, tk, :], in_=ps[bank][:, 0:256], func=AF.Copy),
                 reads=[Bps[bank]], writes=[Bvst])
        S.dma("sp", lambda e, s=s: e.dma_start(out=K.V_s[s], in_=vst), reads=[Bvst], owner=Bvst)

    def stream_B(s):
        for j in range(8):
            w, Bw = load_w("B", [(1536 + j * 128, 128), (2560 + j * 128, 128)])
            for tb in range(4):
                bank = 4 + tb % 2
                inproj(w, Bw, 0, tb, bank)
                S.op("act", lambda e, bank=bank, tb=tb: e.activation(out=xrp[:, 2 + tb * 512:2 + (tb + 1) * 512], in_=ps[bank][:], func=AF.Copy),
                     reads=[Bps[bank]], writes=[Bxrp])
                yield
            cw = lambda jj, j=j: K.pvt[:, PV_CONVW + j * 4 + jj:PV_CONVW + j * 4 + jj + 1]
            cb = K.pvt[:, PV_CONVB + j:PV_CONVB + j + 1]
            S.op("act", lambda e, cw=cw, cb=cb: e.activation(out=cc, in_=xrp[:, 0:SEQ], func=AF.Identity, bias=cb, scale=cw(0)),
                 reads=[Bxrp, K.Bpv], writes=[Bcc])
            for jj in range(1, 4):
                S.op("dve", lambda e, cw=cw, jj=jj: e.scalar_tensor_tensor(out=cc, in0=xrp[:, jj:jj + SEQ], scalar=cw(jj), in1=cc, op0=ALU.mult, op1=ALU.add),
                     reads=[Bxrp, Bcc, K.Bpv], writes=[Bcc])
                yield
            S.op("act", lambda e: e.activation(out=ccb, in_=cc, func=AF.Copy), reads=[Bcc], writes=[Bccb])

            def dir_gen(d, j=j):
                aa_, Baa_ = aas[d]
                bt_, Bbt_ = bts[d]
                hh, Bhh = (hf, Bhf) if d == 0 else (hb, Bhb)
                ba = K.pvt[:, PV_BA + d * 8 + j:PV_BA + d * 8 + j + 1]
                bi = K.pvt[:, PV_BI + d * 8 + j:PV_BI + d * 8 + j + 1]
                kkc = K.kk[:, d * 8 + j:d * 8 + j + 1]
                for tb in range(4):
                    sl = slice(tb * 512, (tb + 1) * 512)
                    mm_group(S, ps[6][:], Bps[6], [(wa_b[:, d * 8 + j, :], ccb[:, sl])], reads=[Bwa, Bccb])
                    S.op("act", lambda e, sl=sl: e.activation(out=aa_[:, sl], in_=ps[6][:], func=AF.Sigmoid, bias=ba),
                         reads=[Bps[6], K.Bpv], writes=[Baa_])
                    mm_group(S, ps[7][:], Bps[7], [(wi_b[:, d * 8 + j, :], ccb[:, sl])], reads=[Bwi, Bccb])
                    S.op("act", lambda e, sl=sl: e.activation(out=bt_[:, sl], in_=ps[7][:], func=AF.Sigmoid, bias=bi),
                         reads=[Bps[7], K.Bpv], writes=[Bbt_])
                    yield
                S.op("act", lambda e: e.activation(out=aa_, in_=aa_, func=AF.Exp, scale=kkc), reads=[Baa_, K.Bkk], writes=[Baa_])
                S.op("dve", lambda e: e.tensor_tensor(out=bt_, in0=bt_, in1=cc, op=ALU.mult), reads=[Bbt_, Bcc], writes=[Bbt_])
                yield
                S.op("act", lambda e: e.activation(out=hh, in_=aa_, func=AF.Square), reads=[Baa_], writes=[Bhh])
                S.op("act", lambda e: e.activation(out=hh, in_=hh, func=AF.Sqrt, bias=1.0, scale=-1.0), reads=[Bhh], writes=[Bhh])
                yield
                S.op("dve", lambda e: e.tensor_tensor(out=bt_, in0=bt_, in1=hh, op=ALU.mult), reads=[Bbt_, Bhh], writes=[Bbt_])
                yield
                if d == 0:
                    S.op("dve", lambda e: e.tensor_tensor_scan(out=hh, data0=aa_, data1=bt_, initial=0.0, op0=ALU.mult, op1=ALU.add),
                         reads=[Baa_, Bbt_], writes=[Bhh])
                else:
                    S.op("dve", lambda e: e.tensor_tensor_scan(out=hh[:, ::-1], data0=aa_[:, ::-1], data1=bt_[:, ::-1], initial=0.0, op0=ALU.mult, op1=ALU.add),
                         reads=[Baa_, Bbt_], writes=[Bhh])
                yield

            def gelu_gen(w=w, Bw=Bw):
                for tb in range(4):
                    sl = slice(tb * 512, (tb + 1) * 512)
                    bank = 4 + tb % 2
                    inproj(w, Bw, 128, tb, bank)
                    S.op("act", lambda e, bank=bank, sl=sl: e.activation(out=t1[:, sl], in_=ps[bank][:], func=AF.Square), reads=[Bps[bank]], writes=[Bt1])
                    S.op("act", lambda e, sl=sl: e.activation(out=t1[:, sl], in_=t1[:, sl], func=AF.Identity, bias=1.0, scale=0.044715),
                         reads=[Bt1], writes=[Bt1])
                    S.op("dve", lambda e, bank=bank, sl=sl: e.tensor_tensor(out=t1[:, sl], in0=t1[:, sl], in1=ps[bank][:], op=ALU.mult),
                         reads=[Bt1, Bps[bank]], writes=[Bt1])
                    S.op("act", lambda e, sl=sl: e.activation(out=t1[:, sl], in_=t1[:, sl], func=AF.Sigmoid, scale=1.5957691216), reads=[Bt1], writes=[Bt1])
                    S.op("dve", lambda e, bank=bank, sl=sl: e.tensor_tensor(out=t1[:, sl], in0=t1[:, sl], in1=ps[bank][:], op=ALU.mult),
                         reads=[Bt1, Bps[bank]], writes=[Bt1])
                    yield

            subs = [dir_gen(0), dir_gen(1), gelu_gen()]
            while subs:
                for g in list(subs):
                    try:
                        next(g)
                    except StopIteration:
                        subs.remove(g)
                yield
            S.op("dve", lambda e: e.tensor_tensor(out=hf, in0=hf, in1=hb, op=ALU.add), reads=[Bhf, Bhb], writes=[Bhf])
            st_t, Bst = stgB[0]
            S.op("dve", lambda e, st_t=st_t: e.tensor_tensor(out=st_t, in0=hf, in1=t1, op=ALU.mult), reads=[Bhf, Bt1], writes=[Bst])
            S.dma("sp", lambda e, st_t=st_t, j=j, s=s: e.dma_start(out=K.yl_s[s, j], in_=st_t), reads=[Bst], owner=Bst)
            yield

    for s in range(NSEQ):
        build_uT(s)
        interleave([stream_A(s), stream_B(s)])
        do_V(s)


def phase2_attn(K):
    S, A, nc = K.S, K.A, K.nc
    ps, Bps = K.ps, K.Bps
    wab, Bwab = A.alloc("wab", [8, D], BF16)
    wlb, Bwlb = A.alloc("wlb", [8, D], BF16)
    wo, Bwo = A.alloc("wo", [8, D], BF16)
    kT, BkT = A.alloc("kT", [2, SEQ], BF16)
    V, BV = A.alloc("V", [16, 256], BF16)
    qT = [A.alloc(f"qT{i}", [8, 512], BF16) for i in range(2)]
    ylb = [A.alloc(f"ylb{i}", [8, 512], BF16) for i in range(2)]
    gab = [A.alloc(f"gab{i}", [8, 512], BF16) for i in range(2)]
    grb = [A.alloc(f"grb{i}", [8, 512], BF16) for i in range(2)]
    PT = [A.alloc(f"PT{i}", [512], BF16) for i in range(6)]
    SBANK = (0, 1, 2, 7)
    attnT, BattnT = A.alloc("attnT", [8, 512], BF16)
    mrg, Bmrg = A.alloc("mrg", [8, 512], BF16)
    rz, Brz = A.alloc("rz", [512])
    zacc = [A.alloc(f"zacc{i}", [512]) for i in range(2)]
    zab = [A.alloc(f"zab{i}", [512], BF16) for i in range(2)]
    m1, Bm1 = A.alloc("m1", [512])
    m2, Bm2 = A.alloc("m2", [512])
    xt = [A.alloc(f"xt{i}", [D]) for i in range(2)]
    ho = [A.alloc(f"ho{i}", [D]) for i in range(2)]
    S.dma("pool", lambda e: e.dma_start(out=wab, in_=K.w_attn_br.rearrange("(c p) n -> p c n", p=128)), writes=[Bwab])
    S.dma("pool", lambda e: e.dma_start(out=wlb, in_=K.w_lru_br.rearrange("(c p) n -> p c n", p=128)), writes=[Bwlb])
    S.dma("pool", lambda e: e.dma_start(out=wo, in_=K.w_out.rearrange("(c p) n -> p c n", p=128)), writes=[Bwo])
    zt, Bzt = A.alloc("zt", [D], BF16)
    S.op("dve", lambda e: e.memset(zt, 0.0), writes=[Bzt])
    zrows = list(range(0, NSLOT + 128, 128))

    def zero_some(n):
        for _ in range(n):
            if zrows:
                r0 = zrows.pop(0)
                S.dma("sp", lambda e, r0=r0: e.dma_start(out=K.xs[r0:r0 + 128, :], in_=zt), reads=[Bzt], owner=Bzt)
    scale = 128.0 ** -0.5
    cnt = 0
    for s in range(NSEQ):
        S.dma("sp", lambda e, s=s: e.dma_start(out=kT, in_=K.kT_s[s].rearrange("h p t -> p h t")), writes=[BkT])
        S.dma("sp", lambda e, s=s: e.dma_start(out=V, in_=K.V_s[s]), writes=[BV])
        for qb in range(4):
            q, Bq = qT[cnt % 2]
            yl, Byl = ylb[cnt % 2]
            ga, Bga = gab[cnt % 2]
            gr, Bgr = grb[cnt % 2]
            cnt += 1
            tsl = slice(qb * 512, (qb + 1) * 512)
            S.dma("sp", lambda e, q=q, s=s, tsl=tsl: e.dma_start(out=q, in_=K.qT_s[s].rearrange("h p t -> p h t")[:, :, tsl]), writes=[Bq])
            S.dma("sp", lambda e, yl=yl, s=s, tsl=tsl: e.dma_start(out=yl, in_=K.yl_s[s].rearrange("h p t -> p h t")[:, :, tsl]), writes=[Byl])
            S.dma("sp", lambda e, ga=ga, s=s, tsl=tsl: e.dma_start(out=ga, in_=K.sga_s[s].rearrange("h p t -> p h t")[:, :, tsl]), writes=[Bga])
            S.dma("sp", lambda e, gr=gr, s=s, tsl=tsl: e.dma_start(out=gr, in_=K.sgr_s[s].rearrange("h p t -> p h t")[:, :, tsl]), writes=[Bgr])
            for h in range(8):
                kv = h // 4
                ob, zb = 3 + h % 2, 5 + h % 2

                def score(kc):
                    bank = SBANK[kc % 4]
                    mm_group(S, ps[bank][:], Bps[bank], [(kT[:, kv, kc * 128:(kc + 1) * 128], q[:, h, :])], reads=[BkT, Bq])
                    pt, Bpt = PT[kc % 6]
                    S.op("act", lambda e, bank=bank, pt=pt: e.activation(out=pt, in_=ps[bank][:], func=AF.Exp, scale=scale),
                         reads=[Bps[bank]], writes=[Bpt])

                za, Bza = zacc[h % 2]
                zb16, Bzb16 = zab[h % 2]

                def pv(kc):
                    pt, Bpt = PT[kc % 6]
                    S.op("pe", lambda e, kc=kc, pt=pt, ob=ob, kv=kv: e.matmul(ps[ob][:], V[:, kc, kv * 128:(kv + 1) * 128], pt, start=(kc == 0), stop=(kc == 15)),
                         reads=[BV, Bpt], writes=[Bps[ob]], signal=(kc == 15))
                    if kc % 2 == 1:
                        S.op("pe", lambda e, kc=kc, pt=pt, zb=zb: e.matmul(ps[zb][:], K.ones_b, pt, start=(kc == 1), stop=False),
                             reads=[K.Bones_b, Bpt], writes=[Bps[zb]], signal=False)
                    elif kc == 0:
                        S.op("dve", lambda e, pt=pt, za=za: e.tensor_copy(out=za, in_=pt), reads=[Bpt], writes=[Bza])
                    else:
                        S.op("dve", lambda e, pt=pt, za=za: e.tensor_tensor(out=za, in0=za, in1=pt, op=ALU.add), reads=[Bpt, Bza], writes=[Bza])
                    if kc == 15:
                        S.op("pe", lambda e, za=za, zb=zb: e.matmul(ps[zb][:], K.ones_f, za, start=False, stop=True),
                             reads=[K.Bones_f, Bza], writes=[Bps[zb]], signal=True)

                score(0)
                score(1)
                score(2)
                for kc in range(16):
                    if kc + 3 < 16:
                        score(kc + 3)
                    pv(kc)
                S.op("dve", lambda e, zb=zb: e.reciprocal(out=rz, in_=ps[zb][:]), reads=[Bps[zb]], writes=[Brz])
                S.op("dve", lambda e, ob=ob, h=h: e.tensor_tensor(out=attnT[:, h, :], in0=ps[ob][:], in1=rz, op=ALU.mult),
                     reads=[Bps[ob], Brz], writes=[BattnT])
                zero_some(6)
            for m in range(8):
                b1, b2 = 0 + m % 2, 2 + m % 2
                mm_group(S, ps[b1][:], Bps[b1], [(wab[:, k, m * 128:(m + 1) * 128], attnT[:, k, :]) for k in range(8)], reads=[Bwab, BattnT])
                mm_group(S, ps[b2][:], Bps[b2], [(wlb[:, k, m * 128:(m + 1) * 128], yl[:, k, :]) for k in range(8)], reads=[Bwlb, Byl])
                S.op("dve", lambda e, b1=b1, m=m, ga=ga: e.tensor_tensor(out=m1, in0=ps[b1][:], in1=ga[:, m, :], op=ALU.mult),
                     reads=[Bps[b1], Bga], writes=[Bm1])
                S.op("dve", lambda e, b2=b2, m=m, gr=gr: e.tensor_tensor(out=m2, in0=ps[b2][:], in1=gr[:, m, :], op=ALU.mult),
                     reads=[Bps[b2], Bgr], writes=[Bm2])
                S.op("dve", lambda e, m=m: e.tensor_tensor(out=mrg[:, m, :], in0=m1, in1=m2, op=ALU.add),
                     reads=[Bm1, Bm2], writes=[Bmrg])
            for tk in range(4):
                x_t, Bx = xt[tk % 2]
                h_t, Bh = ho[tk % 2]
                r0 = s * SEQ + qb * 512 + tk * 128
                S.dma("sp", lambda e, x_t=x_t, r0=r0: e.dma_start(out=x_t, in_=K.x[r0:r0 + 128, :]), writes=[Bx])
                for nh in range(2):
                    bank = 5 + nh
                    mm_group(S, ps[bank][:], Bps[bank],
                             [(mrg[:, k, tk * 128:(tk + 1) * 128], wo[:, k, nh * 512:(nh + 1) * 512]) for k in range(8)], reads=[Bmrg, Bwo])
                    S.op("dve", lambda e, bank=bank, nh=nh, x_t=x_t, h_t=h_t: e.tensor_tensor(
                        out=h_t[:, nh * 512:(nh + 1) * 512], in0=ps[bank][:], in1=x_t[:, nh * 512:(nh + 1) * 512], op=ALU.add),
                        reads=[Bps[bank], Bx], writes=[Bh])
                S.dma("sp", lambda e, h_t=h_t, r0=r0: e.dma_start(out=K.h1_s[r0:r0 + 128, :], in_=h_t), reads=[Bh], owner=Bh)
    zero_some(len(zrows))


def phase3_router(K):
    S, A, nc = K.S, K.A, K.nc
    ps, Bps = K.ps, K.Bps
    gmoe, Bgmoe = A.alloc("gmoe", [D])
    wr, Bwr = A.alloc("wr", [8, E])
    brt, Bbr = A.alloc("brt", [E])
    tri, Btri = A.alloc("tri", [128])
    eC, BeC = A.alloc("eC", [E])
    msum, Bmsum = A.alloc("msum", [E])
    S.dma("sp", lambda e: e.dma_start(out=gmoe, in_=bcast_row(K.g_moe, D)), writes=[Bgmoe])
    S.dma("sp", lambda e: e.dma_start(out=wr, in_=K.w_router.rearrange("(c p) n -> p c n", p=128)), writes=[Bwr])
    S.dma("sp", lambda e: e.dma_start(out=brt, in_=bcast_row(K.b_router, E)), writes=[Bbr])
    S.dma("sp", lambda e: e.dma_start(out=tri, in_=K.tri), writes=[Btri])
    S.dma("sp", lambda e: e.dma_start(out=eC, in_=K.eC), writes=[BeC])
    S.op("dve", lambda e: e.memset(msum, 0.0), writes=[Bmsum])

    def tile_stream(par):
        h_t, Bh = A.alloc(f"ht{par}", [D])
        junk, Bjunk = A.alloc(f"junk{par}", [D], BF16)
        ss_t, Bss = A.alloc(f"ss{par}", [1])
        u2, Bu2 = A.alloc(f"u2{par}", [D])
        ub, Bub = A.alloc(f"u2b{par}", [D], BF16)
        u2T, Bu2T = A.alloc(f"u2T{par}", [8, 128])
        lg, Blg = A.alloc(f"lg{par}", [E])
        top8, Btop8 = A.alloc(f"top8{par}", [8])
        nm, Bnm = A.alloc(f"nm{par}", [1])
        mask, Bmask = A.alloc(f"mask{par}", [E])
        ex, Bex = A.alloc(f"ex{par}", [E])
        den, Bden = A.alloc(f"den{par}", [1])
        gf, Bgf = A.alloc(f"gf{par}", [E])
        rank, Brank = A.alloc(f"rank{par}", [E])
        okm, Bok = A.alloc(f"okm{par}", [E])
        dst, Bdst = A.alloc(f"dst{par}", [E])
        dk, Bdk = A.alloc(f"dk{par}", [4])
        oh, Boh = A.alloc(f"oh{par}", [E])
        b0 = par * 2

        def gen():
            for i in range(par, NTILE, 4):
                r0 = i * 128
                S.dma("sp", lambda e, r0=r0: e.dma_start(out=h_t, in_=K.h1_s[r0:r0 + 128, :]), writes=[Bh])
                rms_rstd(K, h_t, Bh, junk, Bjunk, ss_t, Bss, D)
                S.op("dve", lambda e: e.scalar_tensor_tensor(out=u2, in0=h_t, scalar=ss_t, in1=gmoe, op0=ALU.mult, op1=ALU.mult),
                     reads=[Bh, Bss, Bgmoe], writes=[Bu2])
                yield
                S.op("act", lambda e: e.activation(out=ub, in_=u2, func=AF.Copy), reads=[Bu2], writes=[Bub])
                for hc in range(2):
                    bank = b0
                    for c4 in range(4):
                        c = hc * 4 + c4
                        S.op("pe", lambda e, c=c, c4=c4, bank=bank: e.transpose(ps[bank][:, c4 * 128:(c4 + 1) * 128], u2[:, c * 128:(c + 1) * 128], K.ident_f),
                             reads=[Bu2, K.Bident_f], writes=[Bps[bank]], signal=(c4 == 3))
                    S.op("act", lambda e, bank=bank, hc=hc: e.activation(out=u2T[:, hc * 4:(hc + 1) * 4, :], in_=ps[bank][:].rearrange("p (c t) -> p c t", c=4), func=AF.Copy),
                         reads=[Bps[bank]], writes=[Bu2T])
                yield
                lb, rb = b0 + 1, b0 + 1
                mm_group(S, ps[lb][:, 0:E], Bps[lb], [(u2T[:, k, :], wr[:, k, :]) for k in range(8)], reads=[Bu2T, Bwr])
                S.op("dve", lambda e, lb=lb: e.tensor_tensor(out=lg, in0=ps[lb][:, 0:E], in1=brt, op=ALU.add), reads=[Bps[lb], Bbr], writes=[Blg])
                S.op("dve", lambda e: e.max(out=top8, in_=lg), reads=[Blg], writes=[Btop8])
                S.op("dve", lambda e: e.tensor_scalar(out=mask, in0=lg, scalar1=top8[:, 3:4], scalar2=None, op0=ALU.is_ge),
                     reads=[Blg, Btop8], writes=[Bmask])
                yield
                mm_group(S, ps[rb][:, 64:64 + E], Bps[rb], [(tri, mask), (K.ones_f, msum)], reads=[Btri, Bmask, K.Bones_f, Bmsum])
                S.op("dve", lambda e: e.tensor_tensor(out=msum, in0=msum, in1=mask, op=ALU.add), reads=[Bmsum, Bmask], writes=[Bmsum])
                yield
                S.op("dve", lambda e: e.tensor_scalar(out=nm, in0=top8[:, 0:1], scalar1=-1.0, scalar2=None, op0=ALU.mult),
                     reads=[Btop8], writes=[Bnm])
                S.op("act", lambda e: e.activation(out=ex, in_=lg, func=AF.Exp, bias=nm), reads=[Blg, Bnm], writes=[Bex])
                S.op("dve", lambda e, rb=rb: e.tensor_copy(out=rank, in_=ps[rb][:, 64:64 + E]), reads=[Bps[rb]], writes=[Brank])
                S.op("dve", lambda e: e.tensor_scalar(out=okm, in0=rank, scalar1=float(CAP), scalar2=None, op0=ALU.is_lt),
                     reads=[Brank], writes=[Bok])
                S.op("dve", lambda e: e.tensor_tensor(out=dst, in0=rank, in1=eC, op=ALU.add), reads=[Brank, BeC], writes=[Bdst])
                S.op("dve", lambda e: e.scalar_tensor_tensor(out=dst, in0=dst, scalar=float(-TRASH), in1=okm, op0=ALU.add, op1=ALU.mult),
                     reads=[Bdst, Bok], writes=[Bdst])
                S.op("dve", lambda e: e.tensor_scalar(out=dst, in0=dst, scalar1=float(TRASH), scalar2=None, op0=ALU.add),
                     reads=[Bdst], writes=[Bdst])
                yield
                S.op("dve", lambda e: e.tensor_tensor(out=ex, in0=ex, in1=mask, op=ALU.mult), reads=[Bex, Bmask], writes=[Bex])
                S.op("dve", lambda e: e.reduce_sum(out=den, in_=ex, axis=AX.X), reads=[Bex], writes=[Bden])
                S.op("dve", lambda e: e.reciprocal(out=den, in_=den), reads=[Bden], writes=[Bden])
                S.op("dve", lambda e: e.scalar_tensor_tensor(out=gf, in0=ex, scalar=den, in1=okm, op0=ALU.mult, op1=ALU.mult),
                     reads=[Bex, Bden, Bok], writes=[Bgf])
                yield
                for k in range(4):
                    S.op("dve", lambda e, k=k: e.scalar_tensor_tensor(out=oh, in0=lg, scalar=top8[:, k:k + 1], in1=dst, op0=ALU.is_equal, op1=ALU.mult,
                                                                      accum_out=dk[:, k:k + 1]),
                         reads=[Blg, Btop8, Bdst], writes=[Boh, Bdk])
                    S.op("dve", lambda e, k=k, i=i: e.scalar_tensor_tensor(out=oh, in0=lg, scalar=top8[:, k:k + 1], in1=gf, op0=ALU.is_equal, op1=ALU.mult,
                                                                           accum_out=K.gate_a[:, i * 4 + k:i * 4 + k + 1]),
                         reads=[Blg, Btop8, Bgf], writes=[Boh, K.Bgate])
                S.op("dve", lambda e, i=i: e.tensor_copy(out=K.dest_i[:, i * 4:(i + 1) * 4], in_=dk), reads=[Bdk], writes=[K.Bdest])
                for k in range(4):
                    S.dma("pool", lambda e, k=k, i=i: e.indirect_dma_start(
                        out=K.xs, out_offset=bass.IndirectOffsetOnAxis(ap=K.dest_i[:, i * 4 + k:i * 4 + k + 1], axis=0),
                        in_=ub, in_offset=None), reads=[Bub, K.Bdest], owner=Bub)
                yield
        return gen()

    interleave([tile_stream(q) for q in range(4)], skew=2)


def issue_expert_load(K, e_, w, Bw, wd, Bwd, bd, Bbd):
    S = K.S
    src = K.w_gu[e_].rearrange("(c p) n -> p c n", p=128)
    for hh in range(2):
        S.dma("pool", lambda e, w=w, src=src, hh=hh: e.dma_start(out=w[:, hh * 4:(hh + 1) * 4, :], in_=src[:, hh * 4:(hh + 1) * 4, :]), writes=[Bw])
    S.dma("pool", lambda e, wd=wd, e_=e_: e.dma_start(out=wd, in_=K.w_dn[e_].rearrange("(c p) n -> p c n", p=128)), writes=[Bwd])
    S.dma("pool", lambda e, bd=bd, e_=e_: e.dma_start(out=bd[0:1, :], in_=K.b_dn[e_:e_ + 1, :]), writes=[Bbd])


def phase4_experts(K):
    S, A, nc = K.S, K.A, K.nc
    ps, Bps = K.ps, K.Bps
    wgu = [K.pre4["wgu"], A.alloc("wgu1", [8, 2 * F], BF16)]
    wdn = [K.pre4["wdn"], A.alloc("wdn1", [8, D], BF16)]
    bdn = [K.pre4["bdn"], A.alloc("bdn1", [D], BF16)]
    xTs = [A.alloc(f"xT{i}", [8, CAP], BF16) for i in range(2)]
    resTs = [A.alloc(f"resT{i}", [8, CAP], BF16) for i in range(2)]
    xst = [A.alloc(f"xst{i}", [D], BF16) for i in range(3)]
    yst = [A.alloc(f"yst{i}", [D]) for i in range(2)]
    HW = CAP // 2
    gt = [A.alloc(f"gt{i}", [HW]) for i in range(2)]
    sg = [A.alloc(f"sg{i}", [HW]) for i in range(2)]
    ut = [A.alloc(f"ut{i}", [HW]) for i in range(2)]
    cnts = {"y": 0, "x": 0, "u": 0}
    S.op("dve", lambda e: e.memset(yst[1][0], 0.0), writes=[yst[1][1]])
    S.dma("sp", lambda e: e.dma_start(out=K.ys[NSLOT:NSLOT + 128, :], in_=yst[1][0]), reads=[yst[1][1]], owner=yst[1][1])

    def load_expert(e_):
        issue_expert_load(K, e_, *wgu[e_ % 2], *wdn[e_ % 2], *bdn[e_ % 2])

    def build_xT(e_):
        xT, BxT = xTs[e_ % 2]
        for sb in range(NSB):
            xs_t, Bxs = xst[cnts["x"] % 3]
            cnts["x"] += 1
            r0 = e_ * CAP + sb * 128
            S.dma("sp", lambda e, xs_t=xs_t, r0=r0: e.dma_start(out=xs_t, in_=K.xs[r0:r0 + 128, :]), writes=[Bxs])
            bank = sb % 2
            pv16 = ps[bank][:].bitcast(BF16)
            for c in range(8):
                S.op("pe", lambda e, c=c, pv16=pv16, xs_t=xs_t: e.transpose(pv16[:, c * 128:(c + 1) * 128], xs_t[:, c * 128:(c + 1) * 128], K.ident_b),
                     reads=[Bxs, K.Bident_b], writes=[Bps[bank]], signal=(c == 7))
            S.op("act", lambda e, pv16=pv16, sb=sb, xT=xT: e.activation(out=xT[:, :, sb * 128:(sb + 1) * 128], in_=pv16.rearrange("p (c t) -> p c t", c=8), func=AF.Copy),
                 reads=[Bps[bank]], writes=[BxT])

    def gate_up(e_):
        w, Bw = wgu[e_ % 2]
        xT, BxT = xTs[e_ % 2]
        resT, BresT = resTs[e_ % 2]
        for f in range(8):
            bgc = K.pvt[:, PV_BGU + e_ * 16 + f:PV_BGU + e_ * 16 + f + 1]
            buc = K.pvt[:, PV_BGU + e_ * 16 + 8 + f:PV_BGU + e_ * 16 + 8 + f + 1]
            for hv in range(2):
                nsl = slice(hv * HW, (hv + 1) * HW)
                cnt = cnts["u"]
                cnts["u"] += 1
                gb, ub_ = 2 + cnt % 2, 4 + cnt % 2
                g_t, Bg = gt[cnt % 2]
                s_t, Bs = sg[cnt % 2]
                u_t, Bu = ut[cnt % 2]
                mm_group(S, ps[gb][:, 0:HW], Bps[gb], [(w[:, k, f * 128:(f + 1) * 128], xT[:, k, nsl]) for k in range(8)], reads=[Bw, BxT])
                mm_group(S, ps[ub_][:, 0:HW], Bps[ub_], [(w[:, k, F + f * 128:F + (f + 1) * 128], xT[:, k, nsl]) for k in range(8)], reads=[Bw, BxT])
                S.op("dve", lambda e, gb=gb, g_t=g_t, bgc=bgc: e.tensor_scalar(out=g_t, in0=ps[gb][:, 0:HW], scalar1=bgc, scalar2=7.0, op0=ALU.add, op1=ALU.min),
                     reads=[Bps[gb], K.Bpv], writes=[Bg])
                S.op("act", lambda e, ub_=ub_, u_t=u_t, buc=buc: e.activation(out=u_t, in_=ps[ub_][:, 0:HW], func=AF.Identity, bias=buc),
                     reads=[Bps[ub_], K.Bpv], writes=[Bu])
                S.op("act", lambda e, g_t=g_t, s_t=s_t: e.activation(out=s_t, in_=g_t, func=AF.Sigmoid, scale=1.702), reads=[Bg], writes=[Bs])
                S.op("dve", lambda e, u_t=u_t: e.tensor_scalar(out=u_t, in0=u_t, scalar1=7.0, scalar2=-7.0, op0=ALU.min, op1=ALU.max),
                     reads=[Bu], writes=[Bu])
                S.op("dve", lambda e, g_t=g_t, s_t=s_t: e.tensor_tensor(out=g_t, in0=g_t, in1=s_t, op=ALU.mult), reads=[Bg, Bs], writes=[Bg])
                S.op("dve", lambda e, g_t=g_t, u_t=u_t, f=f, nsl=nsl, resT=resT: e.scalar_tensor_tensor(
                    out=resT[:, f, nsl], in0=u_t, scalar=1.0, in1=g_t, op0=ALU.add, op1=ALU.mult),
                    reads=[Bg, Bu], writes=[BresT])

    def down(e_):
        wd, Bwd = wdn[e_ % 2]
        bd, Bbd = bdn[e_ % 2]
        resT, BresT = resTs[e_ % 2]
        for sb in range(NSB):
            y_t, By = yst[cnts["y"] % 2]
            cnts["y"] += 1
            for nh in range(2):
                bank = 6 + nh
                pairs = [(resT[:, k, sb * 128:(sb + 1) * 128], wd[:, k, nh * 512:(nh + 1) * 512]) for k in range(8)]
                pairs.append((K.ones_b[0:1, :], bd[0:1, nh * 512:(nh + 1) * 512]))
                mm_group(S, ps[bank][:], Bps[bank], pairs, reads=[BresT, Bwd, Bbd, K.Bones_b])
                S.op("act", lambda e, bank=bank, y_t=y_t, nh=nh: e.activation(out=y_t[:, nh * 512:(nh + 1) * 512], in_=ps[bank][:], func=AF.Copy),
                     reads=[Bps[bank]], writes=[By])
            r0 = e_ * CAP + sb * 128
            S.dma("sp", lambda e, y_t=y_t, r0=r0: e.dma_start(out=K.ys[r0:r0 + 128, :], in_=y_t), reads=[By], owner=By)

    build_xT(0)
    for e_ in range(E):
        if e_ + 1 < E:
            load_expert(e_ + 1)
        if e_ == 0:
            wpg, Bwpg = K.pre5["wpg"]
            wpp, Bwpp = K.pre5["wpp"]
            gple, Bgple = K.pre5["gple"]
            S.dma("pool", lambda e: e.dma_start(out=wpg, in_=K.w_ple_gate.rearrange("(c p) n -> p c n", p=128)), writes=[Bwpg])
            S.dma("pool", lambda e: e.dma_start(out=wpp, in_=K.w_ple_proj.rearrange("(c p) n -> p c n", p=128)), writes=[Bwpp])
            S.dma("sp", lambda e: e.dma_start(out=gple, in_=bcast_row(K.g_ple, D)), writes=[Bgple])
        gate_up(e_)
        if e_ + 1 < E:
            build_xT(e_ + 1)
        down(e_)


def phase5_combine(K):
    S, A, nc = K.S, K.A, K.nc
    ps, Bps = K.ps, K.Bps
    wpg, Bwpg = K.pre5["wpg"]
    wpp, Bwpp = K.pre5["wpp"]
    gple, Bgple = K.pre5["gple"]

    def tile_stream(par):
        h_t, Bh = A.alloc(f"ht{par}", [D])
        yg = [A.alloc(f"yg{par}_{k}", [D]) for k in range(4)]
        p_t, Bp = A.alloc(f"pt{par}", [PLE])
        ptb, Bptb = A.alloc(f"ptb{par}", [PLE], BF16)
        pT, BpT = A.alloc(f"pT{par}", [2, 128], BF16)
        junk, Bjunk = A.alloc(f"junk{par}", [D], BF16)
        ss_t, Bss = A.alloc(f"ss{par}", [1])
        u3, Bu3 = A.alloc(f"u3{par}", [D], BF16)
        u3T, Bu3T = A.alloc(f"u3T{par}", [8, 128], BF16)
        sgm, Bsgm = A.alloc(f"sgm{par}", [D])
        o_t, Bo = A.alloc(f"ot{par}", [D])
        b0 = par * 2

        def gen():
            for i in range(par, NTILE, 4):
                r0 = i * 128
                S.dma("sp", lambda e, r0=r0: e.dma_start(out=h_t, in_=K.h1_s[r0:r0 + 128, :]), writes=[Bh])
                S.dma("sp", lambda e, r0=r0: e.dma_start(out=p_t, in_=K.p[r0:r0 + 128, :]), writes=[Bp])
                for k in range(4):
                    y_t, By = yg[k]
                    S.dma("pool", lambda e, y_t=y_t, i=i, k=k: e.indirect_dma_start(
                        out=y_t, out_offset=None, in_=K.ys,
                        in_offset=bass.IndirectOffsetOnAxis(ap=K.dest_i[:, i * 4 + k:i * 4 + k + 1], axis=0)),
                        reads=[K.Bdest], writes=[By])
                yield
                S.op("act", lambda e: e.activation(out=ptb, in_=p_t, func=AF.Copy), reads=[Bp], writes=[Bptb])
                pv1 = ps[b0 + 1][:].bitcast(BF16)
                for c in range(2):
                    S.op("pe", lambda e, c=c, pv1=pv1: e.transpose(pv1[:, c * 128:(c + 1) * 128], ptb[:, c * 128:(c + 1) * 128], K.ident_b),
                         reads=[Bptb, K.Bident_b], writes=[Bps[b0 + 1]], signal=(c == 1))
                S.op("act", lambda e, pv1=pv1: e.activation(out=pT, in_=pv1[:, 0:256].rearrange("p (c t) -> p c t", c=2), func=AF.Copy),
                     reads=[Bps[b0 + 1]], writes=[BpT])
                yield
                for k in range(4):
                    y_t, By = yg[k]
                    S.op("dve", lambda e, y_t=y_t, i=i, k=k: e.scalar_tensor_tensor(
                        out=h_t, in0=y_t, scalar=K.gate_a[:, i * 4 + k:i * 4 + k + 1], in1=h_t, op0=ALU.mult, op1=ALU.add),
                        reads=[By, Bh, K.Bgate], writes=[Bh])
                yield
                rms_rstd(K, h_t, Bh, junk, Bjunk, ss_t, Bss, D)
                S.op("dve", lambda e: e.scalar_tensor_tensor(out=u3, in0=h_t, scalar=ss_t, in1=gple, op0=ALU.mult, op1=ALU.mult),
                     reads=[Bh, Bss, Bgple], writes=[Bu3])
                yield
                pv16 = ps[b0][:].bitcast(BF16)
                for c in range(8):
                    S.op("pe", lambda e, c=c, pv16=pv16: e.transpose(pv16[:, c * 128:(c + 1) * 128], u3[:, c * 128:(c + 1) * 128], K.ident_b),
                         reads=[Bu3, K.Bident_b], writes=[Bps[b0]], signal=(c == 7))
                S.op("act", lambda e, pv16=pv16: e.activation(out=u3T, in_=pv16.rearrange("p (c t) -> p c t", c=8), func=AF.Copy),
                     reads=[Bps[b0]], writes=[Bu3T])
                yield
                for nh in range(2):
                    nsl = slice(nh * 512, (nh + 1) * 512)
                    gbk, pbk = b0, b0 + 1
                    mm_group(S, ps[gbk][:], Bps[gbk], [(u3T[:, k, :], wpg[:, k, nsl]) for k in range(8)], reads=[Bu3T, Bwpg])
                    mm_group(S, ps[pbk][:], Bps[pbk], [(pT[:, k, :], wpp[:, k, nsl]) for k in range(2)], reads=[BpT, Bwpp])
                    S.op("act", lambda e, gbk=gbk, nsl=nsl: e.activation(out=sgm[:, nsl], in_=ps[gbk][:], func=AF.Sigmoid), reads=[Bps[gbk]], writes=[Bsgm])
                    S.op("dve", lambda e, pbk=pbk, nsl=nsl: e.tensor_tensor(out=sgm[:, nsl], in0=sgm[:, nsl], in1=ps[pbk][:], op=ALU.mult),
                         reads=[Bsgm, Bps[pbk]], writes=[Bsgm])
                    S.op("dve", lambda e, nsl=nsl: e.tensor_tensor(out=o_t[:, nsl], in0=sgm[:, nsl], in1=h_t[:, nsl], op=ALU.add),
                         reads=[Bsgm, Bh], writes=[Bo])
                    yield
                S.dma("sp", lambda e, r0=r0: e.dma_start(out=K.y[r0:r0 + 128, :], in_=o_t), reads=[Bo], owner=Bo)
        return gen()

    interleave([tile_stream(q) for q in range(4)], skew=2)


def _rope_tables():
    pos = np.arange(SEQ)
    row = (pos // 64).astype(np.float32)
    col = (pos % 64).astype(np.float32)
    inv = (10000.0 ** (-np.arange(0, 64, 2, dtype=np.float32) / 64.0)).astype(np.float32)
    C = np.zeros((128, SEQ), np.float32)
    Sg = np.zeros((128, SEQ), np.float32)
    for p in range(128):
        ids = row if p < 64 else col
        j = p % 32
        ang = (ids * inv[j]).astype(np.float32)
        C[p] = np.cos(ang)
        sgn = -1.0 if (p % 64) < 32 else 1.0
        Sg[p] = sgn * np.sin(ang)
    perm = np.zeros((128, 128), np.float32)
    for m in range(128):
        partner = m + 32 if (m % 64) < 32 else m - 32
        perm[partner, m] = 1.0
    return C, Sg, perm


_NC_CACHE = {}


def _prep_common(inp):
    f = lambda a: np.ascontiguousarray(np.asarray(a, dtype=np.float32))
    pv = np.zeros((128, NPV), np.float32)
    cw = f(inp["conv_w"])[0]
    pv[:, PV_CONVW:PV_CONVW + 32] = cw.reshape(4, 8, 128).transpose(2, 1, 0).reshape(128, 32)
    pv[:, PV_CONVB:PV_CONVB + 8] = f(inp["conv_b"])[0].reshape(8, 128).T
    pv[:, PV_BA:PV_BA + 16] = f(inp["lru_ba"])[0].reshape(16, 128).T
    pv[:, PV_BI:PV_BI + 16] = f(inp["lru_bi"])[0].reshape(16, 128).T
    pv[:, PV_LAM:PV_LAM + 16] = f(inp["lru_lam"])[0].reshape(16, 128).T
    pv[:, PV_QN] = f(inp["q_norm"])[0]
    pv[:, PV_KN] = f(inp["k_norm"])[0]
    pv[:, PV_BGU:PV_BGU + 512] = f(inp["b_gu"])[0].reshape(E * 16, 128).T
    C, Sg, perm = _rope_tables()
    tri = np.triu(np.ones((128, 128), np.float32), 1)
    eC = np.tile((np.arange(E, dtype=np.float32) * CAP)[None, :], (128, 1))
    com = {
        "w_in": f(inp["w_in"])[0], "lru_wa": f(inp["lru_wa"])[0], "lru_wi": f(inp["lru_wi"])[0],
        "w_attn_br": f(inp["w_attn_br"])[0], "w_lru_br": f(inp["w_lru_br"])[0], "w_out": f(inp["w_out"])[0],
        "w_router": f(inp["w_router"])[0], "w_gu": f(inp["w_gu"])[0], "w_dn": f(inp["w_dn"])[0],
        "b_dn": f(inp["b_dn"])[0], "w_ple_gate": f(inp["w_ple_gate"])[0], "w_ple_proj": f(inp["w_ple_proj"])[0],
        "g_mix": f(inp["g_mix"]), "g_moe": f(inp["g_moe"]), "g_ple": f(inp["g_ple"]), "b_router": f(inp["b_router"]),
        "pv": pv, "ropeC": C, "ropeS": Sg, "perm": perm, "ident": np.eye(128, dtype=np.float32), "tri": tri, "eC": eC,
    }
    return com


def kernel(**inputs):
    dbg = bool(int(os.environ.get("MK_DBG", "0")))
    ncores = int(os.environ.get("MK_NCORES", str(NCORES)))
    key = dbg
    if key not in _NC_CACHE:
        _NC_CACHE[key] = build_program(dbg)
    nc = _NC_CACHE[key]
    com = _prep_common(inputs)
    x = np.asarray(inputs["x"], dtype=np.float32)
    p = np.asarray(inputs["p"], dtype=np.float32)[0]
    in_maps = []
    for c in range(ncores):
        m = dict(com)
        m["x"] = np.ascontiguousarray(x[2 * c:2 * c + 2].reshape(T, D))
        m["p"] = np.ascontiguousarray(p[2 * c:2 * c + 2].reshape(T, PLE))
        in_maps.append(m)
    res = run_bass_kernel_spmd(nc, in_maps, core_ids=list(range(ncores)))
    if dbg:
        kernel.last = res
    out = np.zeros((16, SEQ, D), np.float32)
    for c in range(ncores):
        out[2 * c:2 * c + 2] = np.asarray(res.results[c]["y"], dtype=np.float32).reshape(2, SEQ, D)
    return out
```

```python
import os
import numpy as np
from contextlib import ExitStack
import concourse.bass as bass
import concourse.mybir as mybir
from concourse.bass_utils import run_bass_kernel_spmd

F32 = mybir.dt.float32
BF16 = mybir.dt.bfloat16
I32 = mybir.dt.int32
AF = mybir.ActivationFunctionType
ALU = mybir.AluOpType
AX = mybir.AxisListType

NCORES = 8
D = 1024
SEQ = 2048
NSEQ = 2
T = NSEQ * SEQ
NTILE = T // 128
E = 32
F = 1024
CAP = 640
NSB = CAP // 128
NSLOT = E * CAP
TRASH = NSLOT
PLE = 256
EPS = 1e-6
INW = 5632
NPV = 608
PV_CONVW, PV_CONVB, PV_BA, PV_BI, PV_LAM, PV_QN, PV_KN, PV_BGU = 0, 32, 40, 56, 72, 88, 89, 96

ENGS = ("pe", "act", "dve", "pool", "sp")


class Buf:
    __slots__ = ("name", "last_write", "reads", "sem", "sem_total", "excl")

    def __init__(self, name, excl=False):
        self.name = name
        self.excl = excl
        self.last_write = None
        self.reads = []
        self.sem = None
        self.sem_total = 0


class Sched:
    def __init__(self, nc, stack):
        self.nc = nc
        self.stack = stack
        self.stream = {e: [] for e in ENGS}
        self.sem = {e: stack.enter_context(nc.semaphore("s_" + e)) for e in ENGS}
        self.count = {e: 0 for e in ENGS}
        self.seen = {e: {} for e in ENGS}
        self.dma_bufs = []

    def _wait_tokens(self, e, toks):
        need = {}
        for t in toks:
            if t is None:
                continue
            if t[0] == "e":
                _, src, c = t
                if src == "pe" and e == "pe":
                    continue
                key = ("e", src)
                val = c
                sem = self.sem[src]
            else:
                b = t[1]
                key = ("d", id(b))
                val = b.sem_total
                sem = b.sem
            if self.seen[e].get(key, 0) >= val:
                continue
            if key not in need or need[key][1] < val:
                need[key] = (sem, val)
        for key, (sem, val) in need.items():
            self.seen[e][key] = val
            self.stream[e].append(lambda eng, sem=sem, val=val: eng.wait_ge(sem, val))

    @staticmethod
    def _deps(reads, writes):
        toks = []
        for r in reads:
            toks.append(r.last_write)
            if r.excl:
                toks.extend(r.reads)
        for w in writes:
            toks.append(w.last_write)
            toks.extend(w.reads)
        return toks

    def op(self, e, fn, reads=(), writes=(), signal=True):
        self._wait_tokens(e, self._deps(reads, writes))
        if signal:
            self.count[e] += 1
            tok = ("e", e, self.count[e])
            sem = self.sem[e]
            self.stream[e].append(lambda eng, fn=fn, sem=sem: fn(eng).then_inc(sem, 1))
        else:
            tok = ("e", e, self.count[e] + 1)
            self.stream[e].append(lambda eng, fn=fn: fn(eng))
        for w in writes:
            w.last_write = tok
            w.reads = []
        for r in reads:
            r.reads.append(tok)
        return tok

    def dma(self, e, fn, reads=(), writes=(), owner=None):
        if owner is None:
            owner = writes[0] if writes else reads[0]
        if owner.sem is None:
            owner.sem = self.stack.enter_context(self.nc.semaphore("d%d_%s" % (len(self.dma_bufs), owner.name)))
            self.dma_bufs.append(owner)
        self._wait_tokens(e, self._deps(reads, writes))
        owner.sem_total += 16
        sem = owner.sem
        self.stream[e].append(lambda eng, fn=fn, sem=sem: fn(eng).then_inc(sem, 16))
        tok = ("d", owner)
        for w in writes:
            w.last_write = tok
            w.reads = []
        for r in reads:
            r.reads.append(tok)
        return tok

    def barrier(self):
        toks = [("e", s, self.count[s]) for s in ENGS if self.count[s] > 0]
        toks += [("d", b) for b in self.dma_bufs]
        for e in ENGS:
            self._wait_tokens(e, toks)

    def emit(self):
        nc = self.nc
        self.barrier()
        with nc.Block() as block:
            for e, reg in (("sp", block.sync), ("act", block.scalar), ("pe", block.tensor),
                           ("dve", block.vector), ("pool", block.gpsimd)):
                lst = self.stream[e]
                if not lst:
                    continue

                def body(eng, lst=lst):
                    for f in lst:
                        f(eng)
                reg(body)


def _dsize(dt):
    return 2 if dt == BF16 else 4


class Arena:
    def __init__(self, nc, stack, nbytes):
        self.t = stack.enter_context(nc.sbuf_tensor("arena", [128, nbytes // 4], F32))
        self.off = 0
        self.nbytes = nbytes
        self.peak = 0

    def alloc(self, name, free, dt=F32):
        n = 1
        for f in free:
            n *= f
        sz = (n * _dsize(dt) + 31) // 32 * 32
        assert self.off + sz <= self.nbytes, (name, self.off, sz, self.nbytes)
        a = self.t[:, self.off // 4:(self.off + sz) // 4]
        if dt != F32:
            a = a.bitcast(dt)
        a = a[:, 0:n]
        if len(free) == 2:
            a = a.rearrange("p (a b) -> p a b", a=free[0])
        self.off += sz
        self.peak = max(self.peak, self.off)
        return a, Buf(name)

    def mark(self):
        return self.off

    def release(self, m):
        self.off = m


class Ctx:
    pass


def build_program(dbg=False):
    nc = bass.Bass("TRN2", target_bir_lowering=False)
    K = Ctx()
    K.nc = nc

    def din(name, shape, dt=F32):
        return nc.dram_tensor(name, list(shape), dt, kind="ExternalInput")

    K.x = din("x", [T, D]).ap()
    K.p = din("p", [T, PLE]).ap()
    K.w_in = din("w_in", [D, INW]).ap()
    K.lru_wa = din("lru_wa", [2, 8, 128, 128]).ap()
    K.lru_wi = din("lru_wi", [2, 8, 128, 128]).ap()
    K.w_attn_br = din("w_attn_br", [D, D]).ap()
    K.w_lru_br = din("w_lru_br", [D, D]).ap()
    K.w_out = din("w_out", [D, D]).ap()
    K.w_router = din("w_router", [D, E]).ap()
    K.w_gu = din("w_gu", [E, D, 2 * F]).ap()
    K.w_dn = din("w_dn", [E, F, D]).ap()
    K.b_dn = din("b_dn", [E, D]).ap()
    K.w_ple_gate = din("w_ple_gate", [D, D]).ap()
    K.w_ple_proj = din("w_ple_proj", [PLE, D]).ap()
    K.g_mix = din("g_mix", [1, D])
    K.g_moe = din("g_moe", [1, D])
    K.g_ple = din("g_ple", [1, D])
    K.b_router = din("b_router", [1, E])
    K.pv = din("pv", [128, NPV]).ap()
    K.ropeC = din("ropeC", [128, SEQ]).ap()
    K.ropeS = din("ropeS", [128, SEQ]).ap()
    K.perm = din("perm", [128, 128]).ap()
    K.ident = din("ident", [128, 128]).ap()
    K.tri = din("tri", [128, 128]).ap()
    K.eC = din("eC", [128, E]).ap()
    K.y = nc.dram_tensor("y", [T, D], F32, kind="ExternalOutput").ap()

    kind = "ExternalOutput" if dbg else "Internal"

    def dscr(name, shape, dt):
        if dbg:
            return nc.dram_tensor(name, list(shape), dt, kind="ExternalOutput").ap()
        return nc.dram_tensor(name, list(shape), dt).ap()

    K.qT_s = dscr("qT_s", [NSEQ, 8, 128, SEQ], BF16)
    K.kT_s = dscr("kT_s", [NSEQ, 2, 128, SEQ], BF16)
    K.V_s = dscr("V_s", [NSEQ, 128, 16, 256], BF16)
    K.yl_s = dscr("yl_s", [NSEQ, 8, 128, SEQ], BF16)
    K.sga_s = dscr("sga_s", [NSEQ, 8, 128, SEQ], BF16)
    K.sgr_s = dscr("sgr_s", [NSEQ, 8, 128, SEQ], BF16)
    K.h1_s = dscr("h1_s", [T, D], F32)
    K.xs = dscr("xs_s", [NSLOT + 128, D], BF16)
    K.ys = dscr("ys_s", [NSLOT + 128, D], F32)

    with ExitStack() as st:
        S = Sched(nc, st)
        K.S = S
        A = Arena(nc, st, 204 * 1024)
        K.A = A
        K.ps = []
        K.Bps = []
        for i in range(8):
            K.ps.append(st.enter_context(nc.psum_tensor(f"ps{i}", [128, 512], F32)))
            K.Bps.append(Buf(f"ps{i}", excl=True))
        stop = int(os.environ.get("MK_STOP", "9"))
        phase0_consts(K)
        S.barrier()
        m0 = A.mark()
        if stop >= 1:
            phase1_inproj(K)
            S.barrier()
        A.release(m0)
        if stop >= 2:
            phase2_attn(K)
            S.barrier()
        A.release(m0)
        K.pre5 = {"wpg": A.alloc("wpg", [8, D], BF16), "wpp": A.alloc("wpp", [2, D], BF16), "gple": A.alloc("gple", [D])}
        m5 = A.mark()
        K.pre4 = {"wgu": A.alloc("wgu0", [8, 2 * F], BF16), "wdn": A.alloc("wdn0", [8, D], BF16), "bdn": A.alloc("bdn0", [D])}
        m3 = A.mark()
        if stop >= 3:
            issue_expert_load(K, 0, *K.pre4["wgu"], *K.pre4["wdn"], *K.pre4["bdn"])
            phase3_router(K)
            S.barrier()
        A.release(m3)
        if stop >= 4:
            phase4_experts(K)
            S.barrier()
        A.release(m5)
        if stop >= 5:
            phase5_combine(K)
        S.emit()
    return nc


def bcast_row(dt_tensor, n):
    return bass.AP(dt_tensor, 0, [[0, 128], [1, n]])


def phase0_consts(K):
    S, A = K.S, K.A
    K.ident_f, K.Bident_f = A.alloc("ident_f", [128])
    K.ident_b, K.Bident_b = A.alloc("ident_b", [128], BF16)
    K.ones_b, K.Bones_b = A.alloc("ones_b", [128], BF16)
    K.ones_f, K.Bones_f = A.alloc("ones_f", [128])
    K.pvt, K.Bpv = A.alloc("pvt", [NPV])
    K.kk, K.Bkk = A.alloc("kk", [16])
    K.dest_i, K.Bdest = A.alloc("dest_i", [NTILE * 4], I32)
    K.gate_a, K.Bgate = A.alloc("gate_a", [NTILE * 4])
    S.dma("sp", lambda e: e.dma_start(out=K.ident_f, in_=K.ident), writes=[K.Bident_f])
    S.dma("pool", lambda e: e.dma_start(out=K.ident_b, in_=K.ident), writes=[K.Bident_b])
    S.dma("sp", lambda e: e.dma_start(out=K.pvt, in_=K.pv), writes=[K.Bpv])
    S.op("dve", lambda e: e.memset(K.ones_b, 1.0), writes=[K.Bones_b])
    S.op("dve", lambda e: e.memset(K.ones_f, 1.0), writes=[K.Bones_f])
    lam = K.pvt[:, PV_LAM:PV_LAM + 16]
    S.op("act", lambda e: e.activation(out=K.kk, in_=lam, func=AF.Exp, scale=-1.0), reads=[K.Bpv], writes=[K.Bkk])
    S.op("act", lambda e: e.activation(out=K.kk, in_=K.kk, func=AF.Ln, bias=1.0), reads=[K.Bkk], writes=[K.Bkk])
    S.op("dve", lambda e: e.tensor_scalar(out=K.kk, in0=K.kk, scalar1=-8.0, scalar2=None, op0=ALU.mult),
         reads=[K.Bkk], writes=[K.Bkk])


def mm_group(S, out, Bout, pairs, reads):
    n = len(pairs)
    for i, (l, r) in enumerate(pairs):
        S.op("pe", lambda e, l=l, r=r, i=i: e.matmul(out, l, r, start=(i == 0), stop=(i == n - 1)),
             reads=reads, writes=[Bout], signal=(i == n - 1))


def rms_rstd(K, src, Bsrc, junk, Bjunk, ss, Bss, n):
    S = K.S
    S.op("act", lambda e: e.activation(out=junk, in_=src, func=AF.Square, accum_out=ss),
         reads=[Bsrc], writes=[Bjunk, Bss])
    S.op("act", lambda e: e.activation(out=ss, in_=ss, func=AF.Sqrt, bias=EPS, scale=1.0 / n),
         reads=[Bss], writes=[Bss])
    S.op("dve", lambda e: e.reciprocal(out=ss, in_=ss), reads=[Bss], writes=[Bss])


def interleave(gens, skew=0):
    gens = list(gens)
    start = {id(g): i * skew for i, g in enumerate(gens)}
    rnd = 0
    while gens:
        for g in list(gens):
            if rnd < start[id(g)]:
                continue
            try:
                next(g)
            except StopIteration:
                gens.remove(g)
        rnd += 1


def phase1_inproj(K):
    S, A, nc = K.S, K.A, K.nc
    ps, Bps = K.ps, K.Bps
    uT, BuT = A.alloc("uT", [8, SEQ], BF16)
    wtA = [A.alloc(f"wtA{i}", [8, 512], BF16) for i in range(2)]
    wtB = [A.alloc(f"wtB{i}", [8, 256], BF16) for i in range(2)]
    ropeS, BropeS = A.alloc("ropeS", [SEQ])
    cq, Bcq = A.alloc("cq", [SEQ])
    ck, Bck = A.alloc("ck", [SEQ])
    permf, Bpermf = A.alloc("permf", [128])
    permq, Bpermq = A.alloc("permq", [128], BF16)
    permk, Bpermk = A.alloc("permk", [128], BF16)
    od_b, Bod = A.alloc("od_b", [128], BF16)
    wa_b, Bwa = A.alloc("wa_b", [16, 128], BF16)
    wi_b, Bwi = A.alloc("wi_b", [16, 128], BF16)
    xn, Bxn = A.alloc("xn", [D], BF16)
    junk, Bjunk = xn, Bxn
    xns = [(xn, Bxn), A.alloc("xn2", [D], BF16)]
    ss = [A.alloc(f"ss{i}", [1]) for i in range(2)]
    stgA = [A.alloc(f"stgA{i}", [SEQ], BF16) for i in range(2)]
    stgB = [A.alloc(f"stgB{i}", [SEQ], BF16) for i in range(1)]
    xq = [A.alloc(f"xq{i}", [512], BF16) for i in range(2)]
    sq = [A.alloc(f"sq{i}", [512], BF16) for i in range(2)]
    ta = [A.alloc(f"ta{i}", [512]) for i in range(2)]
    tb_ = [A.alloc(f"tb{i}", [512]) for i in range(2)]
    rst = [A.alloc(f"rst{i}", [512]) for i in range(2)]
    xrp, Bxrp = A.alloc("xrp", [SEQ + 4])
    cc, Bcc = A.alloc("cc", [SEQ])
    ccb, Bccb = A.alloc("ccb", [SEQ], BF16)
    aas = [A.alloc(f"aa{i}", [SEQ]) for i in range(2)]
    bts = [A.alloc(f"bt{i}", [SEQ]) for i in range(2)]
    aa, Baa = aas[0]
    t1, Bt1 = A.alloc("t1", [SEQ])
    gmix, Bgmix = cc[:, 0:D], Bcc
    hf, Bhf = A.alloc("hf", [SEQ])
    hb, Bhb = A.alloc("hb", [SEQ])
    xt = [(hb[:, 0:D], Bhb), (hf[:, 0:D], Bhf)]
    vst, Bvst = aa.bitcast(BF16).rearrange("p (a b) -> p a b", a=16), Baa

    pvt = K.pvt
    S.dma("sp", lambda e: e.dma_start(out=cq, in_=K.ropeC), writes=[Bcq])
    S.dma("sp", lambda e: e.dma_start(out=ck, in_=K.ropeC), writes=[Bck])
    S.dma("sp", lambda e: e.dma_start(out=ropeS, in_=K.ropeS), writes=[BropeS])
    S.dma("sp", lambda e: e.dma_start(out=permf, in_=K.perm), writes=[Bpermf])
    S.dma("pool", lambda e: e.dma_start(out=wa_b, in_=K.lru_wa.rearrange("d c p n -> p (d c) n")), writes=[Bwa])
    S.dma("pool", lambda e: e.dma_start(out=wi_b, in_=K.lru_wi.rearrange("d c p n -> p (d c) n")), writes=[Bwi])
    S.op("dve", lambda e: e.memset(od_b, 1.0 / 128.0), writes=[Bod])
    S.op("dve", lambda e: e.memset(xrp, 0.0), writes=[Bxrp])
    qn = pvt[:, PV_QN:PV_QN + 1]
    kn = pvt[:, PV_KN:PV_KN + 1]
    S.op("dve", lambda e: e.tensor_scalar(out=cq, in0=cq, scalar1=qn, scalar2=None, op0=ALU.mult),
         reads=[Bcq, K.Bpv], writes=[Bcq])
    S.op("dve", lambda e: e.tensor_scalar(out=ck, in0=ck, scalar1=kn, scalar2=None, op0=ALU.mult),
         reads=[Bck, K.Bpv], writes=[Bck])
    S.op("dve", lambda e: e.tensor_scalar(out=permq, in0=permf, scalar1=qn, scalar2=None, op0=ALU.mult),
         reads=[Bpermf, K.Bpv], writes=[Bpermq])
    S.op("dve", lambda e: e.tensor_scalar(out=permk, in0=permf, scalar1=kn, scalar2=None, op0=ALU.mult),
         reads=[Bpermf, K.Bpv], writes=[Bpermk])

    w_in_v = K.w_in.rearrange("(c p) n -> p c n", p=128)
    wcnt = {"A": 0, "B": 0}

    def load_w(which, cols):
        tiles = wtA if which == "A" else wtB
        i = wcnt[which] % 2
        wcnt[which] += 1
        w, Bw = tiles[i]
        off = 0
        for (c0, wd) in cols:
            S.dma("pool", lambda e, w=w, off=off, c0=c0, wd=wd: e.dma_start(
                out=w[:, :, off:off + wd], in_=w_in_v[:, :, c0:c0 + wd]), writes=[Bw])
            off += wd
        return w, Bw

    def inproj(w, Bw, woff, tb, bank):
        mm_group(S, ps[bank][:], Bps[bank],
                 [(w[:, k, woff:woff + 128], uT[:, k, tb * 512:(tb + 1) * 512]) for k in range(8)],
                 reads=[Bw, BuT])

    scnt = {"A": 0, "B": 0}

    def build_uT(s):
        S.dma("sp", lambda e: e.dma_start(out=gmix, in_=bcast_row(K.g_mix, D)), writes=[Bgmix])

        def tiles(par):
            x_t, Bx = xt[par]
            ss_t, Bss = ss[par]
            xn_, Bxn_ = xns[par]
            bank = par
            pv16 = ps[bank][:].bitcast(BF16)
            for i in range(par, 16, 2):
                r0 = s * SEQ + i * 128
                S.dma("sp", lambda e, r0=r0: e.dma_start(out=x_t, in_=K.x[r0:r0 + 128, :]), writes=[Bx])
                rms_rstd(K, x_t, Bx, xn_, Bxn_, ss_t, Bss, D)
                yield
                S.op("dve", lambda e: e.scalar_tensor_tensor(
                    out=xn_, in0=x_t, scalar=ss_t, in1=gmix, op0=ALU.mult, op1=ALU.mult),
                    reads=[Bx, Bss, Bgmix], writes=[Bxn_])
                yield
                for c in range(8):
                    S.op("pe", lambda e, c=c: e.transpose(pv16[:, c * 128:(c + 1) * 128], xn_[:, c * 128:(c + 1) * 128], K.ident_b),
                         reads=[Bxn_, K.Bident_b], writes=[Bps[bank]], signal=(c == 7))
                S.op("act", lambda e, i=i: e.activation(
                    out=uT[:, :, i * 128:(i + 1) * 128], in_=pv16.rearrange("p (c t) -> p c t", c=8), func=AF.Copy),
                    reads=[Bps[bank]], writes=[BuT])
                yield

        interleave([tiles(0), tiles(1)], skew=1)

    def qk_head(s, w, Bw, woff, is_q, hidx):
        cg, Bcg = (cq, Bcq) if is_q else (ck, Bck)
        pm, Bpm = (permq, Bpermq) if is_q else (permk, Bpermk)
        st_t, Bst = stgA[scnt["A"] % 2]
        scnt["A"] += 1
        for tb in range(4):
            p = tb % 2
            sl = slice(tb * 512, (tb + 1) * 512)
            xq_, Bxq = xq[p]
            sq_, Bsq = sq[p]
            ta_, Bta = ta[p]
            tb2, Btb = tb_[p]
            rs_, Brst = rst[p]
            zb = p
            inproj(w, Bw, woff, tb, zb)
            S.op("act", lambda e, xq_=xq_, zb=zb: e.activation(out=xq_, in_=ps[zb][:], func=AF.Copy), reads=[Bps[zb]], writes=[Bxq])
            S.op("act", lambda e, sq_=sq_, zb=zb: e.activation(out=sq_, in_=ps[zb][:], func=AF.Square), reads=[Bps[zb]], writes=[Bsq])
            S.op("dve", lambda e, sl=sl, cg=cg, ta_=ta_, zb=zb: e.tensor_tensor(out=ta_, in0=ps[zb][:], in1=cg[:, sl], op=ALU.mult),
                 reads=[Bps[zb], Bcg], writes=[Bta])
            yield
            mm_group(S, ps[2][:], Bps[2], [(od_b, sq_)], reads=[Bod, Bsq])
            mm_group(S, ps[3][:], Bps[3], [(pm, xq_)], reads=[Bpm, Bxq])
            S.op("act", lambda e, rs_=rs_: e.activation(out=rs_, in_=ps[2][:], func=AF.Sqrt, bias=EPS), reads=[Bps[2]], writes=[Brst])
            S.op("dve", lambda e, sl=sl, tb2=tb2: e.tensor_tensor(out=tb2, in0=ps[3][:], in1=ropeS[:, sl], op=ALU.mult),
                 reads=[Bps[3], BropeS], writes=[Btb])
            yield
            S.op("dve", lambda e, rs_=rs_: e.reciprocal(out=rs_, in_=rs_), reads=[Brst], writes=[Brst])
            S.op("dve", lambda e, ta_=ta_, tb2=tb2: e.tensor_tensor(out=ta_, in0=ta_, in1=tb2, op=ALU.add), reads=[Bta, Btb], writes=[Bta])
            S.op("dve", lambda e, sl=sl, st_t=st_t, ta_=ta_, rs_=rs_: e.tensor_tensor(out=st_t[:, sl], in0=ta_, in1=rs_, op=ALU.mult),
                 reads=[Bta, Brst], writes=[Bst])
            yield
        dst = (K.qT_s if is_q else K.kT_s)[s, hidx]
        S.dma("sp", lambda e, st_t=st_t, dst=dst: e.dma_start(out=dst, in_=st_t), reads=[Bst], owner=Bst)

    def stream_A(s):
        for t2 in range(2):
            w, Bw = load_w("A", [(t2 * 512, 512)])
            for j in range(4):
                yield from qk_head(s, w, Bw, j * 128, True, t2 * 4 + j)
        w, Bw = load_w("A", [(1024, 512)])
        for j in range(2):
            yield from qk_head(s, w, Bw, j * 128, False, j)
        for gi, dst_s in ((0, K.sga_s), (1, K.sgr_s)):
            for t2 in range(2):
                wg, Bwg = load_w("A", [(3584 + gi * 1024 + t2 * 512, 512)])
                for j in range(4):
                    st_t, Bst = stgA[scnt["A"] % 2]
                    scnt["A"] += 1
                    for tb in range(4):
                        bank = tb % 2
                        inproj(wg, Bwg, j * 128, tb, bank)
                        S.op("act", lambda e, bank=bank, tb=tb, st_t=st_t: e.activation(out=st_t[:, tb * 512:(tb + 1) * 512], in_=ps[bank][:], func=AF.Sigmoid),
                             reads=[Bps[bank]], writes=[Bst])
                        yield
                    S.dma("sp", lambda e, st_t=st_t, dst=dst_s[s, t2 * 4 + j]: e.dma_start(out=dst, in_=st_t), reads=[Bst], owner=Bst)

    def do_V(s):
        w, Bw = load_w("A", [(1024, 512)])
        for tk in range(16):
            bank = 2 + tk % 2
            mm_group(S, ps[bank][:, 0:256], Bps[bank],
                     [(uT[:, k, tk * 128:(tk + 1) * 128], w[:, k, 256:512]) for k in range(8)], reads=[Bw, BuT])
            S.op("act", lambda e, bank=bank, tk=tk: e.activation(out=vst[:, tk, :], in_=ps[bank][:, 0:256], func=AF.Copy),
                 reads=[Bps[bank]], writes=[Bvst])
        S.dma("sp", lambda e, s=s: e.dma_start(out=K.V_s[s], in_=vst), reads=[Bvst], owner=Bvst)

    def stream_B(s):
        for j in range(8):
            w, Bw = load_w("B", [(1536 + j * 128, 128), (2560 + j * 128, 128)])
            for tb in range(4):
                bank = 4 + tb % 2
                inproj(w, Bw, 0, tb, bank)
                S.op("act", lambda e, bank=bank, tb=tb: e.activation(out=xrp[:, 2 + tb * 512:2 + (tb + 1) * 512], in_=ps[bank][:], func=AF.Copy),
                     reads=[Bps[bank]], writes=[Bxrp])
                yield
            cw = lambda jj, j=j: K.pvt[:, PV_CONVW + j * 4 + jj:PV_CONVW + j * 4 + jj + 1]
            cb = K.pvt[:, PV_CONVB + j:PV_CONVB + j + 1]
            S.op("act", lambda e, cw=cw, cb=cb: e.activation(out=cc, in_=xrp[:, 0:SEQ], func=AF.Identity, bias=cb, scale=cw(0)),
                 reads=[Bxrp, K.Bpv], writes=[Bcc])
            for jj in range(1, 4):
                S.op("dve", lambda e, cw=cw, jj=jj: e.scalar_tensor_tensor(out=cc, in0=xrp[:, jj:jj + SEQ], scalar=cw(jj), in1=cc, op0=ALU.mult, op1=ALU.add),
                     reads=[Bxrp, Bcc, K.Bpv], writes=[Bcc])
                yield
            S.op("act", lambda e: e.activation(out=ccb, in_=cc, func=AF.Copy), reads=[Bcc], writes=[Bccb])

            def dir_gen(d, j=j):
                aa_, Baa_ = aas[d]
                bt_, Bbt_ = bts[d]
                hh, Bhh = (hf, Bhf) if d == 0 else (hb, Bhb)
                ba = K.pvt[:, PV_BA + d * 8 + j:PV_BA + d * 8 + j + 1]
                bi = K.pvt[:, PV_BI + d * 8 + j:PV_BI + d * 8 + j + 1]
                kkc = K.kk[:, d * 8 + j:d * 8 + j + 1]
                for tb in range(4):
                    sl = slice(tb * 512, (tb + 1) * 512)
                    mm_group(S, ps[6][:], Bps[6], [(wa_b[:, d * 8 + j, :], ccb[:, sl])], reads=[Bwa, Bccb])
                    S.op("act", lambda e, sl=sl: e.activation(out=aa_[:, sl], in_=ps[6][:], func=AF.Sigmoid, bias=ba),
                         reads=[Bps[6], K.Bpv], writes=[Baa_])
                    mm_group(S, ps[7][:], Bps[7], [(wi_b[:, d * 8 + j, :], ccb[:, sl])], reads=[Bwi, Bccb])
                    S.op("act", lambda e, sl=sl: e.activation(out=bt_[:, sl], in_=ps[7][:], func=AF.Sigmoid, bias=bi),
                         reads=[Bps[7], K.Bpv], writes=[Bbt_])
                    yield
                S.op("act", lambda e: e.activation(out=aa_, in_=aa_, func=AF.Exp, scale=kkc), reads=[Baa_, K.Bkk], writes=[Baa_])
                S.op("dve", lambda e: e.tensor_tensor(out=bt_, in0=bt_, in1=cc, op=ALU.mult), reads=[Bbt_, Bcc], writes=[Bbt_])
                yield
                S.op("act", lambda e: e.activation(out=hh, in_=aa_, func=AF.Square), reads=[Baa_], writes=[Bhh])
                S.op("act", lambda e: e.activation(out=hh, in_=hh, func=AF.Sqrt, bias=1.0, scale=-1.0), reads=[Bhh], writes=[Bhh])
                yield
                S.op("dve", lambda e: e.tensor_tensor(out=bt_, in0=bt_, in1=hh, op=ALU.mult), reads=[Bbt_, Bhh], writes=[Bbt_])
                yield
                if d == 0:
                    S.op("dve", lambda e: e.tensor_tensor_scan(out=hh, data0=aa_, data1=bt_, initial=0.0, op0=ALU.mult, op1=ALU.add),
                         reads=[Baa_, Bbt_], writes=[Bhh])
                else:
                    S.op("dve", lambda e: e.tensor_tensor_scan(out=hh[:, ::-1], data0=aa_[:, ::-1], data1=bt_[:, ::-1], initial=0.0, op0=ALU.mult, op1=ALU.add),
                         reads=[Baa_, Bbt_], writes=[Bhh])
                yield

            def gelu_gen(w=w, Bw=Bw):
                for tb in range(4):
                    sl = slice(tb * 512, (tb + 1) * 512)
                    bank = 4 + tb % 2
                    inproj(w, Bw, 128, tb, bank)
                    S.op("act", lambda e, bank=bank, sl=sl: e.activation(out=t1[:, sl], in_=ps[bank][:], func=AF.Square), reads=[Bps[bank]], writes=[Bt1])
                    S.op("act", lambda e, sl=sl: e.activation(out=t1[:, sl], in_=t1[:, sl], func=AF.Identity, bias=1.0, scale=0.044715),
                         reads=[Bt1], writes=[Bt1])
                    S.op("dve", lambda e, bank=bank, sl=sl: e.tensor_tensor(out=t1[:, sl], in0=t1[:, sl], in1=ps[bank][:], op=ALU.mult),
                         reads=[Bt1, Bps[bank]], writes=[Bt1])
                    S.op("act", lambda e, sl=sl: e.activation(out=t1[:, sl], in_=t1[:, sl], func=AF.Sigmoid, scale=1.5957691216), reads=[Bt1], writes=[Bt1])
                    S.op("dve", lambda e, bank=bank, sl=sl: e.tensor_tensor(out=t1[:, sl], in0=t1[:, sl], in1=ps[bank][:], op=ALU.mult),
                         reads=[Bt1, Bps[bank]], writes=[Bt1])
                    yield

            subs = [dir_gen(0), dir_gen(1), gelu_gen()]
            while subs:
                for g in list(subs):
                    try:
                        next(g)
                    except StopIteration:
                        subs.remove(g)
                yield
            S.op("dve", lambda e: e.tensor_tensor(out=hf, in0=hf, in1=hb, op=ALU.add), reads=[Bhf, Bhb], writes=[Bhf])
            st_t, Bst = stgB[0]
            S.op("dve", lambda e, st_t=st_t: e.tensor_tensor(out=st_t, in0=hf, in1=t1, op=ALU.mult), reads=[Bhf, Bt1], writes=[Bst])
            S.dma("sp", lambda e, st_t=st_t, j=j, s=s: e.dma_start(out=K.yl_s[s, j], in_=st_t), reads=[Bst], owner=Bst)
            yield

    for s in range(NSEQ):
        build_uT(s)
        interleave([stream_A(s), stream_B(s)])
        do_V(s)


def phase2_attn(K):
    S, A, nc = K.S, K.A, K.nc
    ps, Bps = K.ps, K.Bps
    wab, Bwab = A.alloc("wab", [8, D], BF16)
    wlb, Bwlb = A.alloc("wlb", [8, D], BF16)
    wo, Bwo = A.alloc("wo", [8, D], BF16)
    kT, BkT = A.alloc("kT", [2, SEQ], BF16)
    V, BV = A.alloc("V", [16, 256], BF16)
    qT = [A.alloc(f"qT{i}", [8, 512], BF16) for i in range(2)]
    ylb = [A.alloc(f"ylb{i}", [8, 512], BF16) for i in range(2)]
    gab = [A.alloc(f"gab{i}", [8, 512], BF16) for i in range(2)]
    grb = [A.alloc(f"grb{i}", [8, 512], BF16) for i in range(2)]
    PT = [A.alloc(f"PT{i}", [512], BF16) for i in range(6)]
    SBANK = (0, 1, 2, 7)
    attnT, BattnT = A.alloc("attnT", [8, 512], BF16)
    mrg, Bmrg = A.alloc("mrg", [8, 512], BF16)
    rz, Brz = A.alloc("rz", [512])
    zacc = [A.alloc(f"zacc{i}", [512]) for i in range(2)]
    zab = [A.alloc(f"zab{i}", [512], BF16) for i in range(2)]
    m1, Bm1 = A.alloc("m1", [512])
    m2, Bm2 = A.alloc("m2", [512])
    xt = [A.alloc(f"xt{i}", [D]) for i in range(2)]
    ho = [A.alloc(f"ho{i}", [D]) for i in range(2)]
    S.dma("pool", lambda e: e.dma_start(out=wab, in_=K.w_attn_br.rearrange("(c p) n -> p c n", p=128)), writes=[Bwab])
    S.dma("pool", lambda e: e.dma_start(out=wlb, in_=K.w_lru_br.rearrange("(c p) n -> p c n", p=128)), writes=[Bwlb])
    S.dma("pool", lambda e: e.dma_start(out=wo, in_=K.w_out.rearrange("(c p) n -> p c n", p=128)), writes=[Bwo])
    zt, Bzt = A.alloc("zt", [D], BF16)
    S.op("dve", lambda e: e.memset(zt, 0.0), writes=[Bzt])
    zrows = list(range(0, NSLOT + 128, 128))

    def zero_some(n):
        for _ in range(n):
            if zrows:
                r0 = zrows.pop(0)
                S.dma("sp", lambda e, r0=r0: e.dma_start(out=K.xs[r0:r0 + 128, :], in_=zt), reads=[Bzt], owner=Bzt)
    scale = 128.0 ** -0.5
    cnt = 0
    for s in range(NSEQ):
        S.dma("sp", lambda e, s=s: e.dma_start(out=kT, in_=K.kT_s[s].rearrange("h p t -> p h t")), writes=[BkT])
        S.dma("sp", lambda e, s=s: e.dma_start(out=V, in_=K.V_s[s]), writes=[BV])
        for qb in range(4):
            q, Bq = qT[cnt % 2]
            yl, Byl = ylb[cnt % 2]
            ga, Bga = gab[cnt % 2]
            gr, Bgr = grb[cnt % 2]
            cnt += 1
            tsl = slice(qb * 512, (qb + 1) * 512)
            S.dma("sp", lambda e, q=q, s=s, tsl=tsl: e.dma_start(out=q, in_=K.qT_s[s].rearrange("h p t -> p h t")[:, :, tsl]), writes=[Bq])
            S.dma("sp", lambda e, yl=yl, s=s, tsl=tsl: e.dma_start(out=yl, in_=K.yl_s[s].rearrange("h p t -> p h t")[:, :, tsl]), writes=[Byl])
            S.dma("sp", lambda e, ga=ga, s=s, tsl=tsl: e.dma_start(out=ga, in_=K.sga_s[s].rearrange("h p t -> p h t")[:, :, tsl]), writes=[Bga])
            S.dma("sp", lambda e, gr=gr, s=s, tsl=tsl: e.dma_start(out=gr, in_=K.sgr_s[s].rearrange("h p t -> p h t")[:, :, tsl]), writes=[Bgr])
            for h in range(8):
                kv = h // 4
                ob, zb = 3 + h % 2, 5 + h % 2

                def score(kc):
                    bank = SBANK[kc % 4]
                    mm_group(S, ps[bank][:], Bps[bank], [(kT[:, kv, kc * 128:(kc + 1) * 128], q[:, h, :])], reads=[BkT, Bq])
                    pt, Bpt = PT[kc % 6]
                    S.op("act", lambda e, bank=bank, pt=pt: e.activation(out=pt, in_=ps[bank][:], func=AF.Exp, scale=scale),
                         reads=[Bps[bank]], writes=[Bpt])

                za, Bza = zacc[h % 2]
                zb16, Bzb16 = zab[h % 2]

                def pv(kc):
                    pt, Bpt = PT[kc % 6]
                    S.op("pe", lambda e, kc=kc, pt=pt, ob=ob, kv=kv: e.matmul(ps[ob][:], V[:, kc, kv * 128:(kv + 1) * 128], pt, start=(kc == 0), stop=(kc == 15)),
                         reads=[BV, Bpt], writes=[Bps[ob]], signal=(kc == 15))
                    if kc % 2 == 1:
                        S.op("pe", lambda e, kc=kc, pt=pt, zb=zb: e.matmul(ps[zb][:], K.ones_b, pt, start=(kc == 1), stop=False),
                             reads=[K.Bones_b, Bpt], writes=[Bps[zb]], signal=False)
                    elif kc == 0:
                        S.op("dve", lambda e, pt=pt, za=za: e.tensor_copy(out=za, in_=pt), reads=[Bpt], writes=[Bza])
                    else:
                        S.op("dve", lambda e, pt=pt, za=za: e.tensor_tensor(out=za, in0=za, in1=pt, op=ALU.add), reads=[Bpt, Bza], writes=[Bza])
                    if kc == 15:
                        S.op("pe", lambda e, za=za, zb=zb: e.matmul(ps[zb][:], K.ones_f, za, start=False, stop=True),
                             reads=[K.Bones_f, Bza], writes=[Bps[zb]], signal=True)

                score(0)
                score(1)
                score(2)
                for kc in range(16):
                    if kc + 3 < 16:
                        score(kc + 3)
                    pv(kc)
                S.op("dve", lambda e, zb=zb: e.reciprocal(out=rz, in_=ps[zb][:]), reads=[Bps[zb]], writes=[Brz])
                S.op("dve", lambda e, ob=ob, h=h: e.tensor_tensor(out=attnT[:, h, :], in0=ps[ob][:], in1=rz, op=ALU.mult),
                     reads=[Bps[ob], Brz], writes=[BattnT])
                zero_some(6)
            for m in range(8):
                b1, b2 = 0 + m % 2, 2 + m % 2
                mm_group(S, ps[b1][:], Bps[b1], [(wab[:, k, m * 128:(m + 1) * 128], attnT[:, k, :]) for k in range(8)], reads=[Bwab, BattnT])
                mm_group(S, ps[b2][:], Bps[b2], [(wlb[:, k, m * 128:(m + 1) * 128], yl[:, k, :]) for k in range(8)], reads=[Bwlb, Byl])
                S.op("dve", lambda e, b1=b1, m=m, ga=ga: e.tensor_tensor(out=m1, in0=ps[b1][:], in1=ga[:, m, :], op=ALU.mult),
                     reads=[Bps[b1], Bga], writes=[Bm1])
                S.op("dve", lambda e, b2=b2, m=m, gr=gr: e.tensor_tensor(out=m2, in0=ps[b2][:], in1=gr[:, m, :], op=ALU.mult),
                     reads=[Bps[b2], Bgr], writes=[Bm2])
                S.op("dve", lambda e, m=m: e.tensor_tensor(out=mrg[:, m, :], in0=m1, in1=m2, op=ALU.add),
                     reads=[Bm1, Bm2], writes=[Bmrg])
            for tk in range(4):
                x_t, Bx = xt[tk % 2]
                h_t, Bh = ho[tk % 2]
                r0 = s * SEQ + qb * 512 + tk * 128
                S.dma("sp", lambda e, x_t=x_t, r0=r0: e.dma_start(out=x_t, in_=K.x[r0:r0 + 128, :]), writes=[Bx])
                for nh in range(2):
                    bank = 5 + nh
                    mm_group(S, ps[bank][:], Bps[bank],
                             [(mrg[:, k, tk * 128:(tk + 1) * 128], wo[:, k, nh * 512:(nh + 1) * 512]) for k in range(8)], reads=[Bmrg, Bwo])
                    S.op("dve", lambda e, bank=bank, nh=nh, x_t=x_t, h_t=h_t: e.tensor_tensor(
                        out=h_t[:, nh * 512:(nh + 1) * 512], in0=ps[bank][:], in1=x_t[:, nh * 512:(nh + 1) * 512], op=ALU.add),
                        reads=[Bps[bank], Bx], writes=[Bh])
                S.dma("sp", lambda e, h_t=h_t, r0=r0: e.dma_start(out=K.h1_s[r0:r0 + 128, :], in_=h_t), reads=[Bh], owner=Bh)
    zero_some(len(zrows))


def phase3_router(K):
    S, A, nc = K.S, K.A, K.nc
    ps, Bps = K.ps, K.Bps
    gmoe, Bgmoe = A.alloc("gmoe", [D])
    wr, Bwr = A.alloc("wr", [8, E])
    brt, Bbr = A.alloc("brt", [E])
    tri, Btri = A.alloc("tri", [128])
    eC, BeC = A.alloc("eC", [E])
    msum, Bmsum = A.alloc("msum", [E])
    S.dma("sp", lambda e: e.dma_start(out=gmoe, in_=bcast_row(K.g_moe, D)), writes=[Bgmoe])
    S.dma("sp", lambda e: e.dma_start(out=wr, in_=K.w_router.rearrange("(c p) n -> p c n", p=128)), writes=[Bwr])
    S.dma("sp", lambda e: e.dma_start(out=brt, in_=bcast_row(K.b_router, E)), writes=[Bbr])
    S.dma("sp", lambda e: e.dma_start(out=tri, in_=K.tri), writes=[Btri])
    S.dma("sp", lambda e: e.dma_start(out=eC, in_=K.eC), writes=[BeC])
    S.op("dve", lambda e: e.memset(msum, 0.0), writes=[Bmsum])

    def tile_stream(par):
        h_t, Bh = A.alloc(f"ht{par}", [D])
        junk, Bjunk = A.alloc(f"junk{par}", [D], BF16)
        ss_t, Bss = A.alloc(f"ss{par}", [1])
        u2, Bu2 = A.alloc(f"u2{par}", [D])
        ub, Bub = A.alloc(f"u2b{par}", [D], BF16)
        u2T, Bu2T = A.alloc(f"u2T{par}", [8, 128])
        lg, Blg = A.alloc(f"lg{par}", [E])
        top8, Btop8 = A.alloc(f"top8{par}", [8])
        nm, Bnm = A.alloc(f"nm{par}", [1])
        mask, Bmask = A.alloc(f"mask{par}", [E])
        ex, Bex = A.alloc(f"ex{par}", [E])
        den, Bden = A.alloc(f"den{par}", [1])
        gf, Bgf = A.alloc(f"gf{par}", [E])
        rank, Brank = A.alloc(f"rank{par}", [E])
        okm, Bok = A.alloc(f"okm{par}", [E])
        dst, Bdst = A.alloc(f"dst{par}", [E])
        dk, Bdk = A.alloc(f"dk{par}", [4])
        oh, Boh = A.alloc(f"oh{par}", [E])
        b0 = par * 2

        def gen():
            for i in range(par, NTILE, 4):
                r0 = i * 128
                S.dma("sp", lambda e, r0=r0: e.dma_start(out=h_t, in_=K.h1_s[r0:r0 + 128, :]), writes=[Bh])
                rms_rstd(K, h_t, Bh, junk, Bjunk, ss_t, Bss, D)
                S.op("dve", lambda e: e.scalar_tensor_tensor(out=u2, in0=h_t, scalar=ss_t, in1=gmoe, op0=ALU.mult, op1=ALU.mult),
                     reads=[Bh, Bss, Bgmoe], writes=[Bu2])
                yield
                S.op("act", lambda e: e.activation(out=ub, in_=u2, func=AF.Copy), reads=[Bu2], writes=[Bub])
                for hc in range(2):
                    bank = b0
                    for c4 in range(4):
                        c = hc * 4 + c4
                        S.op("pe", lambda e, c=c, c4=c4, bank=bank: e.transpose(ps[bank][:, c4 * 128:(c4 + 1) * 128], u2[:, c * 128:(c + 1) * 128], K.ident_f),
                             reads=[Bu2, K.Bident_f], writes=[Bps[bank]], signal=(c4 == 3))
                    S.op("act", lambda e, bank=bank, hc=hc: e.activation(out=u2T[:, hc * 4:(hc + 1) * 4, :], in_=ps[bank][:].rearrange("p (c t) -> p c t", c=4), func=AF.Copy),
                         reads=[Bps[bank]], writes=[Bu2T])
                yield
                lb, rb = b0 + 1, b0 + 1
                mm_group(S, ps[lb][:, 0:E], Bps[lb], [(u2T[:, k, :], wr[:, k, :]) for k in range(8)], reads=[Bu2T, Bwr])
                S.op("dve", lambda e, lb=lb: e.tensor_tensor(out=lg, in0=ps[lb][:, 0:E], in1=brt, op=ALU.add), reads=[Bps[lb], Bbr], writes=[Blg])
                S.op("dve", lambda e: e.max(out=top8, in_=lg), reads=[Blg], writes=[Btop8])
                S.op("dve", lambda e: e.tensor_scalar(out=mask, in0=lg, scalar1=top8[:, 3:4], scalar2=None, op0=ALU.is_ge),
                     reads=[Blg, Btop8], writes=[Bmask])
                yield
                mm_group(S, ps[rb][:, 64:64 + E], Bps[rb], [(tri, mask), (K.ones_f, msum)], reads=[Btri, Bmask, K.Bones_f, Bmsum])
                S.op("dve", lambda e: e.tensor_tensor(out=msum, in0=msum, in1=mask, op=ALU.add), reads=[Bmsum, Bmask], writes=[Bmsum])
                yield
                S.op("dve", lambda e: e.tensor_scalar(out=nm, in0=top8[:, 0:1], scalar1=-1.0, scalar2=None, op0=ALU.mult),
                     reads=[Btop8], writes=[Bnm])
                S.op("act", lambda e: e.activation(out=ex, in_=lg, func=AF.Exp, bias=nm), reads=[Blg, Bnm], writes=[Bex])
                S.op("dve", lambda e, rb=rb: e.tensor_copy(out=rank, in_=ps[rb][:, 64:64 + E]), reads=[Bps[rb]], writes=[Brank])
                S.op("dve", lambda e: e.tensor_scalar(out=okm, in0=rank, scalar1=float(CAP), scalar2=None, op0=ALU.is_lt),
                     reads=[Brank], writes=[Bok])
                S.op("dve", lambda e: e.tensor_tensor(out=dst, in0=rank, in1=eC, op=ALU.add), reads=[Brank, BeC], writes=[Bdst])
                S.op("dve", lambda e: e.scalar_tensor_tensor(out=dst, in0=dst, scalar=float(-TRASH), in1=okm, op0=ALU.add, op1=ALU.mult),
                     reads=[Bdst, Bok], writes=[Bdst])
                S.op("dve", lambda e: e.tensor_scalar(out=dst, in0=dst, scalar1=float(TRASH), scalar2=None, op0=ALU.add),
                     reads=[Bdst], writes=[Bdst])
                yield
                S.op("dve", lambda e: e.tensor_tensor(out=ex, in0=ex, in1=mask, op=ALU.mult), reads=[Bex, Bmask], writes=[Bex])
                S.op("dve", lambda e: e.reduce_sum(out=den, in_=ex, axis=AX.X), reads=[Bex], writes=[Bden])
                S.op("dve", lambda e: e.reciprocal(out=den, in_=den), reads=[Bden], writes=[Bden])
                S.op("dve", lambda e: e.scalar_tensor_tensor(out=gf, in0=ex, scalar=den, in1=okm, op0=ALU.mult, op1=ALU.mult),
                     reads=[Bex, Bden, Bok], writes=[Bgf])
                yield
                for k in range(4):
                    S.op("dve", lambda e, k=k: e.scalar_tensor_tensor(out=oh, in0=lg, scalar=top8[:, k:k + 1], in1=dst, op0=ALU.is_equal, op1=ALU.mult,
                                                                      accum_out=dk[:, k:k + 1]),
                         reads=[Blg, Btop8, Bdst], writes=[Boh, Bdk])
                    S.op("dve", lambda e, k=k, i=i: e.scalar_tensor_tensor(out=oh, in0=lg, scalar=top8[:, k:k + 1], in1=gf, op0=ALU.is_equal, op1=ALU.mult,
                                                                           accum_out=K.gate_a[:, i * 4 + k:i * 4 + k + 1]),
                         reads=[Blg, Btop8, Bgf], writes=[Boh, K.Bgate])
                S.op("dve", lambda e, i=i: e.tensor_copy(out=K.dest_i[:, i * 4:(i + 1) * 4], in_=dk), reads=[Bdk], writes=[K.Bdest])
                for k in range(4):
                    S.dma("pool", lambda e, k=k, i=i: e.indirect_dma_start(
                        out=K.xs, out_offset=bass.IndirectOffsetOnAxis(ap=K.dest_i[:, i * 4 + k:i * 4 + k + 1], axis=0),
                        in_=ub, in_offset=None), reads=[Bub, K.Bdest], owner=Bub)
                yield
        return gen()

    interleave([tile_stream(q) for q in range(4)], skew=2)


def issue_expert_load(K, e_, w, Bw, wd, Bwd, bd, Bbd):
    S = K.S
    src = K.w_gu[e_].rearrange("(c p) n -> p c n", p=128)
    for hh in range(2):
        S.dma("pool", lambda e, w=w, src=src, hh=hh: e.dma_start(out=w[:, hh * 4:(hh + 1) * 4, :], in_=src[:, hh * 4:(hh + 1) * 4, :]), writes=[Bw])
    S.dma("pool", lambda e, wd=wd, e_=e_: e.dma_start(out=wd, in_=K.w_dn[e_].rearrange("(c p) n -> p c n", p=128)), writes=[Bwd])
    S.dma("sp", lambda e, bd=bd, e_=e_: e.dma_start(out=bd, in_=bass.AP(K.b_dn.tensor, e_ * D, [[0, 128], [1, D]])), writes=[Bbd])


def phase4_experts(K):
    S, A, nc = K.S, K.A, K.nc
    ps, Bps = K.ps, K.Bps
    wgu = [K.pre4["wgu"], A.alloc("wgu1", [8, 2 * F], BF16)]
    wdn = [K.pre4["wdn"], A.alloc("wdn1", [8, D], BF16)]
    bdn = [K.pre4["bdn"], A.alloc("bdn1", [D])]
    xTs = [A.alloc(f"xT{i}", [8, CAP], BF16) for i in range(2)]
    resTs = [A.alloc(f"resT{i}", [8, CAP], BF16) for i in range(2)]
    xst = [A.alloc(f"xst{i}", [D], BF16) for i in range(3)]
    yst = [A.alloc(f"yst{i}", [D]) for i in range(2)]
    HW = CAP // 2
    gt = [A.alloc(f"gt{i}", [HW]) for i in range(2)]
    sg = [A.alloc(f"sg{i}", [HW]) for i in range(2)]
    ut = [A.alloc(f"ut{i}", [HW]) for i in range(2)]
    cnts = {"y": 0, "x": 0, "u": 0}
    S.op("dve", lambda e: e.memset(yst[1][0], 0.0), writes=[yst[1][1]])
    S.dma("sp", lambda e: e.dma_start(out=K.ys[NSLOT:NSLOT + 128, :], in_=yst[1][0]), reads=[yst[1][1]], owner=yst[1][1])

    def load_expert(e_):
        issue_expert_load(K, e_, *wgu[e_ % 2], *wdn[e_ % 2], *bdn[e_ % 2])

    def build_xT(e_):
        xT, BxT = xTs[e_ % 2]
        for sb in range(NSB):
            xs_t, Bxs = xst[cnts["x"] % 3]
            cnts["x"] += 1
            r0 = e_ * CAP + sb * 128
            S.dma("sp", lambda e, xs_t=xs_t, r0=r0: e.dma_start(out=xs_t, in_=K.xs[r0:r0 + 128, :]), writes=[Bxs])
            bank = sb % 2
            pv16 = ps[bank][:].bitcast(BF16)
            for c in range(8):
                S.op("pe", lambda e, c=c, pv16=pv16, xs_t=xs_t: e.transpose(pv16[:, c * 128:(c + 1) * 128], xs_t[:, c * 128:(c + 1) * 128], K.ident_b),
                     reads=[Bxs, K.Bident_b], writes=[Bps[bank]], signal=(c == 7))
            S.op("act", lambda e, pv16=pv16, sb=sb, xT=xT: e.activation(out=xT[:, :, sb * 128:(sb + 1) * 128], in_=pv16.rearrange("p (c t) -> p c t", c=8), func=AF.Copy),
                 reads=[Bps[bank]], writes=[BxT])

    def gate_up(e_):
        w, Bw = wgu[e_ % 2]
        xT, BxT = xTs[e_ % 2]
        resT, BresT = resTs[e_ % 2]
        for f in range(8):
            bgc = K.pvt[:, PV_BGU + e_ * 16 + f:PV_BGU + e_ * 16 + f + 1]
            buc = K.pvt[:, PV_BGU + e_ * 16 + 8 + f:PV_BGU + e_ * 16 + 8 + f + 1]
            for hv in range(2):
                nsl = slice(hv * HW, (hv + 1) * HW)
                cnt = cnts["u"]
                cnts["u"] += 1
                gb, ub_ = 2 + cnt % 2, 4 + cnt % 2
                g_t, Bg = gt[cnt % 2]
                s_t, Bs = sg[cnt % 2]
                u_t, Bu = ut[cnt % 2]
                mm_group(S, ps[gb][:, 0:HW], Bps[gb], [(w[:, k, f * 128:(f + 1) * 128], xT[:, k, nsl]) for k in range(8)], reads=[Bw, BxT])
                mm_group(S, ps[ub_][:, 0:HW], Bps[ub_], [(w[:, k, F + f * 128:F + (f + 1) * 128], xT[:, k, nsl]) for k in range(8)], reads=[Bw, BxT])
                S.op("dve", lambda e, gb=gb, g_t=g_t, bgc=bgc: e.tensor_scalar(out=g_t, in0=ps[gb][:, 0:HW], scalar1=bgc, scalar2=7.0, op0=ALU.add, op1=ALU.min),
                     reads=[Bps[gb], K.Bpv], writes=[Bg])
                S.op("act", lambda e, ub_=ub_, u_t=u_t, buc=buc: e.activation(out=u_t, in_=ps[ub_][:, 0:HW], func=AF.Identity, bias=buc),
                     reads=[Bps[ub_], K.Bpv], writes=[Bu])
                S.op("act", lambda e, g_t=g_t, s_t=s_t: e.activation(out=s_t, in_=g_t, func=AF.Sigmoid, scale=1.702), reads=[Bg], writes=[Bs])
                S.op("dve", lambda e, u_t=u_t: e.tensor_scalar(out=u_t, in0=u_t, scalar1=7.0, scalar2=-7.0, op0=ALU.min, op1=ALU.max),
                     reads=[Bu], writes=[Bu])
                S.op("dve", lambda e, g_t=g_t, s_t=s_t: e.tensor_tensor(out=g_t, in0=g_t, in1=s_t, op=ALU.mult), reads=[Bg, Bs], writes=[Bg])
                S.op("dve", lambda e, g_t=g_t, u_t=u_t, f=f, nsl=nsl, resT=resT: e.scalar_tensor_tensor(
                    out=resT[:, f, nsl], in0=u_t, scalar=1.0, in1=g_t, op0=ALU.add, op1=ALU.mult),
                    reads=[Bg, Bu], writes=[BresT])

    def down(e_):
        wd, Bwd = wdn[e_ % 2]
        bd, Bbd = bdn[e_ % 2]
        resT, BresT = resTs[e_ % 2]
        for sb in range(NSB):
            y_t, By = yst[cnts["y"] % 2]
            cnts["y"] += 1
            for nh in range(2):
                bank = 6 + nh
                pairs = [(resT[:, k, sb * 128:(sb + 1) * 128], wd[:, k, nh * 512:(nh + 1) * 512]) for k in range(8)]
                mm_group(S, ps[bank][:], Bps[bank], pairs, reads=[BresT, Bwd])
                S.op("dve", lambda e, bank=bank, y_t=y_t, nh=nh, bd=bd: e.tensor_tensor(
                    out=y_t[:, nh * 512:(nh + 1) * 512], in0=ps[bank][:], in1=bd[:, nh * 512:(nh + 1) * 512], op=ALU.add),
                    reads=[Bps[bank], Bbd], writes=[By])
            r0 = e_ * CAP + sb * 128
            S.dma("sp", lambda e, y_t=y_t, r0=r0: e.dma_start(out=K.ys[r0:r0 + 128, :], in_=y_t), reads=[By], owner=By)

    build_xT(0)
    for e_ in range(E):
        if e_ + 1 < E:
            load_expert(e_ + 1)
        if e_ == 0:
            wpg, Bwpg = K.pre5["wpg"]
            wpp, Bwpp = K.pre5["wpp"]
            gple, Bgple = K.pre5["gple"]
            S.dma("pool", lambda e: e.dma_start(out=wpg, in_=K.w_ple_gate.rearrange("(c p) n -> p c n", p=128)), writes=[Bwpg])
            S.dma("pool", lambda e: e.dma_start(out=wpp, in_=K.w_ple_proj.rearrange("(c p) n -> p c n", p=128)), writes=[Bwpp])
            S.dma("sp", lambda e: e.dma_start(out=gple, in_=bcast_row(K.g_ple, D)), writes=[Bgple])
        gate_up(e_)
        if e_ + 1 < E:
            build_xT(e_ + 1)
        down(e_)


def phase5_combine(K):
    S, A, nc = K.S, K.A, K.nc
    ps, Bps = K.ps, K.Bps
    wpg, Bwpg = K.pre5["wpg"]
    wpp, Bwpp = K.pre5["wpp"]
    gple, Bgple = K.pre5["gple"]

    def tile_stream(par):
        h_t, Bh = A.alloc(f"ht{par}", [D])
        yg = [A.alloc(f"yg{par}_{k}", [D]) for k in range(4)]
        p_t, Bp = A.alloc(f"pt{par}", [PLE])
        ptb, Bptb = A.alloc(f"ptb{par}", [PLE], BF16)
        pT, BpT = A.alloc(f"pT{par}", [2, 128], BF16)
        junk, Bjunk = A.alloc(f"junk{par}", [D], BF16)
        ss_t, Bss = A.alloc(f"ss{par}", [1])
        u3, Bu3 = A.alloc(f"u3{par}", [D], BF16)
        u3T, Bu3T = A.alloc(f"u3T{par}", [8, 128], BF16)
        sgm, Bsgm = A.alloc(f"sgm{par}", [D])
        o_t, Bo = A.alloc(f"ot{par}", [D])
        b0 = par * 2

        def gen():
            for i in range(par, NTILE, 4):
                r0 = i * 128
                S.dma("sp", lambda e, r0=r0: e.dma_start(out=h_t, in_=K.h1_s[r0:r0 + 128, :]), writes=[Bh])
                S.dma("sp", lambda e, r0=r0: e.dma_start(out=p_t, in_=K.p[r0:r0 + 128, :]), writes=[Bp])
                for k in range(4):
                    y_t, By = yg[k]
                    S.dma("pool", lambda e, y_t=y_t, i=i, k=k: e.indirect_dma_start(
                        out=y_t, out_offset=None, in_=K.ys,
                        in_offset=bass.IndirectOffsetOnAxis(ap=K.dest_i[:, i * 4 + k:i * 4 + k + 1], axis=0)),
                        reads=[K.Bdest], writes=[By])
                yield
                S.op("act", lambda e: e.activation(out=ptb, in_=p_t, func=AF.Copy), reads=[Bp], writes=[Bptb])
                pv1 = ps[b0 + 1][:].bitcast(BF16)
                for c in range(2):
                    S.op("pe", lambda e, c=c, pv1=pv1: e.transpose(pv1[:, c * 128:(c + 1) * 128], ptb[:, c * 128:(c + 1) * 128], K.ident_b),
                         reads=[Bptb, K.Bident_b], writes=[Bps[b0 + 1]], signal=(c == 1))
                S.op("act", lambda e, pv1=pv1: e.activation(out=pT, in_=pv1[:, 0:256].rearrange("p (c t) -> p c t", c=2), func=AF.Copy),
                     reads=[Bps[b0 + 1]], writes=[BpT])
                yield
                for k in range(4):
                    y_t, By = yg[k]
                    S.op("dve", lambda e, y_t=y_t, i=i, k=k: e.scalar_tensor_tensor(
                        out=h_t, in0=y_t, scalar=K.gate_a[:, i * 4 + k:i * 4 + k + 1], in1=h_t, op0=ALU.mult, op1=ALU.add),
                        reads=[By, Bh, K.Bgate], writes=[Bh])
                yield
                rms_rstd(K, h_t, Bh, junk, Bjunk, ss_t, Bss, D)
                S.op("dve", lambda e: e.scalar_tensor_tensor(out=u3, in0=h_t, scalar=ss_t, in1=gple, op0=ALU.mult, op1=ALU.mult),
                     reads=[Bh, Bss, Bgple], writes=[Bu3])
                yield
                pv16 = ps[b0][:].bitcast(BF16)
                for c in range(8):
                    S.op("pe", lambda e, c=c, pv16=pv16: e.transpose(pv16[:, c * 128:(c + 1) * 128], u3[:, c * 128:(c + 1) * 128], K.ident_b),
                         reads=[Bu3, K.Bident_b], writes=[Bps[b0]], signal=(c == 7))
                S.op("act", lambda e, pv16=pv16: e.activation(out=u3T, in_=pv16.rearrange("p (c t) -> p c t", c=8), func=AF.Copy),
                     reads=[Bps[b0]], writes=[Bu3T])
                yield
                for nh in range(2):
                    nsl = slice(nh * 512, (nh + 1) * 512)
                    gbk, pbk = b0, b0 + 1
                    mm_group(S, ps[gbk][:], Bps[gbk], [(u3T[:, k, :], wpg[:, k, nsl]) for k in range(8)], reads=[Bu3T, Bwpg])
                    mm_group(S, ps[pbk][:], Bps[pbk], [(pT[:, k, :], wpp[:, k, nsl]) for k in range(2)], reads=[BpT, Bwpp])
                    S.op("act", lambda e, gbk=gbk, nsl=nsl: e.activation(out=sgm[:, nsl], in_=ps[gbk][:], func=AF.Sigmoid), reads=[Bps[gbk]], writes=[Bsgm])
                    S.op("dve", lambda e, pbk=pbk, nsl=nsl: e.tensor_tensor(out=sgm[:, nsl], in0=sgm[:, nsl], in1=ps[pbk][:], op=ALU.mult),
                         reads=[Bsgm, Bps[pbk]], writes=[Bsgm])
                    S.op("dve", lambda e, nsl=nsl: e.tensor_tensor(out=o_t[:, nsl], in0=sgm[:, nsl], in1=h_t[:, nsl], op=ALU.add),
                         reads=[Bsgm, Bh], writes=[Bo])
                    yield
                S.dma("sp", lambda e, r0=r0: e.dma_start(out=K.y[r0:r0 + 128, :], in_=o_t), reads=[Bo], owner=Bo)
        return gen()

    interleave([tile_stream(q) for q in range(4)], skew=2)


def _rope_tables():
    pos = np.arange(SEQ)
    row = (pos // 64).astype(np.float32)
    col = (pos % 64).astype(np.float32)
    inv = (10000.0 ** (-np.arange(0, 64, 2, dtype=np.float32) / 64.0)).astype(np.float32)
    C = np.zeros((128, SEQ), np.float32)
    Sg = np.zeros((128, SEQ), np.float32)
    for p in range(128):
        ids = row if p < 64 else col
        j = p % 32
        ang = (ids * inv[j]).astype(np.float32)
        C[p] = np.cos(ang)
        sgn = -1.0 if (p % 64) < 32 else 1.0
        Sg[p] = sgn * np.sin(ang)
    perm = np.zeros((128, 128), np.float32)
    for m in range(128):
        partner = m + 32 if (m % 64) < 32 else m - 32
        perm[partner, m] = 1.0
    return C, Sg, perm


_NC_CACHE = {}


def _prep_common(inp):
    f = lambda a: np.ascontiguousarray(np.asarray(a, dtype=np.float32))
    pv = np.zeros((128, NPV), np.float32)
    cw = f(inp["conv_w"])[0]
    pv[:, PV_CONVW:PV_CONVW + 32] = cw.reshape(4, 8, 128).transpose(2, 1, 0).reshape(128, 32)
    pv[:, PV_CONVB:PV_CONVB + 8] = f(inp["conv_b"])[0].reshape(8, 128).T
    pv[:, PV_BA:PV_BA + 16] = f(inp["lru_ba"])[0].reshape(16, 128).T
    pv[:, PV_BI:PV_BI + 16] = f(inp["lru_bi"])[0].reshape(16, 128).T
    pv[:, PV_LAM:PV_LAM + 16] = f(inp["lru_lam"])[0].reshape(16, 128).T
    pv[:, PV_QN] = f(inp["q_norm"])[0]
    pv[:, PV_KN] = f(inp["k_norm"])[0]
    pv[:, PV_BGU:PV_BGU + 512] = f(inp["b_gu"])[0].reshape(E * 16, 128).T
    C, Sg, perm = _rope_tables()
    tri = np.triu(np.ones((128, 128), np.float32), 1)
    eC = np.tile((np.arange(E, dtype=np.float32) * CAP)[None, :], (128, 1))
    com = {
        "w_in": f(inp["w_in"])[0], "lru_wa": f(inp["lru_wa"])[0], "lru_wi": f(inp["lru_wi"])[0],
        "w_attn_br": f(inp["w_attn_br"])[0], "w_lru_br": f(inp["w_lru_br"])[0], "w_out": f(inp["w_out"])[0],
        "w_router": f(inp["w_router"])[0], "w_gu": f(inp["w_gu"])[0], "w_dn": f(inp["w_dn"])[0],
        "b_dn": f(inp["b_dn"])[0], "w_ple_gate": f(inp["w_ple_gate"])[0], "w_ple_proj": f(inp["w_ple_proj"])[0],
        "g_mix": f(inp["g_mix"]), "g_moe": f(inp["g_moe"]), "g_ple": f(inp["g_ple"]), "b_router": f(inp["b_router"]),
        "pv": pv, "ropeC": C, "ropeS": Sg, "perm": perm, "ident": np.eye(128, dtype=np.float32), "tri": tri, "eC": eC,
    }
    return com


def kernel(**inputs):
    dbg = bool(int(os.environ.get("MK_DBG", "0")))
    ncores = int(os.environ.get("MK_NCORES", str(NCORES)))
    key = dbg
    if key not in _NC_CACHE:
        _NC_CACHE[key] = build_program(dbg)
    nc = _NC_CACHE[key]
    com = _prep_common(inputs)
    x = np.asarray(inputs["x"], dtype=np.float32)
    p = np.asarray(inputs["p"], dtype=np.float32)[0]
    in_maps = []
    for c in range(ncores):
        m = dict(com)
        m["x"] = np.ascontiguousarray(x[2 * c:2 * c + 2].reshape(T, D))
        m["p"] = np.ascontiguousarray(p[2 * c:2 * c + 2].reshape(T, PLE))
        in_maps.append(m)
    res = run_bass_kernel_spmd(nc, in_maps, core_ids=list(range(ncores)))
    if dbg:
        kernel.last = res
    out = np.zeros((16, SEQ, D), np.float32)
    for c in range(ncores):
        out[2 * c:2 * c + 2] = np.asarray(res.results[c]["y"], dtype=np.float32).reshape(2, SEQ, D)
    return out
```

```python
import os
import numpy as np
from contextlib import ExitStack
import concourse.bass as bass
import concourse.mybir as mybir
from concourse.bass_utils import run_bass_kernel_spmd

F32 = mybir.dt.float32
BF16 = mybir.dt.bfloat16
I32 = mybir.dt.int32
AF = mybir.ActivationFunctionType
ALU = mybir.AluOpType
AX = mybir.AxisListType

NCORES = 8
D = 1024
SEQ = 2048
NSEQ = 2
T = NSEQ * SEQ
NTILE = T // 128
E = 32
F = 1024
CAP = 640
NSB = CAP // 128
NSLOT = E * CAP
TRASH = NSLOT
PLE = 256
EPS = 1e-6
INW = 5632
NPV = 608
PV_CONVW, PV_CONVB, PV_BA, PV_BI, PV_LAM, PV_QN, PV_KN, PV_BGU = 0, 32, 40, 56, 72, 88, 89, 96

ENGS = ("pe", "act", "dve", "pool", "sp")


class Buf:
    __slots__ = ("name", "last_write", "reads", "sem", "sem_total", "excl")

    def __init__(self, name, excl=False):
        self.name = name
        self.excl = excl
        self.last_write = None
        self.reads = []
        self.sem = None
        self.sem_total = 0


class Sched:
    def __init__(self, nc, stack):
        self.nc = nc
        self.stack = stack
        self.stream = {e: [] for e in ENGS}
        self.sem = {e: stack.enter_context(nc.semaphore("s_" + e)) for e in ENGS}
        self.count = {e: 0 for e in ENGS}
        self.seen = {e: {} for e in ENGS}
        self.dma_bufs = []

    def _wait_tokens(self, e, toks):
        need = {}
        for t in toks:
            if t is None:
                continue
            if t[0] == "e":
                _, src, c = t
                if src == "pe" and e == "pe":
                    continue
                key = ("e", src)
                val = c
                sem = self.sem[src]
            else:
                b = t[1]
                key = ("d", id(b))
                val = b.sem_total
                sem = b.sem
            if self.seen[e].get(key, 0) >= val:
                continue
            if key not in need or need[key][1] < val:
                need[key] = (sem, val)
        for key, (sem, val) in need.items():
            self.seen[e][key] = val
            self.stream[e].append(lambda eng, sem=sem, val=val: eng.wait_ge(sem, val))

    @staticmethod
    def _deps(reads, writes):
        toks = []
        for r in reads:
            toks.append(r.last_write)
            if r.excl:
                toks.extend(r.reads)
        for w in writes:
            toks.append(w.last_write)
            toks.extend(w.reads)
        return toks

    def op(self, e, fn, reads=(), writes=(), signal=True):
        self._wait_tokens(e, self._deps(reads, writes))
        if signal:
            self.count[e] += 1
            tok = ("e", e, self.count[e])
            sem = self.sem[e]
            self.stream[e].append(lambda eng, fn=fn, sem=sem: fn(eng).then_inc(sem, 1))
        else:
            tok = ("e", e, self.count[e] + 1)
            self.stream[e].append(lambda eng, fn=fn: fn(eng))
        for w in writes:
            w.last_write = tok
            w.reads = []
        for r in reads:
            r.reads.append(tok)
        return tok

    def dma(self, e, fn, reads=(), writes=(), owner=None):
        if owner is None:
            owner = writes[0] if writes else reads[0]
        if owner.sem is None:
            owner.sem = self.stack.enter_context(self.nc.semaphore("d%d_%s" % (len(self.dma_bufs), owner.name)))
            self.dma_bufs.append(owner)
        self._wait_tokens(e, self._deps(reads, writes))
        owner.sem_total += 16
        sem = owner.sem
        self.stream[e].append(lambda eng, fn=fn, sem=sem: fn(eng).then_inc(sem, 16))
        tok = ("d", owner)
        for w in writes:
            w.last_write = tok
            w.reads = []
        for r in reads:
            r.reads.append(tok)
        return tok

    def barrier(self):
        toks = [("e", s, self.count[s]) for s in ENGS if self.count[s] > 0]
        toks += [("d", b) for b in self.dma_bufs]
        for e in ENGS:
            self._wait_tokens(e, toks)

    def emit(self):
        nc = self.nc
        self.barrier()
        with nc.Block() as block:
            for e, reg in (("sp", block.sync), ("act", block.scalar), ("pe", block.tensor),
                           ("dve", block.vector), ("pool", block.gpsimd)):
                lst = self.stream[e]
                if not lst:
                    continue

                def body(eng, lst=lst):
                    for f in lst:
                        f(eng)
                reg(body)


def _dsize(dt):
    return 2 if dt == BF16 else 4


class Arena:
    def __init__(self, nc, stack, nbytes):
        self.t = stack.enter_context(nc.sbuf_tensor("arena", [128, nbytes // 4], F32))
        self.off = 0
        self.nbytes = nbytes
        self.peak = 0

    def alloc(self, name, free, dt=F32):
        n = 1
        for f in free:
            n *= f
        sz = (n * _dsize(dt) + 31) // 32 * 32
        assert self.off + sz <= self.nbytes, (name, self.off, sz, self.nbytes)
        a = self.t[:, self.off // 4:(self.off + sz) // 4]
        if dt != F32:
            a = a.bitcast(dt)
        a = a[:, 0:n]
        if len(free) == 2:
            a = a.rearrange("p (a b) -> p a b", a=free[0])
        self.off += sz
        self.peak = max(self.peak, self.off)
        return a, Buf(name)

    def mark(self):
        return self.off

    def release(self, m):
        self.off = m


class Ctx:
    pass


def build_program(dbg=False):
    nc = bass.Bass("TRN2", target_bir_lowering=False)
    K = Ctx()
    K.nc = nc

    def din(name, shape, dt=F32):
        return nc.dram_tensor(name, list(shape), dt, kind="ExternalInput")

    K.x = din("x", [T, D]).ap()
    K.p = din("p", [T, PLE]).ap()
    K.w_in = din("w_in", [D, INW]).ap()
    K.lru_wa = din("lru_wa", [2, 8, 128, 128]).ap()
    K.lru_wi = din("lru_wi", [2, 8, 128, 128]).ap()
    K.w_attn_br = din("w_attn_br", [D, D]).ap()
    K.w_lru_br = din("w_lru_br", [D, D]).ap()
    K.w_out = din("w_out", [D, D]).ap()
    K.w_router = din("w_router", [D, E]).ap()
    K.w_gu = din("w_gu", [E, D, 2 * F]).ap()
    K.w_dn = din("w_dn", [E, F, D]).ap()
    K.b_dn = din("b_dn", [E, D]).ap()
    K.w_ple_gate = din("w_ple_gate", [D, D]).ap()
    K.w_ple_proj = din("w_ple_proj", [PLE, D]).ap()
    K.g_mix = din("g_mix", [1, D])
    K.g_moe = din("g_moe", [1, D])
    K.g_ple = din("g_ple", [1, D])
    K.b_router = din("b_router", [1, E])
    K.pv = din("pv", [128, NPV]).ap()
    K.ropeC = din("ropeC", [128, SEQ]).ap()
    K.ropeS = din("ropeS", [128, SEQ]).ap()
    K.perm = din("perm", [128, 128]).ap()
    K.ident = din("ident", [128, 128]).ap()
    K.tri = din("tri", [128, 128]).ap()
    K.eC = din("eC", [128, E]).ap()
    K.y = nc.dram_tensor("y", [T, D], F32, kind="ExternalOutput").ap()

    kind = "ExternalOutput" if dbg else "Internal"

    def dscr(name, shape, dt):
        if dbg:
            return nc.dram_tensor(name, list(shape), dt, kind="ExternalOutput").ap()
        return nc.dram_tensor(name, list(shape), dt).ap()

    K.qT_s = dscr("qT_s", [NSEQ, 8, 128, SEQ], BF16)
    K.kT_s = dscr("kT_s", [NSEQ, 2, 128, SEQ], BF16)
    K.V_s = dscr("V_s", [NSEQ, 128, 16, 256], BF16)
    K.yl_s = dscr("yl_s", [NSEQ, 8, 128, SEQ], BF16)
    K.sga_s = dscr("sga_s", [NSEQ, 8, 128, SEQ], BF16)
    K.sgr_s = dscr("sgr_s", [NSEQ, 8, 128, SEQ], BF16)
    K.h1_s = dscr("h1_s", [T, D], F32)
    K.xs = dscr("xs_s", [NSLOT + 128, D], BF16)
    K.ys = dscr("ys_s", [NSLOT + 128, D], F32)

    with ExitStack() as st:
        S = Sched(nc, st)
        K.S = S
        A = Arena(nc, st, 204 * 1024)
        K.A = A
        K.ps = []
        K.Bps = []
        for i in range(8):
            K.ps.append(st.enter_context(nc.psum_tensor(f"ps{i}", [128, 512], F32)))
            K.Bps.append(Buf(f"ps{i}", excl=True))
        stop = int(os.environ.get("MK_STOP", "9"))
        phase0_consts(K)
        S.barrier()
        m0 = A.mark()
        if stop >= 1:
            phase1_inproj(K)
            S.barrier()
        A.release(m0)
        if stop >= 2:
            phase2_attn(K)
            S.barrier()
        A.release(m0)
        K.pre5 = {"wpg": A.alloc("wpg", [8, D], BF16), "wpp": A.alloc("wpp", [2, D], BF16), "gple": A.alloc("gple", [D])}
        m5 = A.mark()
        K.pre4 = {"wgu": A.alloc("wgu0", [8, 2 * F], BF16), "wdn": A.alloc("wdn0", [8, D], BF16), "bdn": A.alloc("bdn0", [D], BF16)}
        m3 = A.mark()
        if stop >= 3:
            issue_expert_load(K, 0, *K.pre4["wgu"], *K.pre4["wdn"], *K.pre4["bdn"])
            phase3_router(K)
            S.barrier()
        A.release(m3)
        if stop >= 4:
            phase4_experts(K)
            S.barrier()
        A.release(m5)
        if stop >= 5:
            phase5_combine(K)
        S.emit()
    return nc


def bcast_row(dt_tensor, n):
    return bass.AP(dt_tensor, 0, [[0, 128], [1, n]])


def phase0_consts(K):
    S, A = K.S, K.A
    K.ident_f, K.Bident_f = A.alloc("ident_f", [128])
    K.ident_b, K.Bident_b = A.alloc("ident_b", [128], BF16)
    K.ones_b, K.Bones_b = A.alloc("ones_b", [128], BF16)
    K.ones_f, K.Bones_f = A.alloc("ones_f", [128])
    K.pvt, K.Bpv = A.alloc("pvt", [NPV])
    K.kk, K.Bkk = A.alloc("kk", [16])
    K.dest_i, K.Bdest = A.alloc("dest_i", [NTILE * 4], I32)
    K.gate_a, K.Bgate = A.alloc("gate_a", [NTILE * 4])
    S.dma("sp", lambda e: e.dma_start(out=K.ident_f, in_=K.ident), writes=[K.Bident_f])
    S.dma("pool", lambda e: e.dma_start(out=K.ident_b, in_=K.ident), writes=[K.Bident_b])
    S.dma("sp", lambda e: e.dma_start(out=K.pvt, in_=K.pv), writes=[K.Bpv])
    S.op("dve", lambda e: e.memset(K.ones_b, 1.0), writes=[K.Bones_b])
    S.op("dve", lambda e: e.memset(K.ones_f, 1.0), writes=[K.Bones_f])
    lam = K.pvt[:, PV_LAM:PV_LAM + 16]
    S.op("act", lambda e: e.activation(out=K.kk, in_=lam, func=AF.Exp, scale=-1.0), reads=[K.Bpv], writes=[K.Bkk])
    S.op("act", lambda e: e.activation(out=K.kk, in_=K.kk, func=AF.Ln, bias=1.0), reads=[K.Bkk], writes=[K.Bkk])
    S.op("dve", lambda e: e.tensor_scalar(out=K.kk, in0=K.kk, scalar1=-8.0, scalar2=None, op0=ALU.mult),
         reads=[K.Bkk], writes=[K.Bkk])


def mm_group(S, out, Bout, pairs, reads):
    n = len(pairs)
    for i, (l, r) in enumerate(pairs):
        S.op("pe", lambda e, l=l, r=r, i=i: e.matmul(out, l, r, start=(i == 0), stop=(i == n - 1)),
             reads=reads, writes=[Bout], signal=(i == n - 1))


def rms_rstd(K, src, Bsrc, junk, Bjunk, ss, Bss, n):
    S = K.S
    S.op("act", lambda e: e.activation(out=junk, in_=src, func=AF.Square, accum_out=ss),
         reads=[Bsrc], writes=[Bjunk, Bss])
    S.op("act", lambda e: e.activation(out=ss, in_=ss, func=AF.Sqrt, bias=EPS, scale=1.0 / n),
         reads=[Bss], writes=[Bss])
    S.op("dve", lambda e: e.reciprocal(out=ss, in_=ss), reads=[Bss], writes=[Bss])


def interleave(gens, skew=0):
    gens = list(gens)
    start = {id(g): i * skew for i, g in enumerate(gens)}
    rnd = 0
    while gens:
        for g in list(gens):
            if rnd < start[id(g)]:
                continue
            try:
                next(g)
            except StopIteration:
                gens.remove(g)
        rnd += 1


def phase1_inproj(K):
    S, A, nc = K.S, K.A, K.nc
    ps, Bps = K.ps, K.Bps
    uT, BuT = A.alloc("uT", [8, SEQ], BF16)
    wtA = [A.alloc(f"wtA{i}", [8, 512], BF16) for i in range(2)]
    wtB = [A.alloc(f"wtB{i}", [8, 256], BF16) for i in range(2)]
    ropeS, BropeS = A.alloc("ropeS", [SEQ])
    cq, Bcq = A.alloc("cq", [SEQ])
    ck, Bck = A.alloc("ck", [SEQ])
    permf, Bpermf = A.alloc("permf", [128])
    permq, Bpermq = A.alloc("permq", [128], BF16)
    permk, Bpermk = A.alloc("permk", [128], BF16)
    od_b, Bod = A.alloc("od_b", [128], BF16)
    wa_b, Bwa = A.alloc("wa_b", [16, 128], BF16)
    wi_b, Bwi = A.alloc("wi_b", [16, 128], BF16)
    xn, Bxn = A.alloc("xn", [D], BF16)
    junk, Bjunk = xn, Bxn
    xns = [(xn, Bxn), A.alloc("xn2", [D], BF16)]
    ss = [A.alloc(f"ss{i}", [1]) for i in range(2)]
    stgA = [A.alloc(f"stgA{i}", [SEQ], BF16) for i in range(2)]
    stgB = [A.alloc(f"stgB{i}", [SEQ], BF16) for i in range(1)]
    xq = [A.alloc(f"xq{i}", [512], BF16) for i in range(2)]
    sq = [A.alloc(f"sq{i}", [512], BF16) for i in range(2)]
    ta = [A.alloc(f"ta{i}", [512]) for i in range(2)]
    tb_ = [A.alloc(f"tb{i}", [512]) for i in range(2)]
    rst = [A.alloc(f"rst{i}", [512]) for i in range(2)]
    xrp, Bxrp = A.alloc("xrp", [SEQ + 4])
    cc, Bcc = A.alloc("cc", [SEQ])
    ccb, Bccb = A.alloc("ccb", [SEQ], BF16)
    aas = [A.alloc(f"aa{i}", [SEQ]) for i in range(2)]
    bts = [A.alloc(f"bt{i}", [SEQ]) for i in range(2)]
    aa, Baa = aas[0]
    t1, Bt1 = A.alloc("t1", [SEQ])
    gmix, Bgmix = cc[:, 0:D], Bcc
    hf, Bhf = A.alloc("hf", [SEQ])
    hb, Bhb = A.alloc("hb", [SEQ])
    xt = [(hb[:, 0:D], Bhb), (hf[:, 0:D], Bhf)]
    vst, Bvst = aa.bitcast(BF16).rearrange("p (a b) -> p a b", a=16), Baa

    pvt = K.pvt
    S.dma("sp", lambda e: e.dma_start(out=cq, in_=K.ropeC), writes=[Bcq])
    S.dma("sp", lambda e: e.dma_start(out=ck, in_=K.ropeC), writes=[Bck])
    S.dma("sp", lambda e: e.dma_start(out=ropeS, in_=K.ropeS), writes=[BropeS])
    S.dma("sp", lambda e: e.dma_start(out=permf, in_=K.perm), writes=[Bpermf])
    S.dma("pool", lambda e: e.dma_start(out=wa_b, in_=K.lru_wa.rearrange("d c p n -> p (d c) n")), writes=[Bwa])
    S.dma("pool", lambda e: e.dma_start(out=wi_b, in_=K.lru_wi.rearrange("d c p n -> p (d c) n")), writes=[Bwi])
    S.op("dve", lambda e: e.memset(od_b, 1.0 / 128.0), writes=[Bod])
    S.op("dve", lambda e: e.memset(xrp, 0.0), writes=[Bxrp])
    qn = pvt[:, PV_QN:PV_QN + 1]
    kn = pvt[:, PV_KN:PV_KN + 1]
    S.op("dve", lambda e: e.tensor_scalar(out=cq, in0=cq, scalar1=qn, scalar2=None, op0=ALU.mult),
         reads=[Bcq, K.Bpv], writes=[Bcq])
    S.op("dve", lambda e: e.tensor_scalar(out=ck, in0=ck, scalar1=kn, scalar2=None, op0=ALU.mult),
         reads=[Bck, K.Bpv], writes=[Bck])
    S.op("dve", lambda e: e.tensor_scalar(out=permq, in0=permf, scalar1=qn, scalar2=None, op0=ALU.mult),
         reads=[Bpermf, K.Bpv], writes=[Bpermq])
    S.op("dve", lambda e: e.tensor_scalar(out=permk, in0=permf, scalar1=kn, scalar2=None, op0=ALU.mult),
         reads=[Bpermf, K.Bpv], writes=[Bpermk])

    w_in_v = K.w_in.rearrange("(c p) n -> p c n", p=128)
    wcnt = {"A": 0, "B": 0}

    def load_w(which, cols):
        tiles = wtA if which == "A" else wtB
        i = wcnt[which] % 2
        wcnt[which] += 1
        w, Bw = tiles[i]
        off = 0
        for (c0, wd) in cols:
            S.dma("pool", lambda e, w=w, off=off, c0=c0, wd=wd: e.dma_start(
                out=w[:, :, off:off + wd], in_=w_in_v[:, :, c0:c0 + wd]), writes=[Bw])
            off += wd
        return w, Bw

    def inproj(w, Bw, woff, tb, bank):
        mm_group(S, ps[bank][:], Bps[bank],
                 [(w[:, k, woff:woff + 128], uT[:, k, tb * 512:(tb + 1) * 512]) for k in range(8)],
                 reads=[Bw, BuT])

    scnt = {"A": 0, "B": 0}

    def build_uT(s):
        S.dma("sp", lambda e: e.dma_start(out=gmix, in_=bcast_row(K.g_mix, D)), writes=[Bgmix])

        def tiles(par):
            x_t, Bx = xt[par]
            ss_t, Bss = ss[par]
            xn_, Bxn_ = xns[par]
            bank = par
            pv16 = ps[bank][:].bitcast(BF16)
            for i in range(par, 16, 2):
                r0 = s * SEQ + i * 128
                S.dma("sp", lambda e, r0=r0: e.dma_start(out=x_t, in_=K.x[r0:r0 + 128, :]), writes=[Bx])
                rms_rstd(K, x_t, Bx, xn_, Bxn_, ss_t, Bss, D)
                yield
                S.op("dve", lambda e: e.scalar_tensor_tensor(
                    out=xn_, in0=x_t, scalar=ss_t, in1=gmix, op0=ALU.mult, op1=ALU.mult),
                    reads=[Bx, Bss, Bgmix], writes=[Bxn_])
                yield
                for c in range(8):
                    S.op("pe", lambda e, c=c: e.transpose(pv16[:, c * 128:(c + 1) * 128], xn_[:, c * 128:(c + 1) * 128], K.ident_b),
                         reads=[Bxn_, K.Bident_b], writes=[Bps[bank]], signal=(c == 7))
                S.op("act", lambda e, i=i: e.activation(
                    out=uT[:, :, i * 128:(i + 1) * 128], in_=pv16.rearrange("p (c t) -> p c t", c=8), func=AF.Copy),
                    reads=[Bps[bank]], writes=[BuT])
                yield

        interleave([tiles(0), tiles(1)], skew=1)

    def qk_head(s, w, Bw, woff, is_q, hidx):
        cg, Bcg = (cq, Bcq) if is_q else (ck, Bck)
        pm, Bpm = (permq, Bpermq) if is_q else (permk, Bpermk)
        st_t, Bst = stgA[scnt["A"] % 2]
        scnt["A"] += 1
        for tb in range(4):
            p = tb % 2
            sl = slice(tb * 512, (tb + 1) * 512)
            xq_, Bxq = xq[p]
            sq_, Bsq = sq[p]
            ta_, Bta = ta[p]
            tb2, Btb = tb_[p]
            rs_, Brst = rst[p]
            zb = p
            inproj(w, Bw, woff, tb, zb)
            S.op("act", lambda e, xq_=xq_, zb=zb: e.activation(out=xq_, in_=ps[zb][:], func=AF.Copy), reads=[Bps[zb]], writes=[Bxq])
            S.op("act", lambda e, sq_=sq_, zb=zb: e.activation(out=sq_, in_=ps[zb][:], func=AF.Square), reads=[Bps[zb]], writes=[Bsq])
            S.op("dve", lambda e, sl=sl, cg=cg, ta_=ta_, zb=zb: e.tensor_tensor(out=ta_, in0=ps[zb][:], in1=cg[:, sl], op=ALU.mult),
                 reads=[Bps[zb], Bcg], writes=[Bta])
            yield
            mm_group(S, ps[2][:], Bps[2], [(od_b, sq_)], reads=[Bod, Bsq])
            mm_group(S, ps[3][:], Bps[3], [(pm, xq_)], reads=[Bpm, Bxq])
            S.op("act", lambda e, rs_=rs_: e.activation(out=rs_, in_=ps[2][:], func=AF.Sqrt, bias=EPS), reads=[Bps[2]], writes=[Brst])
            S.op("dve", lambda e, sl=sl, tb2=tb2: e.tensor_tensor(out=tb2, in0=ps[3][:], in1=ropeS[:, sl], op=ALU.mult),
                 reads=[Bps[3], BropeS], writes=[Btb])
            yield
            S.op("dve", lambda e, rs_=rs_: e.reciprocal(out=rs_, in_=rs_), reads=[Brst], writes=[Brst])
            S.op("dve", lambda e, ta_=ta_, tb2=tb2: e.tensor_tensor(out=ta_, in0=ta_, in1=tb2, op=ALU.add), reads=[Bta, Btb], writes=[Bta])
            S.op("dve", lambda e, sl=sl, st_t=st_t, ta_=ta_, rs_=rs_: e.tensor_tensor(out=st_t[:, sl], in0=ta_, in1=rs_, op=ALU.mult),
                 reads=[Bta, Brst], writes=[Bst])
            yield
        dst = (K.qT_s if is_q else K.kT_s)[s, hidx]
        S.dma("sp", lambda e, st_t=st_t, dst=dst: e.dma_start(out=dst, in_=st_t), reads=[Bst], owner=Bst)

    def stream_A(s):
        for t2 in range(2):
            w, Bw = load_w("A", [(t2 * 512, 512)])
            for j in range(4):
                yield from qk_head(s, w, Bw, j * 128, True, t2 * 4 + j)
        w, Bw = load_w("A", [(1024, 512)])
        for j in range(2):
            yield from qk_head(s, w, Bw, j * 128, False, j)
        for gi, dst_s in ((0, K.sga_s), (1, K.sgr_s)):
            for t2 in range(2):
                wg, Bwg = load_w("A", [(3584 + gi * 1024 + t2 * 512, 512)])
                for j in range(4):
                    st_t, Bst = stgA[scnt["A"] % 2]
                    scnt["A"] += 1
                    for tb in range(4):
                        bank = tb % 2
                        inproj(wg, Bwg, j * 128, tb, bank)
                        S.op("act", lambda e, bank=bank, tb=tb, st_t=st_t: e.activation(out=st_t[:, tb * 512:(tb + 1) * 512], in_=ps[bank][:], func=AF.Sigmoid),
                             reads=[Bps[bank]], writes=[Bst])
                        yield
                    S.dma("sp", lambda e, st_t=st_t, dst=dst_s[s, t2 * 4 + j]: e.dma_start(out=dst, in_=st_t), reads=[Bst], owner=Bst)

    def do_V(s):
        w, Bw = load_w("A", [(1024, 512)])
        for tk in range(16):
            bank = 2 + tk % 2
            mm_group(S, ps[bank][:, 0:256], Bps[bank],
                     [(uT[:, k, tk * 128:(tk + 1) * 128], w[:, k, 256:512]) for k in range(8)], reads=[Bw, BuT])
            S.op("act", lambda e, bank=bank, tk=tk: e.activation(out=vst[:, tk, :], in_=ps[bank][:, 0:256], func=AF.Copy),
                 reads=[Bps[bank]], writes=[Bvst])
        S.dma("sp", lambda e, s=s: e.dma_start(out=K.V_s[s], in_=vst), reads=[Bvst], owner=Bvst)

    def stream_B(s):
        for j in range(8):
            w, Bw = load_w("B", [(1536 + j * 128, 128), (2560 + j * 128, 128)])
            for tb in range(4):
                bank = 4 + tb % 2
                inproj(w, Bw, 0, tb, bank)
                S.op("act", lambda e, bank=bank, tb=tb: e.activation(out=xrp[:, 2 + tb * 512:2 + (tb + 1) * 512], in_=ps[bank][:], func=AF.Copy),
                     reads=[Bps[bank]], writes=[Bxrp])
                yield
            cw = lambda jj, j=j: K.pvt[:, PV_CONVW + j * 4 + jj:PV_CONVW + j * 4 + jj + 1]
            cb = K.pvt[:, PV_CONVB + j:PV_CONVB + j + 1]
            S.op("act", lambda e, cw=cw, cb=cb: e.activation(out=cc, in_=xrp[:, 0:SEQ], func=AF.Identity, bias=cb, scale=cw(0)),
                 reads=[Bxrp, K.Bpv], writes=[Bcc])
            for jj in range(1, 4):
                S.op("dve", lambda e, cw=cw, jj=jj: e.scalar_tensor_tensor(out=cc, in0=xrp[:, jj:jj + SEQ], scalar=cw(jj), in1=cc, op0=ALU.mult, op1=ALU.add),
                     reads=[Bxrp, Bcc, K.Bpv], writes=[Bcc])
                yield
            S.op("act", lambda e: e.activation(out=ccb, in_=cc, func=AF.Copy), reads=[Bcc], writes=[Bccb])

            def dir_gen(d, j=j):
                aa_, Baa_ = aas[d]
                bt_, Bbt_ = bts[d]
                hh, Bhh = (hf, Bhf) if d == 0 else (hb, Bhb)
                ba = K.pvt[:, PV_BA + d * 8 + j:PV_BA + d * 8 + j + 1]
                bi = K.pvt[:, PV_BI + d * 8 + j:PV_BI + d * 8 + j + 1]
                kkc = K.kk[:, d * 8 + j:d * 8 + j + 1]
                for tb in range(4):
                    sl = slice(tb * 512, (tb + 1) * 512)
                    mm_group(S, ps[6][:], Bps[6], [(wa_b[:, d * 8 + j, :], ccb[:, sl])], reads=[Bwa, Bccb])
                    S.op("act", lambda e, sl=sl: e.activation(out=aa_[:, sl], in_=ps[6][:], func=AF.Sigmoid, bias=ba),
                         reads=[Bps[6], K.Bpv], writes=[Baa_])
                    mm_group(S, ps[7][:], Bps[7], [(wi_b[:, d * 8 + j, :], ccb[:, sl])], reads=[Bwi, Bccb])
                    S.op("act", lambda e, sl=sl: e.activation(out=bt_[:, sl], in_=ps[7][:], func=AF.Sigmoid, bias=bi),
                         reads=[Bps[7], K.Bpv], writes=[Bbt_])
                    yield
                S.op("act", lambda e: e.activation(out=aa_, in_=aa_, func=AF.Exp, scale=kkc), reads=[Baa_, K.Bkk], writes=[Baa_])
                S.op("dve", lambda e: e.tensor_tensor(out=bt_, in0=bt_, in1=cc, op=ALU.mult), reads=[Bbt_, Bcc], writes=[Bbt_])
                yield
                S.op("act", lambda e: e.activation(out=hh, in_=aa_, func=AF.Square), reads=[Baa_], writes=[Bhh])
                S.op("act", lambda e: e.activation(out=hh, in_=hh, func=AF.Sqrt, bias=1.0, scale=-1.0), reads=[Bhh], writes=[Bhh])
                yield
                S.op("dve", lambda e: e.tensor_tensor(out=bt_, in0=bt_, in1=hh, op=ALU.mult), reads=[Bbt_, Bhh], writes=[Bbt_])
                yield
                if d == 0:
                    S.op("dve", lambda e: e.tensor_tensor_scan(out=hh, data0=aa_, data1=bt_, initial=0.0, op0=ALU.mult, op1=ALU.add),
                         reads=[Baa_, Bbt_], writes=[Bhh])
                else:
                    S.op("dve", lambda e: e.tensor_tensor_scan(out=hh[:, ::-1], data0=aa_[:, ::-1], data1=bt_[:, ::-1], initial=0.0, op0=ALU.mult, op1=ALU.add),
                         reads=[Baa_, Bbt_], writes=[Bhh])
                yield

            def gelu_gen(w=w, Bw=Bw):
                for tb in range(4):
                    sl = slice(tb * 512, (tb + 1) * 512)
                    bank = 4 + tb % 2
                    inproj(w, Bw, 128, tb, bank)
                    S.op("act", lambda e, bank=bank, sl=sl: e.activation(out=t1[:, sl], in_=ps[bank][:], func=AF.Square), reads=[Bps[bank]], writes=[Bt1])
                    S.op("act", lambda e, sl=sl: e.activation(out=t1[:, sl], in_=t1[:, sl], func=AF.Identity, bias=1.0, scale=0.044715),
                         reads=[Bt1], writes=[Bt1])
                    S.op("dve", lambda e, bank=bank, sl=sl: e.tensor_tensor(out=t1[:, sl], in0=t1[:, sl], in1=ps[bank][:], op=ALU.mult),
                         reads=[Bt1, Bps[bank]], writes=[Bt1])
                    S.op("act", lambda e, sl=sl: e.activation(out=t1[:, sl], in_=t1[:, sl], func=AF.Sigmoid, scale=1.5957691216), reads=[Bt1], writes=[Bt1])
                    S.op("dve", lambda e, bank=bank, sl=sl: e.tensor_tensor(out=t1[:, sl], in0=t1[:, sl], in1=ps[bank][:], op=ALU.mult),
                         reads=[Bt1, Bps[bank]], writes=[Bt1])
                    yield

            subs = [dir_gen(0), dir_gen(1), gelu_gen()]
            while subs:
                for g in list(subs):
                    try:
                        next(g)
                    except StopIteration:
                        subs.remove(g)
                yield
            S.op("dve", lambda e: e.tensor_tensor(out=hf, in0=hf, in1=hb, op=ALU.add), reads=[Bhf, Bhb], writes=[Bhf])
            st_t, Bst = stgB[0]
            S.op("dve", lambda e, st_t=st_t: e.tensor_tensor(out=st_t, in0=hf, in1=t1, op=ALU.mult), reads=[Bhf, Bt1], writes=[Bst])
            S.dma("sp", lambda e, st_t=st_t, j=j, s=s: e.dma_start(out=K.yl_s[s, j], in_=st_t), reads=[Bst], owner=Bst)
            yield

    for s in range(NSEQ):
        build_uT(s)
        interleave([stream_A(s), stream_B(s)])
        do_V(s)


def phase2_attn(K):
    S, A, nc = K.S, K.A, K.nc
    ps, Bps = K.ps, K.Bps
    wab, Bwab = A.alloc("wab", [8, D], BF16)
    wlb, Bwlb = A.alloc("wlb", [8, D], BF16)
    wo, Bwo = A.alloc("wo", [8, D], BF16)
    kT, BkT = A.alloc("kT", [2, SEQ], BF16)
    V, BV = A.alloc("V", [16, 256], BF16)
    qT = [A.alloc(f"qT{i}", [8, 512], BF16) for i in range(2)]
    ylb = [A.alloc(f"ylb{i}", [8, 512], BF16) for i in range(2)]
    gab = [A.alloc(f"gab{i}", [8, 512], BF16) for i in range(2)]
    grb = [A.alloc(f"grb{i}", [8, 512], BF16) for i in range(2)]
    PT = [A.alloc(f"PT{i}", [512], BF16) for i in range(6)]
    SBANK = (0, 1, 2, 7)
    attnT, BattnT = A.alloc("attnT", [8, 512], BF16)
    mrg, Bmrg = A.alloc("mrg", [8, 512], BF16)
    rz, Brz = A.alloc("rz", [512])
    zacc = [A.alloc(f"zacc{i}", [512]) for i in range(2)]
    zab = [A.alloc(f"zab{i}", [512], BF16) for i in range(2)]
    m1, Bm1 = A.alloc("m1", [512])
    m2, Bm2 = A.alloc("m2", [512])
    xt = [A.alloc(f"xt{i}", [D]) for i in range(2)]
    ho = [A.alloc(f"ho{i}", [D]) for i in range(2)]
    S.dma("pool", lambda e: e.dma_start(out=wab, in_=K.w_attn_br.rearrange("(c p) n -> p c n", p=128)), writes=[Bwab])
    S.dma("pool", lambda e: e.dma_start(out=wlb, in_=K.w_lru_br.rearrange("(c p) n -> p c n", p=128)), writes=[Bwlb])
    S.dma("pool", lambda e: e.dma_start(out=wo, in_=K.w_out.rearrange("(c p) n -> p c n", p=128)), writes=[Bwo])
    zt, Bzt = A.alloc("zt", [D], BF16)
    S.op("dve", lambda e: e.memset(zt, 0.0), writes=[Bzt])
    zrows = list(range(0, NSLOT + 128, 128))

    def zero_some(n):
        for _ in range(n):
            if zrows:
                r0 = zrows.pop(0)
                S.dma("sp", lambda e, r0=r0: e.dma_start(out=K.xs[r0:r0 + 128, :], in_=zt), reads=[Bzt], owner=Bzt)
    scale = 128.0 ** -0.5
    cnt = 0
    for s in range(NSEQ):
        S.dma("sp", lambda e, s=s: e.dma_start(out=kT, in_=K.kT_s[s].rearrange("h p t -> p h t")), writes=[BkT])
        S.dma("sp", lambda e, s=s: e.dma_start(out=V, in_=K.V_s[s]), writes=[BV])
        for qb in range(4):
            q, Bq = qT[cnt % 2]
            yl, Byl = ylb[cnt % 2]
            ga, Bga = gab[cnt % 2]
            gr, Bgr = grb[cnt % 2]
            cnt += 1
            tsl = slice(qb * 512, (qb + 1) * 512)
            S.dma("sp", lambda e, q=q, s=s, tsl=tsl: e.dma_start(out=q, in_=K.qT_s[s].rearrange("h p t -> p h t")[:, :, tsl]), writes=[Bq])
            S.dma("sp", lambda e, yl=yl, s=s, tsl=tsl: e.dma_start(out=yl, in_=K.yl_s[s].rearrange("h p t -> p h t")[:, :, tsl]), writes=[Byl])
            S.dma("sp", lambda e, ga=ga, s=s, tsl=tsl: e.dma_start(out=ga, in_=K.sga_s[s].rearrange("h p t -> p h t")[:, :, tsl]), writes=[Bga])
            S.dma("sp", lambda e, gr=gr, s=s, tsl=tsl: e.dma_start(out=gr, in_=K.sgr_s[s].rearrange("h p t -> p h t")[:, :, tsl]), writes=[Bgr])
            for h in range(8):
                kv = h // 4
                ob, zb = 3 + h % 2, 5 + h % 2

                def score(kc):
                    bank = SBANK[kc % 4]
                    mm_group(S, ps[bank][:], Bps[bank], [(kT[:, kv, kc * 128:(kc + 1) * 128], q[:, h, :])], reads=[BkT, Bq])
                    pt, Bpt = PT[kc % 6]
                    S.op("act", lambda e, bank=bank, pt=pt: e.activation(out=pt, in_=ps[bank][:], func=AF.Exp, scale=scale),
                         reads=[Bps[bank]], writes=[Bpt])

                za, Bza = zacc[h % 2]
                zb16, Bzb16 = zab[h % 2]

                def pv(kc):
                    pt, Bpt = PT[kc % 6]
                    S.op("pe", lambda e, kc=kc, pt=pt, ob=ob, kv=kv: e.matmul(ps[ob][:], V[:, kc, kv * 128:(kv + 1) * 128], pt, start=(kc == 0), stop=(kc == 15)),
                         reads=[BV, Bpt], writes=[Bps[ob]], signal=(kc == 15))
                    if kc % 2 == 1:
                        S.op("pe", lambda e, kc=kc, pt=pt, zb=zb: e.matmul(ps[zb][:], K.ones_b, pt, start=(kc == 1), stop=False),
                             reads=[K.Bones_b, Bpt], writes=[Bps[zb]], signal=False)
                    elif kc == 0:
                        S.op("dve", lambda e, pt=pt, za=za: e.tensor_copy(out=za, in_=pt), reads=[Bpt], writes=[Bza])
                    else:
                        S.op("dve", lambda e, pt=pt, za=za: e.tensor_tensor(out=za, in0=za, in1=pt, op=ALU.add), reads=[Bpt, Bza], writes=[Bza])
                    if kc == 15:
                        S.op("pe", lambda e, za=za, zb=zb: e.matmul(ps[zb][:], K.ones_f, za, start=False, stop=True),
                             reads=[K.Bones_f, Bza], writes=[Bps[zb]], signal=True)

                score(0)
                score(1)
                score(2)
                for kc in range(16):
                    if kc + 3 < 16:
                        score(kc + 3)
                    pv(kc)
                S.op("dve", lambda e, zb=zb: e.reciprocal(out=rz, in_=ps[zb][:]), reads=[Bps[zb]], writes=[Brz])
                S.op("dve", lambda e, ob=ob, h=h: e.tensor_tensor(out=attnT[:, h, :], in0=ps[ob][:], in1=rz, op=ALU.mult),
                     reads=[Bps[ob], Brz], writes=[BattnT])
                zero_some(6)
            for m in range(8):
                b1, b2 = 0 + m % 2, 2 + m % 2
                mm_group(S, ps[b1][:], Bps[b1], [(wab[:, k, m * 128:(m + 1) * 128], attnT[:, k, :]) for k in range(8)], reads=[Bwab, BattnT])
                mm_group(S, ps[b2][:], Bps[b2], [(wlb[:, k, m * 128:(m + 1) * 128], yl[:, k, :]) for k in range(8)], reads=[Bwlb, Byl])
                S.op("dve", lambda e, b1=b1, m=m, ga=ga: e.tensor_tensor(out=m1, in0=ps[b1][:], in1=ga[:, m, :], op=ALU.mult),
                     reads=[Bps[b1], Bga], writes=[Bm1])
                S.op("dve", lambda e, b2=b2, m=m, gr=gr: e.tensor_tensor(out=m2, in0=ps[b2][:], in1=gr[:, m, :], op=ALU.mult),
                     reads=[Bps[b2], Bgr], writes=[Bm2])
                S.op("dve", lambda e, m=m: e.tensor_tensor(out=mrg[:, m, :], in0=m1, in1=m2, op=ALU.add),
                     reads=[Bm1, Bm2], writes=[Bmrg])
            for tk in range(4):
                x_t, Bx = xt[tk % 2]
                h_t, Bh = ho[tk % 2]
                r0 = s * SEQ + qb * 512 + tk * 128
                S.dma("sp", lambda e, x_t=x_t, r0=r0: e.dma_start(out=x_t, in_=K.x[r0:r0 + 128, :]), writes=[Bx])
                for nh in range(2):
                    bank = 5 + nh
                    mm_group(S, ps[bank][:], Bps[bank],
                             [(mrg[:, k, tk * 128:(tk + 1) * 128], wo[:, k, nh * 512:(nh + 1) * 512]) for k in range(8)], reads=[Bmrg, Bwo])
                    S.op("dve", lambda e, bank=bank, nh=nh, x_t=x_t, h_t=h_t: e.tensor_tensor(
                        out=h_t[:, nh * 512:(nh + 1) * 512], in0=ps[bank][:], in1=x_t[:, nh * 512:(nh + 1) * 512], op=ALU.add),
                        reads=[Bps[bank], Bx], writes=[Bh])
                S.dma("sp", lambda e, h_t=h_t, r0=r0: e.dma_start(out=K.h1_s[r0:r0 + 128, :], in_=h_t), reads=[Bh], owner=Bh)
    zero_some(len(zrows))


def phase3_router(K):
    S, A, nc = K.S, K.A, K.nc
    ps, Bps = K.ps, K.Bps
    gmoe, Bgmoe = A.alloc("gmoe", [D])
    wr, Bwr = A.alloc("wr", [8, E])
    brt, Bbr = A.alloc("brt", [E])
    tri, Btri = A.alloc("tri", [128])
    eC, BeC = A.alloc("eC", [E])
    msum, Bmsum = A.alloc("msum", [E])
    S.dma("sp", lambda e: e.dma_start(out=gmoe, in_=bcast_row(K.g_moe, D)), writes=[Bgmoe])
    S.dma("sp", lambda e: e.dma_start(out=wr, in_=K.w_router.rearrange("(c p) n -> p c n", p=128)), writes=[Bwr])
    S.dma("sp", lambda e: e.dma_start(out=brt, in_=bcast_row(K.b_router, E)), writes=[Bbr])
    S.dma("sp", lambda e: e.dma_start(out=tri, in_=K.tri), writes=[Btri])
    S.dma("sp", lambda e: e.dma_start(out=eC, in_=K.eC), writes=[BeC])
    S.op("dve", lambda e: e.memset(msum, 0.0), writes=[Bmsum])

    def tile_stream(par):
        h_t, Bh = A.alloc(f"ht{par}", [D])
        junk, Bjunk = A.alloc(f"junk{par}", [D], BF16)
        ss_t, Bss = A.alloc(f"ss{par}", [1])
        u2, Bu2 = A.alloc(f"u2{par}", [D])
        ub, Bub = A.alloc(f"u2b{par}", [D], BF16)
        u2T, Bu2T = A.alloc(f"u2T{par}", [8, 128])
        lg, Blg = A.alloc(f"lg{par}", [E])
        top8, Btop8 = A.alloc(f"top8{par}", [8])
        nm, Bnm = A.alloc(f"nm{par}", [1])
        mask, Bmask = A.alloc(f"mask{par}", [E])
        ex, Bex = A.alloc(f"ex{par}", [E])
        den, Bden = A.alloc(f"den{par}", [1])
        gf, Bgf = A.alloc(f"gf{par}", [E])
        rank, Brank = A.alloc(f"rank{par}", [E])
        okm, Bok = A.alloc(f"okm{par}", [E])
        dst, Bdst = A.alloc(f"dst{par}", [E])
        dk, Bdk = A.alloc(f"dk{par}", [4])
        oh, Boh = A.alloc(f"oh{par}", [E])
        b0 = par * 2

        def gen():
            for i in range(par, NTILE, 4):
                r0 = i * 128
                S.dma("sp", lambda e, r0=r0: e.dma_start(out=h_t, in_=K.h1_s[r0:r0 + 128, :]), writes=[Bh])
                rms_rstd(K, h_t, Bh, junk, Bjunk, ss_t, Bss, D)
                S.op("dve", lambda e: e.scalar_tensor_tensor(out=u2, in0=h_t, scalar=ss_t, in1=gmoe, op0=ALU.mult, op1=ALU.mult),
                     reads=[Bh, Bss, Bgmoe], writes=[Bu2])
                yield
                S.op("act", lambda e: e.activation(out=ub, in_=u2, func=AF.Copy), reads=[Bu2], writes=[Bub])
                for hc in range(2):
                    bank = b0
                    for c4 in range(4):
                        c = hc * 4 + c4
                        S.op("pe", lambda e, c=c, c4=c4, bank=bank: e.transpose(ps[bank][:, c4 * 128:(c4 + 1) * 128], u2[:, c * 128:(c + 1) * 128], K.ident_f),
                             reads=[Bu2, K.Bident_f], writes=[Bps[bank]], signal=(c4 == 3))
                    S.op("act", lambda e, bank=bank, hc=hc: e.activation(out=u2T[:, hc * 4:(hc + 1) * 4, :], in_=ps[bank][:].rearrange("p (c t) -> p c t", c=4), func=AF.Copy),
                         reads=[Bps[bank]], writes=[Bu2T])
                yield
                lb, rb = b0 + 1, b0 + 1
                mm_group(S, ps[lb][:, 0:E], Bps[lb], [(u2T[:, k, :], wr[:, k, :]) for k in range(8)], reads=[Bu2T, Bwr])
                S.op("dve", lambda e, lb=lb: e.tensor_tensor(out=lg, in0=ps[lb][:, 0:E], in1=brt, op=ALU.add), reads=[Bps[lb], Bbr], writes=[Blg])
                S.op("dve", lambda e: e.max(out=top8, in_=lg), reads=[Blg], writes=[Btop8])
                S.op("dve", lambda e: e.tensor_scalar(out=mask, in0=lg, scalar1=top8[:, 3:4], scalar2=None, op0=ALU.is_ge),
                     reads=[Blg, Btop8], writes=[Bmask])
                yield
                mm_group(S, ps[rb][:, 64:64 + E], Bps[rb], [(tri, mask), (K.ones_f, msum)], reads=[Btri, Bmask, K.Bones_f, Bmsum])
                S.op("dve", lambda e: e.tensor_tensor(out=msum, in0=msum, in1=mask, op=ALU.add), reads=[Bmsum, Bmask], writes=[Bmsum])
                yield
                S.op("dve", lambda e: e.tensor_scalar(out=nm, in0=top8[:, 0:1], scalar1=-1.0, scalar2=None, op0=ALU.mult),
                     reads=[Btop8], writes=[Bnm])
                S.op("act", lambda e: e.activation(out=ex, in_=lg, func=AF.Exp, bias=nm), reads=[Blg, Bnm], writes=[Bex])
                S.op("dve", lambda e, rb=rb: e.tensor_copy(out=rank, in_=ps[rb][:, 64:64 + E]), reads=[Bps[rb]], writes=[Brank])
                S.op("dve", lambda e: e.tensor_scalar(out=okm, in0=rank, scalar1=float(CAP), scalar2=None, op0=ALU.is_lt),
                     reads=[Brank], writes=[Bok])
                S.op("dve", lambda e: e.tensor_tensor(out=dst, in0=rank, in1=eC, op=ALU.add), reads=[Brank, BeC], writes=[Bdst])
                S.op("dve", lambda e: e.scalar_tensor_tensor(out=dst, in0=dst, scalar=float(-TRASH), in1=okm, op0=ALU.add, op1=ALU.mult),
                     reads=[Bdst, Bok], writes=[Bdst])
                S.op("dve", lambda e: e.tensor_scalar(out=dst, in0=dst, scalar1=float(TRASH), scalar2=None, op0=ALU.add),
                     reads=[Bdst], writes=[Bdst])
                yield
                S.op("dve", lambda e: e.tensor_tensor(out=ex, in0=ex, in1=mask, op=ALU.mult), reads=[Bex, Bmask], writes=[Bex])
                S.op("dve", lambda e: e.reduce_sum(out=den, in_=ex, axis=AX.X), reads=[Bex], writes=[Bden])
                S.op("dve", lambda e: e.reciprocal(out=den, in_=den), reads=[Bden], writes=[Bden])
                S.op("dve", lambda e: e.scalar_tensor_tensor(out=gf, in0=ex, scalar=den, in1=okm, op0=ALU.mult, op1=ALU.mult),
                     reads=[Bex, Bden, Bok], writes=[Bgf])
                yield
                for k in range(4):
                    S.op("dve", lambda e, k=k: e.scalar_tensor_tensor(out=oh, in0=lg, scalar=top8[:, k:k + 1], in1=dst, op0=ALU.is_equal, op1=ALU.mult,
                                                                      accum_out=dk[:, k:k + 1]),
                         reads=[Blg, Btop8, Bdst], writes=[Boh, Bdk])
                    S.op("dve", lambda e, k=k, i=i: e.scalar_tensor_tensor(out=oh, in0=lg, scalar=top8[:, k:k + 1], in1=gf, op0=ALU.is_equal, op1=ALU.mult,
                                                                           accum_out=K.gate_a[:, i * 4 + k:i * 4 + k + 1]),
                         reads=[Blg, Btop8, Bgf], writes=[Boh, K.Bgate])
                S.op("dve", lambda e, i=i: e.tensor_copy(out=K.dest_i[:, i * 4:(i + 1) * 4], in_=dk), reads=[Bdk], writes=[K.Bdest])
                for k in range(4):
                    S.dma("pool", lambda e, k=k, i=i: e.indirect_dma_start(
                        out=K.xs, out_offset=bass.IndirectOffsetOnAxis(ap=K.dest_i[:, i * 4 + k:i * 4 + k + 1], axis=0),
                        in_=ub, in_offset=None), reads=[Bub, K.Bdest], owner=Bub)
                yield
        return gen()

    interleave([tile_stream(q) for q in range(4)], skew=2)


def issue_expert_load(K, e_, w, Bw, wd, Bwd, bd, Bbd):
    S = K.S
    src = K.w_gu[e_].rearrange("(c p) n -> p c n", p=128)
    for hh in range(2):
        S.dma("pool", lambda e, w=w, src=src, hh=hh: e.dma_start(out=w[:, hh * 4:(hh + 1) * 4, :], in_=src[:, hh * 4:(hh + 1) * 4, :]), writes=[Bw])
    S.dma("pool", lambda e, wd=wd, e_=e_: e.dma_start(out=wd, in_=K.w_dn[e_].rearrange("(c p) n -> p c n", p=128)), writes=[Bwd])
    S.dma("pool", lambda e, bd=bd, e_=e_: e.dma_start(out=bd[0:1, :], in_=K.b_dn[e_:e_ + 1, :]), writes=[Bbd])


def phase4_experts(K):
    S, A, nc = K.S, K.A, K.nc
    ps, Bps = K.ps, K.Bps
    wgu = [K.pre4["wgu"], A.alloc("wgu1", [8, 2 * F], BF16)]
    wdn = [K.pre4["wdn"], A.alloc("wdn1", [8, D], BF16)]
    bdn = [K.pre4["bdn"], A.alloc("bdn1", [D], BF16)]
    xTs = [A.alloc(f"xT{i}", [8, CAP], BF16) for i in range(2)]
    resTs = [A.alloc(f"resT{i}", [8, CAP], BF16) for i in range(2)]
    xst = [A.alloc(f"xst{i}", [D], BF16) for i in range(3)]
    yst = [A.alloc(f"yst{i}", [D]) for i in range(2)]
    HW = CAP // 2
    gt = [A.alloc(f"gt{i}", [HW]) for i in range(2)]
    sg = [A.alloc(f"sg{i}", [HW]) for i in range(2)]
    ut = [A.alloc(f"ut{i}", [HW]) for i in range(2)]
    cnts = {"y": 0, "x": 0, "u": 0}
    S.op("dve", lambda e: e.memset(yst[1][0], 0.0), writes=[yst[1][1]])
    S.dma("sp", lambda e: e.dma_start(out=K.ys[NSLOT:NSLOT + 128, :], in_=yst[1][0]), reads=[yst[1][1]], owner=yst[1][1])

    def load_expert(e_):
        issue_expert_load(K, e_, *wgu[e_ % 2], *wdn[e_ % 2], *bdn[e_ % 2])

    def build_xT(e_):
        xT, BxT = xTs[e_ % 2]
        for sb in range(NSB):
            xs_t, Bxs = xst[cnts["x"] % 3]
            cnts["x"] += 1
            r0 = e_ * CAP + sb * 128
            S.dma("sp", lambda e, xs_t=xs_t, r0=r0: e.dma_start(out=xs_t, in_=K.xs[r0:r0 + 128, :]), writes=[Bxs])
            bank = sb % 2
            pv16 = ps[bank][:].bitcast(BF16)
            for c in range(8):
                S.op("pe", lambda e, c=c, pv16=pv16, xs_t=xs_t: e.transpose(pv16[:, c * 128:(c + 1) * 128], xs_t[:, c * 128:(c + 1) * 128], K.ident_b),
                     reads=[Bxs, K.Bident_b], writes=[Bps[bank]], signal=(c == 7))
            S.op("act", lambda e, pv16=pv16, sb=sb, xT=xT: e.activation(out=xT[:, :, sb * 128:(sb + 1) * 128], in_=pv16.rearrange("p (c t) -> p c t", c=8), func=AF.Copy),
                 reads=[Bps[bank]], writes=[BxT])

    def gate_up(e_):
        w, Bw = wgu[e_ % 2]
        xT, BxT = xTs[e_ % 2]
        resT, BresT = resTs[e_ % 2]
        for f in range(8):
            bgc = K.pvt[:, PV_BGU + e_ * 16 + f:PV_BGU + e_ * 16 + f + 1]
            buc = K.pvt[:, PV_BGU + e_ * 16 + 8 + f:PV_BGU + e_ * 16 + 8 + f + 1]
            for hv in range(2):
                nsl = slice(hv * HW, (hv + 1) * HW)
                cnt = cnts["u"]
                cnts["u"] += 1
                gb, ub_ = 2 + cnt % 2, 4 + cnt % 2
                g_t, Bg = gt[cnt % 2]
                s_t, Bs = sg[cnt % 2]
                u_t, Bu = ut[cnt % 2]
                mm_group(S, ps[gb][:, 0:HW], Bps[gb], [(w[:, k, f * 128:(f + 1) * 128], xT[:, k, nsl]) for k in range(8)], reads=[Bw, BxT])
                mm_group(S, ps[ub_][:, 0:HW], Bps[ub_], [(w[:, k, F + f * 128:F + (f + 1) * 128], xT[:, k, nsl]) for k in range(8)], reads=[Bw, BxT])
                S.op("dve", lambda e, gb=gb, g_t=g_t, bgc=bgc: e.tensor_scalar(out=g_t, in0=ps[gb][:, 0:HW], scalar1=bgc, scalar2=7.0, op0=ALU.add, op1=ALU.min),
                     reads=[Bps[gb], K.Bpv], writes=[Bg])
                S.op("act", lambda e, ub_=ub_, u_t=u_t, buc=buc: e.activation(out=u_t, in_=ps[ub_][:, 0:HW], func=AF.Identity, bias=buc),
                     reads=[Bps[ub_], K.Bpv], writes=[Bu])
                S.op("act", lambda e, g_t=g_t, s_t=s_t: e.activation(out=s_t, in_=g_t, func=AF.Sigmoid, scale=1.702), reads=[Bg], writes=[Bs])
                S.op("dve", lambda e, u_t=u_t: e.tensor_scalar(out=u_t, in0=u_t, scalar1=7.0, scalar2=-7.0, op0=ALU.min, op1=ALU.max),
                     reads=[Bu], writes=[Bu])
                S.op("dve", lambda e, g_t=g_t, s_t=s_t: e.tensor_tensor(out=g_t, in0=g_t, in1=s_t, op=ALU.mult), reads=[Bg, Bs], writes=[Bg])
                S.op("dve", lambda e, g_t=g_t, u_t=u_t, f=f, nsl=nsl, resT=resT: e.scalar_tensor_tensor(
                    out=resT[:, f, nsl], in0=u_t, scalar=1.0, in1=g_t, op0=ALU.add, op1=ALU.mult),
                    reads=[Bg, Bu], writes=[BresT])

    def down(e_):
        wd, Bwd = wdn[e_ % 2]
        bd, Bbd = bdn[e_ % 2]
        resT, BresT = resTs[e_ % 2]
        for sb in range(NSB):
            y_t, By = yst[cnts["y"] % 2]
            cnts["y"] += 1
            for nh in range(2):
                bank = 6 + nh
                pairs = [(resT[:, k, sb * 128:(sb + 1) * 128], wd[:, k, nh * 512:(nh + 1) * 512]) for k in range(8)]
                pairs.append((K.ones_b[0:1, :], bd[0:1, nh * 512:(nh + 1) * 512]))
                mm_group(S, ps[bank][:], Bps[bank], pairs, reads=[BresT, Bwd, Bbd, K.Bones_b])
                S.op("act", lambda e, bank=bank, y_t=y_t, nh=nh: e.activation(out=y_t[:, nh * 512:(nh + 1) * 512], in_=ps[bank][:], func=AF.Copy),
                     reads=[Bps[bank]], writes=[By])
            r0 = e_ * CAP + sb * 128
            S.dma("act", lambda e, y_t=y_t, r0=r0: e.dma_start(out=K.ys[r0:r0 + 128, :], in_=y_t), reads=[By], owner=By)

    build_xT(0)
    for e_ in range(E):
        if e_ + 1 < E:
            load_expert(e_ + 1)
        if e_ == 0:
            wpg, Bwpg = K.pre5["wpg"]
            wpp, Bwpp = K.pre5["wpp"]
            gple, Bgple = K.pre5["gple"]
            S.dma("pool", lambda e: e.dma_start(out=wpg, in_=K.w_ple_gate.rearrange("(c p) n -> p c n", p=128)), writes=[Bwpg])
            S.dma("pool", lambda e: e.dma_start(out=wpp, in_=K.w_ple_proj.rearrange("(c p) n -> p c n", p=128)), writes=[Bwpp])
            S.dma("sp", lambda e: e.dma_start(out=gple, in_=bcast_row(K.g_ple, D)), writes=[Bgple])
        gate_up(e_)
        if e_ + 1 < E:
            build_xT(e_ + 1)
        down(e_)


def phase5_combine(K):
    S, A, nc = K.S, K.A, K.nc
    ps, Bps = K.ps, K.Bps
    wpg, Bwpg = K.pre5["wpg"]
    wpp, Bwpp = K.pre5["wpp"]
    gple, Bgple = K.pre5["gple"]

    def tile_stream(par):
        h_t, Bh = A.alloc(f"ht{par}", [D])
        yg = [A.alloc(f"yg{par}_{k}", [D]) for k in range(4)]
        p_t, Bp = A.alloc(f"pt{par}", [PLE])
        ptb, Bptb = A.alloc(f"ptb{par}", [PLE], BF16)
        pT, BpT = A.alloc(f"pT{par}", [2, 128], BF16)
        junk, Bjunk = A.alloc(f"junk{par}", [D], BF16)
        ss_t, Bss = A.alloc(f"ss{par}", [1])
        u3, Bu3 = A.alloc(f"u3{par}", [D], BF16)
        u3T, Bu3T = A.alloc(f"u3T{par}", [8, 128], BF16)
        sgm, Bsgm = A.alloc(f"sgm{par}", [D])
        o_t, Bo = A.alloc(f"ot{par}", [D])
        b0 = par * 2

        h_ts = [(h_t, Bh), A.alloc(f"ht{par}b", [D])]
        p_ts = [(p_t, Bp), A.alloc(f"pt{par}b", [PLE])]
        tiles = list(range(par, NTILE, 4))

        def issue_loads(n):
            hh, Bhh = h_ts[n % 2]
            pq, Bpq = p_ts[n % 2]
            r0 = tiles[n] * 128
            S.dma("sp", lambda e, r0=r0, hh=hh: e.dma_start(out=hh, in_=K.h1_s[r0:r0 + 128, :]), writes=[Bhh], owner=h_ts[0][1])
            S.dma("sp", lambda e, r0=r0, pq=pq: e.dma_start(out=pq, in_=K.p[r0:r0 + 128, :]), writes=[Bpq], owner=p_ts[0][1])

        def tile_body(n, i, h_t, Bh, p_t, Bp):
            r0 = i * 128
            if n + 1 < len(tiles):
                issue_loads(n + 1)
            for k in range(4):
                y_t, By = yg[k]
                S.dma("pool", lambda e, y_t=y_t, i=i, k=k: e.indirect_dma_start(
                    out=y_t, out_offset=None, in_=K.ys,
                    in_offset=bass.IndirectOffsetOnAxis(ap=K.dest_i[:, i * 4 + k:i * 4 + k + 1], axis=0)),
                    reads=[K.Bdest], writes=[By])
            yield
            S.op("act", lambda e: e.activation(out=ptb, in_=p_t, func=AF.Copy), reads=[Bp], writes=[Bptb])
            pv1 = ps[b0 + 1][:].bitcast(BF16)
            for c in range(2):
                S.op("pe", lambda e, c=c, pv1=pv1: e.transpose(pv1[:, c * 128:(c + 1) * 128], ptb[:, c * 128:(c + 1) * 128], K.ident_b),
                     reads=[Bptb, K.Bident_b], writes=[Bps[b0 + 1]], signal=(c == 1))
            S.op("act", lambda e, pv1=pv1: e.activation(out=pT, in_=pv1[:, 0:256].rearrange("p (c t) -> p c t", c=2), func=AF.Copy),
                 reads=[Bps[b0 + 1]], writes=[BpT])
            yield
            for k in range(4):
                y_t, By = yg[k]
                S.op("dve", lambda e, y_t=y_t, i=i, k=k: e.scalar_tensor_tensor(
                    out=h_t, in0=y_t, scalar=K.gate_a[:, i * 4 + k:i * 4 + k + 1], in1=h_t, op0=ALU.mult, op1=ALU.add),
                    reads=[By, Bh, K.Bgate], writes=[Bh])
            yield
            rms_rstd(K, h_t, Bh, junk, Bjunk, ss_t, Bss, D)
            S.op("dve", lambda e: e.scalar_tensor_tensor(out=u3, in0=h_t, scalar=ss_t, in1=gple, op0=ALU.mult, op1=ALU.mult),
                 reads=[Bh, Bss, Bgple], writes=[Bu3])
            yield
            pv16 = ps[b0][:].bitcast(BF16)
            for c in range(8):
                S.op("pe", lambda e, c=c, pv16=pv16: e.transpose(pv16[:, c * 128:(c + 1) * 128], u3[:, c * 128:(c + 1) * 128], K.ident_b),
                     reads=[Bu3, K.Bident_b], writes=[Bps[b0]], signal=(c == 7))
            S.op("act", lambda e, pv16=pv16: e.activation(out=u3T, in_=pv16.rearrange("p (c t) -> p c t", c=8), func=AF.Copy),
                 reads=[Bps[b0]], writes=[Bu3T])
            yield
            for nh in range(2):
                nsl = slice(nh * 512, (nh + 1) * 512)
                gbk, pbk = b0, b0 + 1
                mm_group(S, ps[gbk][:], Bps[gbk], [(u3T[:, k, :], wpg[:, k, nsl]) for k in range(8)], reads=[Bu3T, Bwpg])
                mm_group(S, ps[pbk][:], Bps[pbk], [(pT[:, k, :], wpp[:, k, nsl]) for k in range(2)], reads=[BpT, Bwpp])
                S.op("act", lambda e, gbk=gbk, nsl=nsl: e.activation(out=sgm[:, nsl], in_=ps[gbk][:], func=AF.Sigmoid), reads=[Bps[gbk]], writes=[Bsgm])
                S.op("dve", lambda e, pbk=pbk, nsl=nsl: e.tensor_tensor(out=sgm[:, nsl], in0=sgm[:, nsl], in1=ps[pbk][:], op=ALU.mult),
                     reads=[Bsgm, Bps[pbk]], writes=[Bsgm])
                S.op("dve", lambda e, nsl=nsl: e.tensor_tensor(out=o_t[:, nsl], in0=sgm[:, nsl], in1=h_t[:, nsl], op=ALU.add),
                     reads=[Bsgm, Bh], writes=[Bo])
                yield
            S.dma("sp", lambda e, r0=r0: e.dma_start(out=K.y[r0:r0 + 128, :], in_=o_t), reads=[Bo], owner=Bo)

        def gen():
            issue_loads(0)
            for n, i in enumerate(tiles):
                yield from tile_body(n, i, *h_ts[n % 2], *p_ts[n % 2])

        return gen()

    interleave([tile_stream(q) for q in range(4)], skew=2)


def _rope_tables():
    pos = np.arange(SEQ)
    row = (pos // 64).astype(np.float32)
    col = (pos % 64).astype(np.float32)
    inv = (10000.0 ** (-np.arange(0, 64, 2, dtype=np.float32) / 64.0)).astype(np.float32)
    C = np.zeros((128, SEQ), np.float32)
    Sg = np.zeros((128, SEQ), np.float32)
    for p in range(128):
        ids = row if p < 64 else col
        j = p % 32
        ang = (ids * inv[j]).astype(np.float32)
        C[p] = np.cos(ang)
        sgn = -1.0 if (p % 64) < 32 else 1.0
        Sg[p] = sgn * np.sin(ang)
    perm = np.zeros((128, 128), np.float32)
    for m in range(128):
        partner = m + 32 if (m % 64) < 32 else m - 32
        perm[partner, m] = 1.0
    return C, Sg, perm


_NC_CACHE = {}


def _prep_common(inp):
    f = lambda a: np.ascontiguousarray(np.asarray(a, dtype=np.float32))
    pv = np.zeros((128, NPV), np.float32)
    cw = f(inp["conv_w"])[0]
    pv[:, PV_CONVW:PV_CONVW + 32] = cw.reshape(4, 8, 128).transpose(2, 1, 0).reshape(128, 32)
    pv[:, PV_CONVB:PV_CONVB + 8] = f(inp["conv_b"])[0].reshape(8, 128).T
    pv[:, PV_BA:PV_BA + 16] = f(inp["lru_ba"])[0].reshape(16, 128).T
    pv[:, PV_BI:PV_BI + 16] = f(inp["lru_bi"])[0].reshape(16, 128).T
    pv[:, PV_LAM:PV_LAM + 16] = f(inp["lru_lam"])[0].reshape(16, 128).T
    pv[:, PV_QN] = f(inp["q_norm"])[0]
    pv[:, PV_KN] = f(inp["k_norm"])[0]
    pv[:, PV_BGU:PV_BGU + 512] = f(inp["b_gu"])[0].reshape(E * 16, 128).T
    C, Sg, perm = _rope_tables()
    tri = np.triu(np.ones((128, 128), np.float32), 1)
    eC = np.tile((np.arange(E, dtype=np.float32) * CAP)[None, :], (128, 1))
    com = {
        "w_in": f(inp["w_in"])[0], "lru_wa": f(inp["lru_wa"])[0], "lru_wi": f(inp["lru_wi"])[0],
        "w_attn_br": f(inp["w_attn_br"])[0], "w_lru_br": f(inp["w_lru_br"])[0], "w_out": f(inp["w_out"])[0],
        "w_router": f(inp["w_router"])[0], "w_gu": f(inp["w_gu"])[0], "w_dn": f(inp["w_dn"])[0],
        "b_dn": f(inp["b_dn"])[0], "w_ple_gate": f(inp["w_ple_gate"])[0], "w_ple_proj": f(inp["w_ple_proj"])[0],
        "g_mix": f(inp["g_mix"]), "g_moe": f(inp["g_moe"]), "g_ple": f(inp["g_ple"]), "b_router": f(inp["b_router"]),
        "pv": pv, "ropeC": C, "ropeS": Sg, "perm": perm, "ident": np.eye(128, dtype=np.float32), "tri": tri, "eC": eC,
    }
    return com


def kernel(**inputs):
    dbg = bool(int(os.environ.get("MK_DBG", "0")))
    ncores = int(os.environ.get("MK_NCORES", str(NCORES)))
    key = dbg
    if key not in _NC_CACHE:
        _NC_CACHE[key] = build_program(dbg)
    nc = _NC_CACHE[key]
    com = _prep_common(inputs)
    x = np.asarray(inputs["x"], dtype=np.float32)
    p = np.asarray(inputs["p"], dtype=np.float32)[0]
    in_maps = []
    for c in range(ncores):
        m = dict(com)
        m["x"] = np.ascontiguousarray(x[2 * c:2 * c + 2].reshape(T, D))
        m["p"] = np.ascontiguousarray(p[2 * c:2 * c + 2].reshape(T, PLE))
        in_maps.append(m)
    res = run_bass_kernel_spmd(nc, in_maps, core_ids=list(range(ncores)))
    if dbg:
        kernel.last = res
    out = np.zeros((16, SEQ, D), np.float32)
    for c in range(ncores):
        out[2 * c:2 * c + 2] = np.asarray(res.results[c]["y"], dtype=np.float32).reshape(2, SEQ, D)
    return out
```

```python
import os
import numpy as np
from contextlib import ExitStack
import concourse.bass as bass
import concourse.mybir as mybir
from concourse.bass_utils import run_bass_kernel_spmd

F32 = mybir.dt.float32
BF16 = mybir.dt.bfloat16
I32 = mybir.dt.int32
AF = mybir.ActivationFunctionType
ALU = mybir.AluOpType
AX = mybir.AxisListType

NCORES = 8
D = 1024
SEQ = 2048
NSEQ = 2
T = NSEQ * SEQ
NTILE = T // 128
E = 32
F = 1024
CAP = 640
NSB = CAP // 128
NSLOT = E * CAP
TRASH = NSLOT
PLE = 256
EPS = 1e-6
INW = 5632
NPV = 608
PV_CONVW, PV_CONVB, PV_BA, PV_BI, PV_LAM, PV_QN, PV_KN, PV_BGU = 0, 32, 40, 56, 72, 88, 89, 96

ENGS = ("pe", "act", "dve", "pool", "sp")


class Buf:
    __slots__ = ("name", "last_write", "reads", "sem", "sem_total", "excl")

    def __init__(self, name, excl=False):
        self.name = name
        self.excl = excl
        self.last_write = None
        self.reads = []
        self.sem = None
        self.sem_total = 0


class Sched:
    def __init__(self, nc, stack):
        self.nc = nc
        self.stack = stack
        self.stream = {e: [] for e in ENGS}
        self.sem = {e: stack.enter_context(nc.semaphore("s_" + e)) for e in ENGS}
        self.count = {e: 0 for e in ENGS}
        self.seen = {e: {} for e in ENGS}
        self.dma_bufs = []

    def _wait_tokens(self, e, toks):
        need = {}
        for t in toks:
            if t is None:
                continue
            if t[0] == "e":
                _, src, c = t
                if src == "pe" and e == "pe":
                    continue
                key = ("e", src)
                val = c
                sem = self.sem[src]
            else:
                b = t[1]
                key = ("d", id(b))
                val = b.sem_total
                sem = b.sem
            if self.seen[e].get(key, 0) >= val:
                continue
            if key not in need or need[key][1] < val:
                need[key] = (sem, val)
        for key, (sem, val) in need.items():
            self.seen[e][key] = val
            self.stream[e].append(lambda eng, sem=sem, val=val: eng.wait_ge(sem, val))

    @staticmethod
    def _deps(reads, writes):
        toks = []
        for r in reads:
            toks.append(r.last_write)
            if r.excl:
                toks.extend(r.reads)
        for w in writes:
            toks.append(w.last_write)
            toks.extend(w.reads)
        return toks

    def op(self, e, fn, reads=(), writes=(), signal=True):
        self._wait_tokens(e, self._deps(reads, writes))
        if signal:
            self.count[e] += 1
            tok = ("e", e, self.count[e])
            sem = self.sem[e]
            self.stream[e].append(lambda eng, fn=fn, sem=sem: fn(eng).then_inc(sem, 1))
        else:
            tok = ("e", e, self.count[e] + 1)
            self.stream[e].append(lambda eng, fn=fn: fn(eng))
        for w in writes:
            w.last_write = tok
            w.reads = []
        for r in reads:
            r.reads.append(tok)
        return tok

    def dma(self, e, fn, reads=(), writes=(), owner=None):
        if owner is None:
            owner = writes[0] if writes else reads[0]
        if owner.sem is None:
            owner.sem = self.stack.enter_context(self.nc.semaphore("d%d_%s" % (len(self.dma_bufs), owner.name)))
            self.dma_bufs.append(owner)
        self._wait_tokens(e, self._deps(reads, writes))
        owner.sem_total += 16
        sem = owner.sem
        self.stream[e].append(lambda eng, fn=fn, sem=sem: fn(eng).then_inc(sem, 16))
        tok = ("d", owner)
        for w in writes:
            w.last_write = tok
            w.reads = []
        for r in reads:
            r.reads.append(tok)
        return tok

    def barrier(self):
        toks = [("e", s, self.count[s]) for s in ENGS if self.count[s] > 0]
        toks += [("d", b) for b in self.dma_bufs]
        for e in ENGS:
            self._wait_tokens(e, toks)

    def emit(self):
        nc = self.nc
        self.barrier()
        with nc.Block() as block:
            for e, reg in (("sp", block.sync), ("act", block.scalar), ("pe", block.tensor),
                           ("dve", block.vector), ("pool", block.gpsimd)):
                lst = self.stream[e]
                if not lst:
                    continue

                def body(eng, lst=lst):
                    for f in lst:
                        f(eng)
                reg(body)


def _dsize(dt):
    return 2 if dt == BF16 else 4


class Arena:
    def __init__(self, nc, stack, nbytes):
        self.t = stack.enter_context(nc.sbuf_tensor("arena", [128, nbytes // 4], F32))
        self.off = 0
        self.nbytes = nbytes
        self.peak = 0

    def alloc(self, name, free, dt=F32):
        n = 1
        for f in free:
            n *= f
        sz = (n * _dsize(dt) + 31) // 32 * 32
        assert self.off + sz <= self.nbytes, (name, self.off, sz, self.nbytes)
        a = self.t[:, self.off // 4:(self.off + sz) // 4]
        if dt != F32:
            a = a.bitcast(dt)
        a = a[:, 0:n]
        if len(free) == 2:
            a = a.rearrange("p (a b) -> p a b", a=free[0])
        self.off += sz
        self.peak = max(self.peak, self.off)
        return a, Buf(name)

    def mark(self):
        return self.off

    def release(self, m):
        self.off = m


class Ctx:
    pass


def build_program(dbg=False):
    nc = bass.Bass("TRN2", target_bir_lowering=False)
    K = Ctx()
    K.nc = nc

    def din(name, shape, dt=F32):
        return nc.dram_tensor(name, list(shape), dt, kind="ExternalInput")

    K.x = din("x", [T, D]).ap()
    K.p = din("p", [T, PLE]).ap()
    K.w_in = din("w_in", [D, INW]).ap()
    K.lru_wa = din("lru_wa", [2, 8, 128, 128]).ap()
    K.lru_wi = din("lru_wi", [2, 8, 128, 128]).ap()
    K.w_attn_br = din("w_attn_br", [D, D]).ap()
    K.w_lru_br = din("w_lru_br", [D, D]).ap()
    K.w_out = din("w_out", [D, D]).ap()
    K.w_router = din("w_router", [D, E]).ap()
    K.w_gu = din("w_gu", [E, D, 2 * F]).ap()
    K.w_dn = din("w_dn", [E, F, D]).ap()
    K.b_dn = din("b_dn", [E, D]).ap()
    K.w_ple_gate = din("w_ple_gate", [D, D]).ap()
    K.w_ple_proj = din("w_ple_proj", [PLE, D]).ap()
    K.g_mix = din("g_mix", [1, D])
    K.g_moe = din("g_moe", [1, D])
    K.g_ple = din("g_ple", [1, D])
    K.b_router = din("b_router", [1, E])
    K.pv = din("pv", [128, NPV]).ap()
    K.ropeC = din("ropeC", [128, SEQ]).ap()
    K.ropeS = din("ropeS", [128, SEQ]).ap()
    K.perm = din("perm", [128, 128]).ap()
    K.ident = din("ident", [128, 128]).ap()
    K.tri = din("tri", [128, 128]).ap()
    K.eC = din("eC", [128, E]).ap()
    K.y = nc.dram_tensor("y", [T, D], F32, kind="ExternalOutput").ap()

    kind = "ExternalOutput" if dbg else "Internal"

    def dscr(name, shape, dt):
        if dbg:
            return nc.dram_tensor(name, list(shape), dt, kind="ExternalOutput").ap()
        return nc.dram_tensor(name, list(shape), dt).ap()

    K.qT_s = dscr("qT_s", [NSEQ, 8, 128, SEQ], BF16)
    K.kT_s = dscr("kT_s", [NSEQ, 2, 128, SEQ], BF16)
    K.V_s = dscr("V_s", [NSEQ, 128, 16, 256], BF16)
    K.yl_s = dscr("yl_s", [NSEQ, 8, 128, SEQ], BF16)
    K.sga_s = dscr("sga_s", [NSEQ, 8, 128, SEQ], BF16)
    K.sgr_s = dscr("sgr_s", [NSEQ, 8, 128, SEQ], BF16)
    K.h1_s = dscr("h1_s", [T, D], F32)
    K.xs = dscr("xs_s", [NSLOT + 128, D], BF16)
    K.ys = dscr("ys_s", [NSLOT + 128, D], F32)

    with ExitStack() as st:
        S = Sched(nc, st)
        K.S = S
        A = Arena(nc, st, 204 * 1024)
        K.A = A
        K.ps = []
        K.Bps = []
        for i in range(8):
            K.ps.append(st.enter_context(nc.psum_tensor(f"ps{i}", [128, 512], F32)))
            K.Bps.append(Buf(f"ps{i}", excl=True))
        stop = int(os.environ.get("MK_STOP", "9"))
        phase0_consts(K)
        S.barrier()
        m0 = A.mark()
        if stop >= 1:
            phase1_inproj(K)
            S.barrier()
        A.release(m0)
        if stop >= 2:
            phase2_attn(K)
            S.barrier()
        A.release(m0)
        K.pre5 = {"wpg": A.alloc("wpg", [8, D], BF16), "wpp": A.alloc("wpp", [2, D], BF16), "gple": A.alloc("gple", [D])}
        m5 = A.mark()
        K.pre4 = {"wgu": A.alloc("wgu0", [8, 2 * F], BF16), "wdn": A.alloc("wdn0", [8, D], BF16), "bdn": A.alloc("bdn0", [D], BF16)}
        m3 = A.mark()
        if stop >= 3:
            issue_expert_load(K, 0, *K.pre4["wgu"], *K.pre4["wdn"], *K.pre4["bdn"])
            phase3_router(K)
            S.barrier()
        A.release(m3)
        if stop >= 4:
            phase4_experts(K)
            S.barrier()
        A.release(m5)
        if stop >= 5:
            phase5_combine(K)
        S.emit()
    return nc


def bcast_row(dt_tensor, n):
    return bass.AP(dt_tensor, 0, [[0, 128], [1, n]])


def phase0_consts(K):
    S, A = K.S, K.A
    K.ident_f, K.Bident_f = A.alloc("ident_f", [128])
    K.ident_b, K.Bident_b = A.alloc("ident_b", [128], BF16)
    K.ones_b, K.Bones_b = A.alloc("ones_b", [128], BF16)
    K.ones_f, K.Bones_f = A.alloc("ones_f", [128])
    K.pvt, K.Bpv = A.alloc("pvt", [NPV])
    K.kk, K.Bkk = A.alloc("kk", [16])
    K.dest_i, K.Bdest = A.alloc("dest_i", [NTILE * 4], I32)
    K.gate_a, K.Bgate = A.alloc("gate_a", [NTILE * 4])
    S.dma("sp", lambda e: e.dma_start(out=K.ident_f, in_=K.ident), writes=[K.Bident_f])
    S.dma("pool", lambda e: e.dma_start(out=K.ident_b, in_=K.ident), writes=[K.Bident_b])
    S.dma("sp", lambda e: e.dma_start(out=K.pvt, in_=K.pv), writes=[K.Bpv])
    S.op("dve", lambda e: e.memset(K.ones_b, 1.0), writes=[K.Bones_b])
    S.op("dve", lambda e: e.memset(K.ones_f, 1.0), writes=[K.Bones_f])
    lam = K.pvt[:, PV_LAM:PV_LAM + 16]
    S.op("act", lambda e: e.activation(out=K.kk, in_=lam, func=AF.Exp, scale=-1.0), reads=[K.Bpv], writes=[K.Bkk])
    S.op("act", lambda e: e.activation(out=K.kk, in_=K.kk, func=AF.Ln, bias=1.0), reads=[K.Bkk], writes=[K.Bkk])
    S.op("dve", lambda e: e.tensor_scalar(out=K.kk, in0=K.kk, scalar1=-8.0, scalar2=None, op0=ALU.mult),
         reads=[K.Bkk], writes=[K.Bkk])


def mm_group(S, out, Bout, pairs, reads):
    n = len(pairs)
    for i, (l, r) in enumerate(pairs):
        S.op("pe", lambda e, l=l, r=r, i=i: e.matmul(out, l, r, start=(i == 0), stop=(i == n - 1)),
             reads=reads, writes=[Bout], signal=(i == n - 1))


def rms_rstd(K, src, Bsrc, junk, Bjunk, ss, Bss, n):
    S = K.S
    S.op("act", lambda e: e.activation(out=junk, in_=src, func=AF.Square, accum_out=ss),
         reads=[Bsrc], writes=[Bjunk, Bss])
    S.op("act", lambda e: e.activation(out=ss, in_=ss, func=AF.Sqrt, bias=EPS, scale=1.0 / n),
         reads=[Bss], writes=[Bss])
    S.op("dve", lambda e: e.reciprocal(out=ss, in_=ss), reads=[Bss], writes=[Bss])


def interleave(gens, skew=0):
    gens = list(gens)
    start = {id(g): i * skew for i, g in enumerate(gens)}
    rnd = 0
    while gens:
        for g in list(gens):
            if rnd < start[id(g)]:
                continue
            try:
                next(g)
            except StopIteration:
                gens.remove(g)
        rnd += 1


def phase1_inproj(K):
    S, A, nc = K.S, K.A, K.nc
    ps, Bps = K.ps, K.Bps
    uT, BuT = A.alloc("uT", [8, SEQ], BF16)
    wtA = [A.alloc(f"wtA{i}", [8, 512], BF16) for i in range(2)]
    wtB = [A.alloc(f"wtB{i}", [8, 256], BF16) for i in range(2)]
    ropeS, BropeS = A.alloc("ropeS", [SEQ])
    cq, Bcq = A.alloc("cq", [SEQ])
    ck, Bck = A.alloc("ck", [SEQ])
    permf, Bpermf = A.alloc("permf", [128])
    permq, Bpermq = A.alloc("permq", [128], BF16)
    permk, Bpermk = A.alloc("permk", [128], BF16)
    od_b, Bod = A.alloc("od_b", [128], BF16)
    wa_b, Bwa = A.alloc("wa_b", [16, 128], BF16)
    wi_b, Bwi = A.alloc("wi_b", [16, 128], BF16)
    xn, Bxn = A.alloc("xn", [D], BF16)
    junk, Bjunk = xn, Bxn
    xns = [(xn, Bxn), A.alloc("xn2", [D], BF16)]
    ss = [A.alloc(f"ss{i}", [1]) for i in range(2)]
    stgA = [A.alloc(f"stgA{i}", [SEQ], BF16) for i in range(2)]
    stgB = [A.alloc(f"stgB{i}", [SEQ], BF16) for i in range(1)]
    xq = [A.alloc(f"xq{i}", [512], BF16) for i in range(2)]
    sq = [A.alloc(f"sq{i}", [512], BF16) for i in range(2)]
    ta = [A.alloc(f"ta{i}", [512]) for i in range(2)]
    tb_ = [A.alloc(f"tb{i}", [512]) for i in range(2)]
    rst = [A.alloc(f"rst{i}", [512]) for i in range(2)]
    xrp, Bxrp = A.alloc("xrp", [SEQ + 4])
    cc, Bcc = A.alloc("cc", [SEQ])
    ccb, Bccb = A.alloc("ccb", [SEQ], BF16)
    aas = [A.alloc(f"aa{i}", [SEQ]) for i in range(2)]
    bts = [A.alloc(f"bt{i}", [SEQ]) for i in range(2)]
    aa, Baa = aas[0]
    t1, Bt1 = A.alloc("t1", [SEQ])
    gmix, Bgmix = cc[:, 0:D], Bcc
    hf, Bhf = A.alloc("hf", [SEQ])
    hb, Bhb = A.alloc("hb", [SEQ])
    xt = [(hb[:, 0:D], Bhb), (hf[:, 0:D], Bhf)]
    vst, Bvst = aa.bitcast(BF16).rearrange("p (a b) -> p a b", a=16), Baa

    pvt = K.pvt
    S.dma("sp", lambda e: e.dma_start(out=cq, in_=K.ropeC), writes=[Bcq])
    S.dma("sp", lambda e: e.dma_start(out=ck, in_=K.ropeC), writes=[Bck])
    S.dma("sp", lambda e: e.dma_start(out=ropeS, in_=K.ropeS), writes=[BropeS])
    S.dma("sp", lambda e: e.dma_start(out=permf, in_=K.perm), writes=[Bpermf])
    S.dma("pool", lambda e: e.dma_start(out=wa_b, in_=K.lru_wa.rearrange("d c p n -> p (d c) n")), writes=[Bwa])
    S.dma("pool", lambda e: e.dma_start(out=wi_b, in_=K.lru_wi.rearrange("d c p n -> p (d c) n")), writes=[Bwi])
    S.op("dve", lambda e: e.memset(od_b, 1.0 / 128.0), writes=[Bod])
    S.op("dve", lambda e: e.memset(xrp, 0.0), writes=[Bxrp])
    qn = pvt[:, PV_QN:PV_QN + 1]
    kn = pvt[:, PV_KN:PV_KN + 1]
    S.op("dve", lambda e: e.tensor_scalar(out=cq, in0=cq, scalar1=qn, scalar2=None, op0=ALU.mult),
         reads=[Bcq, K.Bpv], writes=[Bcq])
    S.op("dve", lambda e: e.tensor_scalar(out=ck, in0=ck, scalar1=kn, scalar2=None, op0=ALU.mult),
         reads=[Bck, K.Bpv], writes=[Bck])
    S.op("dve", lambda e: e.tensor_scalar(out=permq, in0=permf, scalar1=qn, scalar2=None, op0=ALU.mult),
         reads=[Bpermf, K.Bpv], writes=[Bpermq])
    S.op("dve", lambda e: e.tensor_scalar(out=permk, in0=permf, scalar1=kn, scalar2=None, op0=ALU.mult),
         reads=[Bpermf, K.Bpv], writes=[Bpermk])

    w_in_v = K.w_in.rearrange("(c p) n -> p c n", p=128)
    wcnt = {"A": 0, "B": 0}

    def load_w(which, cols):
        tiles = wtA if which == "A" else wtB
        i = wcnt[which] % 2
        wcnt[which] += 1
        w, Bw = tiles[i]
        off = 0
        for (c0, wd) in cols:
            S.dma("pool", lambda e, w=w, off=off, c0=c0, wd=wd: e.dma_start(
                out=w[:, :, off:off + wd], in_=w_in_v[:, :, c0:c0 + wd]), writes=[Bw])
            off += wd
        return w, Bw

    def inproj(w, Bw, woff, tb, bank):
        mm_group(S, ps[bank][:], Bps[bank],
                 [(w[:, k, woff:woff + 128], uT[:, k, tb * 512:(tb + 1) * 512]) for k in range(8)],
                 reads=[Bw, BuT])

    scnt = {"A": 0, "B": 0}

    def build_uT(s):
        S.dma("sp", lambda e: e.dma_start(out=gmix, in_=bcast_row(K.g_mix, D)), writes=[Bgmix])

        def tiles(par):
            x_t, Bx = xt[par]
            ss_t, Bss = ss[par]
            xn_, Bxn_ = xns[par]
            bank = par
            pv16 = ps[bank][:].bitcast(BF16)
            for i in range(par, 16, 2):
                r0 = s * SEQ + i * 128
                S.dma("sp", lambda e, r0=r0: e.dma_start(out=x_t, in_=K.x[r0:r0 + 128, :]), writes=[Bx])
                rms_rstd(K, x_t, Bx, xn_, Bxn_, ss_t, Bss, D)
                yield
                S.op("dve", lambda e: e.scalar_tensor_tensor(
                    out=xn_, in0=x_t, scalar=ss_t, in1=gmix, op0=ALU.mult, op1=ALU.mult),
                    reads=[Bx, Bss, Bgmix], writes=[Bxn_])
                yield
                for c in range(8):
                    S.op("pe", lambda e, c=c: e.transpose(pv16[:, c * 128:(c + 1) * 128], xn_[:, c * 128:(c + 1) * 128], K.ident_b),
                         reads=[Bxn_, K.Bident_b], writes=[Bps[bank]], signal=(c == 7))
                S.op("act", lambda e, i=i: e.activation(
                    out=uT[:, :, i * 128:(i + 1) * 128], in_=pv16.rearrange("p (c t) -> p c t", c=8), func=AF.Copy),
                    reads=[Bps[bank]], writes=[BuT])
                yield

        interleave([tiles(0), tiles(1)], skew=1)

    def qk_head(s, w, Bw, woff, is_q, hidx):
        cg, Bcg = (cq, Bcq) if is_q else (ck, Bck)
        pm, Bpm = (permq, Bpermq) if is_q else (permk, Bpermk)
        st_t, Bst = stgA[scnt["A"] % 2]
        scnt["A"] += 1
        for tb in range(4):
            p = tb % 2
            sl = slice(tb * 512, (tb + 1) * 512)
            xq_, Bxq = xq[p]
            sq_, Bsq = sq[p]
            ta_, Bta = ta[p]
            tb2, Btb = tb_[p]
            rs_, Brst = rst[p]
            zb = p
            inproj(w, Bw, woff, tb, zb)
            S.op("act", lambda e, xq_=xq_, zb=zb: e.activation(out=xq_, in_=ps[zb][:], func=AF.Copy), reads=[Bps[zb]], writes=[Bxq])
            S.op("act", lambda e, sq_=sq_, zb=zb: e.activation(out=sq_, in_=ps[zb][:], func=AF.Square), reads=[Bps[zb]], writes=[Bsq])
            S.op("dve", lambda e, sl=sl, cg=cg, ta_=ta_, zb=zb: e.tensor_tensor(out=ta_, in0=ps[zb][:], in1=cg[:, sl], op=ALU.mult),
                 reads=[Bps[zb], Bcg], writes=[Bta])
            yield
            mm_group(S, ps[2][:], Bps[2], [(od_b, sq_)], reads=[Bod, Bsq])
            mm_group(S, ps[3][:], Bps[3], [(pm, xq_)], reads=[Bpm, Bxq])
            S.op("act", lambda e, rs_=rs_: e.activation(out=rs_, in_=ps[2][:], func=AF.Sqrt, bias=EPS), reads=[Bps[2]], writes=[Brst])
            S.op("dve", lambda e, sl=sl, tb2=tb2: e.tensor_tensor(out=tb2, in0=ps[3][:], in1=ropeS[:, sl], op=ALU.mult),
                 reads=[Bps[3], BropeS], writes=[Btb])
            yield
            S.op("dve", lambda e, rs_=rs_: e.reciprocal(out=rs_, in_=rs_), reads=[Brst], writes=[Brst])
            S.op("dve", lambda e, ta_=ta_, tb2=tb2: e.tensor_tensor(out=ta_, in0=ta_, in1=tb2, op=ALU.add), reads=[Bta, Btb], writes=[Bta])
            S.op("dve", lambda e, sl=sl, st_t=st_t, ta_=ta_, rs_=rs_: e.tensor_tensor(out=st_t[:, sl], in0=ta_, in1=rs_, op=ALU.mult),
                 reads=[Bta, Brst], writes=[Bst])
            yield
        dst = (K.qT_s if is_q else K.kT_s)[s, hidx]
        S.dma("sp", lambda e, st_t=st_t, dst=dst: e.dma_start(out=dst, in_=st_t), reads=[Bst], owner=Bst)

    def stream_A(s):
        for t2 in range(2):
            w, Bw = load_w("A", [(t2 * 512, 512)])
            for j in range(4):
                yield from qk_head(s, w, Bw, j * 128, True, t2 * 4 + j)
        w, Bw = load_w("A", [(1024, 512)])
        for j in range(2):
            yield from qk_head(s, w, Bw, j * 128, False, j)
        for gi, dst_s in ((0, K.sga_s), (1, K.sgr_s)):
            for t2 in range(2):
                wg, Bwg = load_w("A", [(3584 + gi * 1024 + t2 * 512, 512)])
                for j in range(4):
                    st_t, Bst = stgA[scnt["A"] % 2]
                    scnt["A"] += 1
                    for tb in range(4):
                        bank = tb % 2
                        inproj(wg, Bwg, j * 128, tb, bank)
                        S.op("act", lambda e, bank=bank, tb=tb, st_t=st_t: e.activation(out=st_t[:, tb * 512:(tb + 1) * 512], in_=ps[bank][:], func=AF.Sigmoid),
                             reads=[Bps[bank]], writes=[Bst])
                        yield
                    S.dma("sp", lambda e, st_t=st_t, dst=dst_s[s, t2 * 4 + j]: e.dma_start(out=dst, in_=st_t), reads=[Bst], owner=Bst)

    def do_V(s):
        w, Bw = load_w("A", [(1024, 512)])
        for tk in range(16):
            bank = 2 + tk % 2
            mm_group(S, ps[bank][:, 0:256], Bps[bank],
                     [(uT[:, k, tk * 128:(tk + 1) * 128], w[:, k, 256:512]) for k in range(8)], reads=[Bw, BuT])
            S.op("act", lambda e, bank=bank, tk=tk: e.activation(out=vst[:, tk, :], in_=ps[bank][:, 0:256], func=AF.Copy),
                 reads=[Bps[bank]], writes=[Bvst])
        S.dma("sp", lambda e, s=s: e.dma_start(out=K.V_s[s], in_=vst), reads=[Bvst], owner=Bvst)

    def stream_B(s):
        for j in range(8):
            w, Bw = load_w("B", [(1536 + j * 128, 128), (2560 + j * 128, 128)])
            for tb in range(4):
                bank = 4 + tb % 2
                inproj(w, Bw, 0, tb, bank)
                S.op("act", lambda e, bank=bank, tb=tb: e.activation(out=xrp[:, 2 + tb * 512:2 + (tb + 1) * 512], in_=ps[bank][:], func=AF.Copy),
                     reads=[Bps[bank]], writes=[Bxrp])
                yield
            cw = lambda jj, j=j: K.pvt[:, PV_CONVW + j * 4 + jj:PV_CONVW + j * 4 + jj + 1]
            cb = K.pvt[:, PV_CONVB + j:PV_CONVB + j + 1]
            S.op("act", lambda e, cw=cw, cb=cb: e.activation(out=cc, in_=xrp[:, 0:SEQ], func=AF.Identity, bias=cb, scale=cw(0)),
                 reads=[Bxrp, K.Bpv], writes=[Bcc])
            for jj in range(1, 4):
                S.op("dve", lambda e, cw=cw, jj=jj: e.scalar_tensor_tensor(out=cc, in0=xrp[:, jj:jj + SEQ], scalar=cw(jj), in1=cc, op0=ALU.mult, op1=ALU.add),
                     reads=[Bxrp, Bcc, K.Bpv], writes=[Bcc])
                yield
            S.op("act", lambda e: e.activation(out=ccb, in_=cc, func=AF.Copy), reads=[Bcc], writes=[Bccb])

            def dir_gen(d, j=j):
                aa_, Baa_ = aas[d]
                bt_, Bbt_ = bts[d]
                hh, Bhh = (hf, Bhf) if d == 0 else (hb, Bhb)
                ba = K.pvt[:, PV_BA + d * 8 + j:PV_BA + d * 8 + j + 1]
                bi = K.pvt[:, PV_BI + d * 8 + j:PV_BI + d * 8 + j + 1]
                kkc = K.kk[:, d * 8 + j:d * 8 + j + 1]
                for tb in range(4):
                    sl = slice(tb * 512, (tb + 1) * 512)
                    mm_group(S, ps[6][:], Bps[6], [(wa_b[:, d * 8 + j, :], ccb[:, sl])], reads=[Bwa, Bccb])
                    S.op("act", lambda e, sl=sl: e.activation(out=aa_[:, sl], in_=ps[6][:], func=AF.Sigmoid, bias=ba),
                         reads=[Bps[6], K.Bpv], writes=[Baa_])
                    mm_group(S, ps[7][:], Bps[7], [(wi_b[:, d * 8 + j, :], ccb[:, sl])], reads=[Bwi, Bccb])
                    S.op("act", lambda e, sl=sl: e.activation(out=bt_[:, sl], in_=ps[7][:], func=AF.Sigmoid, bias=bi),
                         reads=[Bps[7], K.Bpv], writes=[Bbt_])
                    yield
                S.op("act", lambda e: e.activation(out=aa_, in_=aa_, func=AF.Exp, scale=kkc), reads=[Baa_, K.Bkk], writes=[Baa_])
                S.op("dve", lambda e: e.tensor_tensor(out=bt_, in0=bt_, in1=cc, op=ALU.mult), reads=[Bbt_, Bcc], writes=[Bbt_])
                yield
                S.op("act", lambda e: e.activation(out=hh, in_=aa_, func=AF.Square), reads=[Baa_], writes=[Bhh])
                S.op("act", lambda e: e.activation(out=hh, in_=hh, func=AF.Sqrt, bias=1.0, scale=-1.0), reads=[Bhh], writes=[Bhh])
                yield
                S.op("dve", lambda e: e.tensor_tensor(out=bt_, in0=bt_, in1=hh, op=ALU.mult), reads=[Bbt_, Bhh], writes=[Bbt_])
                yield
                if d == 0:
                    S.op("dve", lambda e: e.tensor_tensor_scan(out=hh, data0=aa_, data1=bt_, initial=0.0, op0=ALU.mult, op1=ALU.add),
                         reads=[Baa_, Bbt_], writes=[Bhh])
                else:
                    S.op("dve", lambda e: e.tensor_tensor_scan(out=hh[:, ::-1], data0=aa_[:, ::-1], data1=bt_[:, ::-1], initial=0.0, op0=ALU.mult, op1=ALU.add),
                         reads=[Baa_, Bbt_], writes=[Bhh])
                yield

            def gelu_gen(w=w, Bw=Bw):
                for tb in range(4):
                    sl = slice(tb * 512, (tb + 1) * 512)
                    bank = 4 + tb % 2
                    inproj(w, Bw, 128, tb, bank)
                    S.op("act", lambda e, bank=bank, sl=sl: e.activation(out=t1[:, sl], in_=ps[bank][:], func=AF.Square), reads=[Bps[bank]], writes=[Bt1])
                    S.op("act", lambda e, sl=sl: e.activation(out=t1[:, sl], in_=t1[:, sl], func=AF.Identity, bias=1.0, scale=0.044715),
                         reads=[Bt1], writes=[Bt1])
                    S.op("dve", lambda e, bank=bank, sl=sl: e.tensor_tensor(out=t1[:, sl], in0=t1[:, sl], in1=ps[bank][:], op=ALU.mult),
                         reads=[Bt1, Bps[bank]], writes=[Bt1])
                    S.op("act", lambda e, sl=sl: e.activation(out=t1[:, sl], in_=t1[:, sl], func=AF.Sigmoid, scale=1.5957691216), reads=[Bt1], writes=[Bt1])
                    S.op("dve", lambda e, bank=bank, sl=sl: e.tensor_tensor(out=t1[:, sl], in0=t1[:, sl], in1=ps[bank][:], op=ALU.mult),
                         reads=[Bt1, Bps[bank]], writes=[Bt1])
                    yield

            subs = [dir_gen(0), dir_gen(1), gelu_gen()]
            while subs:
                for g in list(subs):
                    try:
                        next(g)
                    except StopIteration:
                        subs.remove(g)
                yield
            S.op("dve", lambda e: e.tensor_tensor(out=hf, in0=hf, in1=hb, op=ALU.add), reads=[Bhf, Bhb], writes=[Bhf])
            st_t, Bst = stgB[0]
            S.op("dve", lambda e, st_t=st_t: e.tensor_tensor(out=st_t, in0=hf, in1=t1, op=ALU.mult), reads=[Bhf, Bt1], writes=[Bst])
            S.dma("sp", lambda e, st_t=st_t, j=j, s=s: e.dma_start(out=K.yl_s[s, j], in_=st_t), reads=[Bst], owner=Bst)
            yield

    for s in range(NSEQ):
        build_uT(s)
        interleave([stream_A(s), stream_B(s)])
        do_V(s)


def phase2_attn(K):
    S, A, nc = K.S, K.A, K.nc
    ps, Bps = K.ps, K.Bps
    wab, Bwab = A.alloc("wab", [8, D], BF16)
    wlb, Bwlb = A.alloc("wlb", [8, D], BF16)
    wo, Bwo = A.alloc("wo", [8, D], BF16)
    kT, BkT = A.alloc("kT", [2, SEQ], BF16)
    V, BV = A.alloc("V", [16, 256], BF16)
    qT = [A.alloc(f"qT{i}", [8, 512], BF16) for i in range(2)]
    ylb = [A.alloc(f"ylb{i}", [8, 512], BF16) for i in range(2)]
    gab = [A.alloc(f"gab{i}", [8, 512], BF16) for i in range(2)]
    grb = [A.alloc(f"grb{i}", [8, 512], BF16) for i in range(2)]
    PT = [A.alloc(f"PT{i}", [512], BF16) for i in range(6)]
    SBANK = (0, 1, 2, 7)
    attnT, BattnT = A.alloc("attnT", [8, 512], BF16)
    mrg, Bmrg = A.alloc("mrg", [8, 512], BF16)
    rz, Brz = A.alloc("rz", [512])
    zacc = [A.alloc(f"zacc{i}", [512]) for i in range(2)]
    zab = [A.alloc(f"zab{i}", [512], BF16) for i in range(2)]
    m1, Bm1 = A.alloc("m1", [512])
    m2, Bm2 = A.alloc("m2", [512])
    xt = [A.alloc(f"xt{i}", [D]) for i in range(2)]
    ho = [A.alloc(f"ho{i}", [D]) for i in range(2)]
    S.dma("pool", lambda e: e.dma_start(out=wab, in_=K.w_attn_br.rearrange("(c p) n -> p c n", p=128)), writes=[Bwab])
    S.dma("pool", lambda e: e.dma_start(out=wlb, in_=K.w_lru_br.rearrange("(c p) n -> p c n", p=128)), writes=[Bwlb])
    S.dma("pool", lambda e: e.dma_start(out=wo, in_=K.w_out.rearrange("(c p) n -> p c n", p=128)), writes=[Bwo])
    zt, Bzt = A.alloc("zt", [D], BF16)
    S.op("dve", lambda e: e.memset(zt, 0.0), writes=[Bzt])
    zrows = list(range(0, NSLOT + 128, 128))

    def zero_some(n):
        for _ in range(n):
            if zrows:
                r0 = zrows.pop(0)
                S.dma("sp", lambda e, r0=r0: e.dma_start(out=K.xs[r0:r0 + 128, :], in_=zt), reads=[Bzt], owner=Bzt)
    scale = 128.0 ** -0.5
    cnt = 0
    for s in range(NSEQ):
        S.dma("sp", lambda e, s=s: e.dma_start(out=kT, in_=K.kT_s[s].rearrange("h p t -> p h t")), writes=[BkT])
        S.dma("sp", lambda e, s=s: e.dma_start(out=V, in_=K.V_s[s]), writes=[BV])
        for qb in range(4):
            q, Bq = qT[cnt % 2]
            yl, Byl = ylb[cnt % 2]
            ga, Bga = gab[cnt % 2]
            gr, Bgr = grb[cnt % 2]
            cnt += 1
            tsl = slice(qb * 512, (qb + 1) * 512)
            S.dma("sp", lambda e, q=q, s=s, tsl=tsl: e.dma_start(out=q, in_=K.qT_s[s].rearrange("h p t -> p h t")[:, :, tsl]), writes=[Bq])
            S.dma("sp", lambda e, yl=yl, s=s, tsl=tsl: e.dma_start(out=yl, in_=K.yl_s[s].rearrange("h p t -> p h t")[:, :, tsl]), writes=[Byl])
            S.dma("sp", lambda e, ga=ga, s=s, tsl=tsl: e.dma_start(out=ga, in_=K.sga_s[s].rearrange("h p t -> p h t")[:, :, tsl]), writes=[Bga])
            S.dma("sp", lambda e, gr=gr, s=s, tsl=tsl: e.dma_start(out=gr, in_=K.sgr_s[s].rearrange("h p t -> p h t")[:, :, tsl]), writes=[Bgr])
            for h in range(8):
                kv = h // 4
                ob, zb = 3 + h % 2, 5 + h % 2

                def score(kc):
                    bank = SBANK[kc % 4]
                    mm_group(S, ps[bank][:], Bps[bank], [(kT[:, kv, kc * 128:(kc + 1) * 128], q[:, h, :])], reads=[BkT, Bq])
                    pt, Bpt = PT[kc % 6]
                    S.op("act", lambda e, bank=bank, pt=pt: e.activation(out=pt, in_=ps[bank][:], func=AF.Exp, scale=scale),
                         reads=[Bps[bank]], writes=[Bpt])

                za, Bza = zacc[h % 2]
                zb16, Bzb16 = zab[h % 2]

                def pv(kc):
                    pt, Bpt = PT[kc % 6]
                    S.op("pe", lambda e, kc=kc, pt=pt, ob=ob, kv=kv: e.matmul(ps[ob][:], V[:, kc, kv * 128:(kv + 1) * 128], pt, start=(kc == 0), stop=(kc == 15)),
                         reads=[BV, Bpt], writes=[Bps[ob]], signal=(kc == 15))
                    if kc % 2 == 1:
                        S.op("pe", lambda e, kc=kc, pt=pt, zb=zb: e.matmul(ps[zb][:], K.ones_b, pt, start=(kc == 1), stop=False),
                             reads=[K.Bones_b, Bpt], writes=[Bps[zb]], signal=False)
                    elif kc == 0:
                        S.op("dve", lambda e, pt=pt, za=za: e.tensor_copy(out=za, in_=pt), reads=[Bpt], writes=[Bza])
                    else:
                        S.op("dve", lambda e, pt=pt, za=za: e.tensor_tensor(out=za, in0=za, in1=pt, op=ALU.add), reads=[Bpt, Bza], writes=[Bza])
                    if kc == 15:
                        S.op("pe", lambda e, za=za, zb=zb: e.matmul(ps[zb][:], K.ones_f, za, start=False, stop=True),
                             reads=[K.Bones_f, Bza], writes=[Bps[zb]], signal=True)

                score(0)
                score(1)
                score(2)
                for kc in range(16):
                    if kc + 3 < 16:
                        score(kc + 3)
                    pv(kc)
                S.op("dve", lambda e, zb=zb: e.reciprocal(out=rz, in_=ps[zb][:]), reads=[Bps[zb]], writes=[Brz])
                S.op("dve", lambda e, ob=ob, h=h: e.tensor_tensor(out=attnT[:, h, :], in0=ps[ob][:], in1=rz, op=ALU.mult),
                     reads=[Bps[ob], Brz], writes=[BattnT])
                zero_some(6)
            for m in range(8):
                b1, b2 = 0 + m % 2, 2 + m % 2
                mm_group(S, ps[b1][:], Bps[b1], [(wab[:, k, m * 128:(m + 1) * 128], attnT[:, k, :]) for k in range(8)], reads=[Bwab, BattnT])
                mm_group(S, ps[b2][:], Bps[b2], [(wlb[:, k, m * 128:(m + 1) * 128], yl[:, k, :]) for k in range(8)], reads=[Bwlb, Byl])
                S.op("dve", lambda e, b1=b1, m=m, ga=ga: e.tensor_tensor(out=m1, in0=ps[b1][:], in1=ga[:, m, :], op=ALU.mult),
                     reads=[Bps[b1], Bga], writes=[Bm1])
                S.op("dve", lambda e, b2=b2, m=m, gr=gr: e.tensor_tensor(out=m2, in0=ps[b2][:], in1=gr[:, m, :], op=ALU.mult),
                     reads=[Bps[b2], Bgr], writes=[Bm2])
                S.op("dve", lambda e, m=m: e.tensor_tensor(out=mrg[:, m, :], in0=m1, in1=m2, op=ALU.add),
                     reads=[Bm1, Bm2], writes=[Bmrg])
            for tk in range(4):
                x_t, Bx = xt[tk % 2]
                h_t, Bh = ho[tk % 2]
                r0 = s * SEQ + qb * 512 + tk * 128
                S.dma("sp", lambda e, x_t=x_t, r0=r0: e.dma_start(out=x_t, in_=K.x[r0:r0 + 128, :]), writes=[Bx])
                for nh in range(2):
                    bank = 5 + nh
                    mm_group(S, ps[bank][:], Bps[bank],
                             [(mrg[:, k, tk * 128:(tk + 1) * 128], wo[:, k, nh * 512:(nh + 1) * 512]) for k in range(8)], reads=[Bmrg, Bwo])
                    S.op("dve", lambda e, bank=bank, nh=nh, x_t=x_t, h_t=h_t: e.tensor_tensor(
                        out=h_t[:, nh * 512:(nh + 1) * 512], in0=ps[bank][:], in1=x_t[:, nh * 512:(nh + 1) * 512], op=ALU.add),
                        reads=[Bps[bank], Bx], writes=[Bh])
                S.dma("sp", lambda e, h_t=h_t, r0=r0: e.dma_start(out=K.h1_s[r0:r0 + 128, :], in_=h_t), reads=[Bh], owner=Bh)
    zero_some(len(zrows))


def phase3_router(K):
    S, A, nc = K.S, K.A, K.nc
    ps, Bps = K.ps, K.Bps
    gmoe, Bgmoe = A.alloc("gmoe", [D])
    wr, Bwr = A.alloc("wr", [8, E])
    brt, Bbr = A.alloc("brt", [E])
    tri, Btri = A.alloc("tri", [128])
    eC, BeC = A.alloc("eC", [E])
    msum, Bmsum = A.alloc("msum", [E])
    S.dma("sp", lambda e: e.dma_start(out=gmoe, in_=bcast_row(K.g_moe, D)), writes=[Bgmoe])
    S.dma("sp", lambda e: e.dma_start(out=wr, in_=K.w_router.rearrange("(c p) n -> p c n", p=128)), writes=[Bwr])
    S.dma("sp", lambda e: e.dma_start(out=brt, in_=bcast_row(K.b_router, E)), writes=[Bbr])
    S.dma("sp", lambda e: e.dma_start(out=tri, in_=K.tri), writes=[Btri])
    S.dma("sp", lambda e: e.dma_start(out=eC, in_=K.eC), writes=[BeC])
    S.op("dve", lambda e: e.memset(msum, 0.0), writes=[Bmsum])

    def tile_stream(par):
        h_t, Bh = A.alloc(f"ht{par}", [D])
        junk, Bjunk = A.alloc(f"junk{par}", [D], BF16)
        ss_t, Bss = A.alloc(f"ss{par}", [1])
        u2, Bu2 = A.alloc(f"u2{par}", [D])
        ub, Bub = A.alloc(f"u2b{par}", [D], BF16)
        u2T, Bu2T = A.alloc(f"u2T{par}", [8, 128])
        lg, Blg = A.alloc(f"lg{par}", [E])
        top8, Btop8 = A.alloc(f"top8{par}", [8])
        nm, Bnm = A.alloc(f"nm{par}", [1])
        mask, Bmask = A.alloc(f"mask{par}", [E])
        ex, Bex = A.alloc(f"ex{par}", [E])
        den, Bden = A.alloc(f"den{par}", [1])
        gf, Bgf = A.alloc(f"gf{par}", [E])
        rank, Brank = A.alloc(f"rank{par}", [E])
        okm, Bok = A.alloc(f"okm{par}", [E])
        dst, Bdst = A.alloc(f"dst{par}", [E])
        dk, Bdk = A.alloc(f"dk{par}", [4])
        oh, Boh = A.alloc(f"oh{par}", [E])
        b0 = par * 2

        def gen():
            for i in range(par, NTILE, 4):
                r0 = i * 128
                S.dma("sp", lambda e, r0=r0: e.dma_start(out=h_t, in_=K.h1_s[r0:r0 + 128, :]), writes=[Bh])
                rms_rstd(K, h_t, Bh, junk, Bjunk, ss_t, Bss, D)
                S.op("dve", lambda e: e.scalar_tensor_tensor(out=u2, in0=h_t, scalar=ss_t, in1=gmoe, op0=ALU.mult, op1=ALU.mult),
                     reads=[Bh, Bss, Bgmoe], writes=[Bu2])
                yield
                S.op("act", lambda e: e.activation(out=ub, in_=u2, func=AF.Copy), reads=[Bu2], writes=[Bub])
                for hc in range(2):
                    bank = b0
                    for c4 in range(4):
                        c = hc * 4 + c4
                        S.op("pe", lambda e, c=c, c4=c4, bank=bank: e.transpose(ps[bank][:, c4 * 128:(c4 + 1) * 128], u2[:, c * 128:(c + 1) * 128], K.ident_f),
                             reads=[Bu2, K.Bident_f], writes=[Bps[bank]], signal=(c4 == 3))
                    S.op("act", lambda e, bank=bank, hc=hc: e.activation(out=u2T[:, hc * 4:(hc + 1) * 4, :], in_=ps[bank][:].rearrange("p (c t) -> p c t", c=4), func=AF.Copy),
                         reads=[Bps[bank]], writes=[Bu2T])
                yield
                lb, rb = b0 + 1, b0 + 1
                mm_group(S, ps[lb][:, 0:E], Bps[lb], [(u2T[:, k, :], wr[:, k, :]) for k in range(8)], reads=[Bu2T, Bwr])
                S.op("dve", lambda e, lb=lb: e.tensor_tensor(out=lg, in0=ps[lb][:, 0:E], in1=brt, op=ALU.add), reads=[Bps[lb], Bbr], writes=[Blg])
                S.op("dve", lambda e: e.max(out=top8, in_=lg), reads=[Blg], writes=[Btop8])
                S.op("dve", lambda e: e.tensor_scalar(out=mask, in0=lg, scalar1=top8[:, 3:4], scalar2=None, op0=ALU.is_ge),
                     reads=[Blg, Btop8], writes=[Bmask])
                yield
                mm_group(S, ps[rb][:, 64:64 + E], Bps[rb], [(tri, mask), (K.ones_f, msum)], reads=[Btri, Bmask, K.Bones_f, Bmsum])
                S.op("dve", lambda e: e.tensor_tensor(out=msum, in0=msum, in1=mask, op=ALU.add), reads=[Bmsum, Bmask], writes=[Bmsum])
                yield
                S.op("dve", lambda e: e.tensor_scalar(out=nm, in0=top8[:, 0:1], scalar1=-1.0, scalar2=None, op0=ALU.mult),
                     reads=[Btop8], writes=[Bnm])
                S.op("act", lambda e: e.activation(out=ex, in_=lg, func=AF.Exp, bias=nm), reads=[Blg, Bnm], writes=[Bex])
                S.op("dve", lambda e, rb=rb: e.tensor_copy(out=rank, in_=ps[rb][:, 64:64 + E]), reads=[Bps[rb]], writes=[Brank])
                S.op("dve", lambda e: e.tensor_scalar(out=okm, in0=rank, scalar1=float(CAP), scalar2=None, op0=ALU.is_lt),
                     reads=[Brank], writes=[Bok])
                S.op("dve", lambda e: e.tensor_tensor(out=dst, in0=rank, in1=eC, op=ALU.add), reads=[Brank, BeC], writes=[Bdst])
                S.op("dve", lambda e: e.scalar_tensor_tensor(out=dst, in0=dst, scalar=float(-TRASH), in1=okm, op0=ALU.add, op1=ALU.mult),
                     reads=[Bdst, Bok], writes=[Bdst])
                S.op("dve", lambda e: e.tensor_scalar(out=dst, in0=dst, scalar1=float(TRASH), scalar2=None, op0=ALU.add),
                     reads=[Bdst], writes=[Bdst])
                yield
                S.op("dve", lambda e: e.tensor_tensor(out=ex, in0=ex, in1=mask, op=ALU.mult), reads=[Bex, Bmask], writes=[Bex])
                S.op("dve", lambda e: e.reduce_sum(out=den, in_=ex, axis=AX.X), reads=[Bex], writes=[Bden])
                S.op("dve", lambda e: e.reciprocal(out=den, in_=den), reads=[Bden], writes=[Bden])
                S.op("dve", lambda e: e.scalar_tensor_tensor(out=gf, in0=ex, scalar=den, in1=okm, op0=ALU.mult, op1=ALU.mult),
                     reads=[Bex, Bden, Bok], writes=[Bgf])
                yield
                for k in range(4):
                    S.op("dve", lambda e, k=k: e.scalar_tensor_tensor(out=oh, in0=lg, scalar=top8[:, k:k + 1], in1=dst, op0=ALU.is_equal, op1=ALU.mult,
                                                                      accum_out=dk[:, k:k + 1]),
                         reads=[Blg, Btop8, Bdst], writes=[Boh, Bdk])
                    S.op("dve", lambda e, k=k, i=i: e.scalar_tensor_tensor(out=oh, in0=lg, scalar=top8[:, k:k + 1], in1=gf, op0=ALU.is_equal, op1=ALU.mult,
                                                                           accum_out=K.gate_a[:, i * 4 + k:i * 4 + k + 1]),
                         reads=[Blg, Btop8, Bgf], writes=[Boh, K.Bgate])
                S.op("dve", lambda e, i=i: e.tensor_copy(out=K.dest_i[:, i * 4:(i + 1) * 4], in_=dk), reads=[Bdk], writes=[K.Bdest])
                for k in range(4):
                    S.dma("pool", lambda e, k=k, i=i: e.indirect_dma_start(
                        out=K.xs, out_offset=bass.IndirectOffsetOnAxis(ap=K.dest_i[:, i * 4 + k:i * 4 + k + 1], axis=0),
                        in_=ub, in_offset=None), reads=[Bub, K.Bdest], owner=Bub)
                yield
        return gen()

    interleave([tile_stream(q) for q in range(4)], skew=2)


def issue_expert_load(K, e_, w, Bw, wd, Bwd, bd, Bbd):
    S = K.S
    src = K.w_gu[e_].rearrange("(c p) n -> p c n", p=128)
    for hh in range(2):
        S.dma("pool", lambda e, w=w, src=src, hh=hh: e.dma_start(out=w[:, hh * 4:(hh + 1) * 4, :], in_=src[:, hh * 4:(hh + 1) * 4, :]), writes=[Bw])
    S.dma("pool", lambda e, wd=wd, e_=e_: e.dma_start(out=wd, in_=K.w_dn[e_].rearrange("(c p) n -> p c n", p=128)), writes=[Bwd])
    S.dma("pool", lambda e, bd=bd, e_=e_: e.dma_start(out=bd[0:1, :], in_=K.b_dn[e_:e_ + 1, :]), writes=[Bbd])


def phase4_experts(K):
    S, A, nc = K.S, K.A, K.nc
    ps, Bps = K.ps, K.Bps
    wgu = [K.pre4["wgu"], A.alloc("wgu1", [8, 2 * F], BF16)]
    wdn = [K.pre4["wdn"], A.alloc("wdn1", [8, D], BF16)]
    bdn = [K.pre4["bdn"], A.alloc("bdn1", [D], BF16)]
    xTs = [A.alloc(f"xT{i}", [8, CAP], BF16) for i in range(2)]
    resTs = [A.alloc(f"resT{i}", [8, CAP], BF16) for i in range(2)]
    xst = [A.alloc(f"xst{i}", [D], BF16) for i in range(3)]
    yst = [A.alloc(f"yst{i}", [D]) for i in range(2)]
    HW = CAP // 2
    gt = [A.alloc(f"gt{i}", [HW]) for i in range(2)]
    sg = [A.alloc(f"sg{i}", [HW]) for i in range(2)]
    ut = [A.alloc(f"ut{i}", [HW]) for i in range(2)]
    cnts = {"y": 0, "x": 0, "u": 0}
    S.op("dve", lambda e: e.memset(yst[1][0], 0.0), writes=[yst[1][1]])
    S.dma("sp", lambda e: e.dma_start(out=K.ys[NSLOT:NSLOT + 128, :], in_=yst[1][0]), reads=[yst[1][1]], owner=yst[1][1])

    def load_expert(e_):
        issue_expert_load(K, e_, *wgu[e_ % 2], *wdn[e_ % 2], *bdn[e_ % 2])

    def build_xT(e_):
        xT, BxT = xTs[e_ % 2]
        for sb in range(NSB):
            xs_t, Bxs = xst[cnts["x"] % 3]
            cnts["x"] += 1
            r0 = e_ * CAP + sb * 128
            S.dma("sp", lambda e, xs_t=xs_t, r0=r0: e.dma_start(out=xs_t, in_=K.xs[r0:r0 + 128, :]), writes=[Bxs])
            bank = sb % 2
            pv16 = ps[bank][:].bitcast(BF16)
            for c in range(8):
                S.op("pe", lambda e, c=c, pv16=pv16, xs_t=xs_t: e.transpose(pv16[:, c * 128:(c + 1) * 128], xs_t[:, c * 128:(c + 1) * 128], K.ident_b),
                     reads=[Bxs, K.Bident_b], writes=[Bps[bank]], signal=(c == 7))
            S.op("act", lambda e, pv16=pv16, sb=sb, xT=xT: e.activation(out=xT[:, :, sb * 128:(sb + 1) * 128], in_=pv16.rearrange("p (c t) -> p c t", c=8), func=AF.Copy),
                 reads=[Bps[bank]], writes=[BxT])

    def gate_up(e_):
        w, Bw = wgu[e_ % 2]
        xT, BxT = xTs[e_ % 2]
        resT, BresT = resTs[e_ % 2]
        for f in range(8):
            bgc = K.pvt[:, PV_BGU + e_ * 16 + f:PV_BGU + e_ * 16 + f + 1]
            buc = K.pvt[:, PV_BGU + e_ * 16 + 8 + f:PV_BGU + e_ * 16 + 8 + f + 1]
            for hv in range(2):
                nsl = slice(hv * HW, (hv + 1) * HW)
                cnt = cnts["u"]
                cnts["u"] += 1
                gb, ub_ = 2 + cnt % 2, 4 + cnt % 2
                g_t, Bg = gt[cnt % 2]
                s_t, Bs = sg[cnt % 2]
                u_t, Bu = ut[cnt % 2]
                mm_group(S, ps[gb][:, 0:HW], Bps[gb], [(w[:, k, f * 128:(f + 1) * 128], xT[:, k, nsl]) for k in range(8)], reads=[Bw, BxT])
                mm_group(S, ps[ub_][:, 0:HW], Bps[ub_], [(w[:, k, F + f * 128:F + (f + 1) * 128], xT[:, k, nsl]) for k in range(8)], reads=[Bw, BxT])
                S.op("dve", lambda e, gb=gb, g_t=g_t, bgc=bgc: e.tensor_scalar(out=g_t, in0=ps[gb][:, 0:HW], scalar1=bgc, scalar2=7.0, op0=ALU.add, op1=ALU.min),
                     reads=[Bps[gb], K.Bpv], writes=[Bg])
                S.op("act", lambda e, ub_=ub_, u_t=u_t, buc=buc: e.activation(out=u_t, in_=ps[ub_][:, 0:HW], func=AF.Identity, bias=buc),
                     reads=[Bps[ub_], K.Bpv], writes=[Bu])
                S.op("act", lambda e, g_t=g_t, s_t=s_t: e.activation(out=s_t, in_=g_t, func=AF.Gelu_apprx_sigmoid), reads=[Bg], writes=[Bs])
                S.op("dve", lambda e, u_t=u_t: e.tensor_scalar(out=u_t, in0=u_t, scalar1=7.0, scalar2=-7.0, op0=ALU.min, op1=ALU.max),
                     reads=[Bu], writes=[Bu])
                S.op("dve", lambda e, s_t=s_t, u_t=u_t, f=f, nsl=nsl, resT=resT: e.scalar_tensor_tensor(
                    out=resT[:, f, nsl], in0=u_t, scalar=1.0, in1=s_t, op0=ALU.add, op1=ALU.mult),
                    reads=[Bs, Bu], writes=[BresT])

    def down(e_):
        wd, Bwd = wdn[e_ % 2]
        bd, Bbd = bdn[e_ % 2]
        resT, BresT = resTs[e_ % 2]
        for sb in range(NSB):
            y_t, By = yst[cnts["y"] % 2]
            cnts["y"] += 1
            for nh in range(2):
                bank = 6 + nh
                pairs = [(resT[:, k, sb * 128:(sb + 1) * 128], wd[:, k, nh * 512:(nh + 1) * 512]) for k in range(8)]
                pairs.append((K.ones_b[0:1, :], bd[0:1, nh * 512:(nh + 1) * 512]))
                mm_group(S, ps[bank][:], Bps[bank], pairs, reads=[BresT, Bwd, Bbd, K.Bones_b])
                S.op("act", lambda e, bank=bank, y_t=y_t, nh=nh: e.activation(out=y_t[:, nh * 512:(nh + 1) * 512], in_=ps[bank][:], func=AF.Copy),
                     reads=[Bps[bank]], writes=[By])
            r0 = e_ * CAP + sb * 128
            S.dma("sp", lambda e, y_t=y_t, r0=r0: e.dma_start(out=K.ys[r0:r0 + 128, :], in_=y_t), reads=[By], owner=By)

    build_xT(0)
    for e_ in range(E):
        if e_ + 1 < E:
            load_expert(e_ + 1)
        if e_ == 0:
            wpg, Bwpg = K.pre5["wpg"]
            wpp, Bwpp = K.pre5["wpp"]
            gple, Bgple = K.pre5["gple"]
            S.dma("pool", lambda e: e.dma_start(out=wpg, in_=K.w_ple_gate.rearrange("(c p) n -> p c n", p=128)), writes=[Bwpg])
            S.dma("pool", lambda e: e.dma_start(out=wpp, in_=K.w_ple_proj.rearrange("(c p) n -> p c n", p=128)), writes=[Bwpp])
            S.dma("sp", lambda e: e.dma_start(out=gple, in_=bcast_row(K.g_ple, D)), writes=[Bgple])
        gate_up(e_)
        if e_ + 1 < E:
            build_xT(e_ + 1)
        down(e_)


def phase5_combine(K):
    S, A, nc = K.S, K.A, K.nc
    ps, Bps = K.ps, K.Bps
    wpg, Bwpg = K.pre5["wpg"]
    wpp, Bwpp = K.pre5["wpp"]
    gple, Bgple = K.pre5["gple"]

    def tile_stream(par):
        h_t, Bh = A.alloc(f"ht{par}", [D])
        yg = [A.alloc(f"yg{par}_{k}", [D]) for k in range(4)]
        p_t, Bp = A.alloc(f"pt{par}", [PLE])
        ptb, Bptb = A.alloc(f"ptb{par}", [PLE], BF16)
        pT, BpT = A.alloc(f"pT{par}", [2, 128], BF16)
        junk, Bjunk = A.alloc(f"junk{par}", [D], BF16)
        ss_t, Bss = A.alloc(f"ss{par}", [1])
        u3, Bu3 = A.alloc(f"u3{par}", [D], BF16)
        u3T, Bu3T = A.alloc(f"u3T{par}", [8, 128], BF16)
        sgm, Bsgm = A.alloc(f"sgm{par}", [D])
        o_t, Bo = A.alloc(f"ot{par}", [D])
        b0 = par * 2

        def gen():
            for i in range(par, NTILE, 4):
                r0 = i * 128
                S.dma("sp", lambda e, r0=r0: e.dma_start(out=h_t, in_=K.h1_s[r0:r0 + 128, :]), writes=[Bh])
                S.dma("sp", lambda e, r0=r0: e.dma_start(out=p_t, in_=K.p[r0:r0 + 128, :]), writes=[Bp])
                for k in range(4):
                    y_t, By = yg[k]
                    S.dma("pool", lambda e, y_t=y_t, i=i, k=k: e.indirect_dma_start(
                        out=y_t, out_offset=None, in_=K.ys,
                        in_offset=bass.IndirectOffsetOnAxis(ap=K.dest_i[:, i * 4 + k:i * 4 + k + 1], axis=0)),
                        reads=[K.Bdest], writes=[By])
                yield
                S.op("act", lambda e: e.activation(out=ptb, in_=p_t, func=AF.Copy), reads=[Bp], writes=[Bptb])
                pv1 = ps[b0 + 1][:].bitcast(BF16)
                for c in range(2):
                    S.op("pe", lambda e, c=c, pv1=pv1: e.transpose(pv1[:, c * 128:(c + 1) * 128], ptb[:, c * 128:(c + 1) * 128], K.ident_b),
                         reads=[Bptb, K.Bident_b], writes=[Bps[b0 + 1]], signal=(c == 1))
                S.op("act", lambda e, pv1=pv1: e.activation(out=pT, in_=pv1[:, 0:256].rearrange("p (c t) -> p c t", c=2), func=AF.Copy),
                     reads=[Bps[b0 + 1]], writes=[BpT])
                yield
                for k in range(4):
                    y_t, By = yg[k]
                    S.op("dve", lambda e, y_t=y_t, i=i, k=k: e.scalar_tensor_tensor(
                        out=h_t, in0=y_t, scalar=K.gate_a[:, i * 4 + k:i * 4 + k + 1], in1=h_t, op0=ALU.mult, op1=ALU.add),
                        reads=[By, Bh, K.Bgate], writes=[Bh])
                yield
                rms_rstd(K, h_t, Bh, junk, Bjunk, ss_t, Bss, D)
                S.op("dve", lambda e: e.scalar_tensor_tensor(out=u3, in0=h_t, scalar=ss_t, in1=gple, op0=ALU.mult, op1=ALU.mult),
                     reads=[Bh, Bss, Bgple], writes=[Bu3])
                yield
                pv16 = ps[b0][:].bitcast(BF16)
                for c in range(8):
                    S.op("pe", lambda e, c=c, pv16=pv16: e.transpose(pv16[:, c * 128:(c + 1) * 128], u3[:, c * 128:(c + 1) * 128], K.ident_b),
                         reads=[Bu3, K.Bident_b], writes=[Bps[b0]], signal=(c == 7))
                S.op("act", lambda e, pv16=pv16: e.activation(out=u3T, in_=pv16.rearrange("p (c t) -> p c t", c=8), func=AF.Copy),
                     reads=[Bps[b0]], writes=[Bu3T])
                yield
                for nh in range(2):
                    nsl = slice(nh * 512, (nh + 1) * 512)
                    gbk, pbk = b0, b0 + 1
                    mm_group(S, ps[gbk][:], Bps[gbk], [(u3T[:, k, :], wpg[:, k, nsl]) for k in range(8)], reads=[Bu3T, Bwpg])
                    mm_group(S, ps[pbk][:], Bps[pbk], [(pT[:, k, :], wpp[:, k, nsl]) for k in range(2)], reads=[BpT, Bwpp])
                    S.op("act", lambda e, gbk=gbk, nsl=nsl: e.activation(out=sgm[:, nsl], in_=ps[gbk][:], func=AF.Sigmoid), reads=[Bps[gbk]], writes=[Bsgm])
                    S.op("dve", lambda e, pbk=pbk, nsl=nsl: e.tensor_tensor(out=sgm[:, nsl], in0=sgm[:, nsl], in1=ps[pbk][:], op=ALU.mult),
                         reads=[Bsgm, Bps[pbk]], writes=[Bsgm])
                    S.op("dve", lambda e, nsl=nsl: e.tensor_tensor(out=o_t[:, nsl], in0=sgm[:, nsl], in1=h_t[:, nsl], op=ALU.add),
                         reads=[Bsgm, Bh], writes=[Bo])
                    yield
                S.dma("sp", lambda e, r0=r0: e.dma_start(out=K.y[r0:r0 + 128, :], in_=o_t), reads=[Bo], owner=Bo)
        return gen()

    interleave([tile_stream(q) for q in range(4)], skew=2)


def _rope_tables():
    pos = np.arange(SEQ)
    row = (pos // 64).astype(np.float32)
    col = (pos % 64).astype(np.float32)
    inv = (10000.0 ** (-np.arange(0, 64, 2, dtype=np.float32) / 64.0)).astype(np.float32)
    C = np.zeros((128, SEQ), np.float32)
    Sg = np.zeros((128, SEQ), np.float32)
    for p in range(128):
        ids = row if p < 64 else col
        j = p % 32
        ang = (ids * inv[j]).astype(np.float32)
        C[p] = np.cos(ang)
        sgn = -1.0 if (p % 64) < 32 else 1.0
        Sg[p] = sgn * np.sin(ang)
    perm = np.zeros((128, 128), np.float32)
    for m in range(128):
        partner = m + 32 if (m % 64) < 32 else m - 32
        perm[partner, m] = 1.0
    return C, Sg, perm


_NC_CACHE = {}


def _prep_common(inp):
    f = lambda a: np.ascontiguousarray(np.asarray(a, dtype=np.float32))
    pv = np.zeros((128, NPV), np.float32)
    cw = f(inp["conv_w"])[0]
    pv[:, PV_CONVW:PV_CONVW + 32] = cw.reshape(4, 8, 128).transpose(2, 1, 0).reshape(128, 32)
    pv[:, PV_CONVB:PV_CONVB + 8] = f(inp["conv_b"])[0].reshape(8, 128).T
    pv[:, PV_BA:PV_BA + 16] = f(inp["lru_ba"])[0].reshape(16, 128).T
    pv[:, PV_BI:PV_BI + 16] = f(inp["lru_bi"])[0].reshape(16, 128).T
    pv[:, PV_LAM:PV_LAM + 16] = f(inp["lru_lam"])[0].reshape(16, 128).T
    pv[:, PV_QN] = f(inp["q_norm"])[0]
    pv[:, PV_KN] = f(inp["k_norm"])[0]
    pv[:, PV_BGU:PV_BGU + 512] = f(inp["b_gu"])[0].reshape(E * 16, 128).T
    C, Sg, perm = _rope_tables()
    tri = np.triu(np.ones((128, 128), np.float32), 1)
    eC = np.tile((np.arange(E, dtype=np.float32) * CAP)[None, :], (128, 1))
    com = {
        "w_in": f(inp["w_in"])[0], "lru_wa": f(inp["lru_wa"])[0], "lru_wi": f(inp["lru_wi"])[0],
        "w_attn_br": f(inp["w_attn_br"])[0], "w_lru_br": f(inp["w_lru_br"])[0], "w_out": f(inp["w_out"])[0],
        "w_router": f(inp["w_router"])[0], "w_gu": f(inp["w_gu"])[0], "w_dn": f(inp["w_dn"])[0],
        "b_dn": f(inp["b_dn"])[0], "w_ple_gate": f(inp["w_ple_gate"])[0], "w_ple_proj": f(inp["w_ple_proj"])[0],
        "g_mix": f(inp["g_mix"]), "g_moe": f(inp["g_moe"]), "g_ple": f(inp["g_ple"]), "b_router": f(inp["b_router"]),
        "pv": pv, "ropeC": C, "ropeS": Sg, "perm": perm, "ident": np.eye(128, dtype=np.float32), "tri": tri, "eC": eC,
    }
    return com


def kernel(**inputs):
    dbg = bool(int(os.environ.get("MK_DBG", "0")))
    ncores = int(os.environ.get("MK_NCORES", str(NCORES)))
    key = dbg
    if key not in _NC_CACHE:
        _NC_CACHE[key] = build_program(dbg)
    nc = _NC_CACHE[key]
    com = _prep_common(inputs)
    x = np.asarray(inputs["x"], dtype=np.float32)
    p = np.asarray(inputs["p"], dtype=np.float32)[0]
    in_maps = []
    for c in range(ncores):
        m = dict(com)
        m["x"] = np.ascontiguousarray(x[2 * c:2 * c + 2].reshape(T, D))
        m["p"] = np.ascontiguousarray(p[2 * c:2 * c + 2].reshape(T, PLE))
        in_maps.append(m)
    res = run_bass_kernel_spmd(nc, in_maps, core_ids=list(range(ncores)))
    if dbg:
        kernel.last = res
    out = np.zeros((16, SEQ, D), np.float32)
    for c in range(ncores):
        out[2 * c:2 * c + 2] = np.asarray(res.results[c]["y"], dtype=np.float32).reshape(2, SEQ, D)
    return out
```
